# Optimizing a Trainium2 kernel written in Bass

```python
import jax, jax.numpy as jnp
from jax import lax
import numpy as np

D_MODEL = 1024
BATCH = 2
SEQ = 8192
DEPTH = 2

N_MIXERS = 2
N_ATTN_LAYERS = (DEPTH + 1) // 2
N_GMLP_LAYERS = DEPTH // 2
HEAD_DIM = 64
N_Q_HEADS = D_MODEL // HEAD_DIM
N_KV_HEADS = 4
GQA_GROUP = N_Q_HEADS // N_KV_HEADS
WINDOW = 128
ROPE_THETA = 10000.0
Q_WIDTH = N_Q_HEADS * HEAD_DIM
KV_WIDTH = N_KV_HEADS * HEAD_DIM
QKV_WIDTH = Q_WIDTH + 2 * KV_WIDTH
CHUNK = 128
GMLP_WIDTH = 2 * D_MODEL
N_SGU_GROUPS = 8
SGU_GROUP_DIM = GMLP_WIDTH // N_SGU_GROUPS
N_EXPERT_GROUPS = 4
EXPERTS_PER_GROUP = 8
TOP_K_INNER = 2
EXPERT_FF = D_MODEL // 4
N_MOD = 6
DEEPNORM_ALPHA = (2.0 * DEPTH) ** 0.25
DEEPNORM_BETA = (8.0 * DEPTH) ** -0.25
LN_EPS = 1e-5

kernel_name = "hybrid_swa_sink_gmlp_hmoe_deepnorm_adaln"


def layer_norm(x, g, b):
    xf = x.astype(jnp.float32)
    mu = jnp.mean(xf, axis=-1, keepdims=True)
    var = jnp.mean(jnp.square(xf - mu), axis=-1, keepdims=True)
    y = (xf - mu) * lax.rsqrt(var + LN_EPS)
    return (y * g.astype(jnp.float32) + b.astype(jnp.float32)).astype(x.dtype)


def rope(x, cos, sin):
    xf = x.astype(jnp.float32)
    x1, x2 = jnp.split(xf, 2, axis=-1)
    return jnp.concatenate([x1 * cos - x2 * sin, x2 * cos + x1 * sin], axis=-1).astype(x.dtype)


def sliding_window_sink_attention(h, cos, sin, w_qkv, b_qkv, sinks, w_o, b_o):
    B, S, _ = h.shape
    nb = S // WINDOW
    qkv = h @ w_qkv + b_qkv
    q, k, v = jnp.split(qkv, [Q_WIDTH, Q_WIDTH + KV_WIDTH], axis=-1)
    q = rope(q.reshape(B, S, N_Q_HEADS, HEAD_DIM), cos, sin)
    k = rope(k.reshape(B, S, N_KV_HEADS, HEAD_DIM), cos, sin)
    v = v.reshape(B, S, N_KV_HEADS, HEAD_DIM)
    qb = q.reshape(B, nb, WINDOW, N_KV_HEADS, GQA_GROUP, HEAD_DIM)

    def band(t):
        tb = t.reshape(B, nb, WINDOW, N_KV_HEADS, HEAD_DIM)
        prev = jnp.pad(tb, ((0, 0), (1, 0), (0, 0), (0, 0), (0, 0)))[:, :-1]
        return jnp.concatenate([prev, tb], axis=2)

    kb, vb = band(k), band(v)
    scores = jnp.einsum('bnqkgd,bnskd->bnkgqs', qb, kb,
                        preferred_element_type=jnp.float32) * (HEAD_DIM ** -0.5)
    qi = jnp.arange(WINDOW)[:, None] + WINDOW
    kj = jnp.arange(2 * WINDOW)[None, :]
    in_band = (kj <= qi) & (kj > qi - WINDOW)
    blk_valid = (jnp.arange(nb)[:, None, None] > 0) | (kj[None] >= WINDOW)
    mask = in_band[None] & blk_valid
    scores = jnp.where(mask[None, :, None, None], scores, -jnp.inf)
    sink = sinks.astype(jnp.float32).reshape(1, 1, N_KV_HEADS, GQA_GROUP, 1, 1)
    m = jnp.maximum(jnp.max(scores, axis=-1, keepdims=True), sink)
    p = jnp.exp(scores - m)
    denom = jnp.sum(p, axis=-1, keepdims=True) + jnp.exp(sink - m)
    probs = (p / denom).astype(v.dtype)
    o = jnp.einsum('bnkgqs,bnskd->bnqkgd', probs, vb)
    return o.reshape(B, S, Q_WIDTH) @ w_o + b_o


def chunked_spatial_gating(h, w_in, b_in, sgu_ln_g, sgu_ln_b, w_s, b_s, w_out, b_out):
    B, S, _ = h.shape
    nc = S // CHUNK
    z = jax.nn.gelu(h @ w_in + b_in, approximate=False)
    u, v = jnp.split(z, 2, axis=-1)
    v = layer_norm(v, sgu_ln_g, sgu_ln_b)
    v = v.reshape(B, nc, CHUNK, N_SGU_GROUPS, SGU_GROUP_DIM)
    causal = jnp.tril(jnp.ones((CHUNK, CHUNK), dtype=bool))
    ws = jnp.where(causal[None], w_s, 0).astype(v.dtype)
    mixed = jnp.einsum('gts,bnsgc->bntgc', ws, v) + b_s.T[None, None, :, :, None]
    out = u * mixed.reshape(B, S, GMLP_WIDTH)
    return out @ w_out + b_out


def hierarchical_moe(h, w_group_router, b_group_router, w_expert_router, b_expert_router,
                     w_gate_up, w_down):
    B, S, D = h.shape
    t = h.reshape(B * S, D)
    group_logits = (t @ w_group_router + b_group_router).astype(jnp.float32)
    group_probs = jax.nn.softmax(group_logits, axis=-1)
    g_p, g_idx = lax.top_k(group_probs, 1)
    group_onehot = jax.nn.one_hot(g_idx[:, 0], N_EXPERT_GROUPS, dtype=jnp.float32)
    expert_logits = (t @ w_expert_router + b_expert_router).astype(jnp.float32)
    expert_logits = expert_logits.reshape(-1, N_EXPERT_GROUPS, EXPERTS_PER_GROUP)
    sel_logits = jnp.sum(expert_logits * group_onehot[:, :, None], axis=1)
    top_vals, top_idx = lax.top_k(sel_logits, TOP_K_INNER)
    top_w = jax.nn.softmax(top_vals, axis=-1) * g_p
    inner = jnp.sum(jax.nn.one_hot(top_idx, EXPERTS_PER_GROUP, dtype=jnp.float32)
                    * top_w[..., None], axis=1)
    combine = (group_onehot[:, :, None] * inner[:, None, :]).astype(h.dtype)
    y = jnp.zeros_like(t)
    for g in range(N_EXPERT_GROUPS):
        gu = jnp.einsum('td,edf->tef', t, w_gate_up[g])
        gate, up = jnp.split(gu, 2, axis=-1)
        act = jax.nn.silu(gate) * up * combine[:, g, :, None]
        y = y + jnp.einsum('tef,efd->td', act, w_down[g])
    return y.reshape(B, S, D)


def setup_inputs(seed: int = 0) -> dict:
    key = jax.random.key(seed)
    ks = list(jax.random.split(key, 32))
    D = D_MODEL

    def nrm(i, shape, scale):
        return jax.random.normal(ks[i], shape, jnp.float32) * scale

    x = nrm(0, (BATCH, SEQ, D), 1.0)
    c = nrm(1, (BATCH, D), 1.0)
    positions = (jnp.arange(SEQ, dtype=jnp.int32)[None, :]
                 + jax.random.randint(ks[2], (BATCH, 1), 0, 4096, dtype=jnp.int32))
    ada_w = nrm(3, (DEPTH, D, N_MOD * D), 0.2 * D ** -0.5)
    ada_b = nrm(4, (DEPTH, N_MOD * D), 0.02)
    post_ln_g = 1.0 + nrm(5, (DEPTH, 2, D), 0.1)
    post_ln_b = nrm(6, (DEPTH, 2, D), 0.02)
    attn_w_qkv = nrm(7, (N_ATTN_LAYERS, D, QKV_WIDTH), D ** -0.5)
    attn_b_qkv = nrm(8, (N_ATTN_LAYERS, QKV_WIDTH), 0.02)
    attn_sinks = nrm(9, (N_ATTN_LAYERS, N_Q_HEADS), 1.0)
    attn_w_o = nrm(10, (N_ATTN_LAYERS, Q_WIDTH, D), Q_WIDTH ** -0.5 * DEEPNORM_BETA)
    attn_b_o = nrm(11, (N_ATTN_LAYERS, D), 0.02)
    gmlp_w_in = nrm(12, (N_GMLP_LAYERS, D, 2 * GMLP_WIDTH), D ** -0.5)
    gmlp_b_in = nrm(13, (N_GMLP_LAYERS, 2 * GMLP_WIDTH), 0.02)
    gmlp_sgu_ln_g = 1.0 + nrm(14, (N_GMLP_LAYERS, GMLP_WIDTH), 0.1)
    gmlp_sgu_ln_b = nrm(15, (N_GMLP_LAYERS, GMLP_WIDTH), 0.02)
    gmlp_w_s = nrm(16, (N_GMLP_LAYERS, N_SGU_GROUPS, CHUNK, CHUNK), CHUNK ** -0.5)
    gmlp_b_s = 1.0 + nrm(17, (N_GMLP_LAYERS, N_SGU_GROUPS, CHUNK), 0.1)
    gmlp_w_out = nrm(18, (N_GMLP_LAYERS, GMLP_WIDTH, D), GMLP_WIDTH ** -0.5 * DEEPNORM_BETA)
    gmlp_b_out = nrm(19, (N_GMLP_LAYERS, D), 0.02)
    moe_w_group_router = nrm(20, (DEPTH, D, N_EXPERT_GROUPS), D ** -0.5)
    moe_b_group_router = nrm(21, (DEPTH, N_EXPERT_GROUPS), 0.01)
    moe_w_expert_router = nrm(22, (DEPTH, D, N_EXPERT_GROUPS * EXPERTS_PER_GROUP), D ** -0.5)
    moe_b_expert_router = nrm(23, (DEPTH, N_EXPERT_GROUPS * EXPERTS_PER_GROUP), 0.01)
    moe_w_gate_up = nrm(24, (DEPTH, N_EXPERT_GROUPS, EXPERTS_PER_GROUP, D, 2 * EXPERT_FF), D ** -0.5)
    moe_w_down = nrm(25, (DEPTH, N_EXPERT_GROUPS, EXPERTS_PER_GROUP, EXPERT_FF, D),
                     EXPERT_FF ** -0.5 * DEEPNORM_BETA)
    return {"x": x, "c": c, "positions": positions, "ada_w": ada_w, "ada_b": ada_b,
            "post_ln_g": post_ln_g, "post_ln_b": post_ln_b,
            "attn_w_qkv": attn_w_qkv, "attn_b_qkv": attn_b_qkv, "attn_sinks": attn_sinks,
            "attn_w_o": attn_w_o, "attn_b_o": attn_b_o,
            "gmlp_w_in": gmlp_w_in, "gmlp_b_in": gmlp_b_in, "gmlp_sgu_ln_g": gmlp_sgu_ln_g,
            "gmlp_sgu_ln_b": gmlp_sgu_ln_b, "gmlp_w_s": gmlp_w_s, "gmlp_b_s": gmlp_b_s,
            "gmlp_w_out": gmlp_w_out, "gmlp_b_out": gmlp_b_out,
            "moe_w_group_router": moe_w_group_router, "moe_b_group_router": moe_b_group_router,
            "moe_w_expert_router": moe_w_expert_router, "moe_b_expert_router": moe_b_expert_router,
            "moe_w_gate_up": moe_w_gate_up, "moe_w_down": moe_w_down}


def reference(x, c, positions, ada_w, ada_b, post_ln_g, post_ln_b,
              attn_w_qkv, attn_b_qkv, attn_sinks, attn_w_o, attn_b_o,
              gmlp_w_in, gmlp_b_in, gmlp_sgu_ln_g, gmlp_sgu_ln_b, gmlp_w_s, gmlp_b_s,
              gmlp_w_out, gmlp_b_out,
              moe_w_group_router, moe_b_group_router, moe_w_expert_router, moe_b_expert_router,
              moe_w_gate_up, moe_w_down):
    inv_freq = ROPE_THETA ** (-jnp.arange(0, HEAD_DIM, 2, dtype=jnp.float32) / HEAD_DIM)
    ang = positions.astype(jnp.float32)[..., None] * inv_freq
    cos = jnp.cos(ang)[:, :, None, :]
    sin = jnp.sin(ang)[:, :, None, :]
    c_act = jax.nn.silu(c)
    for i in range(DEPTH):
        mod = (c_act @ ada_w[i] + ada_b[i])[:, None, :]
        sh_m, sc_m, gt_m, sh_f, sc_f, gt_f = jnp.split(mod, N_MOD, axis=-1)
        h = x * (1 + sc_m) + sh_m
        j = i // N_MIXERS
        if i % N_MIXERS == 0:
            y = sliding_window_sink_attention(h, cos, sin, attn_w_qkv[j], attn_b_qkv[j],
                                              attn_sinks[j], attn_w_o[j], attn_b_o[j])
        else:
            y = chunked_spatial_gating(h, gmlp_w_in[j], gmlp_b_in[j], gmlp_sgu_ln_g[j],
                                       gmlp_sgu_ln_b[j], gmlp_w_s[j], gmlp_b_s[j],
                                       gmlp_w_out[j], gmlp_b_out[j])
        x = layer_norm(DEEPNORM_ALPHA * x + (1 + gt_m) * y, post_ln_g[i, 0], post_ln_b[i, 0])
        h = x * (1 + sc_f) + sh_f
        y = hierarchical_moe(h, moe_w_group_router[i], moe_b_group_router[i],
                             moe_w_expert_router[i], moe_b_expert_router[i],
                             moe_w_gate_up[i], moe_w_down[i])
        x = layer_norm(DEEPNORM_ALPHA * x + (1 + gt_f) * y, post_ln_g[i, 1], post_ln_b[i, 1])
    return x
```

```python
import numpy as np
from contextlib import ExitStack
import concourse.bass as bass
import concourse.mybir as mybir
from concourse.bass_utils import run_bass_kernel_spmd

F32 = mybir.dt.float32
BF16 = mybir.dt.bfloat16
I32 = mybir.dt.int32
AF = mybir.ActivationFunctionType
ALU = mybir.AluOpType
AX = mybir.AxisListType

NT = 16
D = 1024
ALPHA = 4.0 ** 0.25
LN_EPS = 1e-5
TWO_PI = 6.283185307179586
C1 = 6.28125
C2 = TWO_PI - C1
NEG = -30000.0


class Buf:
    __slots__ = ("name", "w", "r")

    def __init__(self, name):
        self.name = name
        self.w = None
        self.r = {}


class Sched:
    ENG = ("pe", "act", "dve", "pool", "sp")
    NSLOT = 8

    def __init__(self, nc, es):
        self.nc = nc
        self.es = es
        self.E = {"pe": nc.tensor, "act": nc.scalar, "dve": nc.vector,
                  "pool": nc.gpsimd, "sp": nc.sync}
        self.cnt = {}
        self.isdma = {}
        self.waited = {e: {} for e in self.ENG}
        self.ops = []
        self.needed = {}
        for e in self.ENG:
            self.cnt[e] = 0
            self.isdma[e] = False
            self.needed[e] = set()

    def dma_proc(self, name):
        self.cnt[name] = 0
        self.isdma[name] = True
        self.needed[name] = set()

    def _add_dep(self, deps, p, v):
        if self.isdma[p]:
            key = (p, (v - 1) % self.NSLOT)
            deps[key] = max(deps.get(key, 0), (v - 1) // self.NSLOT + 1)
        else:
            deps[p] = max(deps.get(p, 0), v)

    def _mk_waits(self, eng, deps):
        waits = []
        wd = self.waited[eng]
        for key, v in deps.items():
            if wd.get(key, 0) >= v:
                continue
            wd[key] = v
            waits.append((key, v))
            if not isinstance(key, tuple):
                self.needed[key].add(v)
        return waits

    def op(self, eng, fn, reads=(), writes=(), proc=None):
        proc = proc or eng
        deps = {}
        for b in reads:
            if b.w is not None:
                p, v = b.w
                if p == eng and eng == "pe":
                    continue
                self._add_dep(deps, p, v)
        for b in writes:
            if b.w is not None:
                p, v = b.w
                if p != eng or eng != "pe":
                    self._add_dep(deps, p, v)
            for p, v in b.r.items():
                if p != eng or eng != "pe":
                    self._add_dep(deps, p, v)
        self.cnt[proc] += 1
        c = self.cnt[proc]
        if self.isdma[proc] and c > self.NSLOT:
            self._add_dep(deps, proc, c - self.NSLOT)
        waits = self._mk_waits(eng, deps)
        self.ops.append((eng, fn, waits, proc, c))
        for b in reads:
            b.r[proc] = c
        for b in writes:
            b.w = (proc, c)
            b.r = {}
        return c

    def _all_deps(self):
        deps = {}
        for p, v in self.cnt.items():
            if v <= 0:
                continue
            if self.isdma[p]:
                for i in range(max(1, v - self.NSLOT + 1), v + 1):
                    self._add_dep(deps, p, i)
            else:
                deps[p] = v
        return deps

    def barrier(self):
        for eng in self.ENG:
            deps = {k: v for k, v in self._all_deps().items() if k != eng}
            waits = self._mk_waits(eng, deps)
            if waits:
                self.ops.append((eng, None, waits, None, 0))

    def wait_all(self, eng, bufs):
        deps = {}
        for b in bufs:
            if b.w is not None:
                self._add_dep(deps, b.w[0], b.w[1])
        for b in bufs:
            if b.w is not None and self.isdma[b.w[0]]:
                p = b.w[0]
                v = self.cnt[p]
                for i in range(max(1, v - self.NSLOT + 1), v + 1):
                    self._add_dep(deps, p, i)
        waits = self._mk_waits(eng, deps)
        self.ops.append((eng, None, waits, None, 0))

    def emit(self):
        nc = self.nc
        sems = {}
        for p in self.cnt:
            if self.isdma[p]:
                for sl in range(self.NSLOT):
                    sems[(p, sl)] = self.es.enter_context(nc.semaphore(f"s_{p}{sl}"))
            else:
                sems[p] = self.es.enter_context(nc.semaphore("s_" + p))
        last_inc = {p: 0 for p in self.cnt}
        for eng, fn, waits, proc, c in self.ops:
            e = self.E[eng]
            for key, v in waits:
                e.wait_ge(sems[key], v * 16 if isinstance(key, tuple) else v)
            if fn is None:
                continue
            ins = fn(e)
            if self.isdma[proc]:
                ins.then_inc(sems[(proc, (c - 1) % self.NSLOT)], 16)
            elif c in self.needed[proc]:
                ins.then_inc(sems[proc], c - last_inc[proc])
                last_inc[proc] = c


def build(stop_after=None):
    nc = bass.Bass("TRN2", target_bir_lowering=False)

    def din(name, shape, dt=F32):
        return nc.dram_tensor(name, list(shape), dt, kind="ExternalInput").ap()

    xin = din("xin", [17 * 128, D])
    pos_d = din("pos", [128, 17], I32)
    cT_d = din("cT", [128, 8])
    ident_d = din("ident", [128, 128])
    masks_d = din("masks", [3, 128, 512])
    invf_d = din("invf", [128, 32])
    tril_d = din("tril", [128, 128])
    ada_w = din("ada_w", [2, D, 6 * D])
    ada_b = din("ada_b", [2, 6 * D])
    lng_d = din("post_ln_g", [4, D])
    lnb_d = din("post_ln_b", [4, D])
    lngT_d = din("post_ln_gT", [4, 128, 8])
    lnbT_d = din("post_ln_bT", [4, 128, 8])
    wqkv_d = din("attn_w_qkv", [D, 1536])
    bqkv_d = din("attn_b_qkv", [1, 1536])
    sinks_d = din("attn_sinks", [1, 16])
    wo_d = din("attn_w_o", [D, D])
    bo_d = din("attn_b_o", [1, D])
    win_d = din("gmlp_w_in", [D, 4096])
    bin_d = din("gmlp_b_in", [1, 4096])
    glng_d = din("gmlp_ln_g", [1, 2048])
    glnb_d = din("gmlp_ln_b", [1, 2048])
    wsT_d = din("gmlp_w_sT", [8, 128, 128])
    bsT_d = din("gmlp_b_sT", [128, 8])
    wout_d = din("gmlp_w_out", [2048, D])
    bout_d = din("gmlp_b_out", [1, D])
    wr_d = din("moe_wr", [2, D, 36])
    br_d = din("moe_br", [2, 1, 36])
    wgu_d = din("moe_w_gate_up", [2, 32, D, 512])
    wdn_d = din("moe_w_down", [2, 32, 256, D])
    out_d = nc.dram_tensor("out", [NT * 128, D], F32, kind="ExternalOutput").ap()

    es = ExitStack()
    with es:
        S = Sched(nc, es)
        for p in ("dx", "dw", "dc", "do"):
            S.dma_proc(p)

        cur_es = [es]

        sb_n = [0]

        def sb(name, shape, dt=F32):
            sb_n[0] += 1
            return cur_es[0].enter_context(nc.sbuf_tensor(f"sb{sb_n[0]}_{name}", list(shape), dt))

        banks = [es.enter_context(nc.psum_tensor(f"bank{i}", [128, 512], F32)) for i in range(8)]
        bankbf = [b[:].bitcast(BF16) for b in banks]
        PB = [Buf(f"bank{i}") for i in range(8)]
        bank_rr = [0]
        bank_pool = [list(range(8))]

        def next_bank():
            bank_rr[0] += 1
            return bank_pool[0][bank_rr[0] % len(bank_pool[0])]

        x = sb("x", [128, NT, D])
        BX = [Buf(f"x{t}") for t in range(NT)]
        hT = sb("hT", [128, 8, NT * 128], BF16)
        BH = [Buf(f"hT{t}") for t in range(NT)]
        W = sb("W", [128, 24576], BF16)
        BW = [Buf(f"W{s}") for s in range(4)]
        A1 = sb("A1", [128, D]); A0 = sb("A0", [128, D])
        BA1, BA0 = Buf("A1"), Buf("A0")
        gtbm = sb("gtbm", [128, D]); gtbf = sb("gtbf", [128, D])
        Bgtbm, Bgtbf = Buf("gtbm"), Buf("gtbf")
        ident = sb("ident", [128, 128], BF16); Bident = Buf("ident")
        ident32 = sb("ident32", [128, 128]); Bident32 = Buf("ident32")
        ones_bf = sb("ones_bf", [1, 128], BF16); Bones = Buf("ones")
        cact_rep = sb("cact_rep", [128, 8, 128], BF16); Bcact = Buf("cact")
        ctmp = sb("ctmp", [128, 8]); Bctmp = Buf("ctmp")
        xnb = [sb(f"xnb{i}", [128, D], BF16) for i in range(2)]
        Bxnb = [Buf(f"xnb{i}") for i in range(2)]
        st_ = [sb(f"st{i}", [128, 2, 6]) for i in range(2)]; mv_ = [sb(f"mv{i}", [128, 2]) for i in range(2)]
        sd_ = [sb(f"sd{i}", [128, 1]) for i in range(2)]
        rstd_ = [sb(f"rstd{i}", [128, 1]) for i in range(2)]; nmr_ = [sb(f"nmr{i}", [128, 1]) for i in range(2)]
        Bst_ = [Buf(f"st{i}") for i in range(2)]; Bmv_ = [Buf(f"mv{i}") for i in range(2)]
        Bsd_ = [Buf(f"sd{i}") for i in range(2)]; Brstd_ = [Buf(f"rstd{i}") for i in range(2)]
        Bnmr_ = [Buf(f"nmr{i}") for i in range(2)]
        fin_i = [0]
        eps_t = sb("eps_t", [128, 1]); Beps = Buf("eps")
        H1 = [sb(f"H1_{i}", [128, 8]) for i in range(4)]
        H0 = [sb(f"H0_{i}", [128, 8]) for i in range(4)]
        BHa = [Buf(f"Haff{i}") for i in range(4)]
        fmt = sb("fmt", [128, 8]); fmt2 = sb("fmt2", [128, 8]); fmg = sb("fmg", [128, 8]); fmb = sb("fmb", [128, 8])
        Bfmt, Bfmt2, Bfmg, Bfmb = Buf("fmt"), Buf("fmt2"), Buf("fmg"), Buf("fmb")
        wr = [sb(f"wr{l}", [128, 8, 36], BF16) for l in range(2)]
        brr = [sb(f"brr{l}", [1, 36], BF16) for l in range(2)]
        Bwr = [Buf(f"wr{l}") for l in range(2)]
        lg = sb("lg", [128, NT, 36]); Blg = Buf("lg")
        comb = sb("comb", [128, NT, 32]); Bcomb = Buf("comb")
        Bout = Buf("out")

        def dma(eng, proc, out, in_, reads=(), writes=()):
            S.op(eng, lambda e: e.dma_start(out=out, in_=in_), reads=reads, writes=writes, proc=proc)

        def bc_load(dst, bdst, row_ap):
            n = row_ap.shape[-1]
            dma("sp", "dc", dst[:, 0:n], row_ap.to_broadcast([128, n]), writes=[bdst])

        MS = {}

        def alloc_mod_scratch():
            MS["vecT"] = sb("vecT", [128, D]); MS["BvecT"] = Buf("vecT")
            MS["bcT"] = sb("bcT", [128, D]); MS["BbcT"] = Buf("bcT")
            MS["stg"] = [sb(f"stg{i}", [128, 8, 256], BF16) for i in range(2)]
            MS["Bstg"] = [Buf(f"stg{i}") for i in range(2)]

        dma("pool", "dw", ident[:], ident_d, writes=[Bident])
        dma("sp", "dc", ident32[:], ident_d, writes=[Bident32])
        S.op("dve", lambda e: e.memset(ones_bf[:], 1.0), writes=[Bones])
        S.op("dve", lambda e: e.memset(eps_t[:], LN_EPS), writes=[Beps])
        dma("sp", "dc", ctmp[:], cT_d, writes=[Bctmp])
        S.op("act", lambda e: e.activation(out=ctmp[:], in_=ctmp[:], func=AF.Silu), reads=[Bctmp], writes=[Bctmp])
        S.op("dve", lambda e: e.tensor_copy(out=cact_rep[:], in_=ctmp[:].unsqueeze(2).to_broadcast([128, 8, 128])),
             reads=[Bctmp], writes=[Bcact])
        for l in range(2):
            dma("pool", "dw", wr[l][:], wr_d[l].rearrange("(k p) n -> p k n", p=128), writes=[Bwr[l]])
            dma("pool", "dw", brr[l][:], br_d[l], writes=[Bwr[l]])

        def compute_mod_gen(l, j, dst, bdst, add_one, lag=0):
            bcT, BbcT, stg, Bstg = MS["bcT"], MS["BbcT"], MS["stg"], MS["Bstg"]
            bc_load(bcT, BbcT, ada_b[l:l + 1, j * D:(j + 1) * D])

            def issue(nb):
                si = nb % 2
                col = j * D + nb * 256
                dma("pool", "dw", stg[si][:], ada_w[l][:, col:col + 256].rearrange("(k p) n -> p k n", p=128),
                    writes=[Bstg[si]])

            def consume(nb):
                si = nb % 2
                bk = next_bank()
                for k in range(8):
                    S.op("pe", (lambda k=k: lambda e: e.matmul(
                        banks[bk][:, 0:256], cact_rep[:, k, :], stg[si][:, k, :], start=(k == 0), stop=(k == 7)))(),
                         reads=[Bcact, Bstg[si]], writes=[PB[bk]])
                S.op("dve", lambda e: e.scalar_tensor_tensor(
                    out=dst[:, nb * 256:(nb + 1) * 256], in0=banks[bk][:, 0:256], scalar=(1.0 if add_one else 0.0),
                    in1=bcT[:, nb * 256:(nb + 1) * 256], op0=ALU.add, op1=ALU.add),
                     reads=[PB[bk], BbcT], writes=[bdst])

            issue(0); issue(1)
            for _ in range(lag):
                yield
            consume(0); consume(1)
            issue(2); issue(3)
            for _ in range(lag):
                yield
            consume(2); consume(3)

        def compute_mod(l, j, dst, bdst, add_one):
            for _ in compute_mod_gen(l, j, dst, bdst, add_one):
                pass

        def to_fm(src, bsrc, dst, bdst):
            tmp, Btmp = MS["bcT"], MS["BbcT"]
            S.op("dve", lambda e: e.tensor_tensor(
                out=tmp[:].rearrange("p (k c) -> p k c", k=8), in0=src[:].rearrange("p (k c) -> p k c", k=8),
                in1=ident32[:].unsqueeze(1).to_broadcast([128, 8, 128]), op=ALU.mult),
                 reads=[bsrc, Bident32], writes=[Btmp])
            S.op("dve", lambda e: e.tensor_reduce(out=dst[:], in_=tmp[:].rearrange("p (k c) -> p k c", k=8),
                                                  axis=AX.X, op=ALU.add),
                 reads=[Btmp], writes=[bdst])

        def make_haff(idx, l, j_sh, j_sc, ln_idx):
            vecT, BvecT = MS["vecT"], MS["BvecT"]
            compute_mod(l, j_sc, vecT, BvecT, True)
            to_fm(vecT, BvecT, fmt, Bfmt)
            compute_mod(l, j_sh, vecT, BvecT, False)
            to_fm(vecT, BvecT, fmt2, Bfmt2)
            haff_combine(idx, ln_idx)

        def haff_combine(idx, ln_idx):
            if ln_idx is None:
                S.op("dve", lambda e: e.tensor_copy(out=H1[idx][:], in_=fmt[:]), reads=[Bfmt], writes=[BHa[idx]])
                S.op("dve", lambda e: e.tensor_copy(out=H0[idx][:], in_=fmt2[:]), reads=[Bfmt2], writes=[BHa[idx]])
            else:
                dma("sp", "dc", fmg[:], lngT_d[ln_idx], writes=[Bfmg])
                dma("sp", "dc", fmb[:], lnbT_d[ln_idx], writes=[Bfmb])
                S.op("dve", lambda e: e.tensor_tensor(out=H1[idx][:], in0=fmt[:], in1=fmg[:], op=ALU.mult),
                     reads=[Bfmt, Bfmg], writes=[BHa[idx]])
                S.op("dve", lambda e: e.tensor_tensor(out=fmb[:], in0=fmt[:], in1=fmb[:], op=ALU.mult),
                     reads=[Bfmt, Bfmb], writes=[Bfmb])
                S.op("dve", lambda e: e.tensor_tensor(out=H0[idx][:], in0=fmb[:], in1=fmt2[:], op=ALU.add),
                     reads=[Bfmb, Bfmt2], writes=[BHa[idx]])

        def make_aset(ln_idx, bias_row, gtb, bgtb, scale=ALPHA):
            bc_load(A1, BA1, lng_d[ln_idx:ln_idx + 1, :])
            bc_load(A0, BA0, lnb_d[ln_idx:ln_idx + 1, :])
            if scale != 1.0:
                S.op("pool", lambda e: e.tensor_scalar(out=A1[:], in0=A1[:], scalar1=scale, scalar2=None, op0=ALU.mult),
                     reads=[BA1], writes=[BA1])
            if bias_row is None:
                if scale != 1.0:
                    S.op("pool", lambda e: e.tensor_scalar(out=A0[:], in0=A0[:], scalar1=scale, scalar2=None, op0=ALU.mult),
                         reads=[BA0], writes=[BA0])
            else:
                vt, bvt = MS["vecT"], MS["BvecT"]
                bc_load(vt, bvt, bias_row)
                S.op("dve", lambda e: e.tensor_tensor(out=vt[:], in0=vt[:], in1=gtb[:], op=ALU.mult),
                     reads=[bvt, bgtb], writes=[bvt])
                S.op("dve", lambda e: e.scalar_tensor_tensor(out=A0[:], in0=A0[:], scalar=scale, in1=vt[:],
                                                             op0=ALU.mult, op1=ALU.add),
                     reads=[BA0, bvt], writes=[BA0])

        def router_logits(l, t):
            bk = next_bank()
            for k in range(8):
                S.op("pe", (lambda k=k: lambda e: e.matmul(
                    banks[bk][:, 0:36], hT[:, k, t * 128:(t + 1) * 128], wr[l][:, k, :], start=(k == 0), stop=False))(),
                     reads=[BH[t], Bwr[l]], writes=[PB[bk]])
            S.op("pe", lambda e: e.matmul(banks[bk][:, 0:36], ones_bf[:], brr[l][:], start=False, stop=True),
                 reads=[Bones, Bwr[l]], writes=[PB[bk]])
            S.op("dve", lambda e: e.tensor_copy(out=lg[:, t, :], in_=banks[bk][:, 0:36]), reads=[PB[bk]], writes=[Blg])

        def finalize_a(t, mode, src=None, bsrc=None, part=0):
            xa = x[:, t, :] if src is None else src
            bxa = BX[t] if bsrc is None else bsrc
            i = fin_i[0] % 2
            fin_i[0] += 1
            st, mv, sd, rstd, nmr = st_[i], mv_[i], sd_[i], rstd_[i], nmr_[i]
            Bst, Bmv, Bsd, Brstd, Bnmr = Bst_[i], Bmv_[i], Bsd_[i], Brstd_[i], Bnmr_[i]
            if mode == "pro":
                S.op("act", lambda e: e.activation(out=xnb[i][:], in_=xa, func=AF.Copy), reads=[bxa], writes=[Bxnb[i]])
                if src is None:
                    S.op("dve", lambda e: e.scalar_tensor_tensor(out=xa, in0=xa, scalar=ALPHA, in1=A0[:],
                                                                 op0=ALU.mult, op1=ALU.add),
                         reads=[bxa, BA0], writes=[bxa])
                return i
            S.op("dve", lambda e: e.bn_stats(out=st[:, 0, :], in_=xa[:, 0:512]), reads=[bxa], writes=[Bst])
            S.op("dve", lambda e: e.bn_stats(out=st[:, 1, :], in_=xa[:, 512:1024]), reads=[bxa], writes=[Bst])
            S.op("dve", lambda e: e.bn_aggr(out=mv[:], in_=st[:]), reads=[Bst], writes=[Bmv])
            if part == 1:
                return i
            return finalize_a2(t, mode, i, src=src, bsrc=bsrc)

        def finalize_a2(t, mode, i, src=None, bsrc=None):
            xa = x[:, t, :] if src is None else src
            bxa = BX[t] if bsrc is None else bsrc
            st, mv, sd, rstd, nmr = st_[i], mv_[i], sd_[i], rstd_[i], nmr_[i]
            Bst, Bmv, Bsd, Brstd, Bnmr = Bst_[i], Bmv_[i], Bsd_[i], Brstd_[i], Bnmr_[i]
            S.op("act", lambda e: e.activation(out=sd[:], in_=mv[:, 1:2], func=AF.Ln, bias=eps_t[:]),
                 reads=[Bmv, Beps], writes=[Bsd])
            S.op("act", lambda e: e.activation(out=rstd[:], in_=sd[:], func=AF.Exp, scale=-0.5), reads=[Bsd], writes=[Brstd])
            S.op("dve", lambda e: e.scalar_tensor_tensor(out=nmr[:], in0=mv[:, 0:1], scalar=-1.0, in1=rstd[:],
                                                         op0=ALU.mult, op1=ALU.mult),
                 reads=[Bmv, Brstd], writes=[Bnmr])
            if mode == "mid":
                S.op("act", lambda e: e.activation(out=xnb[i][:], in_=xa, func=AF.Identity, scale=rstd[:], bias=nmr[:]),
                     reads=[bxa, Brstd, Bnmr], writes=[Bxnb[i]])
            S.op("dve", lambda e: e.tensor_scalar(out=xa, in0=xa, scalar1=rstd[:], scalar2=nmr[:],
                                                  op0=ALU.mult, op1=ALU.add),
                 reads=[bxa, Brstd, Bnmr], writes=[bxa])
            S.op("pool", lambda e: e.tensor_tensor(out=xa, in0=xa, in1=A1[:], op=ALU.mult),
                 reads=[bxa, BA1], writes=[bxa])
            S.op("pool", lambda e: e.tensor_tensor(out=xa, in0=xa, in1=A0[:], op=ALU.add),
                 reads=[bxa, BA0], writes=[bxa])
            if mode == "out":
                dma("sp", "do", out_d[t * 128:(t + 1) * 128, :], xa, reads=[bxa], writes=[Bout])
            return i

        def finalize_b(t, i, haff, hdst=None, bhdst=None):
            bks = [next_bank(), next_bank()]
            for k in range(8):
                S.op("pe", (lambda k=k: lambda e: e.matmul(
                    banks[bks[k // 4]][:, (k % 4) * 128:(k % 4 + 1) * 128], xnb[i][:, k * 128:(k + 1) * 128], ident[:],
                    start=True, stop=True))(),
                     reads=[Bxnb[i], Bident], writes=[PB[bks[k // 4]]])
            hd = hT[:, :, t * 128:(t + 1) * 128] if hdst is None else hdst
            bhd = BH[t] if bhdst is None else bhdst
            for k in range(8):
                if k % 2 == 0:
                    S.op("act", (lambda k=k: lambda e: e.activation(
                        out=hd[:, k, :], in_=banks[bks[k // 4]][:, (k % 4) * 128:(k % 4 + 1) * 128], func=AF.Identity,
                        scale=H1[haff][:, k:k + 1], bias=H0[haff][:, k:k + 1]))(),
                         reads=[PB[bks[k // 4]], BHa[haff]], writes=[bhd])
                else:
                    S.op("dve", (lambda k=k: lambda e: e.tensor_scalar(
                        out=hd[:, k, :], in0=banks[bks[k // 4]][:, (k % 4) * 128:(k % 4 + 1) * 128],
                        scalar1=H1[haff][:, k:k + 1], scalar2=H0[haff][:, k:k + 1], op0=ALU.mult, op1=ALU.add))(),
                         reads=[PB[bks[k // 4]], BHa[haff]], writes=[bhd])

        def finalize(t, mode, haff=None, hdst=None, bhdst=None, route_l=None, src=None, bsrc=None):
            i = finalize_a(t, mode, src=src, bsrc=bsrc)
            if mode == "out":
                return
            finalize_b(t, i, haff, hdst=hdst, bhdst=bhdst)
            if route_l is not None:
                router_logits(route_l, t)

        def finalize_seq(mode, haff=None, route_l=None, hook=None):
            idx = {}
            idx[0] = finalize_a(0, mode)
            for t in range(NT):
                if hook is not None:
                    hook(t)
                if t + 1 < NT:
                    idx[t + 1] = finalize_a(t + 1, mode)
                if mode != "out":
                    finalize_b(t, idx[t], haff)
                    if route_l is not None and t >= 1:
                        router_logits(route_l, t - 1)
            if mode != "out" and route_l is not None:
                router_logits(route_l, NT - 1)

        def make_end_hook(mode, haff=None, route_l=None):
            st = {}

            def stage_b(t):
                if mode != "out":
                    finalize_b(t, st[t], haff)
                    if route_l is not None:
                        router_logits(route_l, t)

            def hook(t):
                st[t] = finalize_a(t, mode, part=1)
                if t >= 1:
                    finalize_a2(t - 1, mode, st[t - 1])
                if t >= 2:
                    stage_b(t - 2)

            def flush():
                finalize_a2(NT - 1, mode, st[NT - 1])
                stage_b(NT - 2)
                stage_b(NT - 1)
            return hook, flush

        def dump_and_end():
            for t in range(NT):
                dma("sp", "do", out_d[t * 128:(t + 1) * 128, :], x[:, t, :], reads=[BX[t]], writes=[Bout])
            S.wait_all("sp", [Bout])
            S.emit()

        esA = ExitStack()
        cur_es[0] = esA
        with esA:
            cosT = sb("cosT", [128, 17, 32]); sinT = sb("sinT", [128, 17, 32])
            Bcos, Bsin = Buf("cos"), Buf("sin")
            hTh = sb("hTh", [128, 8, 128], BF16); BhTh = Buf("hTh")
            bqkv = sb("bqkv", [1, 1536], BF16); Bbqkv = Buf("bqkv")
            esink = sb("esink", [128, 16]); Besink = Buf("esink")
            maskb = sb("maskb", [128, 3, 512], BF16); Bmask = Buf("maskb")
            Wqkv = W[:, 0:12288].rearrange("p (k n) -> p k n", k=8)
            Wo = W[:, 12288:20480].rearrange("p (k n) -> p k n", k=8)

            es0 = ExitStack()
            cur_es[0] = es0
            with es0:
                alloc_mod_scratch()
                posi = sb("posi", [128, 17], I32); posf = sb("posf", [128, 17]); invf = sb("invf", [128, 32])
                ang = sb("ang", [128, 17, 32]); ki = sb("ki", [128, 17, 32], I32)
                kf = W[:, 22528:22528 + 1088].bitcast(F32).rearrange("p (t f) -> p t f", t=17)
                Bpos, Binvf, Bang, Bkf, Bki = Buf("pos"), Buf("invf"), Buf("ang"), Buf("kf"), Buf("ki")
                xh = W[:, 20480:22528].bitcast(F32); Bxh = Buf("xh")

                make_haff(0, 0, 0, 1, None)
                compute_mod(0, 2, gtbm, Bgtbm, True)
                bc_load(A0, BA0, bo_d)
                S.op("dve", lambda e: e.tensor_tensor(out=A0[:], in0=A0[:], in1=gtbm[:], op=ALU.mult),
                     reads=[BA0, Bgtbm], writes=[BA0])
                dma("pool", "dw", Wqkv, wqkv_d.rearrange("(k p) n -> p k n", p=128), writes=[BW[0], BW[1]])
                dma("pool", "dw", Wo, wo_d.rearrange("(k p) n -> p k n", p=128), writes=[BW[2], BW[3]])
                dma("pool", "dw", bqkv[:], bqkv_d, writes=[Bbqkv])
                bc_load(esink, Besink, sinks_d)
                S.op("act", lambda e: e.activation(out=esink[:], in_=esink[:], func=AF.Exp), reads=[Besink], writes=[Besink])
                dma("pool", "dw", maskb[:], masks_d.rearrange("m p n -> p m n"), writes=[Bmask])
                dma("sp", "dc", posi[:], pos_d, writes=[Bpos])
                dma("sp", "dc", invf[:], invf_d, writes=[Binvf])
                S.op("dve", lambda e: e.tensor_copy(out=posf[:], in_=posi[:]), reads=[Bpos], writes=[Bpos])
                S.op("dve", lambda e: e.tensor_tensor(out=ang[:], in0=posf[:].unsqueeze(2).to_broadcast([128, 17, 32]),
                                                      in1=invf[:].unsqueeze(1).to_broadcast([128, 17, 32]), op=ALU.mult),
                     reads=[Bpos, Binvf], writes=[Bang])

                def sin_of(dst, bdst, shift):
                    if shift != 0.0:
                        S.op("dve", lambda e: e.tensor_scalar(out=dst[:], in0=ang[:], scalar1=shift, scalar2=None, op0=ALU.add),
                             reads=[Bang], writes=[bdst])
                        a_src, ba = dst, bdst
                    else:
                        a_src, ba = ang, Bang
                    S.op("dve", lambda e: e.tensor_scalar(out=ki[:], in0=a_src[:], scalar1=1.0 / TWO_PI, scalar2=None,
                                                          op0=ALU.mult), reads=[ba], writes=[Bki])
                    S.op("dve", lambda e: e.tensor_copy(out=kf, in_=ki[:]), reads=[Bki], writes=[Bkf])
                    S.op("dve", lambda e: e.scalar_tensor_tensor(out=dst[:], in0=kf, scalar=-C1, in1=a_src[:],
                                                                 op0=ALU.mult, op1=ALU.add), reads=[Bkf, ba], writes=[bdst])
                    S.op("dve", lambda e: e.scalar_tensor_tensor(out=dst[:], in0=kf, scalar=-C2, in1=dst[:],
                                                                 op0=ALU.mult, op1=ALU.add), reads=[Bkf, bdst], writes=[bdst])
                    S.op("dve", lambda e: e.tensor_scalar(out=dst[:], in0=dst[:], scalar1=3.1415925, scalar2=-3.1415925,
                                                          op0=ALU.min, op1=ALU.max), reads=[bdst], writes=[bdst])
                    S.op("act", lambda e: e.activation(out=dst[:], in_=dst[:], func=AF.Sin), reads=[bdst], writes=[bdst])

                sin_of(sinT, Bsin, 0.0)
                sin_of(cosT, Bcos, TWO_PI / 4)

                dma("sp", "dx", xh, xin[0:128, :], writes=[Bxh])
                for t in range(NT):
                    dma("sp", "dx", x[:, t, :], xin[(t + 1) * 128:(t + 2) * 128, :], writes=[BX[t]])
                finalize(0, "pro", haff=0, hdst=hTh, bhdst=BhTh, src=xh, bsrc=Bxh)
                def mods_b():
                    vecT, BvecT = MS["vecT"], MS["BvecT"]
                    yield from compute_mod_gen(0, 4, vecT, BvecT, True, lag=3)
                    to_fm(vecT, BvecT, fmt, Bfmt)
                    yield from compute_mod_gen(0, 3, vecT, BvecT, False, lag=3)
                    to_fm(vecT, BvecT, fmt2, Bfmt2)
                    haff_combine(1, 0)
                    yield from compute_mod_gen(0, 5, gtbf, Bgtbf, True, lag=2)

                job_b = mods_b()
                finalize_seq("pro", haff=0, hook=lambda t: next(job_b, None))
                for _ in job_b:
                    pass
                S.op("dve", lambda e: e.tensor_tensor(out=Wo, in0=Wo, in1=gtbm[:].unsqueeze(1).to_broadcast([128, 8, D]),
                                                      op=ALU.mult),
                     reads=[BW[2], BW[3], Bgtbm], writes=[BW[2], BW[3]])
                if stop_after == "pro":
                    dump_and_end()
                    return nc
                make_aset(0, None, None, None)
            S.barrier()

            es1 = ExitStack()
            cur_es[0] = es1
            bank_pool[0] = [3, 6, 7]
            with es1:
                qk32 = sb("qk32", [128, 20, 64]); Bqk32 = Buf("qk32")
                rot = sb("rot", [128, 20, 64], BF16); Brot = Buf("rot")
                rot2 = rot[:].rearrange("p h d -> p (h d)")
                tA = sb("tA", [128, 20, 32]); tB = sb("tB", [128, 20, 32])
                BtA, BtB = Buf("tA"), Buf("tB")
                kpad = sb("kpad", [128, 4, 2, 128], BF16); Bkpad = Buf("kpad")
                qT = sb("qT", [128, 8, 128], BF16); BqT = Buf("qT")
                Vaug = [sb(f"Vaug{i}", [128, 4, 65], BF16) for i in range(3)]
                BV = [Buf(f"V{i}") for i in range(3)]
                ao = sb("ao", [128, 16, 64], BF16); Bao = Buf("ao")
                ao2 = ao[:].rearrange("p h d -> p (h d)")
                aoT = sb("aoT", [128, 8, 128], BF16); BaoT = Buf("aoT")
                dent = sb("dent", [128, 16]); Bdent = Buf("dent")
                kT = [W[:, 20480 + i * 1024:20480 + (i + 1) * 1024].rearrange("p (v c) -> p v c", v=8) for i in range(2)]
                BkT = [Buf(f"kT{i}") for i in range(2)]
                PT = [W[:, 22528 + i * 1024:22528 + (i + 1) * 1024].rearrange("p (b c) -> p b c", b=2) for i in range(2)]
                BPT = [Buf(f"PT{i}") for i in range(2)]
                S.op("pool", lambda e: e.memset(kpad[:], 0.0), writes=[Bkpad])
                for i in range(3):
                    S.op("pool", (lambda i=i: lambda e: e.memset(Vaug[i][:], 1.0))(), writes=[BV[i]])

                def X1(t):
                    halo = t < 0
                    tt = t + 1
                    hsrc = hTh if halo else hT[:, :, t * 128:(t + 1) * 128]
                    bh = BhTh if halo else BH[t]
                    qb = []
                    for nb in ([2] if halo else [0, 1, 2]):
                        bk = nb
                        qb.append((nb, bk))
                        for k in range(8):
                            S.op("pe", (lambda k=k, nb=nb, bk=bk: lambda e: e.matmul(
                                banks[bk][:], hsrc[:, k, :], Wqkv[:, k, nb * 512:(nb + 1) * 512], start=(k == 0), stop=False))(),
                                 reads=[bh, BW[0], BW[1]], writes=[PB[bk]])
                        S.op("pe", (lambda nb=nb, bk=bk: lambda e: e.matmul(
                            banks[bk][:], ones_bf[:], bqkv[:, nb * 512:(nb + 1) * 512], start=False, stop=True))(),
                             reads=[Bones, Bbqkv], writes=[PB[bk]])
                    vcur = Vaug[tt % 3]; bvcur = BV[tt % 3]
                    for nb, bk in qb:
                        if nb < 2:
                            S.op("act", (lambda nb=nb, bk=bk: lambda e: e.activation(
                                out=qk32[:, nb * 8:(nb + 1) * 8, :], in_=banks[bk][:].rearrange("p (h d) -> p h d", d=64),
                                func=AF.Copy))(), reads=[PB[bk]], writes=[Bqk32])
                        else:
                            S.op("act", (lambda bk=bk: lambda e: e.activation(
                                out=qk32[:, 16:20, :], in_=banks[bk][:, 0:256].rearrange("p (h d) -> p h d", d=64),
                                func=AF.Copy))(), reads=[PB[bk]], writes=[Bqk32])
                            S.op("act", (lambda bk=bk: lambda e: e.activation(
                                out=vcur[:, :, 0:64], in_=banks[bk][:, 256:512].rearrange("p (h d) -> p h d", d=64),
                                func=AF.Copy))(), reads=[PB[bk]], writes=[bvcur])
                    h0 = 16 if halo else 0
                    nh = 20 - h0
                    x1 = qk32[:, h0:20, 0:32]; x2 = qk32[:, h0:20, 32:64]
                    cb = cosT[:, tt, :].unsqueeze(1).to_broadcast([128, nh, 32])
                    sbc = sinT[:, tt, :].unsqueeze(1).to_broadcast([128, nh, 32])
                    S.op("dve", lambda e: e.tensor_tensor(out=tA[:, h0:20, :], in0=x1, in1=cb, op=ALU.mult),
                         reads=[Bqk32, Bcos], writes=[BtA])
                    S.op("dve", lambda e: e.tensor_tensor(out=tB[:, h0:20, :], in0=x2, in1=sbc, op=ALU.mult),
                         reads=[Bqk32, Bsin], writes=[BtB])
                    if not halo:
                        S.op("dve", lambda e: e.tensor_tensor(out=rot[:, 0:16, 0:32], in0=tA[:, 0:16, :], in1=tB[:, 0:16, :],
                                                              op=ALU.subtract), reads=[BtA, BtB], writes=[Brot])
                    for half in range(2):
                        S.op("dve", (lambda half=half: lambda e: e.tensor_tensor(
                            out=kpad[:, :, half, half * 64:half * 64 + 32], in0=tA[:, 16:20, :], in1=tB[:, 16:20, :],
                            op=ALU.subtract))(), reads=[BtA, BtB], writes=[Bkpad])
                    S.op("dve", lambda e: e.tensor_tensor(out=tA[:, h0:20, :], in0=x2, in1=cb, op=ALU.mult),
                         reads=[Bqk32, Bcos], writes=[BtA])
                    S.op("dve", lambda e: e.tensor_tensor(out=tB[:, h0:20, :], in0=x1, in1=sbc, op=ALU.mult),
                         reads=[Bqk32, Bsin], writes=[BtB])
                    if not halo:
                        S.op("dve", lambda e: e.tensor_tensor(out=rot[:, 0:16, 32:64], in0=tA[:, 0:16, :], in1=tB[:, 0:16, :],
                                                              op=ALU.add), reads=[BtA, BtB], writes=[Brot])
                    for half in range(2):
                        S.op("dve", (lambda half=half: lambda e: e.tensor_tensor(
                            out=kpad[:, :, half, half * 64 + 32:half * 64 + 64], in0=tA[:, 16:20, :], in1=tB[:, 16:20, :],
                            op=ALU.add))(), reads=[BtA, BtB], writes=[Bkpad])

                tb = [3, 6]

                def X2(t):
                    halo = t < 0
                    tt = t + 1
                    cur = tt % 2
                    for v in range(8):
                        S.op("pe", (lambda v=v: lambda e: e.matmul(
                            banks[tb[v // 4]][:, (v % 4) * 128:(v % 4 + 1) * 128], kpad[:, v // 2, v % 2, :], ident[:],
                            start=True, stop=True))(),
                             reads=[Bkpad, Bident], writes=[PB[tb[v // 4]]])
                    for hb in range(2):
                        S.op("act", (lambda hb=hb: lambda e: e.activation(
                            out=kT[cur][:, hb * 4:(hb + 1) * 4, :], in_=banks[tb[hb]][:].rearrange("p (v c) -> p v c", v=4),
                            func=AF.Copy))(), reads=[PB[tb[hb]]], writes=[BkT[cur]])
                    if halo:
                        return
                    for j in range(8):
                        S.op("pe", (lambda j=j: lambda e: e.matmul(
                            banks[tb[j // 4]][:, (j % 4) * 128:(j % 4 + 1) * 128], rot2[:, j * 128:(j + 1) * 128], ident[:],
                            start=True, stop=True))(),
                             reads=[Brot, Bident], writes=[PB[tb[j // 4]]])
                    for hb in range(2):
                        S.op("dve", (lambda hb=hb: lambda e: e.tensor_copy(
                            out=qT[:, hb * 4:(hb + 1) * 4, :], in_=banks[tb[hb]][:].rearrange("p (v c) -> p v c", v=4)))(),
                             reads=[PB[tb[hb]]], writes=[BqT])

                def Y1(t):
                    tt = t + 1
                    cur = tt % 2
                    prv = 1 - cur
                    vcur, bvcur = Vaug[tt % 3], BV[tt % 3]
                    vprv, bvprv = Vaug[(tt - 1) % 3], BV[(tt - 1) % 3]
                    ob = [0, 1, 2]
                    sc_i = [0]
                    first_tile = (t == 0)

                    def scores(g):
                        pi = g % 2
                        for blk, (ktile, bkt, mi) in enumerate([(kT[prv], BkT[prv], 2 if first_tile else 1),
                                                                (kT[cur], BkT[cur], 0)]):
                            bk = 4 + (sc_i[0] % 2)
                            sc_i[0] += 1
                            S.op("pe", (lambda bk=bk, mi=mi: lambda e: e.matmul(
                                banks[bk][:], ident[:], maskb[:, mi, :], start=True, stop=False))(),
                                 reads=[Bident, Bmask], writes=[PB[bk]])
                            for i in range(4):
                                h = 4 * g + i
                                S.op("pe", (lambda bk=bk, i=i, h=h, ktile=ktile, g=g: lambda e: e.matmul(
                                    banks[bk][:, i * 128:(i + 1) * 128], ktile[:, g * 2 + (h % 2), :], qT[:, h // 2, :],
                                    start=False, stop=(i == 3)))(),
                                     reads=[bkt, BqT], writes=[PB[bk]])
                            S.op("act", (lambda bk=bk, blk=blk, pi=pi: lambda e: e.activation(
                                out=PT[pi][:, blk, :], in_=banks[bk][:], func=AF.Exp, scale=0.125))(),
                                 reads=[PB[bk]], writes=[BPT[pi]])

                    def pv(g):
                        pi = g % 2
                        for i in range(4):
                            h = 4 * g + i
                            obk = ob[h // 7]
                            oc = (h % 7) * 65
                            for blk, (vt, bvt) in enumerate([(vprv, bvprv), (vcur, bvcur)]):
                                S.op("pe", (lambda obk=obk, oc=oc, blk=blk, pi=pi, i=i, vt=vt, g=g: lambda e: e.matmul(
                                    banks[obk][:, oc:oc + 65], PT[pi][:, blk, i * 128:(i + 1) * 128], vt[:, g, :],
                                    start=(blk == 0), stop=(blk == 1)))(),
                                     reads=[BPT[pi], bvt], writes=[PB[obk]])

                    scores(0)
                    for g in range(4):
                        if g + 1 < 4:
                            scores(g + 1)
                        pv(g)
                    for b3 in range(3):
                        hs = 7 * b3
                        n = min(7, 16 - hs)
                        ov = banks[ob[b3]][:, 0:n * 65].rearrange("p (h d) -> p h d", d=65)
                        S.op("dve", (lambda ov=ov, hs=hs, n=n: lambda e: e.tensor_tensor(
                            out=dent[:, hs:hs + n], in0=ov[:, :, 64], in1=esink[:, hs:hs + n], op=ALU.add))(),
                             reads=[PB[ob[b3]], Besink], writes=[Bdent])
                        S.op("dve", (lambda hs=hs, n=n: lambda e: e.reciprocal(out=dent[:, hs:hs + n], in_=dent[:, hs:hs + n]))(),
                             reads=[Bdent], writes=[Bdent])
                        S.op("dve", (lambda ov=ov, hs=hs, n=n: lambda e: e.tensor_tensor(
                            out=ao[:, hs:hs + n, :], in0=ov[:, :, 0:64],
                            in1=dent[:, hs:hs + n].unsqueeze(2).to_broadcast([128, n, 64]), op=ALU.mult))(),
                             reads=[PB[ob[b3]], Bdent], writes=[Bao])

                def Y2(t):
                    for j in range(8):
                        S.op("pe", (lambda j=j: lambda e: e.matmul(
                            banks[tb[j // 4]][:, (j % 4) * 128:(j % 4 + 1) * 128], ao2[:, j * 128:(j + 1) * 128], ident[:],
                            start=True, stop=True))(),
                             reads=[Bao, Bident], writes=[PB[tb[j // 4]]])
                    for hb in range(2):
                        S.op("act", (lambda hb=hb: lambda e: e.activation(
                            out=aoT[:, hb * 4:(hb + 1) * 4, :], in_=banks[tb[hb]][:].rearrange("p (v c) -> p v c", v=4),
                            func=AF.Copy))(), reads=[PB[tb[hb]]], writes=[BaoT])
                    for nb in range(2):
                        bk = 4 + nb
                        for k in range(8):
                            S.op("pe", (lambda k=k, nb=nb, bk=bk: lambda e: e.matmul(
                                banks[bk][:], aoT[:, k, :], Wo[:, k, nb * 512:(nb + 1) * 512], start=(k == 0), stop=(k == 7)))(),
                                 reads=[BaoT, BW[2], BW[3]], writes=[PB[bk]])
                        S.op("dve", (lambda nb=nb, bk=bk: lambda e: e.tensor_tensor(
                            out=x[:, t, nb * 512:(nb + 1) * 512], in0=x[:, t, nb * 512:(nb + 1) * 512], in1=banks[bk][:],
                            op=ALU.add))(), reads=[BX[t], PB[bk]], writes=[BX[t]])

                X1(-1); X2(-1)
                X1(0); X2(0)
                fi = {}
                for t in range(NT):
                    if t >= 1:
                        fi[t - 1] = finalize_a(t - 1, "mid")
                    if t + 1 < NT:
                        X1(t + 1)
                    Y1(t)
                    if t + 1 < NT:
                        X2(t + 1)
                    if t >= 1:
                        finalize_b(t - 1, fi[t - 1], 1)
                    Y2(t)
                    if t >= 2:
                        router_logits(0, t - 2)
                fi[NT - 1] = finalize_a(NT - 1, "mid")
                finalize_b(NT - 1, fi[NT - 1], 1)
                router_logits(0, NT - 2)
                router_logits(0, NT - 1)
                if stop_after == "attn":
                    dump_and_end()
                    return nc
            S.barrier()
        cur_es[0] = es

        def bc3(ap2, n):
            return ap2.unsqueeze(2).to_broadcast([128, NT, n])

        def moe_views(s):
            base = s * 6144
            wgu = W[:, base:base + 4096].rearrange("p (k n) -> p k n", k=8)
            wdn = W[:, base + 4096:base + 6144].rearrange("p (j n) -> p j n", j=2)
            return wgu, wdn

        def moe_load(l, ex):
            s = ex % 4
            wgu, wdn = moe_views(s)
            dma("pool", "dw", wgu, wgu_d[l, ex].rearrange("(k p) n -> p k n", p=128), writes=[BW[s]])
            dma("pool", "dw", wdn, wdn_d[l, ex].rearrange("(j p) n -> p j n", p=128), writes=[BW[s]])
            S.op("pool", lambda e: e.tensor_tensor(out=wdn, in0=wdn, in1=gtbf[:].unsqueeze(1).to_broadcast([128, 2, D]),
                                                   op=ALU.mult), reads=[BW[s], Bgtbf], writes=[BW[s]])

        def moe_phase(l, after_chunk=None, nchunks=16, tile_hook=None, first_load=0, end_hook=None):
            gl_m = sb("gl_m", [128, NT]); Bglm = Buf("gl_m")
            goh = sb("goh", [128, NT, 4]); Bgoh = Buf("goh")
            gex = sb("gex", [128, NT, 4]); Bgex = Buf("gex")
            gp = sb("gp", [128, NT]); Bgp = Buf("gp")
            sel4 = sb("sel4", [128, NT, 32]); Bsel4 = Buf("sel4")
            sel = sb("sel", [128, NT, 8]); Bsel = Buf("sel")
            sel2 = sb("sel2", [128, NT, 8]); Bsel2 = Buf("sel2")
            oh1 = sb("oh1", [128, NT, 8]); Boh1 = Buf("oh1")
            oh2 = sb("oh2", [128, NT, 8]); Boh2 = Buf("oh2")
            m1 = sb("m1", [128, NT]); m2 = sb("m2", [128, NT]); Bm1, Bm2 = Buf("m1"), Buf("m2")
            w1 = sb("w1", [128, NT]); w2 = sb("w2", [128, NT]); Bw1, Bw2 = Buf("w1"), Buf("w2")
            sg = [sb(f"sg{i}", [128, 256]) for i in range(2)]
            Bsg = [Buf(f"sg{i}") for i in range(2)]
            actb = [sb(f"actb{i}", [128, 256], BF16) for i in range(2)]
            Bact = [Buf(f"act{i}") for i in range(2)]
            actT = [sb(f"actT{i}", [128, 2, 128], BF16) for i in range(3)]
            BactT = [Buf(f"actT{i}") for i in range(3)]

            for ex in range(first_load, 4):
                moe_load(l, ex)

            gl = lg[:, :, 0:4]
            S.op("dve", lambda e: e.tensor_reduce(out=gl_m[:], in_=gl, axis=AX.X, op=ALU.max), reads=[Blg], writes=[Bglm])
            S.op("dve", lambda e: e.tensor_tensor(out=goh[:], in0=gl, in1=bc3(gl_m[:], 4), op=ALU.is_equal),
                 reads=[Blg, Bglm], writes=[Bgoh])
            S.op("dve", lambda e: e.tensor_tensor(out=gex[:], in0=gl, in1=bc3(gl_m[:], 4), op=ALU.subtract),
                 reads=[Blg, Bglm], writes=[Bgex])
            S.op("act", lambda e: e.activation(out=gex[:], in_=gex[:], func=AF.Exp), reads=[Bgex], writes=[Bgex])
            S.op("dve", lambda e: e.tensor_reduce(out=gp[:], in_=gex[:], axis=AX.X, op=ALU.add), reads=[Bgex], writes=[Bgp])
            S.op("dve", lambda e: e.reciprocal(out=gp[:], in_=gp[:]), reads=[Bgp], writes=[Bgp])
            S.op("dve", lambda e: e.tensor_tensor(
                out=sel4[:].rearrange("p t (g e) -> p t g e", g=4), in0=lg[:, :, 4:36].rearrange("p t (g e) -> p t g e", g=4),
                in1=goh[:].unsqueeze(3).to_broadcast([128, NT, 4, 8]), op=ALU.mult),
                 reads=[Blg, Bgoh], writes=[Bsel4])
            S.op("dve", lambda e: e.tensor_reduce(out=sel[:], in_=sel4[:].rearrange("p t (g e) -> p t e g", g=4),
                                                  axis=AX.X, op=ALU.add), reads=[Bsel4], writes=[Bsel])
            S.op("dve", lambda e: e.tensor_reduce(out=m1[:], in_=sel[:], axis=AX.X, op=ALU.max), reads=[Bsel], writes=[Bm1])
            S.op("dve", lambda e: e.tensor_tensor(out=oh1[:], in0=sel[:], in1=bc3(m1[:], 8), op=ALU.is_equal),
                 reads=[Bsel, Bm1], writes=[Boh1])
            S.op("dve", lambda e: e.scalar_tensor_tensor(out=sel2[:], in0=oh1[:], scalar=-1e30, in1=sel[:],
                                                         op0=ALU.mult, op1=ALU.add), reads=[Boh1, Bsel], writes=[Bsel2])
            S.op("dve", lambda e: e.tensor_reduce(out=m2[:], in_=sel2[:], axis=AX.X, op=ALU.max), reads=[Bsel2], writes=[Bm2])
            S.op("dve", lambda e: e.tensor_tensor(out=oh2[:], in0=sel2[:], in1=bc3(m2[:], 8), op=ALU.is_equal),
                 reads=[Bsel2, Bm2], writes=[Boh2])
            S.op("dve", lambda e: e.tensor_tensor(out=w2[:], in0=m2[:], in1=m1[:], op=ALU.subtract),
                 reads=[Bm1, Bm2], writes=[Bw2])
            S.op("act", lambda e: e.activation(out=w2[:], in_=w2[:], func=AF.Exp), reads=[Bw2], writes=[Bw2])
            S.op("dve", lambda e: e.tensor_scalar(out=w2[:], in0=w2[:], scalar1=1.0, scalar2=None, op0=ALU.add),
                 reads=[Bw2], writes=[Bw2])
            S.op("dve", lambda e: e.reciprocal(out=w1[:], in_=w2[:]), reads=[Bw2], writes=[Bw1])
            S.op("dve", lambda e: e.tensor_tensor(out=w1[:], in0=w1[:], in1=gp[:], op=ALU.mult), reads=[Bw1, Bgp], writes=[Bw1])
            S.op("dve", lambda e: e.tensor_tensor(out=w2[:], in0=gp[:], in1=w1[:], op=ALU.subtract),
                 reads=[Bgp, Bw1], writes=[Bw2])
            S.op("dve", lambda e: e.tensor_tensor(out=oh1[:], in0=oh1[:], in1=bc3(w1[:], 8), op=ALU.mult),
                 reads=[Boh1, Bw1], writes=[Boh1])
            S.op("dve", lambda e: e.tensor_tensor(out=oh2[:], in0=oh2[:], in1=bc3(w2[:], 8), op=ALU.mult),
                 reads=[Boh2, Bw2], writes=[Boh2])
            S.op("dve", lambda e: e.tensor_tensor(out=oh1[:], in0=oh1[:], in1=oh2[:], op=ALU.add),
                 reads=[Boh1, Boh2], writes=[Boh1])
            S.op("dve", lambda e: e.tensor_tensor(
                out=comb[:].rearrange("p t (g e) -> p t g e", g=4),
                in0=goh[:].unsqueeze(3).to_broadcast([128, NT, 4, 8]),
                in1=oh1[:].unsqueeze(2).to_broadcast([128, NT, 4, 8]), op=ALU.mult),
                 reads=[Bgoh, Boh1], writes=[Bcomb])

            GUB = [0, 1]; TB = [2, 3]; YB = [[4, 5], [6, 7]]
            steps = []
            for c in range(nchunks):
                for t in range(NT):
                    for e2 in range(2):
                        steps.append((c, t, e2))
            n = len(steps)

            def GU(i):
                c, t, e2 = steps[i]
                s = (2 * c + e2) % 4
                wgu, _ = moe_views(s)
                bk = GUB[i % 2]
                for k in range(8):
                    S.op("pe", (lambda k=k: lambda e: e.matmul(
                        banks[bk][:], hT[:, k, t * 128:(t + 1) * 128], wgu[:, k, :], start=(k == 0), stop=(k == 7)))(),
                         reads=[BH[t], BW[s]], writes=[PB[bk]])
                si = i % 2
                S.op("act", lambda e: e.activation(out=sg[si][:], in_=banks[bk][:, 0:256], func=AF.Silu),
                     reads=[PB[bk]], writes=[Bsg[si]])
                ex = 2 * c + e2
                S.op("dve", lambda e: e.scalar_tensor_tensor(
                    out=actb[si][:], in0=sg[si][:], scalar=comb[:, t, ex:ex + 1], in1=banks[bk][:, 256:512],
                    op0=ALU.mult, op1=ALU.mult), reads=[Bsg[si], Bcomb, PB[bk]], writes=[Bact[si]])

            def TR(i):
                si = i % 2
                bk = TB[i % 2]
                ti = i % 3
                for j in range(2):
                    S.op("pe", (lambda j=j: lambda e: e.matmul(
                        banks[bk][:, j * 128:(j + 1) * 128], actb[si][:, j * 128:(j + 1) * 128], ident[:],
                        start=True, stop=True))(),
                         reads=[Bact[si], Bident], writes=[PB[bk]])
                S.op("act", lambda e: e.activation(out=actT[ti][:], in_=banks[bk][:, 0:256].rearrange("p (j c) -> p j c", j=2),
                                                   func=AF.Copy), reads=[PB[bk]], writes=[BactT[ti]])

            def DN(i):
                c, t, e2 = steps[i]
                s = (2 * c + e2) % 4
                _, wdn = moe_views(s)
                ti = i % 3
                yb = YB[t % 2]
                for nb in range(2):
                    for j in range(2):
                        S.op("pe", (lambda nb=nb, j=j: lambda e: e.matmul(
                            banks[yb[nb]][:], actT[ti][:, j, :], wdn[:, j, nb * 512:(nb + 1) * 512],
                            start=(e2 == 0 and j == 0), stop=(e2 == 1 and j == 1)))(),
                             reads=[BactT[ti], BW[s]], writes=[PB[yb[nb]]])
                if e2 == 1:
                    for nb in range(2):
                        S.op("dve", (lambda nb=nb: lambda e: e.tensor_tensor(
                            out=x[:, t, nb * 512:(nb + 1) * 512], in0=x[:, t, nb * 512:(nb + 1) * 512],
                            in1=banks[yb[nb]][:], op=ALU.add))(), reads=[BX[t], PB[yb[nb]]], writes=[BX[t]])
                    if tile_hook is not None:
                        tile_hook(c, t)
                    if end_hook is not None and c == nchunks - 1:
                        end_hook(t)
                    if t == NT - 1:
                        if c + 2 < 16 and c + 2 < nchunks:
                            moe_load(l, 2 * (c + 2)); moe_load(l, 2 * (c + 2) + 1)
                        if after_chunk is not None:
                            after_chunk(c)

            for i in range(n + 2):
                if i < n:
                    GU(i)
                if 1 <= i <= n:
                    TR(i - 1)
                if 2 <= i <= n + 1:
                    DN(i - 2)

        def mod_jobs_moe0():
            vecT, BvecT = MS["vecT"], MS["BvecT"]
            yield from compute_mod_gen(1, 1, vecT, BvecT, True, lag=12)
            to_fm(vecT, BvecT, fmt, Bfmt)
            yield from compute_mod_gen(1, 0, vecT, BvecT, False, lag=12)
            to_fm(vecT, BvecT, fmt2, Bfmt2)
            haff_combine(2, 1)
            yield from compute_mod_gen(1, 2, gtbm, Bgtbm, True, lag=12)
            make_aset(1, bout_d, gtbm, Bgtbm)
            yield from compute_mod_gen(1, 4, vecT, BvecT, True, lag=12)
            to_fm(vecT, BvecT, fmt, Bfmt)
            yield from compute_mod_gen(1, 3, vecT, BvecT, False, lag=12)
            to_fm(vecT, BvecT, fmt2, Bfmt2)
            haff_combine(3, 2)
            yield from compute_mod_gen(1, 5, MS["gtmp"], MS["Bgtmp"], True, lag=12)

        moe0_job = [None]

        def tile_hook_moe0(c, t):
            if c < 1:
                return
            if moe0_job[0] is None:
                moe0_job[0] = mod_jobs_moe0()
            next(moe0_job[0], None)

        es2 = ExitStack()
        cur_es[0] = es2
        bank_pool[0] = [0, 1, 2, 3]
        with es2:
            alloc_mod_scratch()
            MS["gtmp"] = sb("gtmp", [128, D]); MS["Bgtmp"] = Buf("gtmp")
            eh0, ef0 = make_end_hook("mid", haff=2)
            moe_phase(0, None, nchunks=(NCHUNK_DBG or 16), tile_hook=tile_hook_moe0, end_hook=(eh0 if USE_END_HOOK else None))
            for _ in (moe0_job[0] or ()):
                pass
            if USE_END_HOOK:
                ef0()
            else:
                finalize_seq("mid", haff=2)
            dma("pool", "dw", W[:, 4096:8192].rearrange("p (k n) -> p k n", k=8),
                win_d[:, 2048:2560].rearrange("(k p) n -> p k n", p=128), writes=[BW[0], BW[1]])
            if stop_after == "moe0":
                dump_and_end()
                return nc
            S.op("pool", lambda e: e.tensor_copy(out=gtbf[:], in_=MS["gtmp"][:]), reads=[MS["Bgtmp"]], writes=[Bgtbf])
            make_aset(2, None, None, None)
        S.barrier()
        cur_es[0] = es

        es3 = ExitStack()
        cur_es[0] = es3
        bank_pool[0] = list(range(8))
        with es3:
            gst = sb("gst", [128, NT, 4, 6]); Bgst = Buf("gst")
            mvg = sb("mvg", [128, NT, 2]); Bmvg = Buf("mvg")
            sdg = sb("sdg", [128, NT]); rstdg = sb("rstdg", [128, NT]); nmrg = sb("nmrg", [128, NT])
            Bsdg, Brstdg, Bnmrg = Buf("sdg"), Buf("rstdg"), Buf("nmrg")
            u32 = [sb(f"u32_{i}", [128, 512]) for i in range(2)]; Bu32 = [Buf(f"u32_{i}") for i in range(2)]
            v32 = [sb(f"v32_{i}", [128, 512]) for i in range(2)]; Bv32 = [Buf(f"v32_{i}") for i in range(2)]
            vln = [sb(f"vln{i}", [128, 512], BF16) for i in range(2)]; Bvln = [Buf(f"vln{i}") for i in range(2)]
            gated = [sb(f"gated{i}", [128, 512], BF16) for i in range(2)]; Bgated = [Buf(f"gated{i}") for i in range(2)]
            gatedT = [sb(f"gatedT{i}", [128, 4, 128], BF16) for i in range(3)]; BgatedT = [Buf(f"gatedT{i}") for i in range(3)]
            lngb = [sb(f"lngb{i}", [128, 2, 512]) for i in range(2)]; Blngb = [Buf(f"lngb{i}") for i in range(2)]
            binr = [sb(f"binr{i}", [1, 2, 512], BF16) for i in range(2)]; Bbinr = [Buf(f"binr{i}") for i in range(2)]
            wsTm = sb("wsTm", [128, 8, 128], BF16); BwsT = Buf("wsTm")
            trilb = sb("trilb", [128, 128], BF16); Btril = Buf("tril")
            bsT = sb("bsT", [128, 8]); BbsT = Buf("bsT")
            dma("pool", "dw", wsTm[:], wsT_d.rearrange("g s t -> s g t"), writes=[BwsT])
            dma("pool", "dw", trilb[:], tril_d, writes=[Btril])
            dma("sp", "dc", bsT[:], bsT_d, writes=[BbsT])
            S.op("dve", lambda e: e.tensor_tensor(out=wsTm[:], in0=wsTm[:], in1=trilb[:].unsqueeze(1).to_broadcast([128, 8, 128]),
                                                  op=ALU.mult), reads=[BwsT, Btril], writes=[BwsT])

            def gviews(b):
                base = b * 12288
                wu = W[:, base:base + 4096].rearrange("p (k n) -> p k n", k=8)
                wv = W[:, base + 4096:base + 8192].rearrange("p (k n) -> p k n", k=8)
                wo2 = W[:, base + 8192:base + 12288].rearrange("p (j n) -> p j n", j=4)
                return wu, wv, wo2

            def gload(cb, b, main, skip_wv=False):
                wu, wv, wo2 = gviews(b)
                bw = [BW[2 * b], BW[2 * b + 1]]
                if not skip_wv:
                    dma("pool", "dw", wv, win_d[:, 2048 + cb * 512:2048 + (cb + 1) * 512].rearrange("(k p) n -> p k n", p=128),
                        writes=bw)
                dma("pool", "dw", binr[b][:, 1, :], bin_d[:, 2048 + cb * 512:2048 + (cb + 1) * 512], writes=[Bbinr[b]])
                if main:
                    dma("pool", "dw", wu, win_d[:, cb * 512:(cb + 1) * 512].rearrange("(k p) n -> p k n", p=128), writes=bw)
                    dma("pool", "dw", binr[b][:, 0, :], bin_d[:, cb * 512:(cb + 1) * 512], writes=[Bbinr[b]])
                    dma("pool", "dw", wo2, wout_d[cb * 512:(cb + 1) * 512, :].rearrange("(j p) n -> p j n", p=128), writes=bw)
                    S.op("pool", lambda e: e.tensor_tensor(out=wo2, in0=wo2,
                                                           in1=gtbm[:].unsqueeze(1).to_broadcast([128, 4, D]), op=ALU.mult),
                         reads=bw + [Bgtbm], writes=bw)
                    dma("sp", "dc", lngb[b][:, 0, :], glng_d[:, cb * 512:(cb + 1) * 512].to_broadcast([128, 512]),
                        writes=[Blngb[b]])
                    dma("sp", "dc", lngb[b][:, 1, :], glnb_d[:, cb * 512:(cb + 1) * 512].to_broadcast([128, 512]),
                        writes=[Blngb[b]])

            gload(0, 0, False, skip_wv=True)
            pi = 0
            for cb in range(4):
                b = cb % 2
                if cb + 1 < 4:
                    gload(cb + 1, (cb + 1) % 2, False)
                _, wv, _ = gviews(b)
                bw = [BW[2 * b], BW[2 * b + 1]]
                for t in range(NT):
                    bk = next_bank()
                    i2 = pi % 2
                    pi += 1
                    for k in range(8):
                        S.op("pe", (lambda k=k, bk=bk, t=t, wv=wv: lambda e: e.matmul(
                            banks[bk][:], hT[:, k, t * 128:(t + 1) * 128], wv[:, k, :], start=(k == 0), stop=False))(),
                             reads=[BH[t]] + bw, writes=[PB[bk]])
                    S.op("pe", (lambda bk=bk, b=b: lambda e: e.matmul(banks[bk][:], ones_bf[:], binr[b][:, 1, :],
                                                                      start=False, stop=True))(),
                         reads=[Bones, Bbinr[b]], writes=[PB[bk]])
                    S.op("act", (lambda bk=bk, i2=i2: lambda e: e.activation(out=v32[i2][:], in_=banks[bk][:], func=AF.Gelu))(),
                         reads=[PB[bk]], writes=[Bv32[i2]])
                    S.op("dve", (lambda i2=i2, t=t, cb=cb: lambda e: e.bn_stats(out=gst[:, t, cb, :], in_=v32[i2][:]))(),
                         reads=[Bv32[i2]], writes=[Bgst])
            for t in range(NT):
                S.op("dve", (lambda t=t: lambda e: e.bn_aggr(out=mvg[:, t, :], in_=gst[:, t, :, :]))(),
                     reads=[Bgst], writes=[Bmvg])
            S.op("act", lambda e: e.activation(out=sdg[:], in_=mvg[:, :, 1], func=AF.Ln, bias=eps_t[:]),
                 reads=[Bmvg, Beps], writes=[Bsdg])
            S.op("act", lambda e: e.activation(out=rstdg[:], in_=sdg[:], func=AF.Exp, scale=-0.5),
                 reads=[Bsdg], writes=[Brstdg])
            S.op("dve", lambda e: e.scalar_tensor_tensor(out=nmrg[:], in0=mvg[:, :, 0], scalar=-1.0, in1=rstdg[:],
                                                         op0=ALU.mult, op1=ALU.mult), reads=[Bmvg, Brstdg], writes=[Bnmrg])

            gsteps = [(cb, t) for cb in range(4) for t in range(NT)]
            ng = len(gsteps)

            def gA(i):
                cb, t = gsteps[i]
                b = cb % 2
                if t == 3 and cb + 1 < 4:
                    gload(cb + 1, (cb + 1) % 2, True)
                wu, wv, _ = gviews(b)
                bw = [BW[2 * b], BW[2 * b + 1]]
                i2 = i % 2
                for which, wmat, dst, bdst in ((0, wu, u32[i2], Bu32[i2]), (1, wv, v32[i2], Bv32[i2])):
                    bk = next_bank()
                    for k in range(8):
                        S.op("pe", (lambda k=k, bk=bk, wmat=wmat: lambda e: e.matmul(
                            banks[bk][:], hT[:, k, t * 128:(t + 1) * 128], wmat[:, k, :], start=(k == 0), stop=False))(),
                             reads=[BH[t]] + bw, writes=[PB[bk]])
                    S.op("pe", (lambda bk=bk, which=which: lambda e: e.matmul(banks[bk][:], ones_bf[:], binr[b][:, which, :],
                                                                              start=False, stop=True))(),
                         reads=[Bones, Bbinr[b]], writes=[PB[bk]])
                    S.op("act", (lambda bk=bk, dst=dst: lambda e: e.activation(out=dst[:], in_=banks[bk][:], func=AF.Gelu))(),
                         reads=[PB[bk]], writes=[bdst])
                S.op("dve", lambda e: e.tensor_scalar(out=v32[i2][:], in0=v32[i2][:], scalar1=rstdg[:, t:t + 1],
                                                      scalar2=nmrg[:, t:t + 1], op0=ALU.mult, op1=ALU.add),
                     reads=[Bv32[i2], Brstdg, Bnmrg], writes=[Bv32[i2]])
                S.op("pool", lambda e: e.tensor_tensor(out=v32[i2][:], in0=v32[i2][:], in1=lngb[b][:, 0, :], op=ALU.mult),
                     reads=[Bv32[i2], Blngb[b]], writes=[Bv32[i2]])
                S.op("pool", lambda e: e.tensor_tensor(out=vln[i2][:], in0=v32[i2][:], in1=lngb[b][:, 1, :], op=ALU.add),
                     reads=[Bv32[i2], Blngb[b]], writes=[Bvln[i2]])

            def gB(i):
                cb, t = gsteps[i]
                i2 = i % 2
                bk = next_bank()
                for gi in range(2):
                    g = 2 * cb + gi
                    S.op("pe", (lambda gi=gi, g=g: lambda e: e.matmul(
                        banks[bk][:, gi * 256:(gi + 1) * 256], wsTm[:, g, :], vln[i2][:, gi * 256:(gi + 1) * 256],
                        start=True, stop=True))(), reads=[BwsT, Bvln[i2]], writes=[PB[bk]])
                for gi in range(2):
                    g = 2 * cb + gi
                    S.op("dve", (lambda gi=gi, g=g: lambda e: e.scalar_tensor_tensor(
                        out=gated[i2][:, gi * 256:(gi + 1) * 256], in0=banks[bk][:, gi * 256:(gi + 1) * 256],
                        scalar=bsT[:, g:g + 1], in1=u32[i2][:, gi * 256:(gi + 1) * 256], op0=ALU.add, op1=ALU.mult))(),
                         reads=[PB[bk], BbsT, Bu32[i2]], writes=[Bgated[i2]])

            def gC(i):
                i2 = i % 2
                bt = next_bank()
                i3 = i % 3
                for j in range(4):
                    S.op("pe", (lambda j=j: lambda e: e.matmul(
                        banks[bt][:, j * 128:(j + 1) * 128], gated[i2][:, j * 128:(j + 1) * 128], ident[:],
                        start=True, stop=True))(),
                         reads=[Bgated[i2], Bident], writes=[PB[bt]])
                S.op("act", lambda e: e.activation(out=gatedT[i3][:], in_=banks[bt][:].rearrange("p (j c) -> p j c", j=4),
                                                   func=AF.Copy), reads=[PB[bt]], writes=[BgatedT[i3]])

            def gD(i):
                cb, t = gsteps[i]
                b = cb % 2
                _, _, wo2 = gviews(b)
                bw = [BW[2 * b], BW[2 * b + 1]]
                i3 = i % 3
                for nb in range(2):
                    bk = next_bank()
                    for j in range(4):
                        S.op("pe", (lambda j=j, nb=nb, bk=bk: lambda e: e.matmul(
                            banks[bk][:], gatedT[i3][:, j, :], wo2[:, j, nb * 512:(nb + 1) * 512],
                            start=(j == 0), stop=(j == 3)))(), reads=[BgatedT[i3]] + bw, writes=[PB[bk]])
                    S.op("dve", (lambda nb=nb, bk=bk: lambda e: e.tensor_tensor(
                        out=x[:, t, nb * 512:(nb + 1) * 512], in0=x[:, t, nb * 512:(nb + 1) * 512], in1=banks[bk][:],
                        op=ALU.add))(), reads=[BX[t], PB[bk]], writes=[BX[t]])
                if cb == 3:
                    if t == 3:
                        moe_load(1, 0); moe_load(1, 1)
                    if USE_END_HOOK:
                        ehg(t)

            ehg, efg = make_end_hook("mid", haff=3, route_l=1)
            gload(0, 0, True)
            for i in range(ng + 3):
                if i < ng:
                    gA(i)
                if 1 <= i <= ng:
                    gB(i - 1)
                if 2 <= i <= ng + 1:
                    gC(i - 2)
                if 3 <= i <= ng + 2:
                    gD(i - 3)
            if USE_END_HOOK:
                efg()
            else:
                finalize_seq("mid", haff=3, route_l=1)
            if stop_after == "gmlp":
                dump_and_end()
                return nc
        S.barrier()
        cur_es[0] = es

        es4 = ExitStack()
        cur_es[0] = es4
        bank_pool[0] = [0, 1, 2, 3]
        with es4:
            make_aset(3, None, None, None, scale=1.0)
            eh1, ef1 = make_end_hook("out")
            moe_phase(1, None, nchunks=(NCHUNK_DBG or 16), first_load=2, end_hook=(eh1 if USE_END_HOOK else None))
            if USE_END_HOOK:
                ef1()
            else:
                finalize_seq("out")
            S.wait_all("sp", [Bout])
            S.emit()
    return nc


NCHUNK_DBG = None
USE_END_HOOK = False
ATT_DBG = [NT, None]


_CACHE = {}


def _host_inputs(inputs):
    f32 = np.float32
    x = np.asarray(inputs["x"], f32)
    c = np.asarray(inputs["c"], f32)
    pos = np.asarray(inputs["positions"], np.int32)
    shared = {}
    shared["ident"] = np.eye(128, dtype=f32)
    s = np.arange(128)[:, None]
    q = np.arange(128)[None, :]
    m_cur = np.where(s <= q, 0.0, NEG).astype(f32)
    m_prev = np.where(s > q, 0.0, NEG).astype(f32)
    m_none = np.full((128, 128), NEG, f32)
    inv_freq = (10000.0 ** (-np.arange(0, 64, 2, dtype=f32) / f32(64))).astype(f32)
    shared["invf"] = np.ascontiguousarray(np.broadcast_to(inv_freq[None, :], (128, 32))).astype(f32)
    shared["tril"] = (s <= q).astype(f32)
    shared["ada_w"] = np.ascontiguousarray(inputs["ada_w"], f32)
    shared["ada_b"] = np.ascontiguousarray(inputs["ada_b"], f32)
    g = np.asarray(inputs["post_ln_g"], f32).reshape(4, D)
    b = np.asarray(inputs["post_ln_b"], f32).reshape(4, D)
    shared["post_ln_g"] = np.ascontiguousarray(g)
    shared["post_ln_b"] = np.ascontiguousarray(b)
    shared["post_ln_gT"] = np.ascontiguousarray(g.reshape(4, 8, 128).transpose(0, 2, 1))
    shared["post_ln_bT"] = np.ascontiguousarray(b.reshape(4, 8, 128).transpose(0, 2, 1))
    shared["attn_w_qkv"] = np.ascontiguousarray(inputs["attn_w_qkv"][0], f32)
    shared["attn_b_qkv"] = np.ascontiguousarray(inputs["attn_b_qkv"], f32).reshape(1, 1536)
    shared["attn_sinks"] = np.ascontiguousarray(inputs["attn_sinks"], f32).reshape(1, 16)
    shared["attn_w_o"] = np.ascontiguousarray(inputs["attn_w_o"][0], f32)
    shared["attn_b_o"] = np.ascontiguousarray(inputs["attn_b_o"], f32).reshape(1, D)
    shared["gmlp_w_in"] = np.ascontiguousarray(inputs["gmlp_w_in"][0], f32)
    shared["gmlp_b_in"] = np.ascontiguousarray(inputs["gmlp_b_in"], f32).reshape(1, 4096)
    shared["gmlp_ln_g"] = np.ascontiguousarray(inputs["gmlp_sgu_ln_g"], f32).reshape(1, 2048)
    shared["gmlp_ln_b"] = np.ascontiguousarray(inputs["gmlp_sgu_ln_b"], f32).reshape(1, 2048)
    shared["gmlp_w_sT"] = np.ascontiguousarray(np.asarray(inputs["gmlp_w_s"][0], f32).transpose(0, 2, 1))
    shared["gmlp_b_sT"] = np.ascontiguousarray(np.asarray(inputs["gmlp_b_s"][0], f32).T)
    shared["gmlp_w_out"] = np.ascontiguousarray(inputs["gmlp_w_out"][0], f32)
    shared["gmlp_b_out"] = np.ascontiguousarray(inputs["gmlp_b_out"], f32).reshape(1, D)
    shared["moe_wr"] = np.ascontiguousarray(np.concatenate(
        [np.asarray(inputs["moe_w_group_router"], f32), np.asarray(inputs["moe_w_expert_router"], f32)], axis=-1))
    shared["moe_br"] = np.ascontiguousarray(np.concatenate(
        [np.asarray(inputs["moe_b_group_router"], f32), np.asarray(inputs["moe_b_expert_router"], f32)], axis=-1)
    ).reshape(2, 1, 36)
    shared["moe_w_gate_up"] = np.ascontiguousarray(inputs["moe_w_gate_up"], f32).reshape(2, 32, D, 512)
    shared["moe_w_down"] = np.ascontiguousarray(inputs["moe_w_down"], f32).reshape(2, 32, 256, D)
    in_maps = []
    for r in range(8):
        bi, qi = r // 4, r % 4
        s0 = qi * 2048
        m = dict(shared)
        xc = np.zeros((17 * 128, D), f32)
        pc = np.zeros((17 * 128,), np.int32)
        if qi > 0:
            xc[:] = x[bi, s0 - 128:s0 + 2048]
            pc[:] = pos[bi, s0 - 128:s0 + 2048]
        else:
            xc[128:] = x[bi, 0:2048]
            pc[128:] = pos[bi, 0:2048]
        m["xin"] = xc
        m["pos"] = np.ascontiguousarray(pc.reshape(17, 128).T)
        m["cT"] = np.ascontiguousarray(c[bi].reshape(8, 128).T)
        mk = np.stack([np.tile(m_cur, (1, 4)), np.tile(m_prev, (1, 4)),
                       np.tile(m_none if qi == 0 else m_prev, (1, 4))]).astype(f32)
        m["masks"] = np.ascontiguousarray(mk)
        in_maps.append(m)
    return in_maps


def kernel(**inputs):
    in_maps = _host_inputs(inputs)
    if "nc" not in _CACHE:
        _CACHE["nc"] = build()
    res = run_bass_kernel_spmd(_CACHE["nc"], in_maps, core_ids=list(range(8)))
    out = np.empty((2, 8192, D), np.float32)
    for r in range(8):
        bi, qi = r // 4, r % 4
        out[bi, qi * 2048:(qi + 1) * 2048] = res.results[r]["out"]
    return out
```

```python
import numpy as np
from contextlib import ExitStack
import concourse.bass as bass
import concourse.mybir as mybir
from concourse.bass_utils import run_bass_kernel_spmd

F32 = mybir.dt.float32
BF16 = mybir.dt.bfloat16
I32 = mybir.dt.int32
AF = mybir.ActivationFunctionType
ALU = mybir.AluOpType
AX = mybir.AxisListType

NT = 16
D = 1024
ALPHA = 4.0 ** 0.25
LN_EPS = 1e-5
TWO_PI = 6.283185307179586
C1 = 6.28125
C2 = TWO_PI - C1
NEG = -30000.0


class Buf:
    __slots__ = ("name", "w", "r")

    def __init__(self, name):
        self.name = name
        self.w = None
        self.r = {}


class Sched:
    ENG = ("pe", "act", "dve", "pool", "sp")
    NSLOT = 8

    def __init__(self, nc, es):
        self.nc = nc
        self.es = es
        self.E = {"pe": nc.tensor, "act": nc.scalar, "dve": nc.vector,
                  "pool": nc.gpsimd, "sp": nc.sync}
        self.cnt = {}
        self.isdma = {}
        self.waited = {e: {} for e in self.ENG}
        self.ops = []
        self.needed = {}
        for e in self.ENG:
            self.cnt[e] = 0
            self.isdma[e] = False
            self.needed[e] = set()

    def dma_proc(self, name):
        self.cnt[name] = 0
        self.isdma[name] = True
        self.needed[name] = set()

    def _add_dep(self, deps, p, v):
        if self.isdma[p]:
            key = (p, (v - 1) % self.NSLOT)
            deps[key] = max(deps.get(key, 0), (v - 1) // self.NSLOT + 1)
        else:
            deps[p] = max(deps.get(p, 0), v)

    def _mk_waits(self, eng, deps):
        waits = []
        wd = self.waited[eng]
        for key, v in deps.items():
            if wd.get(key, 0) >= v:
                continue
            wd[key] = v
            waits.append((key, v))
            if not isinstance(key, tuple):
                self.needed[key].add(v)
        return waits

    def op(self, eng, fn, reads=(), writes=(), proc=None):
        proc = proc or eng
        deps = {}
        for b in reads:
            if b.w is not None:
                p, v = b.w
                if p == eng and eng == "pe":
                    continue
                self._add_dep(deps, p, v)
        for b in writes:
            if b.w is not None:
                p, v = b.w
                if p != eng or eng != "pe":
                    self._add_dep(deps, p, v)
            for p, v in b.r.items():
                if p != eng or eng != "pe":
                    self._add_dep(deps, p, v)
        self.cnt[proc] += 1
        c = self.cnt[proc]
        if self.isdma[proc] and c > self.NSLOT:
            self._add_dep(deps, proc, c - self.NSLOT)
        waits = self._mk_waits(eng, deps)
        self.ops.append((eng, fn, waits, proc, c))
        for b in reads:
            b.r[proc] = c
        for b in writes:
            b.w = (proc, c)
            b.r = {}
        return c

    def _all_deps(self):
        deps = {}
        for p, v in self.cnt.items():
            if v <= 0:
                continue
            if self.isdma[p]:
                for i in range(max(1, v - self.NSLOT + 1), v + 1):
                    self._add_dep(deps, p, i)
            else:
                deps[p] = v
        return deps

    def barrier(self):
        for eng in self.ENG:
            deps = {k: v for k, v in self._all_deps().items() if k != eng}
            waits = self._mk_waits(eng, deps)
            if waits:
                self.ops.append((eng, None, waits, None, 0))

    def wait_all(self, eng, bufs):
        deps = {}
        for b in bufs:
            if b.w is not None:
                self._add_dep(deps, b.w[0], b.w[1])
        for b in bufs:
            if b.w is not None and self.isdma[b.w[0]]:
                p = b.w[0]
                v = self.cnt[p]
                for i in range(max(1, v - self.NSLOT + 1), v + 1):
                    self._add_dep(deps, p, i)
        waits = self._mk_waits(eng, deps)
        self.ops.append((eng, None, waits, None, 0))

    def emit(self):
        nc = self.nc
        sems = {}
        for p in self.cnt:
            if self.isdma[p]:
                for sl in range(self.NSLOT):
                    sems[(p, sl)] = self.es.enter_context(nc.semaphore(f"s_{p}{sl}"))
            else:
                sems[p] = self.es.enter_context(nc.semaphore("s_" + p))
        last_inc = {p: 0 for p in self.cnt}
        for eng, fn, waits, proc, c in self.ops:
            e = self.E[eng]
            for key, v in waits:
                e.wait_ge(sems[key], v * 16 if isinstance(key, tuple) else v)
            if fn is None:
                continue
            ins = fn(e)
            if self.isdma[proc]:
                ins.then_inc(sems[(proc, (c - 1) % self.NSLOT)], 16)
            elif c in self.needed[proc]:
                ins.then_inc(sems[proc], c - last_inc[proc])
                last_inc[proc] = c


def build(stop_after=None):
    nc = bass.Bass("TRN2", target_bir_lowering=False)

    def din(name, shape, dt=F32):
        return nc.dram_tensor(name, list(shape), dt, kind="ExternalInput").ap()

    xin = din("xin", [17 * 128, D])
    pos_d = din("pos", [128, 17], I32)
    cT_d = din("cT", [128, 8])
    ident_d = din("ident", [128, 128])
    masks_d = din("masks", [3, 128, 512])
    invf_d = din("invf", [128, 32])
    tril_d = din("tril", [128, 128])
    ada_w = din("ada_w", [2, D, 6 * D])
    ada_b = din("ada_b", [2, 6 * D])
    lng_d = din("post_ln_g", [4, D])
    lnb_d = din("post_ln_b", [4, D])
    lngT_d = din("post_ln_gT", [4, 128, 8])
    lnbT_d = din("post_ln_bT", [4, 128, 8])
    wqkv_d = din("attn_w_qkv", [D, 1536])
    bqkv_d = din("attn_b_qkv", [1, 1536])
    sinks_d = din("attn_sinks", [1, 16])
    wo_d = din("attn_w_o", [D, D])
    bo_d = din("attn_b_o", [1, D])
    win_d = din("gmlp_w_in", [D, 4096])
    bin_d = din("gmlp_b_in", [1, 4096])
    glng_d = din("gmlp_ln_g", [1, 2048])
    glnb_d = din("gmlp_ln_b", [1, 2048])
    wsT_d = din("gmlp_w_sT", [8, 128, 128])
    bsT_d = din("gmlp_b_sT", [128, 8])
    wout_d = din("gmlp_w_out", [2048, D])
    bout_d = din("gmlp_b_out", [1, D])
    wr_d = din("moe_wr", [2, D, 36])
    br_d = din("moe_br", [2, 1, 36])
    wgu_d = din("moe_w_gate_up", [2, 32, D, 512])
    wdn_d = din("moe_w_down", [2, 32, 256, D])
    out_d = nc.dram_tensor("out", [NT * 128, D], F32, kind="ExternalOutput").ap()

    es = ExitStack()
    with es:
        S = Sched(nc, es)
        for p in ("dx", "dw", "dc", "do"):
            S.dma_proc(p)

        cur_es = [es]

        sb_n = [0]

        def sb(name, shape, dt=F32):
            sb_n[0] += 1
            return cur_es[0].enter_context(nc.sbuf_tensor(f"sb{sb_n[0]}_{name}", list(shape), dt))

        banks = [es.enter_context(nc.psum_tensor(f"bank{i}", [128, 512], F32)) for i in range(8)]
        bankbf = [b[:].bitcast(BF16) for b in banks]
        PB = [Buf(f"bank{i}") for i in range(8)]
        bank_rr = [0]
        bank_pool = [list(range(8))]

        def next_bank():
            bank_rr[0] += 1
            return bank_pool[0][bank_rr[0] % len(bank_pool[0])]

        x = sb("x", [128, NT, D])
        BX = [Buf(f"x{t}") for t in range(NT)]
        hT = sb("hT", [128, 8, NT * 128], BF16)
        BH = [Buf(f"hT{t}") for t in range(NT)]
        W = sb("W", [128, 24576], BF16)
        BW = [Buf(f"W{s}") for s in range(4)]
        A1 = sb("A1", [128, D]); A0 = sb("A0", [128, D])
        BA1, BA0 = Buf("A1"), Buf("A0")
        gtbm = sb("gtbm", [128, D]); gtbf = sb("gtbf", [128, D])
        Bgtbm, Bgtbf = Buf("gtbm"), Buf("gtbf")
        ident = sb("ident", [128, 128], BF16); Bident = Buf("ident")
        ident32 = sb("ident32", [128, 128]); Bident32 = Buf("ident32")
        ones_bf = sb("ones_bf", [1, 128], BF16); Bones = Buf("ones")
        cact_rep = sb("cact_rep", [128, 8, 128], BF16); Bcact = Buf("cact")
        ctmp = sb("ctmp", [128, 8]); Bctmp = Buf("ctmp")
        xnb = [sb(f"xnb{i}", [128, D], BF16) for i in range(2)]
        Bxnb = [Buf(f"xnb{i}") for i in range(2)]
        st_ = [sb(f"st{i}", [128, 2, 6]) for i in range(2)]; mv_ = [sb(f"mv{i}", [128, 2]) for i in range(2)]
        sd_ = [sb(f"sd{i}", [128, 1]) for i in range(2)]
        rstd_ = [sb(f"rstd{i}", [128, 1]) for i in range(2)]; nmr_ = [sb(f"nmr{i}", [128, 1]) for i in range(2)]
        Bst_ = [Buf(f"st{i}") for i in range(2)]; Bmv_ = [Buf(f"mv{i}") for i in range(2)]
        Bsd_ = [Buf(f"sd{i}") for i in range(2)]; Brstd_ = [Buf(f"rstd{i}") for i in range(2)]
        Bnmr_ = [Buf(f"nmr{i}") for i in range(2)]
        fin_i = [0]
        eps_t = sb("eps_t", [128, 1]); Beps = Buf("eps")
        H1 = [sb(f"H1_{i}", [128, 8]) for i in range(4)]
        H0 = [sb(f"H0_{i}", [128, 8]) for i in range(4)]
        BHa = [Buf(f"Haff{i}") for i in range(4)]
        fmt = sb("fmt", [128, 8]); fmt2 = sb("fmt2", [128, 8]); fmg = sb("fmg", [128, 8]); fmb = sb("fmb", [128, 8])
        Bfmt, Bfmt2, Bfmg, Bfmb = Buf("fmt"), Buf("fmt2"), Buf("fmg"), Buf("fmb")
        wr = [sb(f"wr{l}", [128, 8, 36], BF16) for l in range(2)]
        brr = [sb(f"brr{l}", [1, 36], BF16) for l in range(2)]
        Bwr = [Buf(f"wr{l}") for l in range(2)]
        lg = sb("lg", [128, NT, 36]); Blg = Buf("lg")
        comb = sb("comb", [128, NT, 32]); Bcomb = Buf("comb")
        Bout = Buf("out")

        def dma(eng, proc, out, in_, reads=(), writes=()):
            S.op(eng, lambda e: e.dma_start(out=out, in_=in_), reads=reads, writes=writes, proc=proc)

        def bc_load(dst, bdst, row_ap):
            n = row_ap.shape[-1]
            dma("sp", "dc", dst[:, 0:n], row_ap.to_broadcast([128, n]), writes=[bdst])

        MS = {}

        def alloc_mod_scratch():
            MS["vecT"] = sb("vecT", [128, D]); MS["BvecT"] = Buf("vecT")
            MS["bcT"] = sb("bcT", [128, D]); MS["BbcT"] = Buf("bcT")
            MS["stg"] = [sb(f"stg{i}", [128, 8, 256], BF16) for i in range(2)]
            MS["Bstg"] = [Buf(f"stg{i}") for i in range(2)]

        dma("pool", "dw", ident[:], ident_d, writes=[Bident])
        dma("sp", "dc", ident32[:], ident_d, writes=[Bident32])
        S.op("dve", lambda e: e.memset(ones_bf[:], 1.0), writes=[Bones])
        S.op("dve", lambda e: e.memset(eps_t[:], LN_EPS), writes=[Beps])
        dma("sp", "dc", ctmp[:], cT_d, writes=[Bctmp])
        S.op("act", lambda e: e.activation(out=ctmp[:], in_=ctmp[:], func=AF.Silu), reads=[Bctmp], writes=[Bctmp])
        S.op("dve", lambda e: e.tensor_copy(out=cact_rep[:], in_=ctmp[:].unsqueeze(2).to_broadcast([128, 8, 128])),
             reads=[Bctmp], writes=[Bcact])
        for l in range(2):
            dma("pool", "dw", wr[l][:], wr_d[l].rearrange("(k p) n -> p k n", p=128), writes=[Bwr[l]])
            dma("pool", "dw", brr[l][:], br_d[l], writes=[Bwr[l]])

        def compute_mod_gen(l, j, dst, bdst, add_one, lag=0):
            bcT, BbcT, stg, Bstg = MS["bcT"], MS["BbcT"], MS["stg"], MS["Bstg"]
            bc_load(bcT, BbcT, ada_b[l:l + 1, j * D:(j + 1) * D])

            def issue(nb):
                si = nb % 2
                col = j * D + nb * 256
                dma("pool", "dw", stg[si][:], ada_w[l][:, col:col + 256].rearrange("(k p) n -> p k n", p=128),
                    writes=[Bstg[si]])

            def consume(nb):
                si = nb % 2
                bk = next_bank()
                for k in range(8):
                    S.op("pe", (lambda k=k: lambda e: e.matmul(
                        banks[bk][:, 0:256], cact_rep[:, k, :], stg[si][:, k, :], start=(k == 0), stop=(k == 7)))(),
                         reads=[Bcact, Bstg[si]], writes=[PB[bk]])
                S.op("dve", lambda e: e.scalar_tensor_tensor(
                    out=dst[:, nb * 256:(nb + 1) * 256], in0=banks[bk][:, 0:256], scalar=(1.0 if add_one else 0.0),
                    in1=bcT[:, nb * 256:(nb + 1) * 256], op0=ALU.add, op1=ALU.add),
                     reads=[PB[bk], BbcT], writes=[bdst])

            issue(0); issue(1)
            for _ in range(lag):
                yield
            consume(0); consume(1)
            issue(2); issue(3)
            for _ in range(lag):
                yield
            consume(2); consume(3)

        def compute_mod(l, j, dst, bdst, add_one):
            for _ in compute_mod_gen(l, j, dst, bdst, add_one):
                pass

        def to_fm(src, bsrc, dst, bdst):
            tmp, Btmp = MS["bcT"], MS["BbcT"]
            S.op("dve", lambda e: e.tensor_tensor(
                out=tmp[:].rearrange("p (k c) -> p k c", k=8), in0=src[:].rearrange("p (k c) -> p k c", k=8),
                in1=ident32[:].unsqueeze(1).to_broadcast([128, 8, 128]), op=ALU.mult),
                 reads=[bsrc, Bident32], writes=[Btmp])
            S.op("dve", lambda e: e.tensor_reduce(out=dst[:], in_=tmp[:].rearrange("p (k c) -> p k c", k=8),
                                                  axis=AX.X, op=ALU.add),
                 reads=[Btmp], writes=[bdst])

        def make_haff(idx, l, j_sh, j_sc, ln_idx):
            vecT, BvecT = MS["vecT"], MS["BvecT"]
            compute_mod(l, j_sc, vecT, BvecT, True)
            to_fm(vecT, BvecT, fmt, Bfmt)
            compute_mod(l, j_sh, vecT, BvecT, False)
            to_fm(vecT, BvecT, fmt2, Bfmt2)
            haff_combine(idx, ln_idx)

        def haff_combine(idx, ln_idx):
            if ln_idx is None:
                S.op("dve", lambda e: e.tensor_copy(out=H1[idx][:], in_=fmt[:]), reads=[Bfmt], writes=[BHa[idx]])
                S.op("dve", lambda e: e.tensor_copy(out=H0[idx][:], in_=fmt2[:]), reads=[Bfmt2], writes=[BHa[idx]])
            else:
                dma("sp", "dc", fmg[:], lngT_d[ln_idx], writes=[Bfmg])
                dma("sp", "dc", fmb[:], lnbT_d[ln_idx], writes=[Bfmb])
                S.op("dve", lambda e: e.tensor_tensor(out=H1[idx][:], in0=fmt[:], in1=fmg[:], op=ALU.mult),
                     reads=[Bfmt, Bfmg], writes=[BHa[idx]])
                S.op("dve", lambda e: e.tensor_tensor(out=fmb[:], in0=fmt[:], in1=fmb[:], op=ALU.mult),
                     reads=[Bfmt, Bfmb], writes=[Bfmb])
                S.op("dve", lambda e: e.tensor_tensor(out=H0[idx][:], in0=fmb[:], in1=fmt2[:], op=ALU.add),
                     reads=[Bfmb, Bfmt2], writes=[BHa[idx]])

        def make_aset(ln_idx, bias_row, gtb, bgtb, scale=ALPHA):
            bc_load(A1, BA1, lng_d[ln_idx:ln_idx + 1, :])
            bc_load(A0, BA0, lnb_d[ln_idx:ln_idx + 1, :])
            if scale != 1.0:
                S.op("pool", lambda e: e.tensor_scalar(out=A1[:], in0=A1[:], scalar1=scale, scalar2=None, op0=ALU.mult),
                     reads=[BA1], writes=[BA1])
            if bias_row is None:
                if scale != 1.0:
                    S.op("pool", lambda e: e.tensor_scalar(out=A0[:], in0=A0[:], scalar1=scale, scalar2=None, op0=ALU.mult),
                         reads=[BA0], writes=[BA0])
            else:
                vt, bvt = MS["vecT"], MS["BvecT"]
                bc_load(vt, bvt, bias_row)
                S.op("dve", lambda e: e.tensor_tensor(out=vt[:], in0=vt[:], in1=gtb[:], op=ALU.mult),
                     reads=[bvt, bgtb], writes=[bvt])
                S.op("dve", lambda e: e.scalar_tensor_tensor(out=A0[:], in0=A0[:], scalar=scale, in1=vt[:],
                                                             op0=ALU.mult, op1=ALU.add),
                     reads=[BA0, bvt], writes=[BA0])

        def router_logits(l, t):
            bk = next_bank()
            for k in range(8):
                S.op("pe", (lambda k=k: lambda e: e.matmul(
                    banks[bk][:, 0:36], hT[:, k, t * 128:(t + 1) * 128], wr[l][:, k, :], start=(k == 0), stop=False))(),
                     reads=[BH[t], Bwr[l]], writes=[PB[bk]])
            S.op("pe", lambda e: e.matmul(banks[bk][:, 0:36], ones_bf[:], brr[l][:], start=False, stop=True),
                 reads=[Bones, Bwr[l]], writes=[PB[bk]])
            S.op("dve", lambda e: e.tensor_copy(out=lg[:, t, :], in_=banks[bk][:, 0:36]), reads=[PB[bk]], writes=[Blg])

        def finalize_a(t, mode, src=None, bsrc=None, part=0):
            xa = x[:, t, :] if src is None else src
            bxa = BX[t] if bsrc is None else bsrc
            i = fin_i[0] % 2
            fin_i[0] += 1
            st, mv, sd, rstd, nmr = st_[i], mv_[i], sd_[i], rstd_[i], nmr_[i]
            Bst, Bmv, Bsd, Brstd, Bnmr = Bst_[i], Bmv_[i], Bsd_[i], Brstd_[i], Bnmr_[i]
            if mode == "pro":
                S.op("act", lambda e: e.activation(out=xnb[i][:], in_=xa, func=AF.Copy), reads=[bxa], writes=[Bxnb[i]])
                if src is None:
                    S.op("dve", lambda e: e.scalar_tensor_tensor(out=xa, in0=xa, scalar=ALPHA, in1=A0[:],
                                                                 op0=ALU.mult, op1=ALU.add),
                         reads=[bxa, BA0], writes=[bxa])
                return i
            S.op("dve", lambda e: e.bn_stats(out=st[:, 0, :], in_=xa[:, 0:512]), reads=[bxa], writes=[Bst])
            S.op("dve", lambda e: e.bn_stats(out=st[:, 1, :], in_=xa[:, 512:1024]), reads=[bxa], writes=[Bst])
            S.op("dve", lambda e: e.bn_aggr(out=mv[:], in_=st[:]), reads=[Bst], writes=[Bmv])
            if part == 1:
                return i
            return finalize_a2(t, mode, i, src=src, bsrc=bsrc)

        def finalize_a2(t, mode, i, src=None, bsrc=None):
            xa = x[:, t, :] if src is None else src
            bxa = BX[t] if bsrc is None else bsrc
            st, mv, sd, rstd, nmr = st_[i], mv_[i], sd_[i], rstd_[i], nmr_[i]
            Bst, Bmv, Bsd, Brstd, Bnmr = Bst_[i], Bmv_[i], Bsd_[i], Brstd_[i], Bnmr_[i]
            S.op("act", lambda e: e.activation(out=sd[:], in_=mv[:, 1:2], func=AF.Ln, bias=eps_t[:]),
                 reads=[Bmv, Beps], writes=[Bsd])
            S.op("act", lambda e: e.activation(out=rstd[:], in_=sd[:], func=AF.Exp, scale=-0.5), reads=[Bsd], writes=[Brstd])
            S.op("dve", lambda e: e.scalar_tensor_tensor(out=nmr[:], in0=mv[:, 0:1], scalar=-1.0, in1=rstd[:],
                                                         op0=ALU.mult, op1=ALU.mult),
                 reads=[Bmv, Brstd], writes=[Bnmr])
            if mode == "mid":
                S.op("act", lambda e: e.activation(out=xnb[i][:], in_=xa, func=AF.Identity, scale=rstd[:], bias=nmr[:]),
                     reads=[bxa, Brstd, Bnmr], writes=[Bxnb[i]])
            S.op("dve", lambda e: e.tensor_scalar(out=xa, in0=xa, scalar1=rstd[:], scalar2=nmr[:],
                                                  op0=ALU.mult, op1=ALU.add),
                 reads=[bxa, Brstd, Bnmr], writes=[bxa])
            S.op("pool", lambda e: e.tensor_tensor(out=xa, in0=xa, in1=A1[:], op=ALU.mult),
                 reads=[bxa, BA1], writes=[bxa])
            S.op("pool", lambda e: e.tensor_tensor(out=xa, in0=xa, in1=A0[:], op=ALU.add),
                 reads=[bxa, BA0], writes=[bxa])
            if mode == "out":
                dma("sp", "do", out_d[t * 128:(t + 1) * 128, :], xa, reads=[bxa], writes=[Bout])
            return i

        def finalize_b(t, i, haff, hdst=None, bhdst=None):
            bks = [next_bank(), next_bank()]
            for k in range(8):
                S.op("pe", (lambda k=k: lambda e: e.matmul(
                    banks[bks[k // 4]][:, (k % 4) * 128:(k % 4 + 1) * 128], xnb[i][:, k * 128:(k + 1) * 128], ident[:],
                    start=True, stop=True))(),
                     reads=[Bxnb[i], Bident], writes=[PB[bks[k // 4]]])
            hd = hT[:, :, t * 128:(t + 1) * 128] if hdst is None else hdst
            bhd = BH[t] if bhdst is None else bhdst
            for k in range(8):
                if k % 2 == 0:
                    S.op("act", (lambda k=k: lambda e: e.activation(
                        out=hd[:, k, :], in_=banks[bks[k // 4]][:, (k % 4) * 128:(k % 4 + 1) * 128], func=AF.Identity,
                        scale=H1[haff][:, k:k + 1], bias=H0[haff][:, k:k + 1]))(),
                         reads=[PB[bks[k // 4]], BHa[haff]], writes=[bhd])
                else:
                    S.op("dve", (lambda k=k: lambda e: e.tensor_scalar(
                        out=hd[:, k, :], in0=banks[bks[k // 4]][:, (k % 4) * 128:(k % 4 + 1) * 128],
                        scalar1=H1[haff][:, k:k + 1], scalar2=H0[haff][:, k:k + 1], op0=ALU.mult, op1=ALU.add))(),
                         reads=[PB[bks[k // 4]], BHa[haff]], writes=[bhd])

        def finalize(t, mode, haff=None, hdst=None, bhdst=None, route_l=None, src=None, bsrc=None):
            i = finalize_a(t, mode, src=src, bsrc=bsrc)
            if mode == "out":
                return
            finalize_b(t, i, haff, hdst=hdst, bhdst=bhdst)
            if route_l is not None:
                router_logits(route_l, t)

        def finalize_seq(mode, haff=None, route_l=None, hook=None):
            idx = {}
            idx[0] = finalize_a(0, mode)
            for t in range(NT):
                if hook is not None:
                    hook(t)
                if t + 1 < NT:
                    idx[t + 1] = finalize_a(t + 1, mode)
                if mode != "out":
                    finalize_b(t, idx[t], haff)
                    if route_l is not None and t >= 1:
                        router_logits(route_l, t - 1)
            if mode != "out" and route_l is not None:
                router_logits(route_l, NT - 1)

        def make_end_hook(mode, haff=None, route_l=None):
            st = {}

            def stage_b(t):
                if mode != "out":
                    finalize_b(t, st[t], haff)
                    if route_l is not None:
                        router_logits(route_l, t)

            def hook(t):
                st[t] = finalize_a(t, mode, part=1)
                if t >= 1:
                    finalize_a2(t - 1, mode, st[t - 1])
                if t >= 2:
                    stage_b(t - 2)

            def flush():
                finalize_a2(NT - 1, mode, st[NT - 1])
                stage_b(NT - 2)
                stage_b(NT - 1)
            return hook, flush

        def dump_and_end():
            for t in range(NT):
                dma("sp", "do", out_d[t * 128:(t + 1) * 128, :], x[:, t, :], reads=[BX[t]], writes=[Bout])
            S.wait_all("sp", [Bout])
            S.emit()

        esA = ExitStack()
        cur_es[0] = esA
        with esA:
            cosT = sb("cosT", [128, 17, 32]); sinT = sb("sinT", [128, 17, 32])
            Bcos, Bsin = Buf("cos"), Buf("sin")
            hTh = sb("hTh", [128, 8, 128], BF16); BhTh = Buf("hTh")
            bqkv = sb("bqkv", [1, 1536], BF16); Bbqkv = Buf("bqkv")
            esink = sb("esink", [128, 16]); Besink = Buf("esink")
            maskb = sb("maskb", [128, 3, 512], BF16); Bmask = Buf("maskb")
            Wqkv = W[:, 0:12288].rearrange("p (k n) -> p k n", k=8)
            Wo = W[:, 12288:20480].rearrange("p (k n) -> p k n", k=8)

            es0 = ExitStack()
            cur_es[0] = es0
            with es0:
                alloc_mod_scratch()
                posi = sb("posi", [128, 17], I32); posf = sb("posf", [128, 17]); invf = sb("invf", [128, 32])
                ang = sb("ang", [128, 17, 32]); ki = sb("ki", [128, 17, 32], I32)
                kf = W[:, 22528:22528 + 1088].bitcast(F32).rearrange("p (t f) -> p t f", t=17)
                Bpos, Binvf, Bang, Bkf, Bki = Buf("pos"), Buf("invf"), Buf("ang"), Buf("kf"), Buf("ki")
                xh = W[:, 20480:22528].bitcast(F32); Bxh = Buf("xh")

                make_haff(0, 0, 0, 1, None)
                compute_mod(0, 2, gtbm, Bgtbm, True)
                bc_load(A0, BA0, bo_d)
                S.op("dve", lambda e: e.tensor_tensor(out=A0[:], in0=A0[:], in1=gtbm[:], op=ALU.mult),
                     reads=[BA0, Bgtbm], writes=[BA0])
                dma("pool", "dw", Wqkv, wqkv_d.rearrange("(k p) n -> p k n", p=128), writes=[BW[0], BW[1]])
                dma("pool", "dw", Wo, wo_d.rearrange("(k p) n -> p k n", p=128), writes=[BW[2], BW[3]])
                dma("pool", "dw", bqkv[:], bqkv_d, writes=[Bbqkv])
                bc_load(esink, Besink, sinks_d)
                S.op("act", lambda e: e.activation(out=esink[:], in_=esink[:], func=AF.Exp), reads=[Besink], writes=[Besink])
                dma("pool", "dw", maskb[:], masks_d.rearrange("m p n -> p m n"), writes=[Bmask])
                dma("sp", "dc", posi[:], pos_d, writes=[Bpos])
                dma("sp", "dc", invf[:], invf_d, writes=[Binvf])
                S.op("dve", lambda e: e.tensor_copy(out=posf[:], in_=posi[:]), reads=[Bpos], writes=[Bpos])
                S.op("dve", lambda e: e.tensor_tensor(out=ang[:], in0=posf[:].unsqueeze(2).to_broadcast([128, 17, 32]),
                                                      in1=invf[:].unsqueeze(1).to_broadcast([128, 17, 32]), op=ALU.mult),
                     reads=[Bpos, Binvf], writes=[Bang])

                def sin_of(dst, bdst, shift):
                    if shift != 0.0:
                        S.op("dve", lambda e: e.tensor_scalar(out=dst[:], in0=ang[:], scalar1=shift, scalar2=None, op0=ALU.add),
                             reads=[Bang], writes=[bdst])
                        a_src, ba = dst, bdst
                    else:
                        a_src, ba = ang, Bang
                    S.op("dve", lambda e: e.tensor_scalar(out=ki[:], in0=a_src[:], scalar1=1.0 / TWO_PI, scalar2=None,
                                                          op0=ALU.mult), reads=[ba], writes=[Bki])
                    S.op("dve", lambda e: e.tensor_copy(out=kf, in_=ki[:]), reads=[Bki], writes=[Bkf])
                    S.op("dve", lambda e: e.scalar_tensor_tensor(out=dst[:], in0=kf, scalar=-C1, in1=a_src[:],
                                                                 op0=ALU.mult, op1=ALU.add), reads=[Bkf, ba], writes=[bdst])
                    S.op("dve", lambda e: e.scalar_tensor_tensor(out=dst[:], in0=kf, scalar=-C2, in1=dst[:],
                                                                 op0=ALU.mult, op1=ALU.add), reads=[Bkf, bdst], writes=[bdst])
                    S.op("dve", lambda e: e.tensor_scalar(out=dst[:], in0=dst[:], scalar1=3.1415925, scalar2=-3.1415925,
                                                          op0=ALU.min, op1=ALU.max), reads=[bdst], writes=[bdst])
                    S.op("act", lambda e: e.activation(out=dst[:], in_=dst[:], func=AF.Sin), reads=[bdst], writes=[bdst])

                sin_of(sinT, Bsin, 0.0)
                sin_of(cosT, Bcos, TWO_PI / 4)

                dma("sp", "dx", xh, xin[0:128, :], writes=[Bxh])
                for t in range(NT):
                    dma("sp", "dx", x[:, t, :], xin[(t + 1) * 128:(t + 2) * 128, :], writes=[BX[t]])
                finalize(0, "pro", haff=0, hdst=hTh, bhdst=BhTh, src=xh, bsrc=Bxh)
                def mods_b():
                    vecT, BvecT = MS["vecT"], MS["BvecT"]
                    yield from compute_mod_gen(0, 4, vecT, BvecT, True, lag=3)
                    to_fm(vecT, BvecT, fmt, Bfmt)
                    yield from compute_mod_gen(0, 3, vecT, BvecT, False, lag=3)
                    to_fm(vecT, BvecT, fmt2, Bfmt2)
                    haff_combine(1, 0)
                    yield from compute_mod_gen(0, 5, gtbf, Bgtbf, True, lag=2)

                job_b = mods_b()
                finalize_seq("pro", haff=0, hook=lambda t: next(job_b, None))
                for _ in job_b:
                    pass
                S.op("dve", lambda e: e.tensor_tensor(out=Wo, in0=Wo, in1=gtbm[:].unsqueeze(1).to_broadcast([128, 8, D]),
                                                      op=ALU.mult),
                     reads=[BW[2], BW[3], Bgtbm], writes=[BW[2], BW[3]])
                if stop_after == "pro":
                    dump_and_end()
                    return nc
                make_aset(0, None, None, None)
            S.barrier()

            es1 = ExitStack()
            cur_es[0] = es1
            bank_pool[0] = [3, 6, 7]
            with es1:
                qk32 = sb("qk32", [128, 20, 64]); Bqk32 = Buf("qk32")
                rot = sb("rot", [128, 20, 64], BF16); Brot = Buf("rot")
                rot2 = rot[:].rearrange("p h d -> p (h d)")
                tA = sb("tA", [128, 20, 32]); tB = sb("tB", [128, 20, 32])
                BtA, BtB = Buf("tA"), Buf("tB")
                kpad = sb("kpad", [128, 4, 2, 128], BF16); Bkpad = Buf("kpad")
                qT = sb("qT", [128, 8, 128], BF16); BqT = Buf("qT")
                Vaug = [sb(f"Vaug{i}", [128, 4, 65], BF16) for i in range(3)]
                BV = [Buf(f"V{i}") for i in range(3)]
                ao = sb("ao", [128, 16, 64], BF16); Bao = Buf("ao")
                ao2 = ao[:].rearrange("p h d -> p (h d)")
                aoT = sb("aoT", [128, 8, 128], BF16); BaoT = Buf("aoT")
                dent = sb("dent", [128, 16]); Bdent = Buf("dent")
                kT = [W[:, 20480 + i * 1024:20480 + (i + 1) * 1024].rearrange("p (v c) -> p v c", v=8) for i in range(2)]
                BkT = [Buf(f"kT{i}") for i in range(2)]
                PT = [W[:, 22528 + i * 1024:22528 + (i + 1) * 1024].rearrange("p (b c) -> p b c", b=2) for i in range(2)]
                BPT = [Buf(f"PT{i}") for i in range(2)]
                S.op("pool", lambda e: e.memset(kpad[:], 0.0), writes=[Bkpad])
                for i in range(3):
                    S.op("pool", (lambda i=i: lambda e: e.memset(Vaug[i][:], 1.0))(), writes=[BV[i]])

                def X1(t):
                    halo = t < 0
                    tt = t + 1
                    hsrc = hTh if halo else hT[:, :, t * 128:(t + 1) * 128]
                    bh = BhTh if halo else BH[t]
                    qb = []
                    for nb in ([2] if halo else [0, 1, 2]):
                        bk = nb
                        qb.append((nb, bk))
                        for k in range(8):
                            S.op("pe", (lambda k=k, nb=nb, bk=bk: lambda e: e.matmul(
                                banks[bk][:], hsrc[:, k, :], Wqkv[:, k, nb * 512:(nb + 1) * 512], start=(k == 0), stop=False))(),
                                 reads=[bh, BW[0], BW[1]], writes=[PB[bk]])
                        S.op("pe", (lambda nb=nb, bk=bk: lambda e: e.matmul(
                            banks[bk][:], ones_bf[:], bqkv[:, nb * 512:(nb + 1) * 512], start=False, stop=True))(),
                             reads=[Bones, Bbqkv], writes=[PB[bk]])
                    vcur = Vaug[tt % 3]; bvcur = BV[tt % 3]
                    for nb, bk in qb:
                        if nb < 2:
                            S.op("act", (lambda nb=nb, bk=bk: lambda e: e.activation(
                                out=qk32[:, nb * 8:(nb + 1) * 8, :], in_=banks[bk][:].rearrange("p (h d) -> p h d", d=64),
                                func=AF.Copy))(), reads=[PB[bk]], writes=[Bqk32])
                        else:
                            S.op("act", (lambda bk=bk: lambda e: e.activation(
                                out=qk32[:, 16:20, :], in_=banks[bk][:, 0:256].rearrange("p (h d) -> p h d", d=64),
                                func=AF.Copy))(), reads=[PB[bk]], writes=[Bqk32])
                            S.op("act", (lambda bk=bk: lambda e: e.activation(
                                out=vcur[:, :, 0:64], in_=banks[bk][:, 256:512].rearrange("p (h d) -> p h d", d=64),
                                func=AF.Copy))(), reads=[PB[bk]], writes=[bvcur])
                    h0 = 16 if halo else 0
                    nh = 20 - h0
                    x1 = qk32[:, h0:20, 0:32]; x2 = qk32[:, h0:20, 32:64]
                    cb = cosT[:, tt, :].unsqueeze(1).to_broadcast([128, nh, 32])
                    sbc = sinT[:, tt, :].unsqueeze(1).to_broadcast([128, nh, 32])
                    S.op("dve", lambda e: e.tensor_tensor(out=tA[:, h0:20, :], in0=x1, in1=cb, op=ALU.mult),
                         reads=[Bqk32, Bcos], writes=[BtA])
                    S.op("dve", lambda e: e.tensor_tensor(out=tB[:, h0:20, :], in0=x2, in1=sbc, op=ALU.mult),
                         reads=[Bqk32, Bsin], writes=[BtB])
                    if not halo:
                        S.op("dve", lambda e: e.tensor_tensor(out=rot[:, 0:16, 0:32], in0=tA[:, 0:16, :], in1=tB[:, 0:16, :],
                                                              op=ALU.subtract), reads=[BtA, BtB], writes=[Brot])
                    for half in range(2):
                        S.op("dve", (lambda half=half: lambda e: e.tensor_tensor(
                            out=kpad[:, :, half, half * 64:half * 64 + 32], in0=tA[:, 16:20, :], in1=tB[:, 16:20, :],
                            op=ALU.subtract))(), reads=[BtA, BtB], writes=[Bkpad])
                    S.op("dve", lambda e: e.tensor_tensor(out=tA[:, h0:20, :], in0=x2, in1=cb, op=ALU.mult),
                         reads=[Bqk32, Bcos], writes=[BtA])
                    S.op("dve", lambda e: e.tensor_tensor(out=tB[:, h0:20, :], in0=x1, in1=sbc, op=ALU.mult),
                         reads=[Bqk32, Bsin], writes=[BtB])
                    if not halo:
                        S.op("dve", lambda e: e.tensor_tensor(out=rot[:, 0:16, 32:64], in0=tA[:, 0:16, :], in1=tB[:, 0:16, :],
                                                              op=ALU.add), reads=[BtA, BtB], writes=[Brot])
                    for half in range(2):
                        S.op("dve", (lambda half=half: lambda e: e.tensor_tensor(
                            out=kpad[:, :, half, half * 64 + 32:half * 64 + 64], in0=tA[:, 16:20, :], in1=tB[:, 16:20, :],
                            op=ALU.add))(), reads=[BtA, BtB], writes=[Bkpad])

                tb = [3, 6]

                def X2(t):
                    halo = t < 0
                    tt = t + 1
                    cur = tt % 2
                    for v in range(8):
                        S.op("pe", (lambda v=v: lambda e: e.matmul(
                            banks[tb[v // 4]][:, (v % 4) * 128:(v % 4 + 1) * 128], kpad[:, v // 2, v % 2, :], ident[:],
                            start=True, stop=True))(),
                             reads=[Bkpad, Bident], writes=[PB[tb[v // 4]]])
                    for hb in range(2):
                        S.op("act", (lambda hb=hb: lambda e: e.activation(
                            out=kT[cur][:, hb * 4:(hb + 1) * 4, :], in_=banks[tb[hb]][:].rearrange("p (v c) -> p v c", v=4),
                            func=AF.Copy))(), reads=[PB[tb[hb]]], writes=[BkT[cur]])
                    if halo:
                        return
                    for j in range(8):
                        S.op("pe", (lambda j=j: lambda e: e.matmul(
                            banks[tb[j // 4]][:, (j % 4) * 128:(j % 4 + 1) * 128], rot2[:, j * 128:(j + 1) * 128], ident[:],
                            start=True, stop=True))(),
                             reads=[Brot, Bident], writes=[PB[tb[j // 4]]])
                    for hb in range(2):
                        S.op("dve", (lambda hb=hb: lambda e: e.tensor_copy(
                            out=qT[:, hb * 4:(hb + 1) * 4, :], in_=banks[tb[hb]][:].rearrange("p (v c) -> p v c", v=4)))(),
                             reads=[PB[tb[hb]]], writes=[BqT])

                def Y1(t):
                    tt = t + 1
                    cur = tt % 2
                    prv = 1 - cur
                    vcur, bvcur = Vaug[tt % 3], BV[tt % 3]
                    vprv, bvprv = Vaug[(tt - 1) % 3], BV[(tt - 1) % 3]
                    ob = [0, 1, 2]
                    sc_i = [0]
                    first_tile = (t == 0)

                    def scores(g):
                        pi = g % 2
                        for blk, (ktile, bkt, mi) in enumerate([(kT[prv], BkT[prv], 2 if first_tile else 1),
                                                                (kT[cur], BkT[cur], 0)]):
                            bk = 4 + (sc_i[0] % 2)
                            sc_i[0] += 1
                            S.op("pe", (lambda bk=bk, mi=mi: lambda e: e.matmul(
                                banks[bk][:], ident[:], maskb[:, mi, :], start=True, stop=False))(),
                                 reads=[Bident, Bmask], writes=[PB[bk]])
                            for i in range(4):
                                h = 4 * g + i
                                S.op("pe", (lambda bk=bk, i=i, h=h, ktile=ktile, g=g: lambda e: e.matmul(
                                    banks[bk][:, i * 128:(i + 1) * 128], ktile[:, g * 2 + (h % 2), :], qT[:, h // 2, :],
                                    start=False, stop=(i == 3)))(),
                                     reads=[bkt, BqT], writes=[PB[bk]])
                            S.op("act", (lambda bk=bk, blk=blk, pi=pi: lambda e: e.activation(
                                out=PT[pi][:, blk, :], in_=banks[bk][:], func=AF.Exp, scale=0.125))(),
                                 reads=[PB[bk]], writes=[BPT[pi]])

                    def pv(g):
                        pi = g % 2
                        for i in range(4):
                            h = 4 * g + i
                            obk = ob[h // 7]
                            oc = (h % 7) * 65
                            for blk, (vt, bvt) in enumerate([(vprv, bvprv), (vcur, bvcur)]):
                                S.op("pe", (lambda obk=obk, oc=oc, blk=blk, pi=pi, i=i, vt=vt, g=g: lambda e: e.matmul(
                                    banks[obk][:, oc:oc + 65], PT[pi][:, blk, i * 128:(i + 1) * 128], vt[:, g, :],
                                    start=(blk == 0), stop=(blk == 1)))(),
                                     reads=[BPT[pi], bvt], writes=[PB[obk]])

                    scores(0)
                    for g in range(4):
                        if g + 1 < 4:
                            scores(g + 1)
                        pv(g)
                    for b3 in range(3):
                        hs = 7 * b3
                        n = min(7, 16 - hs)
                        ov = banks[ob[b3]][:, 0:n * 65].rearrange("p (h d) -> p h d", d=65)
                        S.op("dve", (lambda ov=ov, hs=hs, n=n: lambda e: e.tensor_tensor(
                            out=dent[:, hs:hs + n], in0=ov[:, :, 64], in1=esink[:, hs:hs + n], op=ALU.add))(),
                             reads=[PB[ob[b3]], Besink], writes=[Bdent])
                        S.op("dve", (lambda hs=hs, n=n: lambda e: e.reciprocal(out=dent[:, hs:hs + n], in_=dent[:, hs:hs + n]))(),
                             reads=[Bdent], writes=[Bdent])
                        S.op("dve", (lambda ov=ov, hs=hs, n=n: lambda e: e.tensor_tensor(
                            out=ao[:, hs:hs + n, :], in0=ov[:, :, 0:64],
                            in1=dent[:, hs:hs + n].unsqueeze(2).to_broadcast([128, n, 64]), op=ALU.mult))(),
                             reads=[PB[ob[b3]], Bdent], writes=[Bao])

                def Y2a(t):
                    for j in range(8):
                        S.op("pe", (lambda j=j: lambda e: e.matmul(
                            banks[tb[j // 4]][:, (j % 4) * 128:(j % 4 + 1) * 128], ao2[:, j * 128:(j + 1) * 128], ident[:],
                            start=True, stop=True))(),
                             reads=[Bao, Bident], writes=[PB[tb[j // 4]]])
                    for hb in range(2):
                        S.op("act", (lambda hb=hb: lambda e: e.activation(
                            out=aoT[:, hb * 4:(hb + 1) * 4, :], in_=banks[tb[hb]][:].rearrange("p (v c) -> p v c", v=4),
                            func=AF.Copy))(), reads=[PB[tb[hb]]], writes=[BaoT])

                def Y2b(t):
                    for nb in range(2):
                        bk = 4 + nb
                        for k in range(8):
                            S.op("pe", (lambda k=k, nb=nb, bk=bk: lambda e: e.matmul(
                                banks[bk][:], aoT[:, k, :], Wo[:, k, nb * 512:(nb + 1) * 512], start=(k == 0), stop=(k == 7)))(),
                                 reads=[BaoT, BW[2], BW[3]], writes=[PB[bk]])
                        S.op("dve", (lambda nb=nb, bk=bk: lambda e: e.tensor_tensor(
                            out=x[:, t, nb * 512:(nb + 1) * 512], in0=x[:, t, nb * 512:(nb + 1) * 512], in1=banks[bk][:],
                            op=ALU.add))(), reads=[BX[t], PB[bk]], writes=[BX[t]])

                X1(-1); X2(-1)
                X1(0); X2(0)
                fi = {}
                for t in range(NT + 3):
                    if 0 <= t - 2 < NT:
                        fi[t - 2] = finalize_a(t - 2, "mid")
                    if t + 1 < NT:
                        X1(t + 1)
                    if 0 <= t - 1 < NT:
                        Y2a(t - 1)
                    if t < NT:
                        Y1(t)
                    if 0 <= t - 1 < NT:
                        Y2b(t - 1)
                    if 0 <= t - 2 < NT:
                        finalize_b(t - 2, fi[t - 2], 1)
                    if t + 1 < NT:
                        X2(t + 1)
                    if 0 <= t - 3 < NT:
                        router_logits(0, t - 3)
                if stop_after == "attn":
                    dump_and_end()
                    return nc
            S.barrier()
        cur_es[0] = es

        def bc3(ap2, n):
            return ap2.unsqueeze(2).to_broadcast([128, NT, n])

        def moe_views(s):
            base = s * 6144
            wgu = W[:, base:base + 4096].rearrange("p (k n) -> p k n", k=8)
            wdn = W[:, base + 4096:base + 6144].rearrange("p (j n) -> p j n", j=2)
            return wgu, wdn

        def moe_load(l, ex):
            s = ex % 4
            wgu, wdn = moe_views(s)
            dma("pool", "dw", wgu, wgu_d[l, ex].rearrange("(k p) n -> p k n", p=128), writes=[BW[s]])
            dma("pool", "dw", wdn, wdn_d[l, ex].rearrange("(j p) n -> p j n", p=128), writes=[BW[s]])
            S.op("pool", lambda e: e.tensor_tensor(out=wdn, in0=wdn, in1=gtbf[:].unsqueeze(1).to_broadcast([128, 2, D]),
                                                   op=ALU.mult), reads=[BW[s], Bgtbf], writes=[BW[s]])

        def moe_phase(l, after_chunk=None, nchunks=16, tile_hook=None, first_load=0, end_hook=None):
            gl_m = sb("gl_m", [128, NT]); Bglm = Buf("gl_m")
            goh = sb("goh", [128, NT, 4]); Bgoh = Buf("goh")
            gex = sb("gex", [128, NT, 4]); Bgex = Buf("gex")
            gp = sb("gp", [128, NT]); Bgp = Buf("gp")
            sel4 = sb("sel4", [128, NT, 32]); Bsel4 = Buf("sel4")
            sel = sb("sel", [128, NT, 8]); Bsel = Buf("sel")
            sel2 = sb("sel2", [128, NT, 8]); Bsel2 = Buf("sel2")
            oh1 = sb("oh1", [128, NT, 8]); Boh1 = Buf("oh1")
            oh2 = sb("oh2", [128, NT, 8]); Boh2 = Buf("oh2")
            m1 = sb("m1", [128, NT]); m2 = sb("m2", [128, NT]); Bm1, Bm2 = Buf("m1"), Buf("m2")
            w1 = sb("w1", [128, NT]); w2 = sb("w2", [128, NT]); Bw1, Bw2 = Buf("w1"), Buf("w2")
            sg = [sb(f"sg{i}", [128, 256]) for i in range(2)]
            Bsg = [Buf(f"sg{i}") for i in range(2)]
            actb = [sb(f"actb{i}", [128, 256], BF16) for i in range(2)]
            Bact = [Buf(f"act{i}") for i in range(2)]
            actT = [sb(f"actT{i}", [128, 2, 128], BF16) for i in range(3)]
            BactT = [Buf(f"actT{i}") for i in range(3)]

            for ex in range(first_load, 4):
                moe_load(l, ex)

            gl = lg[:, :, 0:4]
            S.op("dve", lambda e: e.tensor_reduce(out=gl_m[:], in_=gl, axis=AX.X, op=ALU.max), reads=[Blg], writes=[Bglm])
            S.op("dve", lambda e: e.tensor_tensor(out=goh[:], in0=gl, in1=bc3(gl_m[:], 4), op=ALU.is_equal),
                 reads=[Blg, Bglm], writes=[Bgoh])
            S.op("dve", lambda e: e.tensor_tensor(out=gex[:], in0=gl, in1=bc3(gl_m[:], 4), op=ALU.subtract),
                 reads=[Blg, Bglm], writes=[Bgex])
            S.op("act", lambda e: e.activation(out=gex[:], in_=gex[:], func=AF.Exp), reads=[Bgex], writes=[Bgex])
            S.op("dve", lambda e: e.tensor_reduce(out=gp[:], in_=gex[:], axis=AX.X, op=ALU.add), reads=[Bgex], writes=[Bgp])
            S.op("dve", lambda e: e.reciprocal(out=gp[:], in_=gp[:]), reads=[Bgp], writes=[Bgp])
            S.op("dve", lambda e: e.tensor_tensor(
                out=sel4[:].rearrange("p t (g e) -> p t g e", g=4), in0=lg[:, :, 4:36].rearrange("p t (g e) -> p t g e", g=4),
                in1=goh[:].unsqueeze(3).to_broadcast([128, NT, 4, 8]), op=ALU.mult),
                 reads=[Blg, Bgoh], writes=[Bsel4])
            S.op("dve", lambda e: e.tensor_reduce(out=sel[:], in_=sel4[:].rearrange("p t (g e) -> p t e g", g=4),
                                                  axis=AX.X, op=ALU.add), reads=[Bsel4], writes=[Bsel])
            S.op("dve", lambda e: e.tensor_reduce(out=m1[:], in_=sel[:], axis=AX.X, op=ALU.max), reads=[Bsel], writes=[Bm1])
            S.op("dve", lambda e: e.tensor_tensor(out=oh1[:], in0=sel[:], in1=bc3(m1[:], 8), op=ALU.is_equal),
                 reads=[Bsel, Bm1], writes=[Boh1])
            S.op("dve", lambda e: e.scalar_tensor_tensor(out=sel2[:], in0=oh1[:], scalar=-1e30, in1=sel[:],
                                                         op0=ALU.mult, op1=ALU.add), reads=[Boh1, Bsel], writes=[Bsel2])
            S.op("dve", lambda e: e.tensor_reduce(out=m2[:], in_=sel2[:], axis=AX.X, op=ALU.max), reads=[Bsel2], writes=[Bm2])
            S.op("dve", lambda e: e.tensor_tensor(out=oh2[:], in0=sel2[:], in1=bc3(m2[:], 8), op=ALU.is_equal),
                 reads=[Bsel2, Bm2], writes=[Boh2])
            S.op("dve", lambda e: e.tensor_tensor(out=w2[:], in0=m2[:], in1=m1[:], op=ALU.subtract),
                 reads=[Bm1, Bm2], writes=[Bw2])
            S.op("act", lambda e: e.activation(out=w2[:], in_=w2[:], func=AF.Exp), reads=[Bw2], writes=[Bw2])
            S.op("dve", lambda e: e.tensor_scalar(out=w2[:], in0=w2[:], scalar1=1.0, scalar2=None, op0=ALU.add),
                 reads=[Bw2], writes=[Bw2])
            S.op("dve", lambda e: e.reciprocal(out=w1[:], in_=w2[:]), reads=[Bw2], writes=[Bw1])
            S.op("dve", lambda e: e.tensor_tensor(out=w1[:], in0=w1[:], in1=gp[:], op=ALU.mult), reads=[Bw1, Bgp], writes=[Bw1])
            S.op("dve", lambda e: e.tensor_tensor(out=w2[:], in0=gp[:], in1=w1[:], op=ALU.subtract),
                 reads=[Bgp, Bw1], writes=[Bw2])
            S.op("dve", lambda e: e.tensor_tensor(out=oh1[:], in0=oh1[:], in1=bc3(w1[:], 8), op=ALU.mult),
                 reads=[Boh1, Bw1], writes=[Boh1])
            S.op("dve", lambda e: e.tensor_tensor(out=oh2[:], in0=oh2[:], in1=bc3(w2[:], 8), op=ALU.mult),
                 reads=[Boh2, Bw2], writes=[Boh2])
            S.op("dve", lambda e: e.tensor_tensor(out=oh1[:], in0=oh1[:], in1=oh2[:], op=ALU.add),
                 reads=[Boh1, Boh2], writes=[Boh1])
            S.op("dve", lambda e: e.tensor_tensor(
                out=comb[:].rearrange("p t (g e) -> p t g e", g=4),
                in0=goh[:].unsqueeze(3).to_broadcast([128, NT, 4, 8]),
                in1=oh1[:].unsqueeze(2).to_broadcast([128, NT, 4, 8]), op=ALU.mult),
                 reads=[Bgoh, Boh1], writes=[Bcomb])

            GUB = [0, 1]; TB = [2, 3]; YB = [[4, 5], [6, 7]]
            steps = []
            for c in range(nchunks):
                for t in range(NT):
                    for e2 in range(2):
                        steps.append((c, t, e2))
            n = len(steps)

            def GU(i):
                c, t, e2 = steps[i]
                s = (2 * c + e2) % 4
                wgu, _ = moe_views(s)
                bk = GUB[i % 2]
                for k in range(8):
                    S.op("pe", (lambda k=k: lambda e: e.matmul(
                        banks[bk][:], hT[:, k, t * 128:(t + 1) * 128], wgu[:, k, :], start=(k == 0), stop=(k == 7)))(),
                         reads=[BH[t], BW[s]], writes=[PB[bk]])
                si = i % 2
                S.op("act", lambda e: e.activation(out=sg[si][:], in_=banks[bk][:, 0:256], func=AF.Silu),
                     reads=[PB[bk]], writes=[Bsg[si]])
                ex = 2 * c + e2
                S.op("dve", lambda e: e.scalar_tensor_tensor(
                    out=actb[si][:], in0=sg[si][:], scalar=comb[:, t, ex:ex + 1], in1=banks[bk][:, 256:512],
                    op0=ALU.mult, op1=ALU.mult), reads=[Bsg[si], Bcomb, PB[bk]], writes=[Bact[si]])

            def TR(i):
                si = i % 2
                bk = TB[i % 2]
                ti = i % 3
                for j in range(2):
                    S.op("pe", (lambda j=j: lambda e: e.matmul(
                        banks[bk][:, j * 128:(j + 1) * 128], actb[si][:, j * 128:(j + 1) * 128], ident[:],
                        start=True, stop=True))(),
                         reads=[Bact[si], Bident], writes=[PB[bk]])
                S.op("act", lambda e: e.activation(out=actT[ti][:], in_=banks[bk][:, 0:256].rearrange("p (j c) -> p j c", j=2),
                                                   func=AF.Copy), reads=[PB[bk]], writes=[BactT[ti]])

            def DN(i):
                c, t, e2 = steps[i]
                s = (2 * c + e2) % 4
                _, wdn = moe_views(s)
                ti = i % 3
                yb = YB[t % 2]
                for nb in range(2):
                    for j in range(2):
                        S.op("pe", (lambda nb=nb, j=j: lambda e: e.matmul(
                            banks[yb[nb]][:], actT[ti][:, j, :], wdn[:, j, nb * 512:(nb + 1) * 512],
                            start=(e2 == 0 and j == 0), stop=(e2 == 1 and j == 1)))(),
                             reads=[BactT[ti], BW[s]], writes=[PB[yb[nb]]])
                if e2 == 1:
                    for nb in range(2):
                        S.op("dve", (lambda nb=nb: lambda e: e.tensor_tensor(
                            out=x[:, t, nb * 512:(nb + 1) * 512], in0=x[:, t, nb * 512:(nb + 1) * 512],
                            in1=banks[yb[nb]][:], op=ALU.add))(), reads=[BX[t], PB[yb[nb]]], writes=[BX[t]])
                    if tile_hook is not None:
                        tile_hook(c, t)
                    if end_hook is not None and c == nchunks - 1:
                        end_hook(t)
                    if t == NT - 1:
                        if c + 2 < 16 and c + 2 < nchunks:
                            moe_load(l, 2 * (c + 2)); moe_load(l, 2 * (c + 2) + 1)
                        if after_chunk is not None:
                            after_chunk(c)

            for i in range(n + 2):
                if i < n:
                    GU(i)
                if 1 <= i <= n:
                    TR(i - 1)
                if 2 <= i <= n + 1:
                    DN(i - 2)

        def mod_jobs_moe0():
            vecT, BvecT = MS["vecT"], MS["BvecT"]
            yield from compute_mod_gen(1, 1, vecT, BvecT, True, lag=12)
            to_fm(vecT, BvecT, fmt, Bfmt)
            yield from compute_mod_gen(1, 0, vecT, BvecT, False, lag=12)
            to_fm(vecT, BvecT, fmt2, Bfmt2)
            haff_combine(2, 1)
            yield from compute_mod_gen(1, 2, gtbm, Bgtbm, True, lag=12)
            make_aset(1, bout_d, gtbm, Bgtbm)
            yield from compute_mod_gen(1, 4, vecT, BvecT, True, lag=12)
            to_fm(vecT, BvecT, fmt, Bfmt)
            yield from compute_mod_gen(1, 3, vecT, BvecT, False, lag=12)
            to_fm(vecT, BvecT, fmt2, Bfmt2)
            haff_combine(3, 2)
            yield from compute_mod_gen(1, 5, MS["gtmp"], MS["Bgtmp"], True, lag=12)

        moe0_job = [None]

        def tile_hook_moe0(c, t):
            if c < 1:
                return
            if moe0_job[0] is None:
                moe0_job[0] = mod_jobs_moe0()
            next(moe0_job[0], None)

        es2 = ExitStack()
        cur_es[0] = es2
        bank_pool[0] = [0, 1, 2, 3]
        with es2:
            alloc_mod_scratch()
            MS["gtmp"] = sb("gtmp", [128, D]); MS["Bgtmp"] = Buf("gtmp")
            eh0, ef0 = make_end_hook("mid", haff=2)
            moe_phase(0, None, nchunks=(NCHUNK_DBG or 16), tile_hook=tile_hook_moe0, end_hook=(eh0 if USE_END_HOOK else None))
            for _ in (moe0_job[0] or ()):
                pass
            if USE_END_HOOK:
                ef0()
            else:
                finalize_seq("mid", haff=2)
            dma("pool", "dw", W[:, 4096:8192].rearrange("p (k n) -> p k n", k=8),
                win_d[:, 2048:2560].rearrange("(k p) n -> p k n", p=128), writes=[BW[0], BW[1]])
            if stop_after == "moe0":
                dump_and_end()
                return nc
            S.op("pool", lambda e: e.tensor_copy(out=gtbf[:], in_=MS["gtmp"][:]), reads=[MS["Bgtmp"]], writes=[Bgtbf])
            make_aset(2, None, None, None)
        S.barrier()
        cur_es[0] = es

        es3 = ExitStack()
        cur_es[0] = es3
        bank_pool[0] = list(range(8))
        with es3:
            gst = sb("gst", [128, NT, 4, 6]); Bgst = Buf("gst")
            mvg = sb("mvg", [128, NT, 2]); Bmvg = Buf("mvg")
            sdg = sb("sdg", [128, NT]); rstdg = sb("rstdg", [128, NT]); nmrg = sb("nmrg", [128, NT])
            Bsdg, Brstdg, Bnmrg = Buf("sdg"), Buf("rstdg"), Buf("nmrg")
            u32 = [sb(f"u32_{i}", [128, 512]) for i in range(2)]; Bu32 = [Buf(f"u32_{i}") for i in range(2)]
            v32 = [sb(f"v32_{i}", [128, 512]) for i in range(2)]; Bv32 = [Buf(f"v32_{i}") for i in range(2)]
            vln = [sb(f"vln{i}", [128, 512], BF16) for i in range(2)]; Bvln = [Buf(f"vln{i}") for i in range(2)]
            gated = [sb(f"gated{i}", [128, 512], BF16) for i in range(2)]; Bgated = [Buf(f"gated{i}") for i in range(2)]
            gatedT = [sb(f"gatedT{i}", [128, 4, 128], BF16) for i in range(3)]; BgatedT = [Buf(f"gatedT{i}") for i in range(3)]
            lngb = [sb(f"lngb{i}", [128, 2, 512]) for i in range(2)]; Blngb = [Buf(f"lngb{i}") for i in range(2)]
            binr = [sb(f"binr{i}", [1, 2, 512], BF16) for i in range(2)]; Bbinr = [Buf(f"binr{i}") for i in range(2)]
            wsTm = sb("wsTm", [128, 8, 128], BF16); BwsT = Buf("wsTm")
            trilb = sb("trilb", [128, 128], BF16); Btril = Buf("tril")
            bsT = sb("bsT", [128, 8]); BbsT = Buf("bsT")
            dma("pool", "dw", wsTm[:], wsT_d.rearrange("g s t -> s g t"), writes=[BwsT])
            dma("pool", "dw", trilb[:], tril_d, writes=[Btril])
            dma("sp", "dc", bsT[:], bsT_d, writes=[BbsT])
            S.op("dve", lambda e: e.tensor_tensor(out=wsTm[:], in0=wsTm[:], in1=trilb[:].unsqueeze(1).to_broadcast([128, 8, 128]),
                                                  op=ALU.mult), reads=[BwsT, Btril], writes=[BwsT])

            def gviews(b):
                base = b * 12288
                wu = W[:, base:base + 4096].rearrange("p (k n) -> p k n", k=8)
                wv = W[:, base + 4096:base + 8192].rearrange("p (k n) -> p k n", k=8)
                wo2 = W[:, base + 8192:base + 12288].rearrange("p (j n) -> p j n", j=4)
                return wu, wv, wo2

            def gload(cb, b, main, skip_wv=False):
                wu, wv, wo2 = gviews(b)
                bw = [BW[2 * b], BW[2 * b + 1]]
                if not skip_wv:
                    dma("pool", "dw", wv, win_d[:, 2048 + cb * 512:2048 + (cb + 1) * 512].rearrange("(k p) n -> p k n", p=128),
                        writes=bw)
                dma("pool", "dw", binr[b][:, 1, :], bin_d[:, 2048 + cb * 512:2048 + (cb + 1) * 512], writes=[Bbinr[b]])
                if main:
                    dma("pool", "dw", wu, win_d[:, cb * 512:(cb + 1) * 512].rearrange("(k p) n -> p k n", p=128), writes=bw)
                    dma("pool", "dw", binr[b][:, 0, :], bin_d[:, cb * 512:(cb + 1) * 512], writes=[Bbinr[b]])
                    dma("pool", "dw", wo2, wout_d[cb * 512:(cb + 1) * 512, :].rearrange("(j p) n -> p j n", p=128), writes=bw)
                    S.op("pool", lambda e: e.tensor_tensor(out=wo2, in0=wo2,
                                                           in1=gtbm[:].unsqueeze(1).to_broadcast([128, 4, D]), op=ALU.mult),
                         reads=bw + [Bgtbm], writes=bw)
                    dma("sp", "dc", lngb[b][:, 0, :], glng_d[:, cb * 512:(cb + 1) * 512].to_broadcast([128, 512]),
                        writes=[Blngb[b]])
                    dma("sp", "dc", lngb[b][:, 1, :], glnb_d[:, cb * 512:(cb + 1) * 512].to_broadcast([128, 512]),
                        writes=[Blngb[b]])

            gload(0, 0, False, skip_wv=True)
            pi = 0
            for cb in range(4):
                b = cb % 2
                if cb + 1 < 4:
                    gload(cb + 1, (cb + 1) % 2, False)
                _, wv, _ = gviews(b)
                bw = [BW[2 * b], BW[2 * b + 1]]
                for t in range(NT):
                    bk = next_bank()
                    i2 = pi % 2
                    pi += 1
                    for k in range(8):
                        S.op("pe", (lambda k=k, bk=bk, t=t, wv=wv: lambda e: e.matmul(
                            banks[bk][:], hT[:, k, t * 128:(t + 1) * 128], wv[:, k, :], start=(k == 0), stop=False))(),
                             reads=[BH[t]] + bw, writes=[PB[bk]])
                    S.op("pe", (lambda bk=bk, b=b: lambda e: e.matmul(banks[bk][:], ones_bf[:], binr[b][:, 1, :],
                                                                      start=False, stop=True))(),
                         reads=[Bones, Bbinr[b]], writes=[PB[bk]])
                    S.op("act", (lambda bk=bk, i2=i2: lambda e: e.activation(out=v32[i2][:], in_=banks[bk][:], func=AF.Gelu))(),
                         reads=[PB[bk]], writes=[Bv32[i2]])
                    S.op("dve", (lambda i2=i2, t=t, cb=cb: lambda e: e.bn_stats(out=gst[:, t, cb, :], in_=v32[i2][:]))(),
                         reads=[Bv32[i2]], writes=[Bgst])
            for t in range(NT):
                S.op("dve", (lambda t=t: lambda e: e.bn_aggr(out=mvg[:, t, :], in_=gst[:, t, :, :]))(),
                     reads=[Bgst], writes=[Bmvg])
            S.op("act", lambda e: e.activation(out=sdg[:], in_=mvg[:, :, 1], func=AF.Ln, bias=eps_t[:]),
                 reads=[Bmvg, Beps], writes=[Bsdg])
            S.op("act", lambda e: e.activation(out=rstdg[:], in_=sdg[:], func=AF.Exp, scale=-0.5),
                 reads=[Bsdg], writes=[Brstdg])
            S.op("dve", lambda e: e.scalar_tensor_tensor(out=nmrg[:], in0=mvg[:, :, 0], scalar=-1.0, in1=rstdg[:],
                                                         op0=ALU.mult, op1=ALU.mult), reads=[Bmvg, Brstdg], writes=[Bnmrg])

            gsteps = [(cb, t) for cb in range(4) for t in range(NT)]
            ng = len(gsteps)

            def gA(i):
                cb, t = gsteps[i]
                b = cb % 2
                if t == 3 and cb + 1 < 4:
                    gload(cb + 1, (cb + 1) % 2, True)
                wu, wv, _ = gviews(b)
                bw = [BW[2 * b], BW[2 * b + 1]]
                i2 = i % 2
                for which, wmat, dst, bdst in ((0, wu, u32[i2], Bu32[i2]), (1, wv, v32[i2], Bv32[i2])):
                    bk = next_bank()
                    for k in range(8):
                        S.op("pe", (lambda k=k, bk=bk, wmat=wmat: lambda e: e.matmul(
                            banks[bk][:], hT[:, k, t * 128:(t + 1) * 128], wmat[:, k, :], start=(k == 0), stop=False))(),
                             reads=[BH[t]] + bw, writes=[PB[bk]])
                    S.op("pe", (lambda bk=bk, which=which: lambda e: e.matmul(banks[bk][:], ones_bf[:], binr[b][:, which, :],
                                                                              start=False, stop=True))(),
                         reads=[Bones, Bbinr[b]], writes=[PB[bk]])
                    S.op("act", (lambda bk=bk, dst=dst: lambda e: e.activation(out=dst[:], in_=banks[bk][:], func=AF.Gelu))(),
                         reads=[PB[bk]], writes=[bdst])
                S.op("dve", lambda e: e.tensor_scalar(out=v32[i2][:], in0=v32[i2][:], scalar1=rstdg[:, t:t + 1],
                                                      scalar2=nmrg[:, t:t + 1], op0=ALU.mult, op1=ALU.add),
                     reads=[Bv32[i2], Brstdg, Bnmrg], writes=[Bv32[i2]])
                S.op("pool", lambda e: e.tensor_tensor(out=v32[i2][:], in0=v32[i2][:], in1=lngb[b][:, 0, :], op=ALU.mult),
                     reads=[Bv32[i2], Blngb[b]], writes=[Bv32[i2]])
                S.op("pool", lambda e: e.tensor_tensor(out=vln[i2][:], in0=v32[i2][:], in1=lngb[b][:, 1, :], op=ALU.add),
                     reads=[Bv32[i2], Blngb[b]], writes=[Bvln[i2]])

            def gB(i):
                cb, t = gsteps[i]
                i2 = i % 2
                bk = next_bank()
                for gi in range(2):
                    g = 2 * cb + gi
                    S.op("pe", (lambda gi=gi, g=g: lambda e: e.matmul(
                        banks[bk][:, gi * 256:(gi + 1) * 256], wsTm[:, g, :], vln[i2][:, gi * 256:(gi + 1) * 256],
                        start=True, stop=True))(), reads=[BwsT, Bvln[i2]], writes=[PB[bk]])
                for gi in range(2):
                    g = 2 * cb + gi
                    S.op("dve", (lambda gi=gi, g=g: lambda e: e.scalar_tensor_tensor(
                        out=gated[i2][:, gi * 256:(gi + 1) * 256], in0=banks[bk][:, gi * 256:(gi + 1) * 256],
                        scalar=bsT[:, g:g + 1], in1=u32[i2][:, gi * 256:(gi + 1) * 256], op0=ALU.add, op1=ALU.mult))(),
                         reads=[PB[bk], BbsT, Bu32[i2]], writes=[Bgated[i2]])

            def gC(i):
                i2 = i % 2
                bt = next_bank()
                i3 = i % 3
                for j in range(4):
                    S.op("pe", (lambda j=j: lambda e: e.matmul(
                        banks[bt][:, j * 128:(j + 1) * 128], gated[i2][:, j * 128:(j + 1) * 128], ident[:],
                        start=True, stop=True))(),
                         reads=[Bgated[i2], Bident], writes=[PB[bt]])
                S.op("act", lambda e: e.activation(out=gatedT[i3][:], in_=banks[bt][:].rearrange("p (j c) -> p j c", j=4),
                                                   func=AF.Copy), reads=[PB[bt]], writes=[BgatedT[i3]])

            def gD(i):
                cb, t = gsteps[i]
                b = cb % 2
                _, _, wo2 = gviews(b)
                bw = [BW[2 * b], BW[2 * b + 1]]
                i3 = i % 3
                for nb in range(2):
                    bk = next_bank()
                    for j in range(4):
                        S.op("pe", (lambda j=j, nb=nb, bk=bk: lambda e: e.matmul(
                            banks[bk][:], gatedT[i3][:, j, :], wo2[:, j, nb * 512:(nb + 1) * 512],
                            start=(j == 0), stop=(j == 3)))(), reads=[BgatedT[i3]] + bw, writes=[PB[bk]])
                    S.op("dve", (lambda nb=nb, bk=bk: lambda e: e.tensor_tensor(
                        out=x[:, t, nb * 512:(nb + 1) * 512], in0=x[:, t, nb * 512:(nb + 1) * 512], in1=banks[bk][:],
                        op=ALU.add))(), reads=[BX[t], PB[bk]], writes=[BX[t]])
                if cb == 3:
                    if t == 3:
                        moe_load(1, 0); moe_load(1, 1)
                    if USE_END_HOOK:
                        ehg(t)

            ehg, efg = make_end_hook("mid", haff=3, route_l=1)
            gload(0, 0, True)
            for i in range(ng + 3):
                if i < ng:
                    gA(i)
                if 1 <= i <= ng:
                    gB(i - 1)
                if 2 <= i <= ng + 1:
                    gC(i - 2)
                if 3 <= i <= ng + 2:
                    gD(i - 3)
            if USE_END_HOOK:
                efg()
            else:
                finalize_seq("mid", haff=3, route_l=1)
            if stop_after == "gmlp":
                dump_and_end()
                return nc
        S.barrier()
        cur_es[0] = es

        es4 = ExitStack()
        cur_es[0] = es4
        bank_pool[0] = [0, 1, 2, 3]
        with es4:
            make_aset(3, None, None, None, scale=1.0)
            eh1, ef1 = make_end_hook("out")
            moe_phase(1, None, nchunks=(NCHUNK_DBG or 16), first_load=2, end_hook=(eh1 if USE_END_HOOK else None))
            if USE_END_HOOK:
                ef1()
            else:
                finalize_seq("out")
            S.wait_all("sp", [Bout])
            S.emit()
    return nc


NCHUNK_DBG = None
USE_END_HOOK = False
ATT_DBG = [NT, None]


_CACHE = {}


def _host_inputs(inputs):
    f32 = np.float32
    x = np.asarray(inputs["x"], f32)
    c = np.asarray(inputs["c"], f32)
    pos = np.asarray(inputs["positions"], np.int32)
    shared = {}
    shared["ident"] = np.eye(128, dtype=f32)
    s = np.arange(128)[:, None]
    q = np.arange(128)[None, :]
    m_cur = np.where(s <= q, 0.0, NEG).astype(f32)
    m_prev = np.where(s > q, 0.0, NEG).astype(f32)
    m_none = np.full((128, 128), NEG, f32)
    inv_freq = (10000.0 ** (-np.arange(0, 64, 2, dtype=f32) / f32(64))).astype(f32)
    shared["invf"] = np.ascontiguousarray(np.broadcast_to(inv_freq[None, :], (128, 32))).astype(f32)
    shared["tril"] = (s <= q).astype(f32)
    shared["ada_w"] = np.ascontiguousarray(inputs["ada_w"], f32)
    shared["ada_b"] = np.ascontiguousarray(inputs["ada_b"], f32)
    g = np.asarray(inputs["post_ln_g"], f32).reshape(4, D)
    b = np.asarray(inputs["post_ln_b"], f32).reshape(4, D)
    shared["post_ln_g"] = np.ascontiguousarray(g)
    shared["post_ln_b"] = np.ascontiguousarray(b)
    shared["post_ln_gT"] = np.ascontiguousarray(g.reshape(4, 8, 128).transpose(0, 2, 1))
    shared["post_ln_bT"] = np.ascontiguousarray(b.reshape(4, 8, 128).transpose(0, 2, 1))
    shared["attn_w_qkv"] = np.ascontiguousarray(inputs["attn_w_qkv"][0], f32)
    shared["attn_b_qkv"] = np.ascontiguousarray(inputs["attn_b_qkv"], f32).reshape(1, 1536)
    shared["attn_sinks"] = np.ascontiguousarray(inputs["attn_sinks"], f32).reshape(1, 16)
    shared["attn_w_o"] = np.ascontiguousarray(inputs["attn_w_o"][0], f32)
    shared["attn_b_o"] = np.ascontiguousarray(inputs["attn_b_o"], f32).reshape(1, D)
    shared["gmlp_w_in"] = np.ascontiguousarray(inputs["gmlp_w_in"][0], f32)
    shared["gmlp_b_in"] = np.ascontiguousarray(inputs["gmlp_b_in"], f32).reshape(1, 4096)
    shared["gmlp_ln_g"] = np.ascontiguousarray(inputs["gmlp_sgu_ln_g"], f32).reshape(1, 2048)
    shared["gmlp_ln_b"] = np.ascontiguousarray(inputs["gmlp_sgu_ln_b"], f32).reshape(1, 2048)
    shared["gmlp_w_sT"] = np.ascontiguousarray(np.asarray(inputs["gmlp_w_s"][0], f32).transpose(0, 2, 1))
    shared["gmlp_b_sT"] = np.ascontiguousarray(np.asarray(inputs["gmlp_b_s"][0], f32).T)
    shared["gmlp_w_out"] = np.ascontiguousarray(inputs["gmlp_w_out"][0], f32)
    shared["gmlp_b_out"] = np.ascontiguousarray(inputs["gmlp_b_out"], f32).reshape(1, D)
    shared["moe_wr"] = np.ascontiguousarray(np.concatenate(
        [np.asarray(inputs["moe_w_group_router"], f32), np.asarray(inputs["moe_w_expert_router"], f32)], axis=-1))
    shared["moe_br"] = np.ascontiguousarray(np.concatenate(
        [np.asarray(inputs["moe_b_group_router"], f32), np.asarray(inputs["moe_b_expert_router"], f32)], axis=-1)
    ).reshape(2, 1, 36)
    shared["moe_w_gate_up"] = np.ascontiguousarray(inputs["moe_w_gate_up"], f32).reshape(2, 32, D, 512)
    shared["moe_w_down"] = np.ascontiguousarray(inputs["moe_w_down"], f32).reshape(2, 32, 256, D)
    in_maps = []
    for r in range(8):
        bi, qi = r // 4, r % 4
        s0 = qi * 2048
        m = dict(shared)
        xc = np.zeros((17 * 128, D), f32)
        pc = np.zeros((17 * 128,), np.int32)
        if qi > 0:
            xc[:] = x[bi, s0 - 128:s0 + 2048]
            pc[:] = pos[bi, s0 - 128:s0 + 2048]
        else:
            xc[128:] = x[bi, 0:2048]
            pc[128:] = pos[bi, 0:2048]
        m["xin"] = xc
        m["pos"] = np.ascontiguousarray(pc.reshape(17, 128).T)
        m["cT"] = np.ascontiguousarray(c[bi].reshape(8, 128).T)
        mk = np.stack([np.tile(m_cur, (1, 4)), np.tile(m_prev, (1, 4)),
                       np.tile(m_none if qi == 0 else m_prev, (1, 4))]).astype(f32)
        m["masks"] = np.ascontiguousarray(mk)
        in_maps.append(m)
    return in_maps


def kernel(**inputs):
    in_maps = _host_inputs(inputs)
    if "nc" not in _CACHE:
        _CACHE["nc"] = build()
    res = run_bass_kernel_spmd(_CACHE["nc"], in_maps, core_ids=list(range(8)))
    out = np.empty((2, 8192, D), np.float32)
    for r in range(8):
        bi, qi = r // 4, r % 4
        out[bi, qi * 2048:(qi + 1) * 2048] = res.results[r]["out"]
    return out
```

```python
import numpy as np
from contextlib import ExitStack
import concourse.bass as bass
import concourse.mybir as mybir
from concourse.bass_utils import run_bass_kernel_spmd

F32 = mybir.dt.float32
BF16 = mybir.dt.bfloat16
I32 = mybir.dt.int32
AF = mybir.ActivationFunctionType
ALU = mybir.AluOpType
AX = mybir.AxisListType

NT = 16
D = 1024
ALPHA = 4.0 ** 0.25
LN_EPS = 1e-5
TWO_PI = 6.283185307179586
C1 = 6.28125
C2 = TWO_PI - C1
NEG = -30000.0


class Buf:
    __slots__ = ("name", "w", "r")

    def __init__(self, name):
        self.name = name
        self.w = None
        self.r = {}


class Sched:
    ENG = ("pe", "act", "dve", "pool", "sp")
    NSLOT = 8

    def __init__(self, nc, es):
        self.nc = nc
        self.es = es
        self.E = {"pe": nc.tensor, "act": nc.scalar, "dve": nc.vector,
                  "pool": nc.gpsimd, "sp": nc.sync}
        self.cnt = {}
        self.isdma = {}
        self.waited = {e: {} for e in self.ENG}
        self.ops = []
        self.needed = {}
        for e in self.ENG:
            self.cnt[e] = 0
            self.isdma[e] = False
            self.needed[e] = set()

    def dma_proc(self, name):
        self.cnt[name] = 0
        self.isdma[name] = True
        self.needed[name] = set()

    def _add_dep(self, deps, p, v):
        if self.isdma[p]:
            key = (p, (v - 1) % self.NSLOT)
            deps[key] = max(deps.get(key, 0), (v - 1) // self.NSLOT + 1)
        else:
            deps[p] = max(deps.get(p, 0), v)

    def _mk_waits(self, eng, deps):
        waits = []
        wd = self.waited[eng]
        for key, v in deps.items():
            if wd.get(key, 0) >= v:
                continue
            wd[key] = v
            waits.append((key, v))
            if not isinstance(key, tuple):
                self.needed[key].add(v)
        return waits

    def op(self, eng, fn, reads=(), writes=(), proc=None):
        proc = proc or eng
        deps = {}
        for b in reads:
            if b.w is not None:
                p, v = b.w
                if p == eng and eng == "pe":
                    continue
                self._add_dep(deps, p, v)
        for b in writes:
            if b.w is not None:
                p, v = b.w
                if p != eng or eng != "pe":
                    self._add_dep(deps, p, v)
            for p, v in b.r.items():
                if p != eng or eng != "pe":
                    self._add_dep(deps, p, v)
        self.cnt[proc] += 1
        c = self.cnt[proc]
        if self.isdma[proc] and c > self.NSLOT:
            self._add_dep(deps, proc, c - self.NSLOT)
        waits = self._mk_waits(eng, deps)
        self.ops.append((eng, fn, waits, proc, c))
        for b in reads:
            b.r[proc] = c
        for b in writes:
            b.w = (proc, c)
            b.r = {}
        return c

    def _all_deps(self):
        deps = {}
        for p, v in self.cnt.items():
            if v <= 0:
                continue
            if self.isdma[p]:
                for i in range(max(1, v - self.NSLOT + 1), v + 1):
                    self._add_dep(deps, p, i)
            else:
                deps[p] = v
        return deps

    def barrier(self):
        for eng in self.ENG:
            deps = {k: v for k, v in self._all_deps().items() if k != eng}
            waits = self._mk_waits(eng, deps)
            if waits:
                self.ops.append((eng, None, waits, None, 0))

    def wait_all(self, eng, bufs):
        deps = {}
        for b in bufs:
            if b.w is not None:
                self._add_dep(deps, b.w[0], b.w[1])
        for b in bufs:
            if b.w is not None and self.isdma[b.w[0]]:
                p = b.w[0]
                v = self.cnt[p]
                for i in range(max(1, v - self.NSLOT + 1), v + 1):
                    self._add_dep(deps, p, i)
        waits = self._mk_waits(eng, deps)
        self.ops.append((eng, None, waits, None, 0))

    def emit(self):
        nc = self.nc
        sems = {}
        for p in self.cnt:
            if self.isdma[p]:
                for sl in range(self.NSLOT):
                    sems[(p, sl)] = self.es.enter_context(nc.semaphore(f"s_{p}{sl}"))
            else:
                sems[p] = self.es.enter_context(nc.semaphore("s_" + p))
        last_inc = {p: 0 for p in self.cnt}
        for eng, fn, waits, proc, c in self.ops:
            e = self.E[eng]
            for key, v in waits:
                e.wait_ge(sems[key], v * 16 if isinstance(key, tuple) else v)
            if fn is None:
                continue
            ins = fn(e)
            if self.isdma[proc]:
                ins.then_inc(sems[(proc, (c - 1) % self.NSLOT)], 16)
            elif c in self.needed[proc]:
                ins.then_inc(sems[proc], c - last_inc[proc])
                last_inc[proc] = c


def build(stop_after=None):
    nc = bass.Bass("TRN2", target_bir_lowering=False)

    def din(name, shape, dt=F32):
        return nc.dram_tensor(name, list(shape), dt, kind="ExternalInput").ap()

    xin = din("xin", [17 * 128, D])
    pos_d = din("pos", [128, 17], I32)
    cT_d = din("cT", [128, 8])
    ident_d = din("ident", [128, 128])
    masks_d = din("masks", [3, 128, 512])
    invf_d = din("invf", [128, 32])
    tril_d = din("tril", [128, 128])
    ada_w = din("ada_w", [2, D, 6 * D])
    ada_b = din("ada_b", [2, 6 * D])
    lng_d = din("post_ln_g", [4, D])
    lnb_d = din("post_ln_b", [4, D])
    lngT_d = din("post_ln_gT", [4, 128, 8])
    lnbT_d = din("post_ln_bT", [4, 128, 8])
    wqkv_d = din("attn_w_qkv", [D, 1536])
    bqkv_d = din("attn_b_qkv", [1, 1536])
    sinks_d = din("attn_sinks", [1, 16])
    wo_d = din("attn_w_o", [D, D])
    bo_d = din("attn_b_o", [1, D])
    win_d = din("gmlp_w_in", [D, 4096])
    bin_d = din("gmlp_b_in", [1, 4096])
    glng_d = din("gmlp_ln_g", [1, 2048])
    glnb_d = din("gmlp_ln_b", [1, 2048])
    wsT_d = din("gmlp_w_sT", [8, 128, 128])
    bsT_d = din("gmlp_b_sT", [128, 8])
    wout_d = din("gmlp_w_out", [2048, D])
    bout_d = din("gmlp_b_out", [1, D])
    wr_d = din("moe_wr", [2, D, 36])
    br_d = din("moe_br", [2, 1, 36])
    wgu_d = din("moe_w_gate_up", [2, 32, D, 512])
    wdn_d = din("moe_w_down", [2, 32, 256, D])
    out_d = nc.dram_tensor("out", [NT * 128, D], F32, kind="ExternalOutput").ap()

    es = ExitStack()
    with es:
        S = Sched(nc, es)
        for p in ("dx", "dw", "dc", "do"):
            S.dma_proc(p)

        cur_es = [es]

        sb_n = [0]

        def sb(name, shape, dt=F32):
            sb_n[0] += 1
            return cur_es[0].enter_context(nc.sbuf_tensor(f"sb{sb_n[0]}_{name}", list(shape), dt))

        banks = [es.enter_context(nc.psum_tensor(f"bank{i}", [128, 512], F32)) for i in range(8)]
        bankbf = [b[:].bitcast(BF16) for b in banks]
        PB = [Buf(f"bank{i}") for i in range(8)]
        bank_rr = [0]
        bank_pool = [list(range(8))]

        def next_bank():
            bank_rr[0] += 1
            return bank_pool[0][bank_rr[0] % len(bank_pool[0])]

        x = sb("x", [128, NT, D])
        BX = [Buf(f"x{t}") for t in range(NT)]
        hT = sb("hT", [128, 8, NT * 128], BF16)
        BH = [Buf(f"hT{t}") for t in range(NT)]
        W = sb("W", [128, 24576], BF16)
        BW = [Buf(f"W{s}") for s in range(4)]
        A1 = sb("A1", [128, D]); A0 = sb("A0", [128, D])
        BA1, BA0 = Buf("A1"), Buf("A0")
        gtbm = sb("gtbm", [128, D]); gtbf = sb("gtbf", [128, D])
        Bgtbm, Bgtbf = Buf("gtbm"), Buf("gtbf")
        ident = sb("ident", [128, 128], BF16); Bident = Buf("ident")
        ident32 = sb("ident32", [128, 128]); Bident32 = Buf("ident32")
        ones_bf = sb("ones_bf", [1, 128], BF16); Bones = Buf("ones")
        cact_rep = sb("cact_rep", [128, 8, 128], BF16); Bcact = Buf("cact")
        ctmp = sb("ctmp", [128, 8]); Bctmp = Buf("ctmp")
        xnb = [sb(f"xnb{i}", [128, D], BF16) for i in range(2)]
        Bxnb = [Buf(f"xnb{i}") for i in range(2)]
        st_ = [sb(f"st{i}", [128, 2, 6]) for i in range(2)]; mv_ = [sb(f"mv{i}", [128, 2]) for i in range(2)]
        sd_ = [sb(f"sd{i}", [128, 1]) for i in range(2)]
        rstd_ = [sb(f"rstd{i}", [128, 1]) for i in range(2)]; nmr_ = [sb(f"nmr{i}", [128, 1]) for i in range(2)]
        Bst_ = [Buf(f"st{i}") for i in range(2)]; Bmv_ = [Buf(f"mv{i}") for i in range(2)]
        Bsd_ = [Buf(f"sd{i}") for i in range(2)]; Brstd_ = [Buf(f"rstd{i}") for i in range(2)]
        Bnmr_ = [Buf(f"nmr{i}") for i in range(2)]
        fin_i = [0]
        eps_t = sb("eps_t", [128, 1]); Beps = Buf("eps")
        H1 = [sb(f"H1_{i}", [128, 8]) for i in range(4)]
        H0 = [sb(f"H0_{i}", [128, 8]) for i in range(4)]
        BHa = [Buf(f"Haff{i}") for i in range(4)]
        fmt = sb("fmt", [128, 8]); fmt2 = sb("fmt2", [128, 8]); fmg = sb("fmg", [128, 8]); fmb = sb("fmb", [128, 8])
        Bfmt, Bfmt2, Bfmg, Bfmb = Buf("fmt"), Buf("fmt2"), Buf("fmg"), Buf("fmb")
        wr = [sb(f"wr{l}", [128, 8, 36], BF16) for l in range(2)]
        brr = [sb(f"brr{l}", [1, 36], BF16) for l in range(2)]
        Bwr = [Buf(f"wr{l}") for l in range(2)]
        lg = sb("lg", [128, NT, 36]); Blg = Buf("lg")
        comb = sb("comb", [128, NT, 32]); Bcomb = Buf("comb")
        Bout = Buf("out")

        def dma(eng, proc, out, in_, reads=(), writes=()):
            S.op(eng, lambda e: e.dma_start(out=out, in_=in_), reads=reads, writes=writes, proc=proc)

        def bc_load(dst, bdst, row_ap):
            n = row_ap.shape[-1]
            dma("sp", "dc", dst[:, 0:n], row_ap.to_broadcast([128, n]), writes=[bdst])

        MS = {}

        def alloc_mod_scratch():
            MS["vecT"] = sb("vecT", [128, D]); MS["BvecT"] = Buf("vecT")
            MS["bcT"] = sb("bcT", [128, D]); MS["BbcT"] = Buf("bcT")
            MS["stg"] = [sb(f"stg{i}", [128, 8, 256], BF16) for i in range(2)]
            MS["Bstg"] = [Buf(f"stg{i}") for i in range(2)]

        dma("pool", "dw", ident[:], ident_d, writes=[Bident])
        dma("sp", "dc", ident32[:], ident_d, writes=[Bident32])
        S.op("dve", lambda e: e.memset(ones_bf[:], 1.0), writes=[Bones])
        S.op("dve", lambda e: e.memset(eps_t[:], LN_EPS), writes=[Beps])
        dma("sp", "dc", ctmp[:], cT_d, writes=[Bctmp])
        S.op("act", lambda e: e.activation(out=ctmp[:], in_=ctmp[:], func=AF.Silu), reads=[Bctmp], writes=[Bctmp])
        S.op("dve", lambda e: e.tensor_copy(out=cact_rep[:], in_=ctmp[:].unsqueeze(2).to_broadcast([128, 8, 128])),
             reads=[Bctmp], writes=[Bcact])
        for l in range(2):
            dma("pool", "dw", wr[l][:], wr_d[l].rearrange("(k p) n -> p k n", p=128), writes=[Bwr[l]])
            dma("pool", "dw", brr[l][:], br_d[l], writes=[Bwr[l]])

        def compute_mod_gen(l, j, dst, bdst, add_one, lag=0):
            bcT, BbcT, stg, Bstg = MS["bcT"], MS["BbcT"], MS["stg"], MS["Bstg"]
            bc_load(bcT, BbcT, ada_b[l:l + 1, j * D:(j + 1) * D])

            def issue(nb):
                si = nb % 2
                col = j * D + nb * 256
                dma("pool", "dw", stg[si][:], ada_w[l][:, col:col + 256].rearrange("(k p) n -> p k n", p=128),
                    writes=[Bstg[si]])

            def consume(nb):
                si = nb % 2
                bk = next_bank()
                for k in range(8):
                    S.op("pe", (lambda k=k: lambda e: e.matmul(
                        banks[bk][:, 0:256], cact_rep[:, k, :], stg[si][:, k, :], start=(k == 0), stop=(k == 7)))(),
                         reads=[Bcact, Bstg[si]], writes=[PB[bk]])
                S.op("dve", lambda e: e.scalar_tensor_tensor(
                    out=dst[:, nb * 256:(nb + 1) * 256], in0=banks[bk][:, 0:256], scalar=(1.0 if add_one else 0.0),
                    in1=bcT[:, nb * 256:(nb + 1) * 256], op0=ALU.add, op1=ALU.add),
                     reads=[PB[bk], BbcT], writes=[bdst])

            issue(0); issue(1)
            for _ in range(lag):
                yield
            consume(0); consume(1)
            issue(2); issue(3)
            for _ in range(lag):
                yield
            consume(2); consume(3)

        def compute_mod(l, j, dst, bdst, add_one):
            for _ in compute_mod_gen(l, j, dst, bdst, add_one):
                pass

        def to_fm(src, bsrc, dst, bdst):
            tmp, Btmp = MS["bcT"], MS["BbcT"]
            S.op("dve", lambda e: e.tensor_tensor(
                out=tmp[:].rearrange("p (k c) -> p k c", k=8), in0=src[:].rearrange("p (k c) -> p k c", k=8),
                in1=ident32[:].unsqueeze(1).to_broadcast([128, 8, 128]), op=ALU.mult),
                 reads=[bsrc, Bident32], writes=[Btmp])
            S.op("dve", lambda e: e.tensor_reduce(out=dst[:], in_=tmp[:].rearrange("p (k c) -> p k c", k=8),
                                                  axis=AX.X, op=ALU.add),
                 reads=[Btmp], writes=[bdst])

        def make_haff(idx, l, j_sh, j_sc, ln_idx):
            vecT, BvecT = MS["vecT"], MS["BvecT"]
            compute_mod(l, j_sc, vecT, BvecT, True)
            to_fm(vecT, BvecT, fmt, Bfmt)
            compute_mod(l, j_sh, vecT, BvecT, False)
            to_fm(vecT, BvecT, fmt2, Bfmt2)
            haff_combine(idx, ln_idx)

        def haff_combine(idx, ln_idx):
            if ln_idx is None:
                S.op("dve", lambda e: e.tensor_copy(out=H1[idx][:], in_=fmt[:]), reads=[Bfmt], writes=[BHa[idx]])
                S.op("dve", lambda e: e.tensor_copy(out=H0[idx][:], in_=fmt2[:]), reads=[Bfmt2], writes=[BHa[idx]])
            else:
                dma("sp", "dc", fmg[:], lngT_d[ln_idx], writes=[Bfmg])
                dma("sp", "dc", fmb[:], lnbT_d[ln_idx], writes=[Bfmb])
                S.op("dve", lambda e: e.tensor_tensor(out=H1[idx][:], in0=fmt[:], in1=fmg[:], op=ALU.mult),
                     reads=[Bfmt, Bfmg], writes=[BHa[idx]])
                S.op("dve", lambda e: e.tensor_tensor(out=fmb[:], in0=fmt[:], in1=fmb[:], op=ALU.mult),
                     reads=[Bfmt, Bfmb], writes=[Bfmb])
                S.op("dve", lambda e: e.tensor_tensor(out=H0[idx][:], in0=fmb[:], in1=fmt2[:], op=ALU.add),
                     reads=[Bfmb, Bfmt2], writes=[BHa[idx]])

        def make_aset(ln_idx, bias_row, gtb, bgtb, scale=ALPHA):
            bc_load(A1, BA1, lng_d[ln_idx:ln_idx + 1, :])
            bc_load(A0, BA0, lnb_d[ln_idx:ln_idx + 1, :])
            if scale != 1.0:
                S.op("dve", lambda e: e.tensor_scalar(out=A1[:], in0=A1[:], scalar1=scale, scalar2=None, op0=ALU.mult),
                     reads=[BA1], writes=[BA1])
            if bias_row is None:
                if scale != 1.0:
                    S.op("dve", lambda e: e.tensor_scalar(out=A0[:], in0=A0[:], scalar1=scale, scalar2=None, op0=ALU.mult),
                         reads=[BA0], writes=[BA0])
            else:
                vt, bvt = MS["vecT"], MS["BvecT"]
                bc_load(vt, bvt, bias_row)
                S.op("dve", lambda e: e.tensor_tensor(out=vt[:], in0=vt[:], in1=gtb[:], op=ALU.mult),
                     reads=[bvt, bgtb], writes=[bvt])
                S.op("dve", lambda e: e.scalar_tensor_tensor(out=A0[:], in0=A0[:], scalar=scale, in1=vt[:],
                                                             op0=ALU.mult, op1=ALU.add),
                     reads=[BA0, bvt], writes=[BA0])

        def router_logits(l, t):
            bk = next_bank()
            for k in range(8):
                S.op("pe", (lambda k=k: lambda e: e.matmul(
                    banks[bk][:, 0:36], hT[:, k, t * 128:(t + 1) * 128], wr[l][:, k, :], start=(k == 0), stop=False))(),
                     reads=[BH[t], Bwr[l]], writes=[PB[bk]])
            S.op("pe", lambda e: e.matmul(banks[bk][:, 0:36], ones_bf[:], brr[l][:], start=False, stop=True),
                 reads=[Bones, Bwr[l]], writes=[PB[bk]])
            S.op("dve", lambda e: e.tensor_copy(out=lg[:, t, :], in_=banks[bk][:, 0:36]), reads=[PB[bk]], writes=[Blg])

        def finalize_a(t, mode, src=None, bsrc=None, part=0):
            xa = x[:, t, :] if src is None else src
            bxa = BX[t] if bsrc is None else bsrc
            i = fin_i[0] % 2
            fin_i[0] += 1
            st, mv, sd, rstd, nmr = st_[i], mv_[i], sd_[i], rstd_[i], nmr_[i]
            Bst, Bmv, Bsd, Brstd, Bnmr = Bst_[i], Bmv_[i], Bsd_[i], Brstd_[i], Bnmr_[i]
            if mode == "pro":
                S.op("act", lambda e: e.activation(out=xnb[i][:], in_=xa, func=AF.Copy), reads=[bxa], writes=[Bxnb[i]])
                if src is None:
                    S.op("dve", lambda e: e.scalar_tensor_tensor(out=xa, in0=xa, scalar=ALPHA, in1=A0[:],
                                                                 op0=ALU.mult, op1=ALU.add),
                         reads=[bxa, BA0], writes=[bxa])
                return i
            S.op("dve", lambda e: e.bn_stats(out=st[:, 0, :], in_=xa[:, 0:512]), reads=[bxa], writes=[Bst])
            S.op("dve", lambda e: e.bn_stats(out=st[:, 1, :], in_=xa[:, 512:1024]), reads=[bxa], writes=[Bst])
            S.op("dve", lambda e: e.bn_aggr(out=mv[:], in_=st[:]), reads=[Bst], writes=[Bmv])
            if part == 1:
                return i
            return finalize_a2(t, mode, i, src=src, bsrc=bsrc)

        def finalize_a2(t, mode, i, src=None, bsrc=None):
            xa = x[:, t, :] if src is None else src
            bxa = BX[t] if bsrc is None else bsrc
            st, mv, sd, rstd, nmr = st_[i], mv_[i], sd_[i], rstd_[i], nmr_[i]
            Bst, Bmv, Bsd, Brstd, Bnmr = Bst_[i], Bmv_[i], Bsd_[i], Brstd_[i], Bnmr_[i]
            S.op("act", lambda e: e.activation(out=sd[:], in_=mv[:, 1:2], func=AF.Ln, bias=eps_t[:]),
                 reads=[Bmv, Beps], writes=[Bsd])
            S.op("act", lambda e: e.activation(out=rstd[:], in_=sd[:], func=AF.Exp, scale=-0.5), reads=[Bsd], writes=[Brstd])
            S.op("dve", lambda e: e.scalar_tensor_tensor(out=nmr[:], in0=mv[:, 0:1], scalar=-1.0, in1=rstd[:],
                                                         op0=ALU.mult, op1=ALU.mult),
                 reads=[Bmv, Brstd], writes=[Bnmr])
            if mode == "mid":
                S.op("act", lambda e: e.activation(out=xnb[i][:], in_=xa, func=AF.Identity, scale=rstd[:], bias=nmr[:]),
                     reads=[bxa, Brstd, Bnmr], writes=[Bxnb[i]])
            S.op("dve", lambda e: e.tensor_scalar(out=xa, in0=xa, scalar1=rstd[:], scalar2=nmr[:],
                                                  op0=ALU.mult, op1=ALU.add),
                 reads=[bxa, Brstd, Bnmr], writes=[bxa])
            S.op("pool", lambda e: e.tensor_tensor(out=xa, in0=xa, in1=A1[:], op=ALU.mult),
                 reads=[bxa, BA1], writes=[bxa])
            S.op("pool", lambda e: e.tensor_tensor(out=xa, in0=xa, in1=A0[:], op=ALU.add),
                 reads=[bxa, BA0], writes=[bxa])
            if mode == "out":
                dma("sp", "do", out_d[t * 128:(t + 1) * 128, :], xa, reads=[bxa], writes=[Bout])
            return i

        def finalize_b(t, i, haff, hdst=None, bhdst=None):
            bks = [next_bank(), next_bank()]
            for k in range(8):
                S.op("pe", (lambda k=k: lambda e: e.matmul(
                    banks[bks[k // 4]][:, (k % 4) * 128:(k % 4 + 1) * 128], xnb[i][:, k * 128:(k + 1) * 128], ident[:],
                    start=True, stop=True))(),
                     reads=[Bxnb[i], Bident], writes=[PB[bks[k // 4]]])
            hd = hT[:, :, t * 128:(t + 1) * 128] if hdst is None else hdst
            bhd = BH[t] if bhdst is None else bhdst
            for k in range(8):
                if k % 2 == 0:
                    S.op("act", (lambda k=k: lambda e: e.activation(
                        out=hd[:, k, :], in_=banks[bks[k // 4]][:, (k % 4) * 128:(k % 4 + 1) * 128], func=AF.Identity,
                        scale=H1[haff][:, k:k + 1], bias=H0[haff][:, k:k + 1]))(),
                         reads=[PB[bks[k // 4]], BHa[haff]], writes=[bhd])
                else:
                    S.op("dve", (lambda k=k: lambda e: e.tensor_scalar(
                        out=hd[:, k, :], in0=banks[bks[k // 4]][:, (k % 4) * 128:(k % 4 + 1) * 128],
                        scalar1=H1[haff][:, k:k + 1], scalar2=H0[haff][:, k:k + 1], op0=ALU.mult, op1=ALU.add))(),
                         reads=[PB[bks[k // 4]], BHa[haff]], writes=[bhd])

        def finalize(t, mode, haff=None, hdst=None, bhdst=None, route_l=None, src=None, bsrc=None):
            i = finalize_a(t, mode, src=src, bsrc=bsrc)
            if mode == "out":
                return
            finalize_b(t, i, haff, hdst=hdst, bhdst=bhdst)
            if route_l is not None:
                router_logits(route_l, t)

        def finalize_seq(mode, haff=None, route_l=None, hook=None):
            idx = {}
            if mode == "pro":
                idx[0] = finalize_a(0, mode)
                for t in range(NT):
                    if hook is not None:
                        hook(t)
                    if t + 1 < NT:
                        idx[t + 1] = finalize_a(t + 1, mode)
                    finalize_b(t, idx[t], haff)
                return
            idx[0] = finalize_a(0, mode, part=1)
            idx[1] = finalize_a(1, mode, part=1)
            finalize_a2(0, mode, idx[0])
            for t in range(NT):
                if hook is not None:
                    hook(t)
                if t + 2 < NT:
                    idx[t + 2] = finalize_a(t + 2, mode, part=1)
                if t + 1 < NT:
                    finalize_a2(t + 1, mode, idx[t + 1])
                if mode != "out":
                    finalize_b(t, idx[t], haff)
                    if route_l is not None and t >= 1:
                        router_logits(route_l, t - 1)
            if mode != "out" and route_l is not None:
                router_logits(route_l, NT - 1)

        def make_end_hook(mode, haff=None, route_l=None):
            st = {}

            def stage_b(t):
                if mode != "out":
                    finalize_b(t, st[t], haff)
                    if route_l is not None:
                        router_logits(route_l, t)

            def hook(t):
                st[t] = finalize_a(t, mode, part=1)
                if t >= 1:
                    finalize_a2(t - 1, mode, st[t - 1])
                if t >= 2:
                    stage_b(t - 2)

            def flush():
                finalize_a2(NT - 1, mode, st[NT - 1])
                stage_b(NT - 2)
                stage_b(NT - 1)
            return hook, flush

        def dump_and_end():
            for t in range(NT):
                dma("sp", "do", out_d[t * 128:(t + 1) * 128, :], x[:, t, :], reads=[BX[t]], writes=[Bout])
            S.wait_all("sp", [Bout])
            S.emit()

        esA = ExitStack()
        cur_es[0] = esA
        with esA:
            cosT = sb("cosT", [128, 17, 32]); sinT = sb("sinT", [128, 17, 32])
            Bcos, Bsin = Buf("cos"), Buf("sin")
            hTh = sb("hTh", [128, 8, 128], BF16); BhTh = Buf("hTh")
            bqkv = sb("bqkv", [1, 1536], BF16); Bbqkv = Buf("bqkv")
            esink = sb("esink", [128, 16]); Besink = Buf("esink")
            maskb = sb("maskb", [128, 3, 512], BF16); Bmask = Buf("maskb")
            Wqkv = W[:, 0:12288].rearrange("p (k n) -> p k n", k=8)
            Wo = W[:, 12288:20480].rearrange("p (k n) -> p k n", k=8)

            es0 = ExitStack()
            cur_es[0] = es0
            with es0:
                alloc_mod_scratch()
                posi = sb("posi", [128, 17], I32); posf = sb("posf", [128, 17]); invf = sb("invf", [128, 32])
                ang = sb("ang", [128, 17, 32]); ki = sb("ki", [128, 17, 32], I32)
                kf = W[:, 22528:22528 + 1088].bitcast(F32).rearrange("p (t f) -> p t f", t=17)
                Bpos, Binvf, Bang, Bkf, Bki = Buf("pos"), Buf("invf"), Buf("ang"), Buf("kf"), Buf("ki")
                xh = W[:, 20480:22528].bitcast(F32); Bxh = Buf("xh")

                make_haff(0, 0, 0, 1, None)
                compute_mod(0, 2, gtbm, Bgtbm, True)
                bc_load(A0, BA0, bo_d)
                S.op("dve", lambda e: e.tensor_tensor(out=A0[:], in0=A0[:], in1=gtbm[:], op=ALU.mult),
                     reads=[BA0, Bgtbm], writes=[BA0])
                dma("pool", "dw", Wqkv, wqkv_d.rearrange("(k p) n -> p k n", p=128), writes=[BW[0], BW[1]])
                dma("pool", "dw", Wo, wo_d.rearrange("(k p) n -> p k n", p=128), writes=[BW[2], BW[3]])
                dma("pool", "dw", bqkv[:], bqkv_d, writes=[Bbqkv])
                bc_load(esink, Besink, sinks_d)
                S.op("act", lambda e: e.activation(out=esink[:], in_=esink[:], func=AF.Exp), reads=[Besink], writes=[Besink])
                dma("pool", "dw", maskb[:], masks_d.rearrange("m p n -> p m n"), writes=[Bmask])
                dma("sp", "dc", posi[:], pos_d, writes=[Bpos])
                dma("sp", "dc", invf[:], invf_d, writes=[Binvf])
                S.op("dve", lambda e: e.tensor_copy(out=posf[:], in_=posi[:]), reads=[Bpos], writes=[Bpos])
                S.op("dve", lambda e: e.tensor_tensor(out=ang[:], in0=posf[:].unsqueeze(2).to_broadcast([128, 17, 32]),
                                                      in1=invf[:].unsqueeze(1).to_broadcast([128, 17, 32]), op=ALU.mult),
                     reads=[Bpos, Binvf], writes=[Bang])

                def sin_of(dst, bdst, shift):
                    if shift != 0.0:
                        S.op("dve", lambda e: e.tensor_scalar(out=dst[:], in0=ang[:], scalar1=shift, scalar2=None, op0=ALU.add),
                             reads=[Bang], writes=[bdst])
                        a_src, ba = dst, bdst
                    else:
                        a_src, ba = ang, Bang
                    S.op("dve", lambda e: e.tensor_scalar(out=ki[:], in0=a_src[:], scalar1=1.0 / TWO_PI, scalar2=None,
                                                          op0=ALU.mult), reads=[ba], writes=[Bki])
                    S.op("dve", lambda e: e.tensor_copy(out=kf, in_=ki[:]), reads=[Bki], writes=[Bkf])
                    S.op("dve", lambda e: e.scalar_tensor_tensor(out=dst[:], in0=kf, scalar=-C1, in1=a_src[:],
                                                                 op0=ALU.mult, op1=ALU.add), reads=[Bkf, ba], writes=[bdst])
                    S.op("dve", lambda e: e.scalar_tensor_tensor(out=dst[:], in0=kf, scalar=-C2, in1=dst[:],
                                                                 op0=ALU.mult, op1=ALU.add), reads=[Bkf, bdst], writes=[bdst])
                    S.op("dve", lambda e: e.tensor_scalar(out=dst[:], in0=dst[:], scalar1=3.1415925, scalar2=-3.1415925,
                                                          op0=ALU.min, op1=ALU.max), reads=[bdst], writes=[bdst])
                    S.op("act", lambda e: e.activation(out=dst[:], in_=dst[:], func=AF.Sin), reads=[bdst], writes=[bdst])

                sin_of(sinT, Bsin, 0.0)
                sin_of(cosT, Bcos, TWO_PI / 4)

                dma("sp", "dx", xh, xin[0:128, :], writes=[Bxh])
                for t in range(NT):
                    dma("sp", "dx", x[:, t, :], xin[(t + 1) * 128:(t + 2) * 128, :], writes=[BX[t]])
                finalize(0, "pro", haff=0, hdst=hTh, bhdst=BhTh, src=xh, bsrc=Bxh)
                def mods_b():
                    vecT, BvecT = MS["vecT"], MS["BvecT"]
                    yield from compute_mod_gen(0, 4, vecT, BvecT, True, lag=3)
                    to_fm(vecT, BvecT, fmt, Bfmt)
                    yield from compute_mod_gen(0, 3, vecT, BvecT, False, lag=3)
                    to_fm(vecT, BvecT, fmt2, Bfmt2)
                    haff_combine(1, 0)
                    yield from compute_mod_gen(0, 5, gtbf, Bgtbf, True, lag=2)

                job_b = mods_b()
                finalize_seq("pro", haff=0, hook=lambda t: next(job_b, None))
                for _ in job_b:
                    pass
                S.op("dve", lambda e: e.tensor_tensor(out=Wo, in0=Wo, in1=gtbm[:].unsqueeze(1).to_broadcast([128, 8, D]),
                                                      op=ALU.mult),
                     reads=[BW[2], BW[3], Bgtbm], writes=[BW[2], BW[3]])
                if stop_after == "pro":
                    dump_and_end()
                    return nc
                make_aset(0, None, None, None)
            S.barrier()

            es1 = ExitStack()
            cur_es[0] = es1
            bank_pool[0] = [3, 6, 7]
            with es1:
                qk32 = sb("qk32", [128, 20, 64]); Bqk32 = Buf("qk32")
                rot = sb("rot", [128, 20, 64], BF16); Brot = Buf("rot")
                rot2 = rot[:].rearrange("p h d -> p (h d)")
                tA = sb("tA", [128, 20, 32]); tB = sb("tB", [128, 20, 32])
                BtA, BtB = Buf("tA"), Buf("tB")
                kpad = sb("kpad", [128, 4, 2, 128], BF16); Bkpad = Buf("kpad")
                qT = sb("qT", [128, 8, 128], BF16); BqT = Buf("qT")
                Vaug = [sb(f"Vaug{i}", [128, 4, 65], BF16) for i in range(3)]
                BV = [Buf(f"V{i}") for i in range(3)]
                ao = sb("ao", [128, 16, 64], BF16); Bao = Buf("ao")
                ao2 = ao[:].rearrange("p h d -> p (h d)")
                aoT = sb("aoT", [128, 8, 128], BF16); BaoT = Buf("aoT")
                dent = sb("dent", [128, 16]); Bdent = Buf("dent")
                kT = [W[:, 20480 + i * 1024:20480 + (i + 1) * 1024].rearrange("p (v c) -> p v c", v=8) for i in range(2)]
                BkT = [Buf(f"kT{i}") for i in range(2)]
                PT = [W[:, 22528 + i * 1024:22528 + (i + 1) * 1024].rearrange("p (b c) -> p b c", b=2) for i in range(2)]
                BPT = [Buf(f"PT{i}") for i in range(2)]
                S.op("pool", lambda e: e.memset(kpad[:], 0.0), writes=[Bkpad])
                for i in range(3):
                    S.op("pool", (lambda i=i: lambda e: e.memset(Vaug[i][:], 1.0))(), writes=[BV[i]])

                def X1(t):
                    halo = t < 0
                    tt = t + 1
                    hsrc = hTh if halo else hT[:, :, t * 128:(t + 1) * 128]
                    bh = BhTh if halo else BH[t]
                    qb = []
                    for nb in ([2] if halo else [0, 1, 2]):
                        bk = nb
                        qb.append((nb, bk))
                        for k in range(8):
                            S.op("pe", (lambda k=k, nb=nb, bk=bk: lambda e: e.matmul(
                                banks[bk][:], hsrc[:, k, :], Wqkv[:, k, nb * 512:(nb + 1) * 512], start=(k == 0), stop=False))(),
                                 reads=[bh, BW[0], BW[1]], writes=[PB[bk]])
                        S.op("pe", (lambda nb=nb, bk=bk: lambda e: e.matmul(
                            banks[bk][:], ones_bf[:], bqkv[:, nb * 512:(nb + 1) * 512], start=False, stop=True))(),
                             reads=[Bones, Bbqkv], writes=[PB[bk]])
                    vcur = Vaug[tt % 3]; bvcur = BV[tt % 3]
                    for nb, bk in qb:
                        if nb < 2:
                            S.op("act", (lambda nb=nb, bk=bk: lambda e: e.activation(
                                out=qk32[:, nb * 8:(nb + 1) * 8, :], in_=banks[bk][:].rearrange("p (h d) -> p h d", d=64),
                                func=AF.Copy))(), reads=[PB[bk]], writes=[Bqk32])
                        else:
                            S.op("act", (lambda bk=bk: lambda e: e.activation(
                                out=qk32[:, 16:20, :], in_=banks[bk][:, 0:256].rearrange("p (h d) -> p h d", d=64),
                                func=AF.Copy))(), reads=[PB[bk]], writes=[Bqk32])
                            S.op("act", (lambda bk=bk: lambda e: e.activation(
                                out=vcur[:, :, 0:64], in_=banks[bk][:, 256:512].rearrange("p (h d) -> p h d", d=64),
                                func=AF.Copy))(), reads=[PB[bk]], writes=[bvcur])
                    h0 = 16 if halo else 0
                    nh = 20 - h0
                    x1 = qk32[:, h0:20, 0:32]; x2 = qk32[:, h0:20, 32:64]
                    cb = cosT[:, tt, :].unsqueeze(1).to_broadcast([128, nh, 32])
                    sbc = sinT[:, tt, :].unsqueeze(1).to_broadcast([128, nh, 32])
                    S.op("dve", lambda e: e.tensor_tensor(out=tA[:, h0:20, :], in0=x1, in1=cb, op=ALU.mult),
                         reads=[Bqk32, Bcos], writes=[BtA])
                    S.op("dve", lambda e: e.tensor_tensor(out=tB[:, h0:20, :], in0=x2, in1=sbc, op=ALU.mult),
                         reads=[Bqk32, Bsin], writes=[BtB])
                    if not halo:
                        S.op("dve", lambda e: e.tensor_tensor(out=rot[:, 0:16, 0:32], in0=tA[:, 0:16, :], in1=tB[:, 0:16, :],
                                                              op=ALU.subtract), reads=[BtA, BtB], writes=[Brot])
                    for half in range(2):
                        S.op("dve", (lambda half=half: lambda e: e.tensor_tensor(
                            out=kpad[:, :, half, half * 64:half * 64 + 32], in0=tA[:, 16:20, :], in1=tB[:, 16:20, :],
                            op=ALU.subtract))(), reads=[BtA, BtB], writes=[Bkpad])
                    S.op("dve", lambda e: e.tensor_tensor(out=tA[:, h0:20, :], in0=x2, in1=cb, op=ALU.mult),
                         reads=[Bqk32, Bcos], writes=[BtA])
                    S.op("dve", lambda e: e.tensor_tensor(out=tB[:, h0:20, :], in0=x1, in1=sbc, op=ALU.mult),
                         reads=[Bqk32, Bsin], writes=[BtB])
                    if not halo:
                        S.op("dve", lambda e: e.tensor_tensor(out=rot[:, 0:16, 32:64], in0=tA[:, 0:16, :], in1=tB[:, 0:16, :],
                                                              op=ALU.add), reads=[BtA, BtB], writes=[Brot])
                    for half in range(2):
                        S.op("dve", (lambda half=half: lambda e: e.tensor_tensor(
                            out=kpad[:, :, half, half * 64 + 32:half * 64 + 64], in0=tA[:, 16:20, :], in1=tB[:, 16:20, :],
                            op=ALU.add))(), reads=[BtA, BtB], writes=[Bkpad])

                tb = [3, 6]

                def X2(t):
                    halo = t < 0
                    tt = t + 1
                    cur = tt % 2
                    for v in range(8):
                        S.op("pe", (lambda v=v: lambda e: e.matmul(
                            banks[tb[v // 4]][:, (v % 4) * 128:(v % 4 + 1) * 128], kpad[:, v // 2, v % 2, :], ident[:],
                            start=True, stop=True))(),
                             reads=[Bkpad, Bident], writes=[PB[tb[v // 4]]])
                    for hb in range(2):
                        S.op("act", (lambda hb=hb: lambda e: e.activation(
                            out=kT[cur][:, hb * 4:(hb + 1) * 4, :], in_=banks[tb[hb]][:].rearrange("p (v c) -> p v c", v=4),
                            func=AF.Copy))(), reads=[PB[tb[hb]]], writes=[BkT[cur]])
                    if halo:
                        return
                    for j in range(8):
                        S.op("pe", (lambda j=j: lambda e: e.matmul(
                            banks[tb[j // 4]][:, (j % 4) * 128:(j % 4 + 1) * 128], rot2[:, j * 128:(j + 1) * 128], ident[:],
                            start=True, stop=True))(),
                             reads=[Brot, Bident], writes=[PB[tb[j // 4]]])
                    for hb in range(2):
                        S.op("dve", (lambda hb=hb: lambda e: e.tensor_copy(
                            out=qT[:, hb * 4:(hb + 1) * 4, :], in_=banks[tb[hb]][:].rearrange("p (v c) -> p v c", v=4)))(),
                             reads=[PB[tb[hb]]], writes=[BqT])

                def Y1(t):
                    tt = t + 1
                    cur = tt % 2
                    prv = 1 - cur
                    vcur, bvcur = Vaug[tt % 3], BV[tt % 3]
                    vprv, bvprv = Vaug[(tt - 1) % 3], BV[(tt - 1) % 3]
                    ob = [0, 1, 2]
                    sc_i = [0]
                    first_tile = (t == 0)

                    def scores(g):
                        pi = g % 2
                        for blk, (ktile, bkt, mi) in enumerate([(kT[prv], BkT[prv], 2 if first_tile else 1),
                                                                (kT[cur], BkT[cur], 0)]):
                            bk = 4 + (sc_i[0] % 2)
                            sc_i[0] += 1
                            S.op("pe", (lambda bk=bk, mi=mi: lambda e: e.matmul(
                                banks[bk][:], ident[:], maskb[:, mi, :], start=True, stop=False))(),
                                 reads=[Bident, Bmask], writes=[PB[bk]])
                            for i in range(4):
                                h = 4 * g + i
                                S.op("pe", (lambda bk=bk, i=i, h=h, ktile=ktile, g=g: lambda e: e.matmul(
                                    banks[bk][:, i * 128:(i + 1) * 128], ktile[:, g * 2 + (h % 2), :], qT[:, h // 2, :],
                                    start=False, stop=(i == 3)))(),
                                     reads=[bkt, BqT], writes=[PB[bk]])
                            S.op("act", (lambda bk=bk, blk=blk, pi=pi: lambda e: e.activation(
                                out=PT[pi][:, blk, :], in_=banks[bk][:], func=AF.Exp, scale=0.125))(),
                                 reads=[PB[bk]], writes=[BPT[pi]])

                    def pv(g):
                        pi = g % 2
                        for i in range(4):
                            h = 4 * g + i
                            obk = ob[h // 7]
                            oc = (h % 7) * 65
                            for blk, (vt, bvt) in enumerate([(vprv, bvprv), (vcur, bvcur)]):
                                S.op("pe", (lambda obk=obk, oc=oc, blk=blk, pi=pi, i=i, vt=vt, g=g: lambda e: e.matmul(
                                    banks[obk][:, oc:oc + 65], PT[pi][:, blk, i * 128:(i + 1) * 128], vt[:, g, :],
                                    start=(blk == 0), stop=(blk == 1)))(),
                                     reads=[BPT[pi], bvt], writes=[PB[obk]])

                    scores(0)
                    for g in range(4):
                        if g + 1 < 4:
                            scores(g + 1)
                        pv(g)
                    for b3 in range(3):
                        hs = 7 * b3
                        n = min(7, 16 - hs)
                        ov = banks[ob[b3]][:, 0:n * 65].rearrange("p (h d) -> p h d", d=65)
                        S.op("dve", (lambda ov=ov, hs=hs, n=n: lambda e: e.tensor_tensor(
                            out=dent[:, hs:hs + n], in0=ov[:, :, 64], in1=esink[:, hs:hs + n], op=ALU.add))(),
                             reads=[PB[ob[b3]], Besink], writes=[Bdent])
                        S.op("dve", (lambda hs=hs, n=n: lambda e: e.reciprocal(out=dent[:, hs:hs + n], in_=dent[:, hs:hs + n]))(),
                             reads=[Bdent], writes=[Bdent])
                        S.op("dve", (lambda ov=ov, hs=hs, n=n: lambda e: e.tensor_tensor(
                            out=ao[:, hs:hs + n, :], in0=ov[:, :, 0:64],
                            in1=dent[:, hs:hs + n].unsqueeze(2).to_broadcast([128, n, 64]), op=ALU.mult))(),
                             reads=[PB[ob[b3]], Bdent], writes=[Bao])

                def Y2a(t):
                    for j in range(8):
                        S.op("pe", (lambda j=j: lambda e: e.matmul(
                            banks[tb[j // 4]][:, (j % 4) * 128:(j % 4 + 1) * 128], ao2[:, j * 128:(j + 1) * 128], ident[:],
                            start=True, stop=True))(),
                             reads=[Bao, Bident], writes=[PB[tb[j // 4]]])
                    for hb in range(2):
                        S.op("act", (lambda hb=hb: lambda e: e.activation(
                            out=aoT[:, hb * 4:(hb + 1) * 4, :], in_=banks[tb[hb]][:].rearrange("p (v c) -> p v c", v=4),
                            func=AF.Copy))(), reads=[PB[tb[hb]]], writes=[BaoT])

                def Y2b(t):
                    for nb in range(2):
                        bk = 4 + nb
                        for k in range(8):
                            S.op("pe", (lambda k=k, nb=nb, bk=bk: lambda e: e.matmul(
                                banks[bk][:], aoT[:, k, :], Wo[:, k, nb * 512:(nb + 1) * 512], start=(k == 0), stop=(k == 7)))(),
                                 reads=[BaoT, BW[2], BW[3]], writes=[PB[bk]])
                        S.op("dve", (lambda nb=nb, bk=bk: lambda e: e.tensor_tensor(
                            out=x[:, t, nb * 512:(nb + 1) * 512], in0=x[:, t, nb * 512:(nb + 1) * 512], in1=banks[bk][:],
                            op=ALU.add))(), reads=[BX[t], PB[bk]], writes=[BX[t]])

                X1(-1); X2(-1)
                X1(0); X2(0)
                fi = {}
                for t in range(NT + 3):
                    if 0 <= t - 2 < NT:
                        fi[t - 2] = finalize_a(t - 2, "mid")
                    if t + 1 < NT:
                        X1(t + 1)
                    if 0 <= t - 1 < NT:
                        Y2a(t - 1)
                    if t < NT:
                        Y1(t)
                    if 0 <= t - 1 < NT:
                        Y2b(t - 1)
                    if 0 <= t - 2 < NT:
                        finalize_b(t - 2, fi[t - 2], 1)
                    if t + 1 < NT:
                        X2(t + 1)
                    if 0 <= t - 3 < NT:
                        router_logits(0, t - 3)
                if stop_after == "attn":
                    dump_and_end()
                    return nc
            S.barrier()
        cur_es[0] = es

        def bc3(ap2, n):
            return ap2.unsqueeze(2).to_broadcast([128, NT, n])

        def moe_views(s):
            base = s * 6144
            wgu = W[:, base:base + 4096].rearrange("p (k n) -> p k n", k=8)
            wdn = W[:, base + 4096:base + 6144].rearrange("p (j n) -> p j n", j=2)
            return wgu, wdn

        def moe_load(l, ex):
            s = ex % 4
            wgu, wdn = moe_views(s)
            dma("pool", "dw", wgu, wgu_d[l, ex].rearrange("(k p) n -> p k n", p=128), writes=[BW[s]])
            dma("pool", "dw", wdn, wdn_d[l, ex].rearrange("(j p) n -> p j n", p=128), writes=[BW[s]])
            S.op("pool", lambda e: e.tensor_tensor(out=wdn, in0=wdn, in1=gtbf[:].unsqueeze(1).to_broadcast([128, 2, D]),
                                                   op=ALU.mult), reads=[BW[s], Bgtbf], writes=[BW[s]])

        def moe_phase(l, after_chunk=None, nchunks=16, tile_hook=None, first_load=0, end_hook=None):
            gl_m = sb("gl_m", [128, NT]); Bglm = Buf("gl_m")
            goh = sb("goh", [128, NT, 4]); Bgoh = Buf("goh")
            gex = sb("gex", [128, NT, 4]); Bgex = Buf("gex")
            gp = sb("gp", [128, NT]); Bgp = Buf("gp")
            sel4 = sb("sel4", [128, NT, 32]); Bsel4 = Buf("sel4")
            sel = sb("sel", [128, NT, 8]); Bsel = Buf("sel")
            sel2 = sb("sel2", [128, NT, 8]); Bsel2 = Buf("sel2")
            oh1 = sb("oh1", [128, NT, 8]); Boh1 = Buf("oh1")
            oh2 = sb("oh2", [128, NT, 8]); Boh2 = Buf("oh2")
            m1 = sb("m1", [128, NT]); m2 = sb("m2", [128, NT]); Bm1, Bm2 = Buf("m1"), Buf("m2")
            w1 = sb("w1", [128, NT]); w2 = sb("w2", [128, NT]); Bw1, Bw2 = Buf("w1"), Buf("w2")
            sg = [sb(f"sg{i}", [128, 256]) for i in range(2)]
            Bsg = [Buf(f"sg{i}") for i in range(2)]
            actb = [sb(f"actb{i}", [128, 256], BF16) for i in range(2)]
            Bact = [Buf(f"act{i}") for i in range(2)]
            actT = [sb(f"actT{i}", [128, 2, 128], BF16) for i in range(3)]
            BactT = [Buf(f"actT{i}") for i in range(3)]

            for ex in range(first_load, 4):
                moe_load(l, ex)

            gl = lg[:, :, 0:4]
            S.op("dve", lambda e: e.tensor_reduce(out=gl_m[:], in_=gl, axis=AX.X, op=ALU.max), reads=[Blg], writes=[Bglm])
            S.op("dve", lambda e: e.tensor_tensor(out=goh[:], in0=gl, in1=bc3(gl_m[:], 4), op=ALU.is_equal),
                 reads=[Blg, Bglm], writes=[Bgoh])
            S.op("dve", lambda e: e.tensor_tensor(out=gex[:], in0=gl, in1=bc3(gl_m[:], 4), op=ALU.subtract),
                 reads=[Blg, Bglm], writes=[Bgex])
            S.op("act", lambda e: e.activation(out=gex[:], in_=gex[:], func=AF.Exp), reads=[Bgex], writes=[Bgex])
            S.op("dve", lambda e: e.tensor_reduce(out=gp[:], in_=gex[:], axis=AX.X, op=ALU.add), reads=[Bgex], writes=[Bgp])
            S.op("dve", lambda e: e.reciprocal(out=gp[:], in_=gp[:]), reads=[Bgp], writes=[Bgp])
            S.op("dve", lambda e: e.tensor_tensor(
                out=sel4[:].rearrange("p t (g e) -> p t g e", g=4), in0=lg[:, :, 4:36].rearrange("p t (g e) -> p t g e", g=4),
                in1=goh[:].unsqueeze(3).to_broadcast([128, NT, 4, 8]), op=ALU.mult),
                 reads=[Blg, Bgoh], writes=[Bsel4])
            S.op("dve", lambda e: e.tensor_reduce(out=sel[:], in_=sel4[:].rearrange("p t (g e) -> p t e g", g=4),
                                                  axis=AX.X, op=ALU.add), reads=[Bsel4], writes=[Bsel])
            S.op("dve", lambda e: e.tensor_reduce(out=m1[:], in_=sel[:], axis=AX.X, op=ALU.max), reads=[Bsel], writes=[Bm1])
            S.op("dve", lambda e: e.tensor_tensor(out=oh1[:], in0=sel[:], in1=bc3(m1[:], 8), op=ALU.is_equal),
                 reads=[Bsel, Bm1], writes=[Boh1])
            S.op("dve", lambda e: e.scalar_tensor_tensor(out=sel2[:], in0=oh1[:], scalar=-1e30, in1=sel[:],
                                                         op0=ALU.mult, op1=ALU.add), reads=[Boh1, Bsel], writes=[Bsel2])
            S.op("dve", lambda e: e.tensor_reduce(out=m2[:], in_=sel2[:], axis=AX.X, op=ALU.max), reads=[Bsel2], writes=[Bm2])
            S.op("dve", lambda e: e.tensor_tensor(out=oh2[:], in0=sel2[:], in1=bc3(m2[:], 8), op=ALU.is_equal),
                 reads=[Bsel2, Bm2], writes=[Boh2])
            S.op("dve", lambda e: e.tensor_tensor(out=w2[:], in0=m2[:], in1=m1[:], op=ALU.subtract),
                 reads=[Bm1, Bm2], writes=[Bw2])
            S.op("act", lambda e: e.activation(out=w2[:], in_=w2[:], func=AF.Exp), reads=[Bw2], writes=[Bw2])
            S.op("dve", lambda e: e.tensor_scalar(out=w2[:], in0=w2[:], scalar1=1.0, scalar2=None, op0=ALU.add),
                 reads=[Bw2], writes=[Bw2])
            S.op("dve", lambda e: e.reciprocal(out=w1[:], in_=w2[:]), reads=[Bw2], writes=[Bw1])
            S.op("dve", lambda e: e.tensor_tensor(out=w1[:], in0=w1[:], in1=gp[:], op=ALU.mult), reads=[Bw1, Bgp], writes=[Bw1])
            S.op("dve", lambda e: e.tensor_tensor(out=w2[:], in0=gp[:], in1=w1[:], op=ALU.subtract),
                 reads=[Bgp, Bw1], writes=[Bw2])
            S.op("dve", lambda e: e.tensor_tensor(out=oh1[:], in0=oh1[:], in1=bc3(w1[:], 8), op=ALU.mult),
                 reads=[Boh1, Bw1], writes=[Boh1])
            S.op("dve", lambda e: e.tensor_tensor(out=oh2[:], in0=oh2[:], in1=bc3(w2[:], 8), op=ALU.mult),
                 reads=[Boh2, Bw2], writes=[Boh2])
            S.op("dve", lambda e: e.tensor_tensor(out=oh1[:], in0=oh1[:], in1=oh2[:], op=ALU.add),
                 reads=[Boh1, Boh2], writes=[Boh1])
            S.op("dve", lambda e: e.tensor_tensor(
                out=comb[:].rearrange("p t (g e) -> p t g e", g=4),
                in0=goh[:].unsqueeze(3).to_broadcast([128, NT, 4, 8]),
                in1=oh1[:].unsqueeze(2).to_broadcast([128, NT, 4, 8]), op=ALU.mult),
                 reads=[Bgoh, Boh1], writes=[Bcomb])

            GUB = [0, 1]; TB = [2, 3]; YB = [[4, 5], [6, 7]]
            steps = []
            for c in range(nchunks):
                for t in range(NT):
                    for e2 in range(2):
                        steps.append((c, t, e2))
            n = len(steps)

            def GU(i):
                c, t, e2 = steps[i]
                s = (2 * c + e2) % 4
                wgu, _ = moe_views(s)
                bk = GUB[i % 2]
                for k in range(8):
                    S.op("pe", (lambda k=k: lambda e: e.matmul(
                        banks[bk][:], hT[:, k, t * 128:(t + 1) * 128], wgu[:, k, :], start=(k == 0), stop=(k == 7)))(),
                         reads=[BH[t], BW[s]], writes=[PB[bk]])
                si = i % 2
                S.op("act", lambda e: e.activation(out=sg[si][:], in_=banks[bk][:, 0:256], func=AF.Silu),
                     reads=[PB[bk]], writes=[Bsg[si]])
                ex = 2 * c + e2
                S.op("dve", lambda e: e.scalar_tensor_tensor(
                    out=actb[si][:], in0=sg[si][:], scalar=comb[:, t, ex:ex + 1], in1=banks[bk][:, 256:512],
                    op0=ALU.mult, op1=ALU.mult), reads=[Bsg[si], Bcomb, PB[bk]], writes=[Bact[si]])

            def TR(i):
                si = i % 2
                bk = TB[i % 2]
                ti = i % 3
                for j in range(2):
                    S.op("pe", (lambda j=j: lambda e: e.matmul(
                        banks[bk][:, j * 128:(j + 1) * 128], actb[si][:, j * 128:(j + 1) * 128], ident[:],
                        start=True, stop=True))(),
                         reads=[Bact[si], Bident], writes=[PB[bk]])
                S.op("act", lambda e: e.activation(out=actT[ti][:], in_=banks[bk][:, 0:256].rearrange("p (j c) -> p j c", j=2),
                                                   func=AF.Copy), reads=[PB[bk]], writes=[BactT[ti]])

            def DN(i):
                c, t, e2 = steps[i]
                s = (2 * c + e2) % 4
                _, wdn = moe_views(s)
                ti = i % 3
                yb = YB[t % 2]
                for nb in range(2):
                    for j in range(2):
                        S.op("pe", (lambda nb=nb, j=j: lambda e: e.matmul(
                            banks[yb[nb]][:], actT[ti][:, j, :], wdn[:, j, nb * 512:(nb + 1) * 512],
                            start=(e2 == 0 and j == 0), stop=(e2 == 1 and j == 1)))(),
                             reads=[BactT[ti], BW[s]], writes=[PB[yb[nb]]])
                if e2 == 1:
                    for nb in range(2):
                        S.op("dve", (lambda nb=nb: lambda e: e.tensor_tensor(
                            out=x[:, t, nb * 512:(nb + 1) * 512], in0=x[:, t, nb * 512:(nb + 1) * 512],
                            in1=banks[yb[nb]][:], op=ALU.add))(), reads=[BX[t], PB[yb[nb]]], writes=[BX[t]])
                    if tile_hook is not None:
                        tile_hook(c, t)
                    if end_hook is not None and c == nchunks - 1:
                        end_hook(t)
                    if t == NT - 1:
                        if c + 2 < 16 and c + 2 < nchunks:
                            moe_load(l, 2 * (c + 2)); moe_load(l, 2 * (c + 2) + 1)
                        if after_chunk is not None:
                            after_chunk(c)

            for i in range(n + 2):
                if i < n:
                    GU(i)
                if 1 <= i <= n:
                    TR(i - 1)
                if 2 <= i <= n + 1:
                    DN(i - 2)

        def mod_jobs_moe0():
            vecT, BvecT = MS["vecT"], MS["BvecT"]
            yield from compute_mod_gen(1, 1, vecT, BvecT, True, lag=12)
            to_fm(vecT, BvecT, fmt, Bfmt)
            yield from compute_mod_gen(1, 0, vecT, BvecT, False, lag=12)
            to_fm(vecT, BvecT, fmt2, Bfmt2)
            haff_combine(2, 1)
            yield from compute_mod_gen(1, 2, gtbm, Bgtbm, True, lag=12)
            make_aset(1, bout_d, gtbm, Bgtbm)
            yield from compute_mod_gen(1, 4, vecT, BvecT, True, lag=12)
            to_fm(vecT, BvecT, fmt, Bfmt)
            yield from compute_mod_gen(1, 3, vecT, BvecT, False, lag=12)
            to_fm(vecT, BvecT, fmt2, Bfmt2)
            haff_combine(3, 2)
            yield from compute_mod_gen(1, 5, MS["gtmp"], MS["Bgtmp"], True, lag=12)

        moe0_job = [None]

        def tile_hook_moe0(c, t):
            if c < 1:
                return
            if moe0_job[0] is None:
                moe0_job[0] = mod_jobs_moe0()
            next(moe0_job[0], None)

        es2 = ExitStack()
        cur_es[0] = es2
        bank_pool[0] = [0, 1, 2, 3]
        with es2:
            alloc_mod_scratch()
            MS["gtmp"] = sb("gtmp", [128, D]); MS["Bgtmp"] = Buf("gtmp")
            eh0, ef0 = make_end_hook("mid", haff=2)
            moe_phase(0, None, nchunks=(NCHUNK_DBG or 16), tile_hook=tile_hook_moe0, end_hook=(eh0 if USE_END_HOOK else None))
            for _ in (moe0_job[0] or ()):
                pass
            if USE_END_HOOK:
                ef0()
            else:
                finalize_seq("mid", haff=2)
            dma("pool", "dw", W[:, 4096:8192].rearrange("p (k n) -> p k n", k=8),
                win_d[:, 2048:2560].rearrange("(k p) n -> p k n", p=128), writes=[BW[0], BW[1]])
            if stop_after == "moe0":
                dump_and_end()
                return nc
            S.op("pool", lambda e: e.tensor_copy(out=gtbf[:], in_=MS["gtmp"][:]), reads=[MS["Bgtmp"]], writes=[Bgtbf])
            make_aset(2, None, None, None)
        S.barrier()
        cur_es[0] = es

        es3 = ExitStack()
        cur_es[0] = es3
        bank_pool[0] = list(range(8))
        with es3:
            gst = sb("gst", [128, NT, 4, 6]); Bgst = Buf("gst")
            mvg = sb("mvg", [128, NT, 2]); Bmvg = Buf("mvg")
            sdg = sb("sdg", [128, NT]); rstdg = sb("rstdg", [128, NT]); nmrg = sb("nmrg", [128, NT])
            Bsdg, Brstdg, Bnmrg = Buf("sdg"), Buf("rstdg"), Buf("nmrg")
            u32 = [sb(f"u32_{i}", [128, 512]) for i in range(2)]; Bu32 = [Buf(f"u32_{i}") for i in range(2)]
            v32 = [sb(f"v32_{i}", [128, 512]) for i in range(2)]; Bv32 = [Buf(f"v32_{i}") for i in range(2)]
            vln = [sb(f"vln{i}", [128, 512], BF16) for i in range(2)]; Bvln = [Buf(f"vln{i}") for i in range(2)]
            gated = [sb(f"gated{i}", [128, 512], BF16) for i in range(2)]; Bgated = [Buf(f"gated{i}") for i in range(2)]
            gatedT = [sb(f"gatedT{i}", [128, 4, 128], BF16) for i in range(3)]; BgatedT = [Buf(f"gatedT{i}") for i in range(3)]
            lngb = [sb(f"lngb{i}", [128, 2, 512]) for i in range(2)]; Blngb = [Buf(f"lngb{i}") for i in range(2)]
            binr = [sb(f"binr{i}", [1, 2, 512], BF16) for i in range(2)]; Bbinr = [Buf(f"binr{i}") for i in range(2)]
            wsTm = sb("wsTm", [128, 8, 128], BF16); BwsT = Buf("wsTm")
            trilb = sb("trilb", [128, 128], BF16); Btril = Buf("tril")
            bsT = sb("bsT", [128, 8]); BbsT = Buf("bsT")
            dma("pool", "dw", wsTm[:], wsT_d.rearrange("g s t -> s g t"), writes=[BwsT])
            dma("pool", "dw", trilb[:], tril_d, writes=[Btril])
            dma("sp", "dc", bsT[:], bsT_d, writes=[BbsT])
            S.op("dve", lambda e: e.tensor_tensor(out=wsTm[:], in0=wsTm[:], in1=trilb[:].unsqueeze(1).to_broadcast([128, 8, 128]),
                                                  op=ALU.mult), reads=[BwsT, Btril], writes=[BwsT])

            def gviews(b):
                base = b * 12288
                wu = W[:, base:base + 4096].rearrange("p (k n) -> p k n", k=8)
                wv = W[:, base + 4096:base + 8192].rearrange("p (k n) -> p k n", k=8)
                wo2 = W[:, base + 8192:base + 12288].rearrange("p (j n) -> p j n", j=4)
                return wu, wv, wo2

            def gfold(b):
                _, _, wo2 = gviews(b)
                bw = [BW[2 * b], BW[2 * b + 1]]
                S.op("pool", lambda e: e.tensor_tensor(out=wo2, in0=wo2,
                                                       in1=gtbm[:].unsqueeze(1).to_broadcast([128, 4, D]), op=ALU.mult),
                     reads=bw + [Bgtbm], writes=bw)

            def gload(cb, b, main, skip_wv=False, fold_now=True):
                wu, wv, wo2 = gviews(b)
                bw = [BW[2 * b], BW[2 * b + 1]]
                if not skip_wv:
                    dma("pool", "dw", wv, win_d[:, 2048 + cb * 512:2048 + (cb + 1) * 512].rearrange("(k p) n -> p k n", p=128),
                        writes=bw)
                dma("pool", "dw", binr[b][:, 1, :], bin_d[:, 2048 + cb * 512:2048 + (cb + 1) * 512], writes=[Bbinr[b]])
                if main:
                    dma("pool", "dw", wu, win_d[:, cb * 512:(cb + 1) * 512].rearrange("(k p) n -> p k n", p=128), writes=bw)
                    dma("pool", "dw", binr[b][:, 0, :], bin_d[:, cb * 512:(cb + 1) * 512], writes=[Bbinr[b]])
                    dma("pool", "dw", wo2, wout_d[cb * 512:(cb + 1) * 512, :].rearrange("(j p) n -> p j n", p=128), writes=bw)
                    if fold_now:
                        gfold(b)
                    dma("sp", "dc", lngb[b][:, 0, :], glng_d[:, cb * 512:(cb + 1) * 512].to_broadcast([128, 512]),
                        writes=[Blngb[b]])
                    dma("sp", "dc", lngb[b][:, 1, :], glnb_d[:, cb * 512:(cb + 1) * 512].to_broadcast([128, 512]),
                        writes=[Blngb[b]])

            gload(0, 0, False, skip_wv=True)
            pi = 0
            for cb in range(4):
                b = cb % 2
                if cb + 1 < 4:
                    gload(cb + 1, (cb + 1) % 2, False)
                _, wv, _ = gviews(b)
                bw = [BW[2 * b], BW[2 * b + 1]]
                for t in range(NT):
                    bk = next_bank()
                    i2 = pi % 2
                    pi += 1
                    for k in range(8):
                        S.op("pe", (lambda k=k, bk=bk, t=t, wv=wv: lambda e: e.matmul(
                            banks[bk][:], hT[:, k, t * 128:(t + 1) * 128], wv[:, k, :], start=(k == 0), stop=False))(),
                             reads=[BH[t]] + bw, writes=[PB[bk]])
                    S.op("pe", (lambda bk=bk, b=b: lambda e: e.matmul(banks[bk][:], ones_bf[:], binr[b][:, 1, :],
                                                                      start=False, stop=True))(),
                         reads=[Bones, Bbinr[b]], writes=[PB[bk]])
                    S.op("act", (lambda bk=bk, i2=i2: lambda e: e.activation(out=v32[i2][:], in_=banks[bk][:], func=AF.Gelu))(),
                         reads=[PB[bk]], writes=[Bv32[i2]])
                    S.op("dve", (lambda i2=i2, t=t, cb=cb: lambda e: e.bn_stats(out=gst[:, t, cb, :], in_=v32[i2][:]))(),
                         reads=[Bv32[i2]], writes=[Bgst])
            for t in range(NT):
                S.op("dve", (lambda t=t: lambda e: e.bn_aggr(out=mvg[:, t, :], in_=gst[:, t, :, :]))(),
                     reads=[Bgst], writes=[Bmvg])
            S.op("act", lambda e: e.activation(out=sdg[:], in_=mvg[:, :, 1], func=AF.Ln, bias=eps_t[:]),
                 reads=[Bmvg, Beps], writes=[Bsdg])
            S.op("act", lambda e: e.activation(out=rstdg[:], in_=sdg[:], func=AF.Exp, scale=-0.5),
                 reads=[Bsdg], writes=[Brstdg])
            S.op("dve", lambda e: e.scalar_tensor_tensor(out=nmrg[:], in0=mvg[:, :, 0], scalar=-1.0, in1=rstdg[:],
                                                         op0=ALU.mult, op1=ALU.mult), reads=[Bmvg, Brstdg], writes=[Bnmrg])

            gsteps = [(cb, t) for cb in range(4) for t in range(NT)]
            ng = len(gsteps)

            def gA(i):
                cb, t = gsteps[i]
                b = cb % 2
                if t == 3 and cb + 1 < 4:
                    gload(cb + 1, (cb + 1) % 2, True, fold_now=False)
                if t == 13 and cb + 1 < 4:
                    gfold((cb + 1) % 2)
                wu, wv, _ = gviews(b)
                bw = [BW[2 * b], BW[2 * b + 1]]
                i2 = i % 2
                for which, wmat, dst, bdst in ((0, wu, u32[i2], Bu32[i2]), (1, wv, v32[i2], Bv32[i2])):
                    bk = next_bank()
                    for k in range(8):
                        S.op("pe", (lambda k=k, bk=bk, wmat=wmat: lambda e: e.matmul(
                            banks[bk][:], hT[:, k, t * 128:(t + 1) * 128], wmat[:, k, :], start=(k == 0), stop=False))(),
                             reads=[BH[t]] + bw, writes=[PB[bk]])
                    S.op("pe", (lambda bk=bk, which=which: lambda e: e.matmul(banks[bk][:], ones_bf[:], binr[b][:, which, :],
                                                                              start=False, stop=True))(),
                         reads=[Bones, Bbinr[b]], writes=[PB[bk]])
                    S.op("act", (lambda bk=bk, dst=dst: lambda e: e.activation(out=dst[:], in_=banks[bk][:], func=AF.Gelu))(),
                         reads=[PB[bk]], writes=[bdst])
                S.op("dve", lambda e: e.tensor_scalar(out=v32[i2][:], in0=v32[i2][:], scalar1=rstdg[:, t:t + 1],
                                                      scalar2=nmrg[:, t:t + 1], op0=ALU.mult, op1=ALU.add),
                     reads=[Bv32[i2], Brstdg, Bnmrg], writes=[Bv32[i2]])
                S.op("pool", lambda e: e.tensor_tensor(out=v32[i2][:], in0=v32[i2][:], in1=lngb[b][:, 0, :], op=ALU.mult),
                     reads=[Bv32[i2], Blngb[b]], writes=[Bv32[i2]])
                S.op("pool", lambda e: e.tensor_tensor(out=vln[i2][:], in0=v32[i2][:], in1=lngb[b][:, 1, :], op=ALU.add),
                     reads=[Bv32[i2], Blngb[b]], writes=[Bvln[i2]])

            def gB(i):
                cb, t = gsteps[i]
                i2 = i % 2
                bk = next_bank()
                for gi in range(2):
                    g = 2 * cb + gi
                    S.op("pe", (lambda gi=gi, g=g: lambda e: e.matmul(
                        banks[bk][:, gi * 256:(gi + 1) * 256], wsTm[:, g, :], vln[i2][:, gi * 256:(gi + 1) * 256],
                        start=True, stop=True))(), reads=[BwsT, Bvln[i2]], writes=[PB[bk]])
                for gi in range(2):
                    g = 2 * cb + gi
                    S.op("dve", (lambda gi=gi, g=g: lambda e: e.scalar_tensor_tensor(
                        out=gated[i2][:, gi * 256:(gi + 1) * 256], in0=banks[bk][:, gi * 256:(gi + 1) * 256],
                        scalar=bsT[:, g:g + 1], in1=u32[i2][:, gi * 256:(gi + 1) * 256], op0=ALU.add, op1=ALU.mult))(),
                         reads=[PB[bk], BbsT, Bu32[i2]], writes=[Bgated[i2]])

            def gC(i):
                i2 = i % 2
                bt = next_bank()
                i3 = i % 3
                for j in range(4):
                    S.op("pe", (lambda j=j: lambda e: e.matmul(
                        banks[bt][:, j * 128:(j + 1) * 128], gated[i2][:, j * 128:(j + 1) * 128], ident[:],
                        start=True, stop=True))(),
                         reads=[Bgated[i2], Bident], writes=[PB[bt]])
                S.op("act", lambda e: e.activation(out=gatedT[i3][:], in_=banks[bt][:].rearrange("p (j c) -> p j c", j=4),
                                                   func=AF.Copy), reads=[PB[bt]], writes=[BgatedT[i3]])

            def gD(i):
                cb, t = gsteps[i]
                b = cb % 2
                _, _, wo2 = gviews(b)
                bw = [BW[2 * b], BW[2 * b + 1]]
                i3 = i % 3
                for nb in range(2):
                    bk = next_bank()
                    for j in range(4):
                        S.op("pe", (lambda j=j, nb=nb, bk=bk: lambda e: e.matmul(
                            banks[bk][:], gatedT[i3][:, j, :], wo2[:, j, nb * 512:(nb + 1) * 512],
                            start=(j == 0), stop=(j == 3)))(), reads=[BgatedT[i3]] + bw, writes=[PB[bk]])
                    S.op("dve", (lambda nb=nb, bk=bk: lambda e: e.tensor_tensor(
                        out=x[:, t, nb * 512:(nb + 1) * 512], in0=x[:, t, nb * 512:(nb + 1) * 512], in1=banks[bk][:],
                        op=ALU.add))(), reads=[BX[t], PB[bk]], writes=[BX[t]])
                if cb == 3:
                    if t == 3:
                        moe_load(1, 0); moe_load(1, 1)
                    if USE_END_HOOK:
                        ehg(t)

            ehg, efg = make_end_hook("mid", haff=3, route_l=1)
            gload(0, 0, True)
            for i in range(ng + 3):
                if i < ng:
                    gA(i)
                if 1 <= i <= ng:
                    gB(i - 1)
                if 2 <= i <= ng + 1:
                    gC(i - 2)
                if 3 <= i <= ng + 2:
                    gD(i - 3)
            if USE_END_HOOK:
                efg()
            else:
                finalize_seq("mid", haff=3, route_l=1)
            if stop_after == "gmlp":
                dump_and_end()
                return nc
        S.barrier()
        cur_es[0] = es

        es4 = ExitStack()
        cur_es[0] = es4
        bank_pool[0] = [0, 1, 2, 3]
        with es4:
            make_aset(3, None, None, None, scale=1.0)
            eh1, ef1 = make_end_hook("out")
            moe_phase(1, None, nchunks=(NCHUNK_DBG or 16), first_load=2, end_hook=(eh1 if USE_END_HOOK else None))
            if USE_END_HOOK:
                ef1()
            else:
                finalize_seq("out")
            S.wait_all("sp", [Bout])
            S.emit()
    return nc


NCHUNK_DBG = None
USE_END_HOOK = False
ATT_DBG = [NT, None]


_CACHE = {}


def _host_inputs(inputs):
    f32 = np.float32
    x = np.asarray(inputs["x"], f32)
    c = np.asarray(inputs["c"], f32)
    pos = np.asarray(inputs["positions"], np.int32)
    shared = {}
    shared["ident"] = np.eye(128, dtype=f32)
    s = np.arange(128)[:, None]
    q = np.arange(128)[None, :]
    m_cur = np.where(s <= q, 0.0, NEG).astype(f32)
    m_prev = np.where(s > q, 0.0, NEG).astype(f32)
    m_none = np.full((128, 128), NEG, f32)
    inv_freq = (10000.0 ** (-np.arange(0, 64, 2, dtype=f32) / f32(64))).astype(f32)
    shared["invf"] = np.ascontiguousarray(np.broadcast_to(inv_freq[None, :], (128, 32))).astype(f32)
    shared["tril"] = (s <= q).astype(f32)
    shared["ada_w"] = np.ascontiguousarray(inputs["ada_w"], f32)
    shared["ada_b"] = np.ascontiguousarray(inputs["ada_b"], f32)
    g = np.asarray(inputs["post_ln_g"], f32).reshape(4, D)
    b = np.asarray(inputs["post_ln_b"], f32).reshape(4, D)
    shared["post_ln_g"] = np.ascontiguousarray(g)
    shared["post_ln_b"] = np.ascontiguousarray(b)
    shared["post_ln_gT"] = np.ascontiguousarray(g.reshape(4, 8, 128).transpose(0, 2, 1))
    shared["post_ln_bT"] = np.ascontiguousarray(b.reshape(4, 8, 128).transpose(0, 2, 1))
    shared["attn_w_qkv"] = np.ascontiguousarray(inputs["attn_w_qkv"][0], f32)
    shared["attn_b_qkv"] = np.ascontiguousarray(inputs["attn_b_qkv"], f32).reshape(1, 1536)
    shared["attn_sinks"] = np.ascontiguousarray(inputs["attn_sinks"], f32).reshape(1, 16)
    shared["attn_w_o"] = np.ascontiguousarray(inputs["attn_w_o"][0], f32)
    shared["attn_b_o"] = np.ascontiguousarray(inputs["attn_b_o"], f32).reshape(1, D)
    shared["gmlp_w_in"] = np.ascontiguousarray(inputs["gmlp_w_in"][0], f32)
    shared["gmlp_b_in"] = np.ascontiguousarray(inputs["gmlp_b_in"], f32).reshape(1, 4096)
    shared["gmlp_ln_g"] = np.ascontiguousarray(inputs["gmlp_sgu_ln_g"], f32).reshape(1, 2048)
    shared["gmlp_ln_b"] = np.ascontiguousarray(inputs["gmlp_sgu_ln_b"], f32).reshape(1, 2048)
    shared["gmlp_w_sT"] = np.ascontiguousarray(np.asarray(inputs["gmlp_w_s"][0], f32).transpose(0, 2, 1))
    shared["gmlp_b_sT"] = np.ascontiguousarray(np.asarray(inputs["gmlp_b_s"][0], f32).T)
    shared["gmlp_w_out"] = np.ascontiguousarray(inputs["gmlp_w_out"][0], f32)
    shared["gmlp_b_out"] = np.ascontiguousarray(inputs["gmlp_b_out"], f32).reshape(1, D)
    shared["moe_wr"] = np.ascontiguousarray(np.concatenate(
        [np.asarray(inputs["moe_w_group_router"], f32), np.asarray(inputs["moe_w_expert_router"], f32)], axis=-1))
    shared["moe_br"] = np.ascontiguousarray(np.concatenate(
        [np.asarray(inputs["moe_b_group_router"], f32), np.asarray(inputs["moe_b_expert_router"], f32)], axis=-1)
    ).reshape(2, 1, 36)
    shared["moe_w_gate_up"] = np.ascontiguousarray(inputs["moe_w_gate_up"], f32).reshape(2, 32, D, 512)
    shared["moe_w_down"] = np.ascontiguousarray(inputs["moe_w_down"], f32).reshape(2, 32, 256, D)
    in_maps = []
    for r in range(8):
        bi, qi = r // 4, r % 4
        s0 = qi * 2048
        m = dict(shared)
        xc = np.zeros((17 * 128, D), f32)
        pc = np.zeros((17 * 128,), np.int32)
        if qi > 0:
            xc[:] = x[bi, s0 - 128:s0 + 2048]
            pc[:] = pos[bi, s0 - 128:s0 + 2048]
        else:
            xc[128:] = x[bi, 0:2048]
            pc[128:] = pos[bi, 0:2048]
        m["xin"] = xc
        m["pos"] = np.ascontiguousarray(pc.reshape(17, 128).T)
        m["cT"] = np.ascontiguousarray(c[bi].reshape(8, 128).T)
        mk = np.stack([np.tile(m_cur, (1, 4)), np.tile(m_prev, (1, 4)),
                       np.tile(m_none if qi == 0 else m_prev, (1, 4))]).astype(f32)
        m["masks"] = np.ascontiguousarray(mk)
        in_maps.append(m)
    return in_maps


def kernel(**inputs):
    in_maps = _host_inputs(inputs)
    if "nc" not in _CACHE:
        _CACHE["nc"] = build()
    res = run_bass_kernel_spmd(_CACHE["nc"], in_maps, core_ids=list(range(8)))
    out = np.empty((2, 8192, D), np.float32)
    for r in range(8):
        bi, qi = r // 4, r % 4
        out[bi, qi * 2048:(qi + 1) * 2048] = res.results[r]["out"]
    return out
```

```python
import numpy as np
from contextlib import ExitStack
import concourse.bass as bass
import concourse.mybir as mybir
from concourse.bass_utils import run_bass_kernel_spmd

F32 = mybir.dt.float32
BF16 = mybir.dt.bfloat16
I32 = mybir.dt.int32
AF = mybir.ActivationFunctionType
ALU = mybir.AluOpType
AX = mybir.AxisListType

NT = 16
D = 1024
ALPHA = 4.0 ** 0.25
LN_EPS = 1e-5
TWO_PI = 6.283185307179586
C1 = 6.28125
C2 = TWO_PI - C1
NEG = -30000.0


class Buf:
    __slots__ = ("name", "w", "r")

    def __init__(self, name):
        self.name = name
        self.w = None
        self.r = {}


class Sched:
    ENG = ("pe", "act", "dve", "pool", "sp")
    NSLOT = 8

    def __init__(self, nc, es):
        self.nc = nc
        self.es = es
        self.E = {"pe": nc.tensor, "act": nc.scalar, "dve": nc.vector,
                  "pool": nc.gpsimd, "sp": nc.sync}
        self.cnt = {}
        self.isdma = {}
        self.waited = {e: {} for e in self.ENG}
        self.ops = []
        self.needed = {}
        for e in self.ENG:
            self.cnt[e] = 0
            self.isdma[e] = False
            self.needed[e] = set()

    def dma_proc(self, name):
        self.cnt[name] = 0
        self.isdma[name] = True
        self.needed[name] = set()

    def _add_dep(self, deps, p, v):
        if self.isdma[p]:
            key = (p, (v - 1) % self.NSLOT)
            deps[key] = max(deps.get(key, 0), (v - 1) // self.NSLOT + 1)
        else:
            deps[p] = max(deps.get(p, 0), v)

    def _mk_waits(self, eng, deps):
        waits = []
        wd = self.waited[eng]
        for key, v in deps.items():
            if wd.get(key, 0) >= v:
                continue
            wd[key] = v
            waits.append((key, v))
            if not isinstance(key, tuple):
                self.needed[key].add(v)
        return waits

    def op(self, eng, fn, reads=(), writes=(), proc=None):
        proc = proc or eng
        deps = {}
        for b in reads:
            if b.w is not None:
                p, v = b.w
                if p == eng and eng == "pe":
                    continue
                self._add_dep(deps, p, v)
        for b in writes:
            if b.w is not None:
                p, v = b.w
                if p != eng or eng != "pe":
                    self._add_dep(deps, p, v)
            for p, v in b.r.items():
                if p != eng or eng != "pe":
                    self._add_dep(deps, p, v)
        self.cnt[proc] += 1
        c = self.cnt[proc]
        if self.isdma[proc] and c > self.NSLOT:
            self._add_dep(deps, proc, c - self.NSLOT)
        waits = self._mk_waits(eng, deps)
        self.ops.append((eng, fn, waits, proc, c))
        for b in reads:
            b.r[proc] = c
        for b in writes:
            b.w = (proc, c)
            b.r = {}
        return c

    def _all_deps(self):
        deps = {}
        for p, v in self.cnt.items():
            if v <= 0:
                continue
            if self.isdma[p]:
                for i in range(max(1, v - self.NSLOT + 1), v + 1):
                    self._add_dep(deps, p, i)
            else:
                deps[p] = v
        return deps

    def barrier(self):
        for eng in self.ENG:
            deps = {k: v for k, v in self._all_deps().items() if k != eng}
            waits = self._mk_waits(eng, deps)
            if waits:
                self.ops.append((eng, None, waits, None, 0))

    def wait_all(self, eng, bufs):
        deps = {}
        for b in bufs:
            if b.w is not None:
                self._add_dep(deps, b.w[0], b.w[1])
        for b in bufs:
            if b.w is not None and self.isdma[b.w[0]]:
                p = b.w[0]
                v = self.cnt[p]
                for i in range(max(1, v - self.NSLOT + 1), v + 1):
                    self._add_dep(deps, p, i)
        waits = self._mk_waits(eng, deps)
        self.ops.append((eng, None, waits, None, 0))

    def emit(self):
        nc = self.nc
        sems = {}
        for p in self.cnt:
            if self.isdma[p]:
                for sl in range(self.NSLOT):
                    sems[(p, sl)] = self.es.enter_context(nc.semaphore(f"s_{p}{sl}"))
            else:
                sems[p] = self.es.enter_context(nc.semaphore("s_" + p))
        last_inc = {p: 0 for p in self.cnt}
        for eng, fn, waits, proc, c in self.ops:
            e = self.E[eng]
            for key, v in waits:
                e.wait_ge(sems[key], v * 16 if isinstance(key, tuple) else v)
            if fn is None:
                continue
            ins = fn(e)
            if self.isdma[proc]:
                ins.then_inc(sems[(proc, (c - 1) % self.NSLOT)], 16)
            elif c in self.needed[proc]:
                ins.then_inc(sems[proc], c - last_inc[proc])
                last_inc[proc] = c


def build(stop_after=None):
    nc = bass.Bass("TRN2", target_bir_lowering=False)

    def din(name, shape, dt=F32):
        return nc.dram_tensor(name, list(shape), dt, kind="ExternalInput").ap()

    xin = din("xin", [17 * 128, D])
    pos_d = din("pos", [128, 17], I32)
    cT_d = din("cT", [128, 8])
    ident_d = din("ident", [128, 128])
    masks_d = din("masks", [3, 128, 512])
    invf_d = din("invf", [128, 32])
    tril_d = din("tril", [128, 128])
    ada_w = din("ada_w", [2, D, 6 * D])
    ada_b = din("ada_b", [2, 6 * D])
    lng_d = din("post_ln_g", [4, D])
    lnb_d = din("post_ln_b", [4, D])
    lngT_d = din("post_ln_gT", [4, 128, 8])
    lnbT_d = din("post_ln_bT", [4, 128, 8])
    wqkv_d = din("attn_w_qkv", [D, 1536])
    bqkv_d = din("attn_b_qkv", [1, 1536])
    sinks_d = din("attn_sinks", [1, 16])
    wo_d = din("attn_w_o", [D, D])
    bo_d = din("attn_b_o", [1, D])
    win_d = din("gmlp_w_in", [D, 4096])
    bin_d = din("gmlp_b_in", [1, 4096])
    glng_d = din("gmlp_ln_g", [1, 2048])
    glnb_d = din("gmlp_ln_b", [1, 2048])
    wsT_d = din("gmlp_w_sT", [8, 128, 128])
    bsT_d = din("gmlp_b_sT", [128, 8])
    wout_d = din("gmlp_w_out", [2048, D])
    bout_d = din("gmlp_b_out", [1, D])
    wr_d = din("moe_wr", [2, D, 36])
    br_d = din("moe_br", [2, 1, 36])
    wgu_d = din("moe_w_gate_up", [2, 32, D, 512])
    wdn_d = din("moe_w_down", [2, 32, 256, D])
    out_d = nc.dram_tensor("out", [NT * 128, D], F32, kind="ExternalOutput").ap()

    es = ExitStack()
    with es:
        S = Sched(nc, es)
        for p in ("dx", "dw", "dc", "do"):
            S.dma_proc(p)

        cur_es = [es]

        sb_n = [0]

        def sb(name, shape, dt=F32):
            sb_n[0] += 1
            return cur_es[0].enter_context(nc.sbuf_tensor(f"sb{sb_n[0]}_{name}", list(shape), dt))

        banks = [es.enter_context(nc.psum_tensor(f"bank{i}", [128, 512], F32)) for i in range(8)]
        bankbf = [b[:].bitcast(BF16) for b in banks]
        PB = [Buf(f"bank{i}") for i in range(8)]
        bank_rr = [0]
        bank_pool = [list(range(8))]

        def next_bank():
            bank_rr[0] += 1
            return bank_pool[0][bank_rr[0] % len(bank_pool[0])]

        x = sb("x", [128, NT, D])
        BX = [Buf(f"x{t}") for t in range(NT)]
        hT = sb("hT", [128, 8, NT * 128], BF16)
        BH = [Buf(f"hT{t}") for t in range(NT)]
        W = sb("W", [128, 24576], BF16)
        BW = [Buf(f"W{s}") for s in range(4)]
        A1 = sb("A1", [128, D]); A0 = sb("A0", [128, D])
        BA1, BA0 = Buf("A1"), Buf("A0")
        gtbm = sb("gtbm", [128, D]); gtbf = sb("gtbf", [128, D])
        Bgtbm, Bgtbf = Buf("gtbm"), Buf("gtbf")
        ident = sb("ident", [128, 128], BF16); Bident = Buf("ident")
        ident32 = sb("ident32", [128, 128]); Bident32 = Buf("ident32")
        onesrow = sb("onesrow", [128, 128], BF16); Bonesrow = Buf("onesrow")
        cact_rep = sb("cact_rep", [128, 8, 128], BF16); Bcact = Buf("cact")
        ctmp = sb("ctmp", [128, 8]); Bctmp = Buf("ctmp")
        xnb = [sb(f"xnb{i}", [128, D], BF16) for i in range(2)]
        Bxnb = [Buf(f"xnb{i}") for i in range(2)]
        st_ = [sb(f"st{i}", [128, 2, 6]) for i in range(2)]; mv_ = [sb(f"mv{i}", [128, 2]) for i in range(2)]
        sd_ = [sb(f"sd{i}", [128, 1]) for i in range(2)]
        rstd_ = [sb(f"rstd{i}", [128, 1]) for i in range(2)]; nmr_ = [sb(f"nmr{i}", [128, 1]) for i in range(2)]
        Bst_ = [Buf(f"st{i}") for i in range(2)]; Bmv_ = [Buf(f"mv{i}") for i in range(2)]
        Bsd_ = [Buf(f"sd{i}") for i in range(2)]; Brstd_ = [Buf(f"rstd{i}") for i in range(2)]
        Bnmr_ = [Buf(f"nmr{i}") for i in range(2)]
        fin_i = [0]
        eps_t = sb("eps_t", [128, 1]); Beps = Buf("eps")
        H1 = [sb(f"H1_{i}", [128, 8]) for i in range(4)]
        H0 = [sb(f"H0_{i}", [128, 8]) for i in range(4)]
        BHa = [Buf(f"Haff{i}") for i in range(4)]
        fmt = sb("fmt", [128, 8]); fmt2 = sb("fmt2", [128, 8]); fmg = sb("fmg", [128, 8]); fmb = sb("fmb", [128, 8])
        Bfmt, Bfmt2, Bfmg, Bfmb = Buf("fmt"), Buf("fmt2"), Buf("fmg"), Buf("fmb")
        wr = [sb(f"wr{l}", [128, 8, 36], BF16) for l in range(2)]
        brr = [sb(f"brr{l}", [128, 36], BF16) for l in range(2)]
        Bwr = [Buf(f"wr{l}") for l in range(2)]
        lg = sb("lg", [128, NT, 36]); Blg = Buf("lg")
        comb = sb("comb", [128, NT, 32]); Bcomb = Buf("comb")
        Bout = Buf("out")

        def dma(eng, proc, out, in_, reads=(), writes=()):
            S.op(eng, lambda e: e.dma_start(out=out, in_=in_), reads=reads, writes=writes, proc=proc)

        def bc_load(dst, bdst, row_ap):
            n = row_ap.shape[-1]
            dma("sp", "dc", dst[:, 0:n], row_ap.to_broadcast([128, n]), writes=[bdst])

        MS = {}

        def alloc_mod_scratch():
            MS["vecT"] = sb("vecT", [128, D]); MS["BvecT"] = Buf("vecT")
            MS["bcT"] = sb("bcT", [128, D]); MS["BbcT"] = Buf("bcT")
            MS["stg"] = [sb(f"stg{i}", [128, 8, 256], BF16) for i in range(2)]
            MS["Bstg"] = [Buf(f"stg{i}") for i in range(2)]

        dma("pool", "dw", ident[:], ident_d, writes=[Bident])
        dma("sp", "dc", ident32[:], ident_d, writes=[Bident32])
        S.op("dve", lambda e: e.memset(eps_t[:], LN_EPS), writes=[Beps])
        S.op("dve", lambda e: e.memset(onesrow[:], 0.0), writes=[Bonesrow])
        S.op("dve", lambda e: e.memset(onesrow[0:1, :], 1.0), writes=[Bonesrow])
        dma("sp", "dc", ctmp[:], cT_d, writes=[Bctmp])
        S.op("act", lambda e: e.activation(out=ctmp[:], in_=ctmp[:], func=AF.Silu), reads=[Bctmp], writes=[Bctmp])
        S.op("dve", lambda e: e.tensor_copy(out=cact_rep[:], in_=ctmp[:].unsqueeze(2).to_broadcast([128, 8, 128])),
             reads=[Bctmp], writes=[Bcact])
        for l in range(2):
            dma("pool", "dw", wr[l][:], wr_d[l].rearrange("(k p) n -> p k n", p=128), writes=[Bwr[l]])
            S.op("pool", (lambda l=l: lambda e: e.memset(brr[l][:], 0.0))(), writes=[Bwr[l]])
            dma("pool", "dw", brr[l][0:1, :], br_d[l], writes=[Bwr[l]])

        def compute_mod_gen(l, j, dst, bdst, add_one, lag=0):
            bcT, BbcT, stg, Bstg = MS["bcT"], MS["BbcT"], MS["stg"], MS["Bstg"]
            bc_load(bcT, BbcT, ada_b[l:l + 1, j * D:(j + 1) * D])

            def issue(nb):
                si = nb % 2
                col = j * D + nb * 256
                dma("pool", "dw", stg[si][:], ada_w[l][:, col:col + 256].rearrange("(k p) n -> p k n", p=128),
                    writes=[Bstg[si]])

            def consume(nb):
                si = nb % 2
                bk = next_bank()
                for k in range(8):
                    S.op("pe", (lambda k=k: lambda e: e.matmul(
                        banks[bk][:, 0:256], cact_rep[:, k, :], stg[si][:, k, :], start=(k == 0), stop=(k == 7)))(),
                         reads=[Bcact, Bstg[si]], writes=[PB[bk]])
                S.op("dve", lambda e: e.scalar_tensor_tensor(
                    out=dst[:, nb * 256:(nb + 1) * 256], in0=banks[bk][:, 0:256], scalar=(1.0 if add_one else 0.0),
                    in1=bcT[:, nb * 256:(nb + 1) * 256], op0=ALU.add, op1=ALU.add),
                     reads=[PB[bk], BbcT], writes=[bdst])

            issue(0); issue(1)
            for _ in range(lag):
                yield
            consume(0); consume(1)
            issue(2); issue(3)
            for _ in range(lag):
                yield
            consume(2); consume(3)

        def compute_mod(l, j, dst, bdst, add_one):
            for _ in compute_mod_gen(l, j, dst, bdst, add_one):
                pass

        def to_fm(src, bsrc, dst, bdst):
            tmp, Btmp = MS["bcT"], MS["BbcT"]
            S.op("dve", lambda e: e.tensor_tensor(
                out=tmp[:].rearrange("p (k c) -> p k c", k=8), in0=src[:].rearrange("p (k c) -> p k c", k=8),
                in1=ident32[:].unsqueeze(1).to_broadcast([128, 8, 128]), op=ALU.mult),
                 reads=[bsrc, Bident32], writes=[Btmp])
            S.op("dve", lambda e: e.tensor_reduce(out=dst[:], in_=tmp[:].rearrange("p (k c) -> p k c", k=8),
                                                  axis=AX.X, op=ALU.add),
                 reads=[Btmp], writes=[bdst])

        def make_haff(idx, l, j_sh, j_sc, ln_idx):
            vecT, BvecT = MS["vecT"], MS["BvecT"]
            compute_mod(l, j_sc, vecT, BvecT, True)
            to_fm(vecT, BvecT, fmt, Bfmt)
            compute_mod(l, j_sh, vecT, BvecT, False)
            to_fm(vecT, BvecT, fmt2, Bfmt2)
            haff_combine(idx, ln_idx)

        def haff_combine(idx, ln_idx):
            if ln_idx is None:
                S.op("dve", lambda e: e.tensor_copy(out=H1[idx][:], in_=fmt[:]), reads=[Bfmt], writes=[BHa[idx]])
                S.op("dve", lambda e: e.tensor_copy(out=H0[idx][:], in_=fmt2[:]), reads=[Bfmt2], writes=[BHa[idx]])
            else:
                dma("sp", "dc", fmg[:], lngT_d[ln_idx], writes=[Bfmg])
                dma("sp", "dc", fmb[:], lnbT_d[ln_idx], writes=[Bfmb])
                S.op("dve", lambda e: e.tensor_tensor(out=H1[idx][:], in0=fmt[:], in1=fmg[:], op=ALU.mult),
                     reads=[Bfmt, Bfmg], writes=[BHa[idx]])
                S.op("dve", lambda e: e.tensor_tensor(out=fmb[:], in0=fmt[:], in1=fmb[:], op=ALU.mult),
                     reads=[Bfmt, Bfmb], writes=[Bfmb])
                S.op("dve", lambda e: e.tensor_tensor(out=H0[idx][:], in0=fmb[:], in1=fmt2[:], op=ALU.add),
                     reads=[Bfmb, Bfmt2], writes=[BHa[idx]])

        def make_aset(ln_idx, bias_row, gtb, bgtb, scale=ALPHA):
            bc_load(A1, BA1, lng_d[ln_idx:ln_idx + 1, :])
            bc_load(A0, BA0, lnb_d[ln_idx:ln_idx + 1, :])
            if scale != 1.0:
                S.op("dve", lambda e: e.tensor_scalar(out=A1[:], in0=A1[:], scalar1=scale, scalar2=None, op0=ALU.mult),
                     reads=[BA1], writes=[BA1])
            if bias_row is None:
                if scale != 1.0:
                    S.op("dve", lambda e: e.tensor_scalar(out=A0[:], in0=A0[:], scalar1=scale, scalar2=None, op0=ALU.mult),
                         reads=[BA0], writes=[BA0])
            else:
                vt, bvt = MS["vecT"], MS["BvecT"]
                bc_load(vt, bvt, bias_row)
                S.op("dve", lambda e: e.tensor_tensor(out=vt[:], in0=vt[:], in1=gtb[:], op=ALU.mult),
                     reads=[bvt, bgtb], writes=[bvt])
                S.op("dve", lambda e: e.scalar_tensor_tensor(out=A0[:], in0=A0[:], scalar=scale, in1=vt[:],
                                                             op0=ALU.mult, op1=ALU.add),
                     reads=[BA0, bvt], writes=[BA0])

        def router_logits(l, t):
            bk = next_bank()
            for k in range(8):
                S.op("pe", (lambda k=k: lambda e: e.matmul(
                    banks[bk][:, 0:36], hT[:, k, t * 128:(t + 1) * 128], wr[l][:, k, :], start=(k == 0), stop=False))(),
                     reads=[BH[t], Bwr[l]], writes=[PB[bk]])
            S.op("pe", lambda e: e.matmul(banks[bk][:, 0:36], onesrow[:], brr[l][:], start=False, stop=True),
                 reads=[Bonesrow, Bwr[l]], writes=[PB[bk]])
            S.op("dve", lambda e: e.tensor_copy(out=lg[:, t, :], in_=banks[bk][:, 0:36]), reads=[PB[bk]], writes=[Blg])

        def finalize_a(t, mode, src=None, bsrc=None, part=0):
            xa = x[:, t, :] if src is None else src
            bxa = BX[t] if bsrc is None else bsrc
            i = fin_i[0] % 2
            fin_i[0] += 1
            st, mv, sd, rstd, nmr = st_[i], mv_[i], sd_[i], rstd_[i], nmr_[i]
            Bst, Bmv, Bsd, Brstd, Bnmr = Bst_[i], Bmv_[i], Bsd_[i], Brstd_[i], Bnmr_[i]
            if mode == "pro":
                S.op("act", lambda e: e.activation(out=xnb[i][:], in_=xa, func=AF.Copy), reads=[bxa], writes=[Bxnb[i]])
                if src is None:
                    S.op("dve", lambda e: e.scalar_tensor_tensor(out=xa, in0=xa, scalar=ALPHA, in1=A0[:],
                                                                 op0=ALU.mult, op1=ALU.add),
                         reads=[bxa, BA0], writes=[bxa])
                return i
            S.op("dve", lambda e: e.bn_stats(out=st[:, 0, :], in_=xa[:, 0:512]), reads=[bxa], writes=[Bst])
            S.op("dve", lambda e: e.bn_stats(out=st[:, 1, :], in_=xa[:, 512:1024]), reads=[bxa], writes=[Bst])
            S.op("dve", lambda e: e.bn_aggr(out=mv[:], in_=st[:]), reads=[Bst], writes=[Bmv])
            if part == 1:
                return i
            return finalize_a2(t, mode, i, src=src, bsrc=bsrc)

        def finalize_a2(t, mode, i, src=None, bsrc=None):
            xa = x[:, t, :] if src is None else src
            bxa = BX[t] if bsrc is None else bsrc
            st, mv, sd, rstd, nmr = st_[i], mv_[i], sd_[i], rstd_[i], nmr_[i]
            Bst, Bmv, Bsd, Brstd, Bnmr = Bst_[i], Bmv_[i], Bsd_[i], Brstd_[i], Bnmr_[i]
            S.op("act", lambda e: e.activation(out=sd[:], in_=mv[:, 1:2], func=AF.Ln, bias=eps_t[:]),
                 reads=[Bmv, Beps], writes=[Bsd])
            S.op("act", lambda e: e.activation(out=rstd[:], in_=sd[:], func=AF.Exp, scale=-0.5), reads=[Bsd], writes=[Brstd])
            S.op("dve", lambda e: e.scalar_tensor_tensor(out=nmr[:], in0=mv[:, 0:1], scalar=-1.0, in1=rstd[:],
                                                         op0=ALU.mult, op1=ALU.mult),
                 reads=[Bmv, Brstd], writes=[Bnmr])
            if mode == "mid":
                S.op("act", lambda e: e.activation(out=xnb[i][:], in_=xa, func=AF.Identity, scale=rstd[:], bias=nmr[:]),
                     reads=[bxa, Brstd, Bnmr], writes=[Bxnb[i]])
            S.op("dve", lambda e: e.tensor_scalar(out=xa, in0=xa, scalar1=rstd[:], scalar2=nmr[:],
                                                  op0=ALU.mult, op1=ALU.add),
                 reads=[bxa, Brstd, Bnmr], writes=[bxa])
            S.op("dve" if mode == "out" else "pool", lambda e: e.tensor_tensor(out=xa, in0=xa, in1=A1[:], op=ALU.mult),
                 reads=[bxa, BA1], writes=[bxa])
            S.op("pool", lambda e: e.tensor_tensor(out=xa, in0=xa, in1=A0[:], op=ALU.add),
                 reads=[bxa, BA0], writes=[bxa])
            if mode == "out":
                dma("sp", "do", out_d[t * 128:(t + 1) * 128, :], xa, reads=[bxa], writes=[Bout])
            return i

        def finalize_b(t, i, haff, hdst=None, bhdst=None):
            bks = [next_bank(), next_bank()]
            for k in range(8):
                S.op("pe", (lambda k=k: lambda e: e.matmul(
                    banks[bks[k // 4]][:, (k % 4) * 128:(k % 4 + 1) * 128], xnb[i][:, k * 128:(k + 1) * 128], ident[:],
                    start=True, stop=True))(),
                     reads=[Bxnb[i], Bident], writes=[PB[bks[k // 4]]])
            hd = hT[:, :, t * 128:(t + 1) * 128] if hdst is None else hdst
            bhd = BH[t] if bhdst is None else bhdst
            for k in range(8):
                if k % 2 == 0:
                    S.op("act", (lambda k=k: lambda e: e.activation(
                        out=hd[:, k, :], in_=banks[bks[k // 4]][:, (k % 4) * 128:(k % 4 + 1) * 128], func=AF.Identity,
                        scale=H1[haff][:, k:k + 1], bias=H0[haff][:, k:k + 1]))(),
                         reads=[PB[bks[k // 4]], BHa[haff]], writes=[bhd])
                else:
                    S.op("dve", (lambda k=k: lambda e: e.tensor_scalar(
                        out=hd[:, k, :], in0=banks[bks[k // 4]][:, (k % 4) * 128:(k % 4 + 1) * 128],
                        scalar1=H1[haff][:, k:k + 1], scalar2=H0[haff][:, k:k + 1], op0=ALU.mult, op1=ALU.add))(),
                         reads=[PB[bks[k // 4]], BHa[haff]], writes=[bhd])

        def finalize(t, mode, haff=None, hdst=None, bhdst=None, route_l=None, src=None, bsrc=None):
            i = finalize_a(t, mode, src=src, bsrc=bsrc)
            if mode == "out":
                return
            finalize_b(t, i, haff, hdst=hdst, bhdst=bhdst)
            if route_l is not None:
                router_logits(route_l, t)

        def finalize_seq(mode, haff=None, route_l=None, hook=None):
            idx = {}
            if mode == "pro":
                idx[0] = finalize_a(0, mode)
                for t in range(NT):
                    if hook is not None:
                        hook(t)
                    if t + 1 < NT:
                        idx[t + 1] = finalize_a(t + 1, mode)
                    finalize_b(t, idx[t], haff)
                return
            idx[0] = finalize_a(0, mode, part=1)
            idx[1] = finalize_a(1, mode, part=1)
            finalize_a2(0, mode, idx[0])
            for t in range(NT):
                if hook is not None:
                    hook(t)
                if t + 2 < NT:
                    idx[t + 2] = finalize_a(t + 2, mode, part=1)
                if t + 1 < NT:
                    finalize_a2(t + 1, mode, idx[t + 1])
                if mode != "out":
                    finalize_b(t, idx[t], haff)
                    if route_l is not None and t >= 1:
                        router_logits(route_l, t - 1)
            if mode != "out" and route_l is not None:
                router_logits(route_l, NT - 1)

        def make_end_hook(mode, haff=None, route_l=None):
            st = {}

            def stage_b(t):
                if mode != "out":
                    finalize_b(t, st[t], haff)
                    if route_l is not None:
                        router_logits(route_l, t)

            def hook(t):
                st[t] = finalize_a(t, mode, part=1)
                if t >= 1:
                    finalize_a2(t - 1, mode, st[t - 1])
                if t >= 2:
                    stage_b(t - 2)

            def flush():
                finalize_a2(NT - 1, mode, st[NT - 1])
                stage_b(NT - 2)
                stage_b(NT - 1)
            return hook, flush

        def dump_and_end():
            for t in range(NT):
                dma("sp", "do", out_d[t * 128:(t + 1) * 128, :], x[:, t, :], reads=[BX[t]], writes=[Bout])
            S.wait_all("sp", [Bout])
            S.emit()

        def moe_views(s):
            base = s * 6144
            wgu = W[:, base:base + 4096].rearrange("p (k n) -> p k n", k=8)
            wdn = W[:, base + 4096:base + 6144].rearrange("p (j n) -> p j n", j=2)
            return wgu, wdn

        def moe_load(l, ex):
            s = ex % 4
            wgu, wdn = moe_views(s)
            dma("pool", "dw", wgu, wgu_d[l, ex].rearrange("(k p) n -> p k n", p=128), writes=[BW[s]])
            dma("pool", "dw", wdn, wdn_d[l, ex].rearrange("(j p) n -> p j n", p=128), writes=[BW[s]])
            S.op("pool", lambda e: e.tensor_tensor(out=wdn, in0=wdn, in1=gtbf[:].unsqueeze(1).to_broadcast([128, 2, D]),
                                                   op=ALU.mult), reads=[BW[s], Bgtbf], writes=[BW[s]])

        esA = ExitStack()
        cur_es[0] = esA
        with esA:
            cosT = sb("cosT", [128, 17, 32]); sinT = sb("sinT", [128, 17, 32])
            Bcos, Bsin = Buf("cos"), Buf("sin")
            hTh = sb("hTh", [128, 8, 128], BF16); BhTh = Buf("hTh")
            bqkv = sb("bqkv", [128, 1536], BF16); Bbqkv = Buf("bqkv")
            esink = sb("esink", [128, 16]); Besink = Buf("esink")
            maskb = sb("maskb", [128, 3, 512], BF16); Bmask = Buf("maskb")
            Wqkv = W[:, 0:12288].rearrange("p (k n) -> p k n", k=8)
            Wo = W[:, 12288:20480].rearrange("p (k n) -> p k n", k=8)

            es0 = ExitStack()
            cur_es[0] = es0
            with es0:
                alloc_mod_scratch()
                posi = sb("posi", [128, 17], I32); posf = sb("posf", [128, 17]); invf = sb("invf", [128, 32])
                ang = sb("ang", [128, 17, 32]); ki = sb("ki", [128, 17, 32], I32)
                kf = W[:, 22528:22528 + 1088].bitcast(F32).rearrange("p (t f) -> p t f", t=17)
                Bpos, Binvf, Bang, Bkf, Bki = Buf("pos"), Buf("invf"), Buf("ang"), Buf("kf"), Buf("ki")
                xh = W[:, 20480:22528].bitcast(F32); Bxh = Buf("xh")

                make_haff(0, 0, 0, 1, None)
                compute_mod(0, 2, gtbm, Bgtbm, True)
                bc_load(A0, BA0, bo_d)
                S.op("dve", lambda e: e.tensor_tensor(out=A0[:], in0=A0[:], in1=gtbm[:], op=ALU.mult),
                     reads=[BA0, Bgtbm], writes=[BA0])
                dma("pool", "dw", Wqkv, wqkv_d.rearrange("(k p) n -> p k n", p=128), writes=[BW[0], BW[1]])
                dma("pool", "dw", Wo, wo_d.rearrange("(k p) n -> p k n", p=128), writes=[BW[2], BW[3]])
                S.op("pool", lambda e: e.memset(bqkv[:], 0.0), writes=[Bbqkv])
                dma("pool", "dw", bqkv[0:1, :], bqkv_d, writes=[Bbqkv])
                bc_load(esink, Besink, sinks_d)
                S.op("act", lambda e: e.activation(out=esink[:], in_=esink[:], func=AF.Exp), reads=[Besink], writes=[Besink])
                dma("pool", "dw", maskb[:], masks_d.rearrange("m p n -> p m n"), writes=[Bmask])
                dma("sp", "dc", posi[:], pos_d, writes=[Bpos])
                dma("sp", "dc", invf[:], invf_d, writes=[Binvf])
                S.op("dve", lambda e: e.tensor_copy(out=posf[:], in_=posi[:]), reads=[Bpos], writes=[Bpos])
                S.op("dve", lambda e: e.tensor_tensor(out=ang[:], in0=posf[:].unsqueeze(2).to_broadcast([128, 17, 32]),
                                                      in1=invf[:].unsqueeze(1).to_broadcast([128, 17, 32]), op=ALU.mult),
                     reads=[Bpos, Binvf], writes=[Bang])

                def sin_of(dst, bdst, shift):
                    if shift != 0.0:
                        S.op("dve", lambda e: e.tensor_scalar(out=dst[:], in0=ang[:], scalar1=shift, scalar2=None, op0=ALU.add),
                             reads=[Bang], writes=[bdst])
                        a_src, ba = dst, bdst
                    else:
                        a_src, ba = ang, Bang
                    S.op("dve", lambda e: e.tensor_scalar(out=ki[:], in0=a_src[:], scalar1=1.0 / TWO_PI, scalar2=None,
                                                          op0=ALU.mult), reads=[ba], writes=[Bki])
                    S.op("dve", lambda e: e.tensor_copy(out=kf, in_=ki[:]), reads=[Bki], writes=[Bkf])
                    S.op("dve", lambda e: e.scalar_tensor_tensor(out=dst[:], in0=kf, scalar=-C1, in1=a_src[:],
                                                                 op0=ALU.mult, op1=ALU.add), reads=[Bkf, ba], writes=[bdst])
                    S.op("dve", lambda e: e.scalar_tensor_tensor(out=dst[:], in0=kf, scalar=-C2, in1=dst[:],
                                                                 op0=ALU.mult, op1=ALU.add), reads=[Bkf, bdst], writes=[bdst])
                    S.op("dve", lambda e: e.tensor_scalar(out=dst[:], in0=dst[:], scalar1=3.1415925, scalar2=-3.1415925,
                                                          op0=ALU.min, op1=ALU.max), reads=[bdst], writes=[bdst])
                    S.op("act", lambda e: e.activation(out=dst[:], in_=dst[:], func=AF.Sin), reads=[bdst], writes=[bdst])

                sin_of(sinT, Bsin, 0.0)
                sin_of(cosT, Bcos, TWO_PI / 4)

                dma("sp", "dx", xh, xin[0:128, :], writes=[Bxh])
                for t in range(NT):
                    dma("sp", "dx", x[:, t, :], xin[(t + 1) * 128:(t + 2) * 128, :], writes=[BX[t]])
                finalize(0, "pro", haff=0, hdst=hTh, bhdst=BhTh, src=xh, bsrc=Bxh)
                def mods_b():
                    vecT, BvecT = MS["vecT"], MS["BvecT"]
                    yield from compute_mod_gen(0, 4, vecT, BvecT, True, lag=3)
                    to_fm(vecT, BvecT, fmt, Bfmt)
                    yield from compute_mod_gen(0, 3, vecT, BvecT, False, lag=3)
                    to_fm(vecT, BvecT, fmt2, Bfmt2)
                    haff_combine(1, 0)
                    yield from compute_mod_gen(0, 5, gtbf, Bgtbf, True, lag=2)

                job_b = mods_b()
                finalize_seq("pro", haff=0, hook=lambda t: next(job_b, None))
                for _ in job_b:
                    pass
                S.op("dve", lambda e: e.tensor_tensor(out=Wo, in0=Wo, in1=gtbm[:].unsqueeze(1).to_broadcast([128, 8, D]),
                                                      op=ALU.mult),
                     reads=[BW[2], BW[3], Bgtbm], writes=[BW[2], BW[3]])
                if stop_after == "pro":
                    dump_and_end()
                    return nc
                make_aset(0, None, None, None)
            S.barrier()

            es1 = ExitStack()
            cur_es[0] = es1
            bank_pool[0] = [3, 6, 7]
            with es1:
                qk32 = sb("qk32", [128, 20, 64]); Bqk32 = Buf("qk32")
                rot = sb("rot", [128, 20, 64], BF16); Brot = Buf("rot")
                rot2 = rot[:].rearrange("p h d -> p (h d)")
                tA = sb("tA", [128, 20, 32]); tB = sb("tB", [128, 20, 32])
                BtA, BtB = Buf("tA"), Buf("tB")
                kpad = sb("kpad", [128, 4, 2, 128], BF16); Bkpad = Buf("kpad")
                qT = sb("qT", [128, 8, 128], BF16); BqT = Buf("qT")
                Vaug = [sb(f"Vaug{i}", [128, 4, 65], BF16) for i in range(3)]
                BV = [Buf(f"V{i}") for i in range(3)]
                ao = sb("ao", [128, 16, 64], BF16); Bao = Buf("ao")
                ao2 = ao[:].rearrange("p h d -> p (h d)")
                aoT = sb("aoT", [128, 8, 128], BF16); BaoT = Buf("aoT")
                dent = sb("dent", [128, 16]); Bdent = Buf("dent")
                kT = [W[:, 20480 + i * 1024:20480 + (i + 1) * 1024].rearrange("p (v c) -> p v c", v=8) for i in range(2)]
                BkT = [Buf(f"kT{i}") for i in range(2)]
                PT = [W[:, 22528 + i * 1024:22528 + (i + 1) * 1024].rearrange("p (b c) -> p b c", b=2) for i in range(2)]
                BPT = [Buf(f"PT{i}") for i in range(2)]
                S.op("pool", lambda e: e.memset(kpad[:], 0.0), writes=[Bkpad])
                for i in range(3):
                    S.op("pool", (lambda i=i: lambda e: e.memset(Vaug[i][:], 1.0))(), writes=[BV[i]])

                def X1(t):
                    halo = t < 0
                    tt = t + 1
                    hsrc = hTh if halo else hT[:, :, t * 128:(t + 1) * 128]
                    bh = BhTh if halo else BH[t]
                    qb = []
                    for nb in ([2] if halo else [0, 1, 2]):
                        bk = nb
                        qb.append((nb, bk))
                        for k in range(8):
                            S.op("pe", (lambda k=k, nb=nb, bk=bk: lambda e: e.matmul(
                                banks[bk][:], hsrc[:, k, :], Wqkv[:, k, nb * 512:(nb + 1) * 512], start=(k == 0), stop=False))(),
                                 reads=[bh, BW[0], BW[1]], writes=[PB[bk]])
                        S.op("pe", (lambda nb=nb, bk=bk: lambda e: e.matmul(
                            banks[bk][:], onesrow[:], bqkv[:, nb * 512:(nb + 1) * 512], start=False, stop=True))(),
                             reads=[Bonesrow, Bbqkv], writes=[PB[bk]])
                    vcur = Vaug[tt % 3]; bvcur = BV[tt % 3]
                    for nb, bk in qb:
                        if nb < 2:
                            S.op("act", (lambda nb=nb, bk=bk: lambda e: e.activation(
                                out=qk32[:, nb * 8:(nb + 1) * 8, :], in_=banks[bk][:].rearrange("p (h d) -> p h d", d=64),
                                func=AF.Copy))(), reads=[PB[bk]], writes=[Bqk32])
                        else:
                            S.op("act", (lambda bk=bk: lambda e: e.activation(
                                out=qk32[:, 16:20, :], in_=banks[bk][:, 0:256].rearrange("p (h d) -> p h d", d=64),
                                func=AF.Copy))(), reads=[PB[bk]], writes=[Bqk32])
                            S.op("act", (lambda bk=bk: lambda e: e.activation(
                                out=vcur[:, :, 0:64], in_=banks[bk][:, 256:512].rearrange("p (h d) -> p h d", d=64),
                                func=AF.Copy))(), reads=[PB[bk]], writes=[bvcur])
                    h0 = 16 if halo else 0
                    nh = 20 - h0
                    x1 = qk32[:, h0:20, 0:32]; x2 = qk32[:, h0:20, 32:64]
                    cb = cosT[:, tt, :].unsqueeze(1).to_broadcast([128, nh, 32])
                    sbc = sinT[:, tt, :].unsqueeze(1).to_broadcast([128, nh, 32])
                    S.op("dve", lambda e: e.tensor_tensor(out=tA[:, h0:20, :], in0=x1, in1=cb, op=ALU.mult),
                         reads=[Bqk32, Bcos], writes=[BtA])
                    S.op("dve", lambda e: e.tensor_tensor(out=tB[:, h0:20, :], in0=x2, in1=sbc, op=ALU.mult),
                         reads=[Bqk32, Bsin], writes=[BtB])
                    if not halo:
                        S.op("dve", lambda e: e.tensor_tensor(out=rot[:, 0:16, 0:32], in0=tA[:, 0:16, :], in1=tB[:, 0:16, :],
                                                              op=ALU.subtract), reads=[BtA, BtB], writes=[Brot])
                    for half in range(2):
                        S.op("dve", (lambda half=half: lambda e: e.tensor_tensor(
                            out=kpad[:, :, half, half * 64:half * 64 + 32], in0=tA[:, 16:20, :], in1=tB[:, 16:20, :],
                            op=ALU.subtract))(), reads=[BtA, BtB], writes=[Bkpad])
                    S.op("dve", lambda e: e.tensor_tensor(out=tA[:, h0:20, :], in0=x2, in1=cb, op=ALU.mult),
                         reads=[Bqk32, Bcos], writes=[BtA])
                    S.op("dve", lambda e: e.tensor_tensor(out=tB[:, h0:20, :], in0=x1, in1=sbc, op=ALU.mult),
                         reads=[Bqk32, Bsin], writes=[BtB])
                    if not halo:
                        S.op("dve", lambda e: e.tensor_tensor(out=rot[:, 0:16, 32:64], in0=tA[:, 0:16, :], in1=tB[:, 0:16, :],
                                                              op=ALU.add), reads=[BtA, BtB], writes=[Brot])
                    for half in range(2):
                        S.op("dve", (lambda half=half: lambda e: e.tensor_tensor(
                            out=kpad[:, :, half, half * 64 + 32:half * 64 + 64], in0=tA[:, 16:20, :], in1=tB[:, 16:20, :],
                            op=ALU.add))(), reads=[BtA, BtB], writes=[Bkpad])

                tb = [3, 6]

                def X2(t):
                    halo = t < 0
                    tt = t + 1
                    cur = tt % 2
                    for v in range(8):
                        S.op("pe", (lambda v=v: lambda e: e.matmul(
                            banks[tb[v // 4]][:, (v % 4) * 128:(v % 4 + 1) * 128], kpad[:, v // 2, v % 2, :], ident[:],
                            start=True, stop=True))(),
                             reads=[Bkpad, Bident], writes=[PB[tb[v // 4]]])
                    for hb in range(2):
                        S.op("act", (lambda hb=hb: lambda e: e.activation(
                            out=kT[cur][:, hb * 4:(hb + 1) * 4, :], in_=banks[tb[hb]][:].rearrange("p (v c) -> p v c", v=4),
                            func=AF.Copy))(), reads=[PB[tb[hb]]], writes=[BkT[cur]])
                    if halo:
                        return
                    for j in range(8):
                        S.op("pe", (lambda j=j: lambda e: e.matmul(
                            banks[tb[j // 4]][:, (j % 4) * 128:(j % 4 + 1) * 128], rot2[:, j * 128:(j + 1) * 128], ident[:],
                            start=True, stop=True))(),
                             reads=[Brot, Bident], writes=[PB[tb[j // 4]]])
                    for hb in range(2):
                        S.op("dve", (lambda hb=hb: lambda e: e.tensor_copy(
                            out=qT[:, hb * 4:(hb + 1) * 4, :], in_=banks[tb[hb]][:].rearrange("p (v c) -> p v c", v=4)))(),
                             reads=[PB[tb[hb]]], writes=[BqT])

                def Y1(t):
                    tt = t + 1
                    cur = tt % 2
                    prv = 1 - cur
                    vcur, bvcur = Vaug[tt % 3], BV[tt % 3]
                    vprv, bvprv = Vaug[(tt - 1) % 3], BV[(tt - 1) % 3]
                    ob = [0, 1, 2]
                    sc_i = [0]
                    first_tile = (t == 0)

                    def scores(g):
                        pi = g % 2
                        for blk, (ktile, bkt, mi) in enumerate([(kT[prv], BkT[prv], 2 if first_tile else 1),
                                                                (kT[cur], BkT[cur], 0)]):
                            bk = 4 + (sc_i[0] % 2)
                            sc_i[0] += 1
                            S.op("pe", (lambda bk=bk, mi=mi: lambda e: e.matmul(
                                banks[bk][:], ident[:], maskb[:, mi, :], start=True, stop=False))(),
                                 reads=[Bident, Bmask], writes=[PB[bk]])
                            for i in range(4):
                                h = 4 * g + i
                                S.op("pe", (lambda bk=bk, i=i, h=h, ktile=ktile, g=g: lambda e: e.matmul(
                                    banks[bk][:, i * 128:(i + 1) * 128], ktile[:, g * 2 + (h % 2), :], qT[:, h // 2, :],
                                    start=False, stop=(i == 3)))(),
                                     reads=[bkt, BqT], writes=[PB[bk]])
                            S.op("act", (lambda bk=bk, blk=blk, pi=pi: lambda e: e.activation(
                                out=PT[pi][:, blk, :], in_=banks[bk][:], func=AF.Exp, scale=0.125))(),
                                 reads=[PB[bk]], writes=[BPT[pi]])

                    def pv(g):
                        pi = g % 2
                        for i in range(4):
                            h = 4 * g + i
                            obk = ob[h // 7]
                            oc = (h % 7) * 65
                            for blk, (vt, bvt) in enumerate([(vprv, bvprv), (vcur, bvcur)]):
                                S.op("pe", (lambda obk=obk, oc=oc, blk=blk, pi=pi, i=i, vt=vt, g=g: lambda e: e.matmul(
                                    banks[obk][:, oc:oc + 65], PT[pi][:, blk, i * 128:(i + 1) * 128], vt[:, g, :],
                                    start=(blk == 0), stop=(blk == 1)))(),
                                     reads=[BPT[pi], bvt], writes=[PB[obk]])

                    scores(0)
                    for g in range(4):
                        if g + 1 < 4:
                            scores(g + 1)
                        pv(g)
                    for b3 in range(3):
                        hs = 7 * b3
                        n = min(7, 16 - hs)
                        ov = banks[ob[b3]][:, 0:n * 65].rearrange("p (h d) -> p h d", d=65)
                        S.op("dve", (lambda ov=ov, hs=hs, n=n: lambda e: e.tensor_tensor(
                            out=dent[:, hs:hs + n], in0=ov[:, :, 64], in1=esink[:, hs:hs + n], op=ALU.add))(),
                             reads=[PB[ob[b3]], Besink], writes=[Bdent])
                        S.op("dve", (lambda hs=hs, n=n: lambda e: e.reciprocal(out=dent[:, hs:hs + n], in_=dent[:, hs:hs + n]))(),
                             reads=[Bdent], writes=[Bdent])
                        S.op("dve", (lambda ov=ov, hs=hs, n=n: lambda e: e.tensor_tensor(
                            out=ao[:, hs:hs + n, :], in0=ov[:, :, 0:64],
                            in1=dent[:, hs:hs + n].unsqueeze(2).to_broadcast([128, n, 64]), op=ALU.mult))(),
                             reads=[PB[ob[b3]], Bdent], writes=[Bao])

                def Y2a(t):
                    for j in range(8):
                        S.op("pe", (lambda j=j: lambda e: e.matmul(
                            banks[tb[j // 4]][:, (j % 4) * 128:(j % 4 + 1) * 128], ao2[:, j * 128:(j + 1) * 128], ident[:],
                            start=True, stop=True))(),
                             reads=[Bao, Bident], writes=[PB[tb[j // 4]]])
                    for hb in range(2):
                        S.op("act", (lambda hb=hb: lambda e: e.activation(
                            out=aoT[:, hb * 4:(hb + 1) * 4, :], in_=banks[tb[hb]][:].rearrange("p (v c) -> p v c", v=4),
                            func=AF.Copy))(), reads=[PB[tb[hb]]], writes=[BaoT])

                def Y2b(t):
                    for nb in range(2):
                        bk = 4 + nb
                        for k in range(8):
                            S.op("pe", (lambda k=k, nb=nb, bk=bk: lambda e: e.matmul(
                                banks[bk][:], aoT[:, k, :], Wo[:, k, nb * 512:(nb + 1) * 512], start=(k == 0), stop=(k == 7)))(),
                                 reads=[BaoT, BW[2], BW[3]], writes=[PB[bk]])
                        S.op("dve", (lambda nb=nb, bk=bk: lambda e: e.tensor_tensor(
                            out=x[:, t, nb * 512:(nb + 1) * 512], in0=x[:, t, nb * 512:(nb + 1) * 512], in1=banks[bk][:],
                            op=ALU.add))(), reads=[BX[t], PB[bk]], writes=[BX[t]])

                X1(-1); X2(-1)
                X1(0); X2(0)
                fi = {}
                for t in range(NT + 3):
                    if 0 <= t - 2 < NT:
                        fi[t - 2] = finalize_a(t - 2, "mid")
                    if t + 1 < NT:
                        X1(t + 1)
                    if t + 1 == NT:
                        moe_load(0, 0); moe_load(0, 1)
                    if 0 <= t - 1 < NT:
                        Y2a(t - 1)
                    if t < NT:
                        Y1(t)
                    if 0 <= t - 1 < NT:
                        Y2b(t - 1)
                    if 0 <= t - 2 < NT:
                        finalize_b(t - 2, fi[t - 2], 1)
                    if t + 1 < NT:
                        X2(t + 1)
                    if 0 <= t - 3 < NT:
                        router_logits(0, t - 3)
                if stop_after == "attn":
                    dump_and_end()
                    return nc
            S.barrier()
        cur_es[0] = es

        def bc3(ap2, n):
            return ap2.unsqueeze(2).to_broadcast([128, NT, n])

        def moe_phase(l, after_chunk=None, nchunks=16, tile_hook=None, first_load=0, end_hook=None):
            gl_m = sb("gl_m", [128, NT]); Bglm = Buf("gl_m")
            goh = sb("goh", [128, NT, 4]); Bgoh = Buf("goh")
            gex = sb("gex", [128, NT, 4]); Bgex = Buf("gex")
            gp = sb("gp", [128, NT]); Bgp = Buf("gp")
            sel4 = sb("sel4", [128, NT, 32]); Bsel4 = Buf("sel4")
            sel = sb("sel", [128, NT, 8]); Bsel = Buf("sel")
            sel2 = sb("sel2", [128, NT, 8]); Bsel2 = Buf("sel2")
            oh1 = sb("oh1", [128, NT, 8]); Boh1 = Buf("oh1")
            oh2 = sb("oh2", [128, NT, 8]); Boh2 = Buf("oh2")
            m1 = sb("m1", [128, NT]); m2 = sb("m2", [128, NT]); Bm1, Bm2 = Buf("m1"), Buf("m2")
            w1 = sb("w1", [128, NT]); w2 = sb("w2", [128, NT]); Bw1, Bw2 = Buf("w1"), Buf("w2")
            sg = [sb(f"sg{i}", [128, 256]) for i in range(2)]
            Bsg = [Buf(f"sg{i}") for i in range(2)]
            actb = [sb(f"actb{i}", [128, 256], BF16) for i in range(2)]
            Bact = [Buf(f"act{i}") for i in range(2)]
            actT = [sb(f"actT{i}", [128, 2, 128], BF16) for i in range(3)]
            BactT = [Buf(f"actT{i}") for i in range(3)]

            for ex in range(first_load, 4):
                moe_load(l, ex)

            gl = lg[:, :, 0:4]
            S.op("dve", lambda e: e.tensor_reduce(out=gl_m[:], in_=gl, axis=AX.X, op=ALU.max), reads=[Blg], writes=[Bglm])
            S.op("dve", lambda e: e.tensor_tensor(out=goh[:], in0=gl, in1=bc3(gl_m[:], 4), op=ALU.is_equal),
                 reads=[Blg, Bglm], writes=[Bgoh])
            S.op("dve", lambda e: e.tensor_tensor(out=gex[:], in0=gl, in1=bc3(gl_m[:], 4), op=ALU.subtract),
                 reads=[Blg, Bglm], writes=[Bgex])
            S.op("act", lambda e: e.activation(out=gex[:], in_=gex[:], func=AF.Exp), reads=[Bgex], writes=[Bgex])
            S.op("dve", lambda e: e.tensor_reduce(out=gp[:], in_=gex[:], axis=AX.X, op=ALU.add), reads=[Bgex], writes=[Bgp])
            S.op("dve", lambda e: e.reciprocal(out=gp[:], in_=gp[:]), reads=[Bgp], writes=[Bgp])
            S.op("dve", lambda e: e.tensor_tensor(
                out=sel4[:].rearrange("p t (g e) -> p t g e", g=4), in0=lg[:, :, 4:36].rearrange("p t (g e) -> p t g e", g=4),
                in1=goh[:].unsqueeze(3).to_broadcast([128, NT, 4, 8]), op=ALU.mult),
                 reads=[Blg, Bgoh], writes=[Bsel4])
            S.op("dve", lambda e: e.tensor_reduce(out=sel[:], in_=sel4[:].rearrange("p t (g e) -> p t e g", g=4),
                                                  axis=AX.X, op=ALU.add), reads=[Bsel4], writes=[Bsel])
            S.op("dve", lambda e: e.tensor_reduce(out=m1[:], in_=sel[:], axis=AX.X, op=ALU.max), reads=[Bsel], writes=[Bm1])
            S.op("dve", lambda e: e.tensor_tensor(out=oh1[:], in0=sel[:], in1=bc3(m1[:], 8), op=ALU.is_equal),
                 reads=[Bsel, Bm1], writes=[Boh1])
            S.op("dve", lambda e: e.scalar_tensor_tensor(out=sel2[:], in0=oh1[:], scalar=-1e30, in1=sel[:],
                                                         op0=ALU.mult, op1=ALU.add), reads=[Boh1, Bsel], writes=[Bsel2])
            S.op("dve", lambda e: e.tensor_reduce(out=m2[:], in_=sel2[:], axis=AX.X, op=ALU.max), reads=[Bsel2], writes=[Bm2])
            S.op("dve", lambda e: e.tensor_tensor(out=oh2[:], in0=sel2[:], in1=bc3(m2[:], 8), op=ALU.is_equal),
                 reads=[Bsel2, Bm2], writes=[Boh2])
            S.op("dve", lambda e: e.tensor_tensor(out=w2[:], in0=m2[:], in1=m1[:], op=ALU.subtract),
                 reads=[Bm1, Bm2], writes=[Bw2])
            S.op("act", lambda e: e.activation(out=w2[:], in_=w2[:], func=AF.Exp), reads=[Bw2], writes=[Bw2])
            S.op("dve", lambda e: e.tensor_scalar(out=w2[:], in0=w2[:], scalar1=1.0, scalar2=None, op0=ALU.add),
                 reads=[Bw2], writes=[Bw2])
            S.op("dve", lambda e: e.reciprocal(out=w1[:], in_=w2[:]), reads=[Bw2], writes=[Bw1])
            S.op("dve", lambda e: e.tensor_tensor(out=w1[:], in0=w1[:], in1=gp[:], op=ALU.mult), reads=[Bw1, Bgp], writes=[Bw1])
            S.op("dve", lambda e: e.tensor_tensor(out=w2[:], in0=gp[:], in1=w1[:], op=ALU.subtract),
                 reads=[Bgp, Bw1], writes=[Bw2])
            S.op("dve", lambda e: e.tensor_tensor(out=oh1[:], in0=oh1[:], in1=bc3(w1[:], 8), op=ALU.mult),
                 reads=[Boh1, Bw1], writes=[Boh1])
            S.op("dve", lambda e: e.tensor_tensor(out=oh2[:], in0=oh2[:], in1=bc3(w2[:], 8), op=ALU.mult),
                 reads=[Boh2, Bw2], writes=[Boh2])
            S.op("dve", lambda e: e.tensor_tensor(out=oh1[:], in0=oh1[:], in1=oh2[:], op=ALU.add),
                 reads=[Boh1, Boh2], writes=[Boh1])
            S.op("dve", lambda e: e.tensor_tensor(
                out=comb[:].rearrange("p t (g e) -> p t g e", g=4),
                in0=goh[:].unsqueeze(3).to_broadcast([128, NT, 4, 8]),
                in1=oh1[:].unsqueeze(2).to_broadcast([128, NT, 4, 8]), op=ALU.mult),
                 reads=[Bgoh, Boh1], writes=[Bcomb])

            GUB = [0, 1]; TB = [2, 3]; YB = [[4, 5], [6, 7]]
            steps = []
            for c in range(nchunks):
                for t in range(NT):
                    for e2 in range(2):
                        steps.append((c, t, e2))
            n = len(steps)

            def GU(i):
                c, t, e2 = steps[i]
                s = (2 * c + e2) % 4
                wgu, _ = moe_views(s)
                bk = GUB[i % 2]
                for k in range(8):
                    S.op("pe", (lambda k=k: lambda e: e.matmul(
                        banks[bk][:], hT[:, k, t * 128:(t + 1) * 128], wgu[:, k, :], start=(k == 0), stop=(k == 7)))(),
                         reads=[BH[t], BW[s]], writes=[PB[bk]])
                si = i % 2
                S.op("act", lambda e: e.activation(out=sg[si][:], in_=banks[bk][:, 0:256], func=AF.Silu),
                     reads=[PB[bk]], writes=[Bsg[si]])
                ex = 2 * c + e2
                S.op("dve", lambda e: e.scalar_tensor_tensor(
                    out=actb[si][:], in0=sg[si][:], scalar=comb[:, t, ex:ex + 1], in1=banks[bk][:, 256:512],
                    op0=ALU.mult, op1=ALU.mult), reads=[Bsg[si], Bcomb, PB[bk]], writes=[Bact[si]])

            def TR(i):
                si = i % 2
                bk = TB[i % 2]
                ti = i % 3
                for j in range(2):
                    S.op("pe", (lambda j=j: lambda e: e.matmul(
                        banks[bk][:, j * 128:(j + 1) * 128], actb[si][:, j * 128:(j + 1) * 128], ident[:],
                        start=True, stop=True))(),
                         reads=[Bact[si], Bident], writes=[PB[bk]])
                S.op("act", lambda e: e.activation(out=actT[ti][:], in_=banks[bk][:, 0:256].rearrange("p (j c) -> p j c", j=2),
                                                   func=AF.Copy), reads=[PB[bk]], writes=[BactT[ti]])

            def DN(i):
                c, t, e2 = steps[i]
                s = (2 * c + e2) % 4
                _, wdn = moe_views(s)
                ti = i % 3
                yb = YB[t % 2]
                for nb in range(2):
                    for j in range(2):
                        S.op("pe", (lambda nb=nb, j=j: lambda e: e.matmul(
                            banks[yb[nb]][:], actT[ti][:, j, :], wdn[:, j, nb * 512:(nb + 1) * 512],
                            start=(e2 == 0 and j == 0), stop=(e2 == 1 and j == 1)))(),
                             reads=[BactT[ti], BW[s]], writes=[PB[yb[nb]]])
                if e2 == 1:
                    for nb in range(2):
                        S.op("dve", (lambda nb=nb: lambda e: e.tensor_tensor(
                            out=x[:, t, nb * 512:(nb + 1) * 512], in0=x[:, t, nb * 512:(nb + 1) * 512],
                            in1=banks[yb[nb]][:], op=ALU.add))(), reads=[BX[t], PB[yb[nb]]], writes=[BX[t]])
                    if tile_hook is not None:
                        tile_hook(c, t)
                    if end_hook is not None and c == nchunks - 1:
                        end_hook(t)
                    if t == NT - 1:
                        if c + 2 < 16 and c + 2 < nchunks:
                            moe_load(l, 2 * (c + 2)); moe_load(l, 2 * (c + 2) + 1)
                        if after_chunk is not None:
                            after_chunk(c)

            for i in range(n + 2):
                if i < n:
                    GU(i)
                if 1 <= i <= n:
                    TR(i - 1)
                if 2 <= i <= n + 1:
                    DN(i - 2)

        def mod_jobs_moe0():
            vecT, BvecT = MS["vecT"], MS["BvecT"]
            yield from compute_mod_gen(1, 1, vecT, BvecT, True, lag=12)
            to_fm(vecT, BvecT, fmt, Bfmt)
            yield from compute_mod_gen(1, 0, vecT, BvecT, False, lag=12)
            to_fm(vecT, BvecT, fmt2, Bfmt2)
            haff_combine(2, 1)
            yield from compute_mod_gen(1, 2, gtbm, Bgtbm, True, lag=12)
            make_aset(1, bout_d, gtbm, Bgtbm)
            yield from compute_mod_gen(1, 4, vecT, BvecT, True, lag=12)
            to_fm(vecT, BvecT, fmt, Bfmt)
            yield from compute_mod_gen(1, 3, vecT, BvecT, False, lag=12)
            to_fm(vecT, BvecT, fmt2, Bfmt2)
            haff_combine(3, 2)
            yield from compute_mod_gen(1, 5, MS["gtmp"], MS["Bgtmp"], True, lag=12)

        moe0_job = [None]

        def tile_hook_moe0(c, t):
            if c < 1:
                return
            if moe0_job[0] is None:
                moe0_job[0] = mod_jobs_moe0()
            next(moe0_job[0], None)

        es2 = ExitStack()
        cur_es[0] = es2
        bank_pool[0] = [0, 1, 2, 3]
        with es2:
            alloc_mod_scratch()
            MS["gtmp"] = sb("gtmp", [128, D]); MS["Bgtmp"] = Buf("gtmp")
            eh0, ef0 = make_end_hook("mid", haff=2)
            moe_phase(0, None, nchunks=(NCHUNK_DBG or 16), tile_hook=tile_hook_moe0, end_hook=(eh0 if USE_END_HOOK else None), first_load=2)
            for _ in (moe0_job[0] or ()):
                pass
            if USE_END_HOOK:
                ef0()
            else:
                finalize_seq("mid", haff=2)
            dma("pool", "dw", W[:, 4096:8192].rearrange("p (k n) -> p k n", k=8),
                win_d[:, 2048:2560].rearrange("(k p) n -> p k n", p=128), writes=[BW[0], BW[1]])
            if stop_after == "moe0":
                dump_and_end()
                return nc
            S.op("pool", lambda e: e.tensor_copy(out=gtbf[:], in_=MS["gtmp"][:]), reads=[MS["Bgtmp"]], writes=[Bgtbf])
            make_aset(2, None, None, None)
        S.barrier()
        cur_es[0] = es

        es3 = ExitStack()
        cur_es[0] = es3
        bank_pool[0] = list(range(8))
        with es3:
            gst = sb("gst", [128, NT, 4, 6]); Bgst = Buf("gst")
            mvg = sb("mvg", [128, NT, 2]); Bmvg = Buf("mvg")
            sdg = sb("sdg", [128, NT]); rstdg = sb("rstdg", [128, NT]); nmrg = sb("nmrg", [128, NT])
            Bsdg, Brstdg, Bnmrg = Buf("sdg"), Buf("rstdg"), Buf("nmrg")
            u32 = [sb(f"u32_{i}", [128, 512]) for i in range(2)]; Bu32 = [Buf(f"u32_{i}") for i in range(2)]
            v32 = [sb(f"v32_{i}", [128, 512]) for i in range(2)]; Bv32 = [Buf(f"v32_{i}") for i in range(2)]
            vln = [sb(f"vln{i}", [128, 512], BF16) for i in range(2)]; Bvln = [Buf(f"vln{i}") for i in range(2)]
            gated = [sb(f"gated{i}", [128, 512], BF16) for i in range(2)]; Bgated = [Buf(f"gated{i}") for i in range(2)]
            gatedT = [sb(f"gatedT{i}", [128, 4, 128], BF16) for i in range(3)]; BgatedT = [Buf(f"gatedT{i}") for i in range(3)]
            lngb = [sb(f"lngb{i}", [128, 2, 512]) for i in range(2)]; Blngb = [Buf(f"lngb{i}") for i in range(2)]
            binr = [sb(f"binr{i}", [128, 2, 512], BF16) for i in range(2)]; Bbinr = [Buf(f"binr{i}") for i in range(2)]
            for i in range(2):
                S.op("pool", (lambda i=i: lambda e: e.memset(binr[i][:], 0.0))(), writes=[Bbinr[i]])
            wsTm = sb("wsTm", [128, 8, 128], BF16); BwsT = Buf("wsTm")
            trilb = sb("trilb", [128, 128], BF16); Btril = Buf("tril")
            bsT = sb("bsT", [128, 8]); BbsT = Buf("bsT")
            dma("pool", "dw", wsTm[:], wsT_d.rearrange("g s t -> s g t"), writes=[BwsT])
            dma("pool", "dw", trilb[:], tril_d, writes=[Btril])
            dma("sp", "dc", bsT[:], bsT_d, writes=[BbsT])
            S.op("dve", lambda e: e.tensor_tensor(out=wsTm[:], in0=wsTm[:], in1=trilb[:].unsqueeze(1).to_broadcast([128, 8, 128]),
                                                  op=ALU.mult), reads=[BwsT, Btril], writes=[BwsT])

            def gviews(b):
                base = b * 12288
                wu = W[:, base:base + 4096].rearrange("p (k n) -> p k n", k=8)
                wv = W[:, base + 4096:base + 8192].rearrange("p (k n) -> p k n", k=8)
                wo2 = W[:, base + 8192:base + 12288].rearrange("p (j n) -> p j n", j=4)
                return wu, wv, wo2

            def gfold(b):
                _, _, wo2 = gviews(b)
                bw = [BW[2 * b], BW[2 * b + 1]]
                S.op("pool", lambda e: e.tensor_tensor(out=wo2, in0=wo2,
                                                       in1=gtbm[:].unsqueeze(1).to_broadcast([128, 4, D]), op=ALU.mult),
                     reads=bw + [Bgtbm], writes=bw)

            def gload(cb, b, main, skip_wv=False, fold_now=True):
                wu, wv, wo2 = gviews(b)
                bw = [BW[2 * b], BW[2 * b + 1]]
                if not skip_wv:
                    dma("pool", "dw", wv, win_d[:, 2048 + cb * 512:2048 + (cb + 1) * 512].rearrange("(k p) n -> p k n", p=128),
                        writes=bw)
                dma("pool", "dw", binr[b][0:1, 1, :], bin_d[:, 2048 + cb * 512:2048 + (cb + 1) * 512], writes=[Bbinr[b]])
                if main:
                    dma("pool", "dw", wu, win_d[:, cb * 512:(cb + 1) * 512].rearrange("(k p) n -> p k n", p=128), writes=bw)
                    dma("pool", "dw", binr[b][0:1, 0, :], bin_d[:, cb * 512:(cb + 1) * 512], writes=[Bbinr[b]])
                    dma("pool", "dw", wo2, wout_d[cb * 512:(cb + 1) * 512, :].rearrange("(j p) n -> p j n", p=128), writes=bw)
                    if fold_now:
                        gfold(b)
                    dma("sp", "dc", lngb[b][:, 0, :], glng_d[:, cb * 512:(cb + 1) * 512].to_broadcast([128, 512]),
                        writes=[Blngb[b]])
                    dma("sp", "dc", lngb[b][:, 1, :], glnb_d[:, cb * 512:(cb + 1) * 512].to_broadcast([128, 512]),
                        writes=[Blngb[b]])

            gload(0, 0, False, skip_wv=True)
            pi = 0
            for cb in range(4):
                b = cb % 2
                if cb + 1 < 4:
                    gload(cb + 1, (cb + 1) % 2, False)
                _, wv, _ = gviews(b)
                bw = [BW[2 * b], BW[2 * b + 1]]
                for t in range(NT):
                    bk = next_bank()
                    i2 = pi % 2
                    pi += 1
                    for k in range(8):
                        S.op("pe", (lambda k=k, bk=bk, t=t, wv=wv: lambda e: e.matmul(
                            banks[bk][:], hT[:, k, t * 128:(t + 1) * 128], wv[:, k, :], start=(k == 0), stop=False))(),
                             reads=[BH[t]] + bw, writes=[PB[bk]])
                    S.op("pe", (lambda bk=bk, b=b: lambda e: e.matmul(banks[bk][:], onesrow[:], binr[b][:, 1, :],
                                                                      start=False, stop=True))(),
                         reads=[Bonesrow, Bbinr[b]], writes=[PB[bk]])
                    S.op("act", (lambda bk=bk, i2=i2: lambda e: e.activation(out=v32[i2][:], in_=banks[bk][:], func=AF.Gelu))(),
                         reads=[PB[bk]], writes=[Bv32[i2]])
                    S.op("dve", (lambda i2=i2, t=t, cb=cb: lambda e: e.bn_stats(out=gst[:, t, cb, :], in_=v32[i2][:]))(),
                         reads=[Bv32[i2]], writes=[Bgst])
            for t in range(NT):
                S.op("dve", (lambda t=t: lambda e: e.bn_aggr(out=mvg[:, t, :], in_=gst[:, t, :, :]))(),
                     reads=[Bgst], writes=[Bmvg])
            S.op("act", lambda e: e.activation(out=sdg[:], in_=mvg[:, :, 1], func=AF.Ln, bias=eps_t[:]),
                 reads=[Bmvg, Beps], writes=[Bsdg])
            S.op("act", lambda e: e.activation(out=rstdg[:], in_=sdg[:], func=AF.Exp, scale=-0.5),
                 reads=[Bsdg], writes=[Brstdg])
            S.op("dve", lambda e: e.scalar_tensor_tensor(out=nmrg[:], in0=mvg[:, :, 0], scalar=-1.0, in1=rstdg[:],
                                                         op0=ALU.mult, op1=ALU.mult), reads=[Bmvg, Brstdg], writes=[Bnmrg])

            gsteps = [(cb, t) for cb in range(4) for t in range(NT)]
            ng = len(gsteps)

            def gA(i):
                cb, t = gsteps[i]
                b = cb % 2
                if t == 3 and cb + 1 < 4:
                    gload(cb + 1, (cb + 1) % 2, True, fold_now=False)
                if t == 13 and cb + 1 < 4:
                    gfold((cb + 1) % 2)
                wu, wv, _ = gviews(b)
                bw = [BW[2 * b], BW[2 * b + 1]]
                i2 = i % 2
                for which, wmat, dst, bdst in ((0, wu, u32[i2], Bu32[i2]), (1, wv, v32[i2], Bv32[i2])):
                    bk = next_bank()
                    for k in range(8):
                        S.op("pe", (lambda k=k, bk=bk, wmat=wmat: lambda e: e.matmul(
                            banks[bk][:], hT[:, k, t * 128:(t + 1) * 128], wmat[:, k, :], start=(k == 0), stop=False))(),
                             reads=[BH[t]] + bw, writes=[PB[bk]])
                    S.op("pe", (lambda bk=bk, which=which: lambda e: e.matmul(banks[bk][:], onesrow[:], binr[b][:, which, :],
                                                                              start=False, stop=True))(),
                         reads=[Bonesrow, Bbinr[b]], writes=[PB[bk]])
                    S.op("act", (lambda bk=bk, dst=dst: lambda e: e.activation(out=dst[:], in_=banks[bk][:], func=AF.Gelu))(),
                         reads=[PB[bk]], writes=[bdst])
                S.op("dve", lambda e: e.tensor_scalar(out=v32[i2][:], in0=v32[i2][:], scalar1=rstdg[:, t:t + 1],
                                                      scalar2=nmrg[:, t:t + 1], op0=ALU.mult, op1=ALU.add),
                     reads=[Bv32[i2], Brstdg, Bnmrg], writes=[Bv32[i2]])
                S.op("pool", lambda e: e.tensor_tensor(out=v32[i2][:], in0=v32[i2][:], in1=lngb[b][:, 0, :], op=ALU.mult),
                     reads=[Bv32[i2], Blngb[b]], writes=[Bv32[i2]])
                S.op("pool", lambda e: e.tensor_tensor(out=vln[i2][:], in0=v32[i2][:], in1=lngb[b][:, 1, :], op=ALU.add),
                     reads=[Bv32[i2], Blngb[b]], writes=[Bvln[i2]])

            def gB(i):
                cb, t = gsteps[i]
                i2 = i % 2
                bk = next_bank()
                for gi in range(2):
                    g = 2 * cb + gi
                    S.op("pe", (lambda gi=gi, g=g: lambda e: e.matmul(
                        banks[bk][:, gi * 256:(gi + 1) * 256], wsTm[:, g, :], vln[i2][:, gi * 256:(gi + 1) * 256],
                        start=True, stop=True))(), reads=[BwsT, Bvln[i2]], writes=[PB[bk]])
                for gi in range(2):
                    g = 2 * cb + gi
                    S.op("dve", (lambda gi=gi, g=g: lambda e: e.scalar_tensor_tensor(
                        out=gated[i2][:, gi * 256:(gi + 1) * 256], in0=banks[bk][:, gi * 256:(gi + 1) * 256],
                        scalar=bsT[:, g:g + 1], in1=u32[i2][:, gi * 256:(gi + 1) * 256], op0=ALU.add, op1=ALU.mult))(),
                         reads=[PB[bk], BbsT, Bu32[i2]], writes=[Bgated[i2]])

            def gC(i):
                i2 = i % 2
                bt = next_bank()
                i3 = i % 3
                for j in range(4):
                    S.op("pe", (lambda j=j: lambda e: e.matmul(
                        banks[bt][:, j * 128:(j + 1) * 128], gated[i2][:, j * 128:(j + 1) * 128], ident[:],
                        start=True, stop=True))(),
                         reads=[Bgated[i2], Bident], writes=[PB[bt]])
                S.op("act", lambda e: e.activation(out=gatedT[i3][:], in_=banks[bt][:].rearrange("p (j c) -> p j c", j=4),
                                                   func=AF.Copy), reads=[PB[bt]], writes=[BgatedT[i3]])

            def gD(i):
                cb, t = gsteps[i]
                b = cb % 2
                _, _, wo2 = gviews(b)
                bw = [BW[2 * b], BW[2 * b + 1]]
                i3 = i % 3
                for nb in range(2):
                    bk = next_bank()
                    for j in range(4):
                        S.op("pe", (lambda j=j, nb=nb, bk=bk: lambda e: e.matmul(
                            banks[bk][:], gatedT[i3][:, j, :], wo2[:, j, nb * 512:(nb + 1) * 512],
                            start=(j == 0), stop=(j == 3)))(), reads=[BgatedT[i3]] + bw, writes=[PB[bk]])
                    S.op("dve", (lambda nb=nb, bk=bk: lambda e: e.tensor_tensor(
                        out=x[:, t, nb * 512:(nb + 1) * 512], in0=x[:, t, nb * 512:(nb + 1) * 512], in1=banks[bk][:],
                        op=ALU.add))(), reads=[BX[t], PB[bk]], writes=[BX[t]])
                if cb == 3:
                    if t == 3:
                        moe_load(1, 0); moe_load(1, 1)
                    if USE_END_HOOK:
                        ehg(t)

            ehg, efg = make_end_hook("mid", haff=3, route_l=1)
            gload(0, 0, True)
            for i in range(ng + 3):
                if i < ng:
                    gA(i)
                if 1 <= i <= ng:
                    gB(i - 1)
                if 2 <= i <= ng + 1:
                    gC(i - 2)
                if 3 <= i <= ng + 2:
                    gD(i - 3)
            if USE_END_HOOK:
                efg()
            else:
                finalize_seq("mid", haff=3, route_l=1)
            if stop_after == "gmlp":
                dump_and_end()
                return nc
        S.barrier()
        cur_es[0] = es

        es4 = ExitStack()
        cur_es[0] = es4
        bank_pool[0] = [0, 1, 2, 3]
        with es4:
            make_aset(3, None, None, None, scale=1.0)
            eh1, ef1 = make_end_hook("out")
            moe_phase(1, None, nchunks=(NCHUNK_DBG or 16), first_load=2, end_hook=(eh1 if USE_END_HOOK else None))
            if USE_END_HOOK:
                ef1()
            else:
                finalize_seq("out")
            S.wait_all("sp", [Bout])
            S.emit()
    return nc


NCHUNK_DBG = None
USE_END_HOOK = False
ATT_DBG = [NT, None]


_CACHE = {}


def _host_inputs(inputs):
    f32 = np.float32
    x = np.asarray(inputs["x"], f32)
    c = np.asarray(inputs["c"], f32)
    pos = np.asarray(inputs["positions"], np.int32)
    shared = {}
    shared["ident"] = np.eye(128, dtype=f32)
    s = np.arange(128)[:, None]
    q = np.arange(128)[None, :]
    m_cur = np.where(s <= q, 0.0, NEG).astype(f32)
    m_prev = np.where(s > q, 0.0, NEG).astype(f32)
    m_none = np.full((128, 128), NEG, f32)
    inv_freq = (10000.0 ** (-np.arange(0, 64, 2, dtype=f32) / f32(64))).astype(f32)
    shared["invf"] = np.ascontiguousarray(np.broadcast_to(inv_freq[None, :], (128, 32))).astype(f32)
    shared["tril"] = (s <= q).astype(f32)
    shared["ada_w"] = np.ascontiguousarray(inputs["ada_w"], f32)
    shared["ada_b"] = np.ascontiguousarray(inputs["ada_b"], f32)
    g = np.asarray(inputs["post_ln_g"], f32).reshape(4, D)
    b = np.asarray(inputs["post_ln_b"], f32).reshape(4, D)
    shared["post_ln_g"] = np.ascontiguousarray(g)
    shared["post_ln_b"] = np.ascontiguousarray(b)
    shared["post_ln_gT"] = np.ascontiguousarray(g.reshape(4, 8, 128).transpose(0, 2, 1))
    shared["post_ln_bT"] = np.ascontiguousarray(b.reshape(4, 8, 128).transpose(0, 2, 1))
    shared["attn_w_qkv"] = np.ascontiguousarray(inputs["attn_w_qkv"][0], f32)
    shared["attn_b_qkv"] = np.ascontiguousarray(inputs["attn_b_qkv"], f32).reshape(1, 1536)
    shared["attn_sinks"] = np.ascontiguousarray(inputs["attn_sinks"], f32).reshape(1, 16)
    shared["attn_w_o"] = np.ascontiguousarray(inputs["attn_w_o"][0], f32)
    shared["attn_b_o"] = np.ascontiguousarray(inputs["attn_b_o"], f32).reshape(1, D)
    shared["gmlp_w_in"] = np.ascontiguousarray(inputs["gmlp_w_in"][0], f32)
    shared["gmlp_b_in"] = np.ascontiguousarray(inputs["gmlp_b_in"], f32).reshape(1, 4096)
    shared["gmlp_ln_g"] = np.ascontiguousarray(inputs["gmlp_sgu_ln_g"], f32).reshape(1, 2048)
    shared["gmlp_ln_b"] = np.ascontiguousarray(inputs["gmlp_sgu_ln_b"], f32).reshape(1, 2048)
    shared["gmlp_w_sT"] = np.ascontiguousarray(np.asarray(inputs["gmlp_w_s"][0], f32).transpose(0, 2, 1))
    shared["gmlp_b_sT"] = np.ascontiguousarray(np.asarray(inputs["gmlp_b_s"][0], f32).T)
    shared["gmlp_w_out"] = np.ascontiguousarray(inputs["gmlp_w_out"][0], f32)
    shared["gmlp_b_out"] = np.ascontiguousarray(inputs["gmlp_b_out"], f32).reshape(1, D)
    shared["moe_wr"] = np.ascontiguousarray(np.concatenate(
        [np.asarray(inputs["moe_w_group_router"], f32), np.asarray(inputs["moe_w_expert_router"], f32)], axis=-1))
    shared["moe_br"] = np.ascontiguousarray(np.concatenate(
        [np.asarray(inputs["moe_b_group_router"], f32), np.asarray(inputs["moe_b_expert_router"], f32)], axis=-1)
    ).reshape(2, 1, 36)
    shared["moe_w_gate_up"] = np.ascontiguousarray(inputs["moe_w_gate_up"], f32).reshape(2, 32, D, 512)
    shared["moe_w_down"] = np.ascontiguousarray(inputs["moe_w_down"], f32).reshape(2, 32, 256, D)
    in_maps = []
    for r in range(8):
        bi, qi = r // 4, r % 4
        s0 = qi * 2048
        m = dict(shared)
        xc = np.zeros((17 * 128, D), f32)
        pc = np.zeros((17 * 128,), np.int32)
        if qi > 0:
            xc[:] = x[bi, s0 - 128:s0 + 2048]
            pc[:] = pos[bi, s0 - 128:s0 + 2048]
        else:
            xc[128:] = x[bi, 0:2048]
            pc[128:] = pos[bi, 0:2048]
        m["xin"] = xc
        m["pos"] = np.ascontiguousarray(pc.reshape(17, 128).T)
        m["cT"] = np.ascontiguousarray(c[bi].reshape(8, 128).T)
        mk = np.stack([np.tile(m_cur, (1, 4)), np.tile(m_prev, (1, 4)),
                       np.tile(m_none if qi == 0 else m_prev, (1, 4))]).astype(f32)
        m["masks"] = np.ascontiguousarray(mk)
        in_maps.append(m)
    return in_maps


def kernel(**inputs):
    in_maps = _host_inputs(inputs)
    if "nc" not in _CACHE:
        _CACHE["nc"] = build()
    res = run_bass_kernel_spmd(_CACHE["nc"], in_maps, core_ids=list(range(8)))
    out = np.empty((2, 8192, D), np.float32)
    for r in range(8):
        bi, qi = r // 4, r % 4
        out[bi, qi * 2048:(qi + 1) * 2048] = res.results[r]["out"]
    return out
```

```python
import numpy as np
from contextlib import ExitStack
import concourse.bass as bass
import concourse.mybir as mybir
from concourse.bass_utils import run_bass_kernel_spmd

F32 = mybir.dt.float32
BF16 = mybir.dt.bfloat16
I32 = mybir.dt.int32
AF = mybir.ActivationFunctionType
ALU = mybir.AluOpType
AX = mybir.AxisListType

NT = 16
D = 1024
ALPHA = 4.0 ** 0.25
LN_EPS = 1e-5
TWO_PI = 6.283185307179586
C1 = 6.28125
C2 = TWO_PI - C1
NEG = -30000.0


class Buf:
    __slots__ = ("name", "w", "r")

    def __init__(self, name):
        self.name = name
        self.w = None
        self.r = {}


class Sched:
    ENG = ("pe", "act", "dve", "pool", "sp")
    NSLOT = 8

    def __init__(self, nc, es):
        self.nc = nc
        self.es = es
        self.E = {"pe": nc.tensor, "act": nc.scalar, "dve": nc.vector,
                  "pool": nc.gpsimd, "sp": nc.sync}
        self.cnt = {}
        self.isdma = {}
        self.waited = {e: {} for e in self.ENG}
        self.ops = []
        self.needed = {}
        for e in self.ENG:
            self.cnt[e] = 0
            self.isdma[e] = False
            self.needed[e] = set()

    def dma_proc(self, name):
        self.cnt[name] = 0
        self.isdma[name] = True
        self.needed[name] = set()

    def _add_dep(self, deps, p, v):
        if self.isdma[p]:
            key = (p, (v - 1) % self.NSLOT)
            deps[key] = max(deps.get(key, 0), (v - 1) // self.NSLOT + 1)
        else:
            deps[p] = max(deps.get(p, 0), v)

    def _mk_waits(self, eng, deps):
        waits = []
        wd = self.waited[eng]
        for key, v in deps.items():
            if wd.get(key, 0) >= v:
                continue
            wd[key] = v
            waits.append((key, v))
            if not isinstance(key, tuple):
                self.needed[key].add(v)
        return waits

    def op(self, eng, fn, reads=(), writes=(), proc=None):
        proc = proc or eng
        deps = {}
        for b in reads:
            if b.w is not None:
                p, v = b.w
                if p == eng and eng == "pe":
                    continue
                self._add_dep(deps, p, v)
        for b in writes:
            if b.w is not None:
                p, v = b.w
                if p != eng or eng != "pe":
                    self._add_dep(deps, p, v)
            for p, v in b.r.items():
                if p != eng or eng != "pe":
                    self._add_dep(deps, p, v)
        self.cnt[proc] += 1
        c = self.cnt[proc]
        if self.isdma[proc] and c > self.NSLOT:
            self._add_dep(deps, proc, c - self.NSLOT)
        waits = self._mk_waits(eng, deps)
        self.ops.append((eng, fn, waits, proc, c))
        for b in reads:
            b.r[proc] = c
        for b in writes:
            b.w = (proc, c)
            b.r = {}
        return c

    def _all_deps(self):
        deps = {}
        for p, v in self.cnt.items():
            if v <= 0:
                continue
            if self.isdma[p]:
                for i in range(max(1, v - self.NSLOT + 1), v + 1):
                    self._add_dep(deps, p, i)
            else:
                deps[p] = v
        return deps

    def barrier(self):
        for eng in self.ENG:
            deps = {k: v for k, v in self._all_deps().items() if k != eng}
            waits = self._mk_waits(eng, deps)
            if waits:
                self.ops.append((eng, None, waits, None, 0))

    def wait_all(self, eng, bufs):
        deps = {}
        for b in bufs:
            if b.w is not None:
                self._add_dep(deps, b.w[0], b.w[1])
        for b in bufs:
            if b.w is not None and self.isdma[b.w[0]]:
                p = b.w[0]
                v = self.cnt[p]
                for i in range(max(1, v - self.NSLOT + 1), v + 1):
                    self._add_dep(deps, p, i)
        waits = self._mk_waits(eng, deps)
        self.ops.append((eng, None, waits, None, 0))

    def emit(self):
        nc = self.nc
        sems = {}
        for p in self.cnt:
            if self.isdma[p]:
                for sl in range(self.NSLOT):
                    sems[(p, sl)] = self.es.enter_context(nc.semaphore(f"s_{p}{sl}"))
            else:
                sems[p] = self.es.enter_context(nc.semaphore("s_" + p))
        last_inc = {p: 0 for p in self.cnt}
        for eng, fn, waits, proc, c in self.ops:
            e = self.E[eng]
            for key, v in waits:
                e.wait_ge(sems[key], v * 16 if isinstance(key, tuple) else v)
            if fn is None:
                continue
            ins = fn(e)
            if self.isdma[proc]:
                ins.then_inc(sems[(proc, (c - 1) % self.NSLOT)], 16)
            elif c in self.needed[proc]:
                ins.then_inc(sems[proc], c - last_inc[proc])
                last_inc[proc] = c


def build(stop_after=None):
    nc = bass.Bass("TRN2", target_bir_lowering=False)

    def din(name, shape, dt=F32):
        return nc.dram_tensor(name, list(shape), dt, kind="ExternalInput").ap()

    xin = din("xin", [17 * 128, D])
    pos_d = din("pos", [128, 17], I32)
    cT_d = din("cT", [128, 8])
    ident_d = din("ident", [128, 128])
    masks_d = din("masks", [3, 128, 512])
    invf_d = din("invf", [128, 32])
    tril_d = din("tril", [128, 128])
    ada_w = din("ada_w", [2, D, 6 * D])
    ada_b = din("ada_b", [2, 6 * D])
    lng_d = din("post_ln_g", [4, D])
    lnb_d = din("post_ln_b", [4, D])
    lngT_d = din("post_ln_gT", [4, 128, 8])
    lnbT_d = din("post_ln_bT", [4, 128, 8])
    wqkv_d = din("attn_w_qkv", [D, 1536])
    bqkv_d = din("attn_b_qkv", [1, 1536])
    sinks_d = din("attn_sinks", [1, 16])
    wo_d = din("attn_w_o", [D, D])
    bo_d = din("attn_b_o", [1, D])
    win_d = din("gmlp_w_in", [D, 4096])
    bin_d = din("gmlp_b_in", [1, 4096])
    glng_d = din("gmlp_ln_g", [1, 2048])
    glnb_d = din("gmlp_ln_b", [1, 2048])
    wsT_d = din("gmlp_w_sT", [8, 128, 128])
    bsT_d = din("gmlp_b_sT", [128, 8])
    wout_d = din("gmlp_w_out", [2048, D])
    bout_d = din("gmlp_b_out", [1, D])
    wr_d = din("moe_wr", [2, D, 36])
    br_d = din("moe_br", [2, 1, 36])
    wgu_d = din("moe_w_gate_up", [2, 32, D, 512])
    wdn_d = din("moe_w_down", [2, 32, 256, D])
    out_d = nc.dram_tensor("out", [NT * 128, D], F32, kind="ExternalOutput").ap()

    es = ExitStack()
    with es:
        S = Sched(nc, es)
        for p in ("dx", "dw", "dc", "do"):
            S.dma_proc(p)

        cur_es = [es]

        sb_n = [0]

        def sb(name, shape, dt=F32):
            sb_n[0] += 1
            return cur_es[0].enter_context(nc.sbuf_tensor(f"sb{sb_n[0]}_{name}", list(shape), dt))

        banks = [es.enter_context(nc.psum_tensor(f"bank{i}", [128, 512], F32)) for i in range(8)]
        bankbf = [b[:].bitcast(BF16) for b in banks]
        PB = [Buf(f"bank{i}") for i in range(8)]
        bank_rr = [0]
        bank_pool = [list(range(8))]

        def next_bank():
            bank_rr[0] += 1
            return bank_pool[0][bank_rr[0] % len(bank_pool[0])]

        x = sb("x", [128, NT, D])
        BX = [Buf(f"x{t}") for t in range(NT)]
        hT = sb("hT", [128, 8, NT * 128], BF16)
        BH = [Buf(f"hT{t}") for t in range(NT)]
        W = sb("W", [128, 24576], BF16)
        BW = [Buf(f"W{s}") for s in range(4)]
        A1 = sb("A1", [128, D]); A0 = sb("A0", [128, D])
        BA1, BA0 = Buf("A1"), Buf("A0")
        gtbm = sb("gtbm", [128, D]); gtbf = sb("gtbf", [128, D])
        Bgtbm, Bgtbf = Buf("gtbm"), Buf("gtbf")
        ident = sb("ident", [128, 128], BF16); Bident = Buf("ident")
        ident32 = sb("ident32", [128, 128]); Bident32 = Buf("ident32")
        onesrow = sb("onesrow", [128, 128], BF16); Bonesrow = Buf("onesrow")
        cact_rep = sb("cact_rep", [128, 8, 128], BF16); Bcact = Buf("cact")
        ctmp = sb("ctmp", [128, 8]); Bctmp = Buf("ctmp")
        xnb = [sb(f"xnb{i}", [128, D], BF16) for i in range(2)]
        Bxnb = [Buf(f"xnb{i}") for i in range(2)]
        st_ = [sb(f"st{i}", [128, 2, 6]) for i in range(2)]; mv_ = [sb(f"mv{i}", [128, 2]) for i in range(2)]
        sd_ = [sb(f"sd{i}", [128, 1]) for i in range(2)]
        rstd_ = [sb(f"rstd{i}", [128, 1]) for i in range(2)]; nmr_ = [sb(f"nmr{i}", [128, 1]) for i in range(2)]
        Bst_ = [Buf(f"st{i}") for i in range(2)]; Bmv_ = [Buf(f"mv{i}") for i in range(2)]
        Bsd_ = [Buf(f"sd{i}") for i in range(2)]; Brstd_ = [Buf(f"rstd{i}") for i in range(2)]
        Bnmr_ = [Buf(f"nmr{i}") for i in range(2)]
        fin_i = [0]
        eps_t = sb("eps_t", [128, 1]); Beps = Buf("eps")
        H1 = [sb(f"H1_{i}", [128, 8]) for i in range(4)]
        H0 = [sb(f"H0_{i}", [128, 8]) for i in range(4)]
        BHa = [Buf(f"Haff{i}") for i in range(4)]
        fmt = sb("fmt", [128, 8]); fmt2 = sb("fmt2", [128, 8]); fmg = sb("fmg", [128, 8]); fmb = sb("fmb", [128, 8])
        Bfmt, Bfmt2, Bfmg, Bfmb = Buf("fmt"), Buf("fmt2"), Buf("fmg"), Buf("fmb")
        wr = [sb(f"wr{l}", [128, 8, 36], BF16) for l in range(2)]
        brr = [sb(f"brr{l}", [128, 36], BF16) for l in range(2)]
        Bwr = [Buf(f"wr{l}") for l in range(2)]
        lg = sb("lg", [128, NT, 36]); Blg = Buf("lg")
        comb = sb("comb", [128, NT, 32]); Bcomb = Buf("comb")
        Bout = Buf("out")

        def dma(eng, proc, out, in_, reads=(), writes=()):
            S.op(eng, lambda e: e.dma_start(out=out, in_=in_), reads=reads, writes=writes, proc=proc)

        def bc_load(dst, bdst, row_ap):
            n = row_ap.shape[-1]
            dma("sp", "dc", dst[:, 0:n], row_ap.to_broadcast([128, n]), writes=[bdst])

        MS = {}

        def alloc_mod_scratch():
            MS["vecT"] = sb("vecT", [128, D]); MS["BvecT"] = Buf("vecT")
            MS["bcT"] = sb("bcT", [128, D]); MS["BbcT"] = Buf("bcT")
            MS["stg"] = [sb(f"stg{i}", [128, 8, 256], BF16) for i in range(2)]
            MS["Bstg"] = [Buf(f"stg{i}") for i in range(2)]

        dma("pool", "dw", ident[:], ident_d, writes=[Bident])
        dma("sp", "dc", ident32[:], ident_d, writes=[Bident32])
        S.op("dve", lambda e: e.memset(eps_t[:], LN_EPS), writes=[Beps])
        S.op("dve", lambda e: e.memset(onesrow[:], 0.0), writes=[Bonesrow])
        S.op("dve", lambda e: e.memset(onesrow[0:1, :], 1.0), writes=[Bonesrow])
        dma("sp", "dc", ctmp[:], cT_d, writes=[Bctmp])
        S.op("act", lambda e: e.activation(out=ctmp[:], in_=ctmp[:], func=AF.Silu), reads=[Bctmp], writes=[Bctmp])
        S.op("dve", lambda e: e.tensor_copy(out=cact_rep[:], in_=ctmp[:].unsqueeze(2).to_broadcast([128, 8, 128])),
             reads=[Bctmp], writes=[Bcact])
        for l in range(2):
            dma("pool", "dw", wr[l][:], wr_d[l].rearrange("(k p) n -> p k n", p=128), writes=[Bwr[l]])
            S.op("pool", (lambda l=l: lambda e: e.memset(brr[l][:], 0.0))(), writes=[Bwr[l]])
            dma("pool", "dw", brr[l][0:1, :], br_d[l], writes=[Bwr[l]])

        def compute_mod_gen(l, j, dst, bdst, add_one, lag=0):
            bcT, BbcT, stg, Bstg = MS["bcT"], MS["BbcT"], MS["stg"], MS["Bstg"]
            bc_load(bcT, BbcT, ada_b[l:l + 1, j * D:(j + 1) * D])

            def issue(nb):
                si = nb % 2
                col = j * D + nb * 256
                dma("pool", "dw", stg[si][:], ada_w[l][:, col:col + 256].rearrange("(k p) n -> p k n", p=128),
                    writes=[Bstg[si]])

            def consume(nb):
                si = nb % 2
                bk = next_bank()
                for k in range(8):
                    S.op("pe", (lambda k=k: lambda e: e.matmul(
                        banks[bk][:, 0:256], cact_rep[:, k, :], stg[si][:, k, :], start=(k == 0), stop=(k == 7)))(),
                         reads=[Bcact, Bstg[si]], writes=[PB[bk]])
                S.op("dve", lambda e: e.scalar_tensor_tensor(
                    out=dst[:, nb * 256:(nb + 1) * 256], in0=banks[bk][:, 0:256], scalar=(1.0 if add_one else 0.0),
                    in1=bcT[:, nb * 256:(nb + 1) * 256], op0=ALU.add, op1=ALU.add),
                     reads=[PB[bk], BbcT], writes=[bdst])

            issue(0); issue(1)
            for _ in range(lag):
                yield
            consume(0); consume(1)
            issue(2); issue(3)
            for _ in range(lag):
                yield
            consume(2); consume(3)

        def compute_mod(l, j, dst, bdst, add_one):
            for _ in compute_mod_gen(l, j, dst, bdst, add_one):
                pass

        def to_fm(src, bsrc, dst, bdst):
            tmp, Btmp = MS["bcT"], MS["BbcT"]
            S.op("dve", lambda e: e.tensor_tensor(
                out=tmp[:].rearrange("p (k c) -> p k c", k=8), in0=src[:].rearrange("p (k c) -> p k c", k=8),
                in1=ident32[:].unsqueeze(1).to_broadcast([128, 8, 128]), op=ALU.mult),
                 reads=[bsrc, Bident32], writes=[Btmp])
            S.op("dve", lambda e: e.tensor_reduce(out=dst[:], in_=tmp[:].rearrange("p (k c) -> p k c", k=8),
                                                  axis=AX.X, op=ALU.add),
                 reads=[Btmp], writes=[bdst])

        def make_haff(idx, l, j_sh, j_sc, ln_idx):
            vecT, BvecT = MS["vecT"], MS["BvecT"]
            compute_mod(l, j_sc, vecT, BvecT, True)
            to_fm(vecT, BvecT, fmt, Bfmt)
            compute_mod(l, j_sh, vecT, BvecT, False)
            to_fm(vecT, BvecT, fmt2, Bfmt2)
            haff_combine(idx, ln_idx)

        def haff_combine(idx, ln_idx):
            if ln_idx is None:
                S.op("dve", lambda e: e.tensor_copy(out=H1[idx][:], in_=fmt[:]), reads=[Bfmt], writes=[BHa[idx]])
                S.op("dve", lambda e: e.tensor_copy(out=H0[idx][:], in_=fmt2[:]), reads=[Bfmt2], writes=[BHa[idx]])
            else:
                dma("sp", "dc", fmg[:], lngT_d[ln_idx], writes=[Bfmg])
                dma("sp", "dc", fmb[:], lnbT_d[ln_idx], writes=[Bfmb])
                S.op("dve", lambda e: e.tensor_tensor(out=H1[idx][:], in0=fmt[:], in1=fmg[:], op=ALU.mult),
                     reads=[Bfmt, Bfmg], writes=[BHa[idx]])
                S.op("dve", lambda e: e.tensor_tensor(out=fmb[:], in0=fmt[:], in1=fmb[:], op=ALU.mult),
                     reads=[Bfmt, Bfmb], writes=[Bfmb])
                S.op("dve", lambda e: e.tensor_tensor(out=H0[idx][:], in0=fmb[:], in1=fmt2[:], op=ALU.add),
                     reads=[Bfmb, Bfmt2], writes=[BHa[idx]])

        def make_aset(ln_idx, bias_row, gtb, bgtb, scale=ALPHA):
            bc_load(A1, BA1, lng_d[ln_idx:ln_idx + 1, :])
            bc_load(A0, BA0, lnb_d[ln_idx:ln_idx + 1, :])
            if scale != 1.0:
                S.op("dve", lambda e: e.tensor_scalar(out=A1[:], in0=A1[:], scalar1=scale, scalar2=None, op0=ALU.mult),
                     reads=[BA1], writes=[BA1])
            if bias_row is None:
                if scale != 1.0:
                    S.op("dve", lambda e: e.tensor_scalar(out=A0[:], in0=A0[:], scalar1=scale, scalar2=None, op0=ALU.mult),
                         reads=[BA0], writes=[BA0])
            else:
                vt, bvt = MS["vecT"], MS["BvecT"]
                bc_load(vt, bvt, bias_row)
                S.op("dve", lambda e: e.tensor_tensor(out=vt[:], in0=vt[:], in1=gtb[:], op=ALU.mult),
                     reads=[bvt, bgtb], writes=[bvt])
                S.op("dve", lambda e: e.scalar_tensor_tensor(out=A0[:], in0=A0[:], scalar=scale, in1=vt[:],
                                                             op0=ALU.mult, op1=ALU.add),
                     reads=[BA0, bvt], writes=[BA0])

        def router_logits(l, t):
            bk = next_bank()
            for k in range(8):
                S.op("pe", (lambda k=k: lambda e: e.matmul(
                    banks[bk][:, 0:36], hT[:, k, t * 128:(t + 1) * 128], wr[l][:, k, :], start=(k == 0), stop=False))(),
                     reads=[BH[t], Bwr[l]], writes=[PB[bk]])
            S.op("pe", lambda e: e.matmul(banks[bk][:, 0:36], onesrow[:], brr[l][:], start=False, stop=True),
                 reads=[Bonesrow, Bwr[l]], writes=[PB[bk]])
            S.op("dve", lambda e: e.tensor_copy(out=lg[:, t, :], in_=banks[bk][:, 0:36]), reads=[PB[bk]], writes=[Blg])

        def finalize_a(t, mode, src=None, bsrc=None, part=0):
            xa = x[:, t, :] if src is None else src
            bxa = BX[t] if bsrc is None else bsrc
            i = fin_i[0] % 2
            fin_i[0] += 1
            st, mv, sd, rstd, nmr = st_[i], mv_[i], sd_[i], rstd_[i], nmr_[i]
            Bst, Bmv, Bsd, Brstd, Bnmr = Bst_[i], Bmv_[i], Bsd_[i], Brstd_[i], Bnmr_[i]
            if mode == "pro":
                S.op("act", lambda e: e.activation(out=xnb[i][:], in_=xa, func=AF.Copy), reads=[bxa], writes=[Bxnb[i]])
                if src is None:
                    S.op("dve", lambda e: e.scalar_tensor_tensor(out=xa, in0=xa, scalar=ALPHA, in1=A0[:],
                                                                 op0=ALU.mult, op1=ALU.add),
                         reads=[bxa, BA0], writes=[bxa])
                return i
            S.op("dve", lambda e: e.bn_stats(out=st[:, 0, :], in_=xa[:, 0:512]), reads=[bxa], writes=[Bst])
            S.op("dve", lambda e: e.bn_stats(out=st[:, 1, :], in_=xa[:, 512:1024]), reads=[bxa], writes=[Bst])
            S.op("dve", lambda e: e.bn_aggr(out=mv[:], in_=st[:]), reads=[Bst], writes=[Bmv])
            if part == 1:
                return i
            return finalize_a2(t, mode, i, src=src, bsrc=bsrc)

        def finalize_a2(t, mode, i, src=None, bsrc=None):
            xa = x[:, t, :] if src is None else src
            bxa = BX[t] if bsrc is None else bsrc
            st, mv, sd, rstd, nmr = st_[i], mv_[i], sd_[i], rstd_[i], nmr_[i]
            Bst, Bmv, Bsd, Brstd, Bnmr = Bst_[i], Bmv_[i], Bsd_[i], Brstd_[i], Bnmr_[i]
            S.op("act", lambda e: e.activation(out=sd[:], in_=mv[:, 1:2], func=AF.Ln, bias=eps_t[:]),
                 reads=[Bmv, Beps], writes=[Bsd])
            S.op("act", lambda e: e.activation(out=rstd[:], in_=sd[:], func=AF.Exp, scale=-0.5), reads=[Bsd], writes=[Brstd])
            S.op("dve", lambda e: e.scalar_tensor_tensor(out=nmr[:], in0=mv[:, 0:1], scalar=-1.0, in1=rstd[:],
                                                         op0=ALU.mult, op1=ALU.mult),
                 reads=[Bmv, Brstd], writes=[Bnmr])
            if mode == "mid":
                S.op("act", lambda e: e.activation(out=xnb[i][:], in_=xa, func=AF.Identity, scale=rstd[:], bias=nmr[:]),
                     reads=[bxa, Brstd, Bnmr], writes=[Bxnb[i]])
            S.op("dve", lambda e: e.tensor_scalar(out=xa, in0=xa, scalar1=rstd[:], scalar2=nmr[:],
                                                  op0=ALU.mult, op1=ALU.add),
                 reads=[bxa, Brstd, Bnmr], writes=[bxa])
            S.op("dve" if mode == "out" else "pool", lambda e: e.tensor_tensor(out=xa, in0=xa, in1=A1[:], op=ALU.mult),
                 reads=[bxa, BA1], writes=[bxa])
            S.op("pool", lambda e: e.tensor_tensor(out=xa, in0=xa, in1=A0[:], op=ALU.add),
                 reads=[bxa, BA0], writes=[bxa])
            if mode == "out":
                dma("sp", "do", out_d[t * 128:(t + 1) * 128, :], xa, reads=[bxa], writes=[Bout])
            return i

        def finalize_b(t, i, haff, hdst=None, bhdst=None):
            bks = [next_bank(), next_bank()]
            for k in range(8):
                S.op("pe", (lambda k=k: lambda e: e.matmul(
                    banks[bks[k // 4]][:, (k % 4) * 128:(k % 4 + 1) * 128], xnb[i][:, k * 128:(k + 1) * 128], ident[:],
                    start=True, stop=True))(),
                     reads=[Bxnb[i], Bident], writes=[PB[bks[k // 4]]])
            hd = hT[:, :, t * 128:(t + 1) * 128] if hdst is None else hdst
            bhd = BH[t] if bhdst is None else bhdst
            for k in range(8):
                if k % 2 == 0:
                    S.op("act", (lambda k=k: lambda e: e.activation(
                        out=hd[:, k, :], in_=banks[bks[k // 4]][:, (k % 4) * 128:(k % 4 + 1) * 128], func=AF.Identity,
                        scale=H1[haff][:, k:k + 1], bias=H0[haff][:, k:k + 1]))(),
                         reads=[PB[bks[k // 4]], BHa[haff]], writes=[bhd])
                else:
                    S.op("dve", (lambda k=k: lambda e: e.tensor_scalar(
                        out=hd[:, k, :], in0=banks[bks[k // 4]][:, (k % 4) * 128:(k % 4 + 1) * 128],
                        scalar1=H1[haff][:, k:k + 1], scalar2=H0[haff][:, k:k + 1], op0=ALU.mult, op1=ALU.add))(),
                         reads=[PB[bks[k // 4]], BHa[haff]], writes=[bhd])

        def finalize(t, mode, haff=None, hdst=None, bhdst=None, route_l=None, src=None, bsrc=None):
            i = finalize_a(t, mode, src=src, bsrc=bsrc)
            if mode == "out":
                return
            finalize_b(t, i, haff, hdst=hdst, bhdst=bhdst)
            if route_l is not None:
                router_logits(route_l, t)

        def finalize_seq(mode, haff=None, route_l=None, hook=None):
            idx = {}
            if mode == "pro":
                idx[0] = finalize_a(0, mode)
                for t in range(NT):
                    if hook is not None:
                        hook(t)
                    if t + 1 < NT:
                        idx[t + 1] = finalize_a(t + 1, mode)
                    finalize_b(t, idx[t], haff)
                return
            idx[0] = finalize_a(0, mode, part=1)
            idx[1] = finalize_a(1, mode, part=1)
            finalize_a2(0, mode, idx[0])
            for t in range(NT):
                if hook is not None:
                    hook(t)
                if t + 2 < NT:
                    idx[t + 2] = finalize_a(t + 2, mode, part=1)
                if t + 1 < NT:
                    finalize_a2(t + 1, mode, idx[t + 1])
                if mode != "out":
                    finalize_b(t, idx[t], haff)
                    if route_l is not None and t >= 1:
                        router_logits(route_l, t - 1)
            if mode != "out" and route_l is not None:
                router_logits(route_l, NT - 1)

        def make_end_hook(mode, haff=None, route_l=None):
            st = {}

            def stage_b(t):
                if mode != "out":
                    finalize_b(t, st[t], haff)
                    if route_l is not None:
                        router_logits(route_l, t)

            def hook(t):
                st[t] = finalize_a(t, mode, part=1)
                if t >= 1:
                    finalize_a2(t - 1, mode, st[t - 1])
                if t >= 2:
                    stage_b(t - 2)

            def flush():
                finalize_a2(NT - 1, mode, st[NT - 1])
                stage_b(NT - 2)
                stage_b(NT - 1)
            return hook, flush

        def dump_and_end():
            for t in range(NT):
                dma("sp", "do", out_d[t * 128:(t + 1) * 128, :], x[:, t, :], reads=[BX[t]], writes=[Bout])
            S.wait_all("sp", [Bout])
            S.emit()

        def moe_views(s):
            base = s * 6144
            wgu = W[:, base:base + 4096].rearrange("p (k n) -> p k n", k=8)
            wdn = W[:, base + 4096:base + 6144].rearrange("p (j n) -> p j n", j=2)
            return wgu, wdn

        def moe_load_gu(l, ex):
            s_ = ex % 4
            wgu, _ = moe_views(s_)
            dma("pool", "dw", wgu, wgu_d[l, ex].rearrange("(k p) n -> p k n", p=128), writes=[BW[s_]])

        def moe_load_dn(l, ex):
            s_ = ex % 4
            _, wdn = moe_views(s_)
            dma("pool", "dw", wdn, wdn_d[l, ex].rearrange("(j p) n -> p j n", p=128), writes=[BW[s_]])

        def moe_fold(l, ex):
            s_ = ex % 4
            _, wdn = moe_views(s_)
            S.op("pool", lambda e: e.tensor_tensor(out=wdn, in0=wdn, in1=gtbf[:].unsqueeze(1).to_broadcast([128, 2, D]),
                                                   op=ALU.mult), reads=[BW[s_], Bgtbf], writes=[BW[s_]])

        def moe_load(l, ex):
            moe_load_gu(l, ex); moe_load_dn(l, ex); moe_fold(l, ex)

        esA = ExitStack()
        cur_es[0] = esA
        with esA:
            cosT = sb("cosT", [128, 17, 32]); sinT = sb("sinT", [128, 17, 32])
            Bcos, Bsin = Buf("cos"), Buf("sin")
            hTh = sb("hTh", [128, 8, 128], BF16); BhTh = Buf("hTh")
            bqkv = sb("bqkv", [128, 1536], BF16); Bbqkv = Buf("bqkv")
            esink = sb("esink", [128, 16]); Besink = Buf("esink")
            maskb = sb("maskb", [128, 3, 512], BF16); Bmask = Buf("maskb")
            Wqkv = W[:, 0:12288].rearrange("p (k n) -> p k n", k=8)
            Wo = W[:, 12288:20480].rearrange("p (k n) -> p k n", k=8)

            es0 = ExitStack()
            cur_es[0] = es0
            with es0:
                alloc_mod_scratch()
                posi = sb("posi", [128, 17], I32); posf = sb("posf", [128, 17]); invf = sb("invf", [128, 32])
                ang = sb("ang", [128, 17, 32]); ki = sb("ki", [128, 17, 32], I32)
                kf = W[:, 22528:22528 + 1088].bitcast(F32).rearrange("p (t f) -> p t f", t=17)
                Bpos, Binvf, Bang, Bkf, Bki = Buf("pos"), Buf("invf"), Buf("ang"), Buf("kf"), Buf("ki")
                xh = W[:, 20480:22528].bitcast(F32); Bxh = Buf("xh")
                dma("sp", "dx", xh, xin[0:128, :], writes=[Bxh])
                for t in range(NT):
                    dma("sp", "dx", x[:, t, :], xin[(t + 1) * 128:(t + 2) * 128, :], writes=[BX[t]])

                make_haff(0, 0, 0, 1, None)
                compute_mod(0, 2, gtbm, Bgtbm, True)
                bc_load(A0, BA0, bo_d)
                S.op("dve", lambda e: e.tensor_tensor(out=A0[:], in0=A0[:], in1=gtbm[:], op=ALU.mult),
                     reads=[BA0, Bgtbm], writes=[BA0])
                dma("pool", "dw", Wqkv, wqkv_d.rearrange("(k p) n -> p k n", p=128), writes=[BW[0], BW[1]])
                dma("pool", "dw", Wo, wo_d.rearrange("(k p) n -> p k n", p=128), writes=[BW[2], BW[3]])
                S.op("pool", lambda e: e.memset(bqkv[:], 0.0), writes=[Bbqkv])
                dma("pool", "dw", bqkv[0:1, :], bqkv_d, writes=[Bbqkv])
                bc_load(esink, Besink, sinks_d)
                S.op("act", lambda e: e.activation(out=esink[:], in_=esink[:], func=AF.Exp), reads=[Besink], writes=[Besink])
                dma("pool", "dw", maskb[:], masks_d.rearrange("m p n -> p m n"), writes=[Bmask])
                dma("sp", "dc", posi[:], pos_d, writes=[Bpos])
                dma("sp", "dc", invf[:], invf_d, writes=[Binvf])
                S.op("dve", lambda e: e.tensor_copy(out=posf[:], in_=posi[:]), reads=[Bpos], writes=[Bpos])
                S.op("dve", lambda e: e.tensor_tensor(out=ang[:], in0=posf[:].unsqueeze(2).to_broadcast([128, 17, 32]),
                                                      in1=invf[:].unsqueeze(1).to_broadcast([128, 17, 32]), op=ALU.mult),
                     reads=[Bpos, Binvf], writes=[Bang])

                def sin_of(dst, bdst, shift):
                    if shift != 0.0:
                        S.op("dve", lambda e: e.tensor_scalar(out=dst[:], in0=ang[:], scalar1=shift, scalar2=None, op0=ALU.add),
                             reads=[Bang], writes=[bdst])
                        a_src, ba = dst, bdst
                    else:
                        a_src, ba = ang, Bang
                    S.op("dve", lambda e: e.tensor_scalar(out=ki[:], in0=a_src[:], scalar1=1.0 / TWO_PI, scalar2=None,
                                                          op0=ALU.mult), reads=[ba], writes=[Bki])
                    S.op("dve", lambda e: e.tensor_copy(out=kf, in_=ki[:]), reads=[Bki], writes=[Bkf])
                    S.op("dve", lambda e: e.scalar_tensor_tensor(out=dst[:], in0=kf, scalar=-C1, in1=a_src[:],
                                                                 op0=ALU.mult, op1=ALU.add), reads=[Bkf, ba], writes=[bdst])
                    S.op("dve", lambda e: e.scalar_tensor_tensor(out=dst[:], in0=kf, scalar=-C2, in1=dst[:],
                                                                 op0=ALU.mult, op1=ALU.add), reads=[Bkf, bdst], writes=[bdst])
                    S.op("dve", lambda e: e.tensor_scalar(out=dst[:], in0=dst[:], scalar1=3.1415925, scalar2=-3.1415925,
                                                          op0=ALU.min, op1=ALU.max), reads=[bdst], writes=[bdst])
                    S.op("act", lambda e: e.activation(out=dst[:], in_=dst[:], func=AF.Sin), reads=[bdst], writes=[bdst])

                sin_of(sinT, Bsin, 0.0)
                sin_of(cosT, Bcos, TWO_PI / 4)

                finalize(0, "pro", haff=0, hdst=hTh, bhdst=BhTh, src=xh, bsrc=Bxh)
                def mods_b():
                    vecT, BvecT = MS["vecT"], MS["BvecT"]
                    yield from compute_mod_gen(0, 4, vecT, BvecT, True, lag=3)
                    to_fm(vecT, BvecT, fmt, Bfmt)
                    yield from compute_mod_gen(0, 3, vecT, BvecT, False, lag=3)
                    to_fm(vecT, BvecT, fmt2, Bfmt2)
                    haff_combine(1, 0)
                    yield from compute_mod_gen(0, 5, gtbf, Bgtbf, True, lag=2)

                job_b = mods_b()
                finalize_seq("pro", haff=0, hook=lambda t: next(job_b, None))
                for _ in job_b:
                    pass
                S.op("dve", lambda e: e.tensor_tensor(out=Wo, in0=Wo, in1=gtbm[:].unsqueeze(1).to_broadcast([128, 8, D]),
                                                      op=ALU.mult),
                     reads=[BW[2], BW[3], Bgtbm], writes=[BW[2], BW[3]])
                if stop_after == "pro":
                    dump_and_end()
                    return nc
                make_aset(0, None, None, None)
            S.barrier()

            es1 = ExitStack()
            cur_es[0] = es1
            bank_pool[0] = [3, 6, 7]
            with es1:
                qk32 = sb("qk32", [128, 20, 64]); Bqk32 = Buf("qk32")
                rot = sb("rot", [128, 20, 64], BF16); Brot = Buf("rot")
                rot2 = rot[:].rearrange("p h d -> p (h d)")
                tA = sb("tA", [128, 20, 32]); tB = sb("tB", [128, 20, 32])
                BtA, BtB = Buf("tA"), Buf("tB")
                kpad = sb("kpad", [128, 4, 2, 128], BF16); Bkpad = Buf("kpad")
                qT = sb("qT", [128, 8, 128], BF16); BqT = Buf("qT")
                Vaug = [sb(f"Vaug{i}", [128, 4, 65], BF16) for i in range(3)]
                BV = [Buf(f"V{i}") for i in range(3)]
                ao = sb("ao", [128, 16, 64], BF16); Bao = Buf("ao")
                ao2 = ao[:].rearrange("p h d -> p (h d)")
                aoT = sb("aoT", [128, 8, 128], BF16); BaoT = Buf("aoT")
                dent = sb("dent", [128, 16]); Bdent = Buf("dent")
                kT = [W[:, 20480 + i * 1024:20480 + (i + 1) * 1024].rearrange("p (v c) -> p v c", v=8) for i in range(2)]
                BkT = [Buf(f"kT{i}") for i in range(2)]
                PT = [W[:, 22528 + i * 1024:22528 + (i + 1) * 1024].rearrange("p (b c) -> p b c", b=2) for i in range(2)]
                BPT = [Buf(f"PT{i}") for i in range(2)]
                S.op("pool", lambda e: e.memset(kpad[:], 0.0), writes=[Bkpad])
                for i in range(3):
                    S.op("pool", (lambda i=i: lambda e: e.memset(Vaug[i][:], 1.0))(), writes=[BV[i]])

                def X1(t):
                    halo = t < 0
                    tt = t + 1
                    hsrc = hTh if halo else hT[:, :, t * 128:(t + 1) * 128]
                    bh = BhTh if halo else BH[t]
                    qb = []
                    for nb in ([2] if halo else [0, 1, 2]):
                        bk = nb
                        qb.append((nb, bk))
                        for k in range(8):
                            S.op("pe", (lambda k=k, nb=nb, bk=bk: lambda e: e.matmul(
                                banks[bk][:], hsrc[:, k, :], Wqkv[:, k, nb * 512:(nb + 1) * 512], start=(k == 0), stop=False))(),
                                 reads=[bh, BW[0], BW[1]], writes=[PB[bk]])
                        S.op("pe", (lambda nb=nb, bk=bk: lambda e: e.matmul(
                            banks[bk][:], onesrow[:], bqkv[:, nb * 512:(nb + 1) * 512], start=False, stop=True))(),
                             reads=[Bonesrow, Bbqkv], writes=[PB[bk]])
                    vcur = Vaug[tt % 3]; bvcur = BV[tt % 3]
                    for nb, bk in qb:
                        if nb < 2:
                            S.op("act", (lambda nb=nb, bk=bk: lambda e: e.activation(
                                out=qk32[:, nb * 8:(nb + 1) * 8, :], in_=banks[bk][:].rearrange("p (h d) -> p h d", d=64),
                                func=AF.Copy))(), reads=[PB[bk]], writes=[Bqk32])
                        else:
                            S.op("act", (lambda bk=bk: lambda e: e.activation(
                                out=qk32[:, 16:20, :], in_=banks[bk][:, 0:256].rearrange("p (h d) -> p h d", d=64),
                                func=AF.Copy))(), reads=[PB[bk]], writes=[Bqk32])
                            S.op("act", (lambda bk=bk: lambda e: e.activation(
                                out=vcur[:, :, 0:64], in_=banks[bk][:, 256:512].rearrange("p (h d) -> p h d", d=64),
                                func=AF.Copy))(), reads=[PB[bk]], writes=[bvcur])
                    h0 = 16 if halo else 0
                    nh = 20 - h0
                    x1 = qk32[:, h0:20, 0:32]; x2 = qk32[:, h0:20, 32:64]
                    cb = cosT[:, tt, :].unsqueeze(1).to_broadcast([128, nh, 32])
                    sbc = sinT[:, tt, :].unsqueeze(1).to_broadcast([128, nh, 32])
                    S.op("dve", lambda e: e.tensor_tensor(out=tA[:, h0:20, :], in0=x1, in1=cb, op=ALU.mult),
                         reads=[Bqk32, Bcos], writes=[BtA])
                    S.op("dve", lambda e: e.tensor_tensor(out=tB[:, h0:20, :], in0=x2, in1=sbc, op=ALU.mult),
                         reads=[Bqk32, Bsin], writes=[BtB])
                    if not halo:
                        S.op("dve", lambda e: e.tensor_tensor(out=rot[:, 0:16, 0:32], in0=tA[:, 0:16, :], in1=tB[:, 0:16, :],
                                                              op=ALU.subtract), reads=[BtA, BtB], writes=[Brot])
                    for half in range(2):
                        S.op("dve", (lambda half=half: lambda e: e.tensor_tensor(
                            out=kpad[:, :, half, half * 64:half * 64 + 32], in0=tA[:, 16:20, :], in1=tB[:, 16:20, :],
                            op=ALU.subtract))(), reads=[BtA, BtB], writes=[Bkpad])
                    S.op("dve", lambda e: e.tensor_tensor(out=tA[:, h0:20, :], in0=x2, in1=cb, op=ALU.mult),
                         reads=[Bqk32, Bcos], writes=[BtA])
                    S.op("dve", lambda e: e.tensor_tensor(out=tB[:, h0:20, :], in0=x1, in1=sbc, op=ALU.mult),
                         reads=[Bqk32, Bsin], writes=[BtB])
                    if not halo:
                        S.op("dve", lambda e: e.tensor_tensor(out=rot[:, 0:16, 32:64], in0=tA[:, 0:16, :], in1=tB[:, 0:16, :],
                                                              op=ALU.add), reads=[BtA, BtB], writes=[Brot])
                    for half in range(2):
                        S.op("dve", (lambda half=half: lambda e: e.tensor_tensor(
                            out=kpad[:, :, half, half * 64 + 32:half * 64 + 64], in0=tA[:, 16:20, :], in1=tB[:, 16:20, :],
                            op=ALU.add))(), reads=[BtA, BtB], writes=[Bkpad])

                tb = [3, 6]

                def X2(t):
                    halo = t < 0
                    tt = t + 1
                    cur = tt % 2
                    for v in range(8):
                        S.op("pe", (lambda v=v: lambda e: e.matmul(
                            banks[tb[v // 4]][:, (v % 4) * 128:(v % 4 + 1) * 128], kpad[:, v // 2, v % 2, :], ident[:],
                            start=True, stop=True))(),
                             reads=[Bkpad, Bident], writes=[PB[tb[v // 4]]])
                    for hb in range(2):
                        S.op("act", (lambda hb=hb: lambda e: e.activation(
                            out=kT[cur][:, hb * 4:(hb + 1) * 4, :], in_=banks[tb[hb]][:].rearrange("p (v c) -> p v c", v=4),
                            func=AF.Copy))(), reads=[PB[tb[hb]]], writes=[BkT[cur]])
                    if halo:
                        return
                    for j in range(8):
                        S.op("pe", (lambda j=j: lambda e: e.matmul(
                            banks[tb[j // 4]][:, (j % 4) * 128:(j % 4 + 1) * 128], rot2[:, j * 128:(j + 1) * 128], ident[:],
                            start=True, stop=True))(),
                             reads=[Brot, Bident], writes=[PB[tb[j // 4]]])
                    for hb in range(2):
                        S.op("dve", (lambda hb=hb: lambda e: e.tensor_copy(
                            out=qT[:, hb * 4:(hb + 1) * 4, :], in_=banks[tb[hb]][:].rearrange("p (v c) -> p v c", v=4)))(),
                             reads=[PB[tb[hb]]], writes=[BqT])

                def Y1(t):
                    tt = t + 1
                    cur = tt % 2
                    prv = 1 - cur
                    vcur, bvcur = Vaug[tt % 3], BV[tt % 3]
                    vprv, bvprv = Vaug[(tt - 1) % 3], BV[(tt - 1) % 3]
                    ob = [0, 1, 2]
                    sc_i = [0]
                    first_tile = (t == 0)

                    def scores(g):
                        pi = g % 2
                        for blk, (ktile, bkt, mi) in enumerate([(kT[prv], BkT[prv], 2 if first_tile else 1),
                                                                (kT[cur], BkT[cur], 0)]):
                            bk = 4 + (sc_i[0] % 2)
                            sc_i[0] += 1
                            S.op("pe", (lambda bk=bk, mi=mi: lambda e: e.matmul(
                                banks[bk][:], ident[:], maskb[:, mi, :], start=True, stop=False))(),
                                 reads=[Bident, Bmask], writes=[PB[bk]])
                            for i in range(4):
                                h = 4 * g + i
                                S.op("pe", (lambda bk=bk, i=i, h=h, ktile=ktile, g=g: lambda e: e.matmul(
                                    banks[bk][:, i * 128:(i + 1) * 128], ktile[:, g * 2 + (h % 2), :], qT[:, h // 2, :],
                                    start=False, stop=(i == 3)))(),
                                     reads=[bkt, BqT], writes=[PB[bk]])
                            S.op("act", (lambda bk=bk, blk=blk, pi=pi: lambda e: e.activation(
                                out=PT[pi][:, blk, :], in_=banks[bk][:], func=AF.Exp, scale=0.125))(),
                                 reads=[PB[bk]], writes=[BPT[pi]])

                    def pv(g):
                        pi = g % 2
                        for i in range(4):
                            h = 4 * g + i
                            obk = ob[h // 7]
                            oc = (h % 7) * 65
                            for blk, (vt, bvt) in enumerate([(vprv, bvprv), (vcur, bvcur)]):
                                S.op("pe", (lambda obk=obk, oc=oc, blk=blk, pi=pi, i=i, vt=vt, g=g: lambda e: e.matmul(
                                    banks[obk][:, oc:oc + 65], PT[pi][:, blk, i * 128:(i + 1) * 128], vt[:, g, :],
                                    start=(blk == 0), stop=(blk == 1)))(),
                                     reads=[BPT[pi], bvt], writes=[PB[obk]])

                    scores(0)
                    for g in range(4):
                        if g + 1 < 4:
                            scores(g + 1)
                        pv(g)
                    for b3 in range(3):
                        hs = 7 * b3
                        n = min(7, 16 - hs)
                        ov = banks[ob[b3]][:, 0:n * 65].rearrange("p (h d) -> p h d", d=65)
                        S.op("dve", (lambda ov=ov, hs=hs, n=n: lambda e: e.tensor_tensor(
                            out=dent[:, hs:hs + n], in0=ov[:, :, 64], in1=esink[:, hs:hs + n], op=ALU.add))(),
                             reads=[PB[ob[b3]], Besink], writes=[Bdent])
                        S.op("dve", (lambda hs=hs, n=n: lambda e: e.reciprocal(out=dent[:, hs:hs + n], in_=dent[:, hs:hs + n]))(),
                             reads=[Bdent], writes=[Bdent])
                        S.op("dve", (lambda ov=ov, hs=hs, n=n: lambda e: e.tensor_tensor(
                            out=ao[:, hs:hs + n, :], in0=ov[:, :, 0:64],
                            in1=dent[:, hs:hs + n].unsqueeze(2).to_broadcast([128, n, 64]), op=ALU.mult))(),
                             reads=[PB[ob[b3]], Bdent], writes=[Bao])

                def Y2a(t):
                    for j in range(8):
                        S.op("pe", (lambda j=j: lambda e: e.matmul(
                            banks[tb[j // 4]][:, (j % 4) * 128:(j % 4 + 1) * 128], ao2[:, j * 128:(j + 1) * 128], ident[:],
                            start=True, stop=True))(),
                             reads=[Bao, Bident], writes=[PB[tb[j // 4]]])
                    for hb in range(2):
                        S.op("act", (lambda hb=hb: lambda e: e.activation(
                            out=aoT[:, hb * 4:(hb + 1) * 4, :], in_=banks[tb[hb]][:].rearrange("p (v c) -> p v c", v=4),
                            func=AF.Copy))(), reads=[PB[tb[hb]]], writes=[BaoT])

                def Y2b(t):
                    for nb in range(2):
                        bk = 4 + nb
                        for k in range(8):
                            S.op("pe", (lambda k=k, nb=nb, bk=bk: lambda e: e.matmul(
                                banks[bk][:], aoT[:, k, :], Wo[:, k, nb * 512:(nb + 1) * 512], start=(k == 0), stop=(k == 7)))(),
                                 reads=[BaoT, BW[2], BW[3]], writes=[PB[bk]])
                        S.op("dve", (lambda nb=nb, bk=bk: lambda e: e.tensor_tensor(
                            out=x[:, t, nb * 512:(nb + 1) * 512], in0=x[:, t, nb * 512:(nb + 1) * 512], in1=banks[bk][:],
                            op=ALU.add))(), reads=[BX[t], PB[bk]], writes=[BX[t]])

                X1(-1); X2(-1)
                X1(0); X2(0)
                fi = {}
                for t in range(NT + 3):
                    if 0 <= t - 2 < NT:
                        fi[t - 2] = finalize_a(t - 2, "mid")
                    if t + 1 < NT:
                        X1(t + 1)
                    if t == NT - 1:
                        moe_load_gu(0, 0); moe_load_gu(0, 1)
                    if t == NT:
                        moe_load_dn(0, 0); moe_load_dn(0, 1)
                    if t == NT + 2:
                        moe_fold(0, 0); moe_fold(0, 1)
                    if 0 <= t - 1 < NT:
                        Y2a(t - 1)
                    if t < NT:
                        Y1(t)
                    if 0 <= t - 1 < NT:
                        Y2b(t - 1)
                    if 0 <= t - 2 < NT:
                        finalize_b(t - 2, fi[t - 2], 1)
                    if t + 1 < NT:
                        X2(t + 1)
                    if 0 <= t - 3 < NT:
                        router_logits(0, t - 3)
                if stop_after == "attn":
                    dump_and_end()
                    return nc
            S.barrier()
        cur_es[0] = es

        def bc3(ap2, n):
            return ap2.unsqueeze(2).to_broadcast([128, NT, n])

        def moe_phase(l, after_chunk=None, nchunks=16, tile_hook=None, first_load=0, end_hook=None):
            gl_m = sb("gl_m", [128, NT]); Bglm = Buf("gl_m")
            goh = sb("goh", [128, NT, 4]); Bgoh = Buf("goh")
            gex = sb("gex", [128, NT, 4]); Bgex = Buf("gex")
            gp = sb("gp", [128, NT]); Bgp = Buf("gp")
            sel4 = sb("sel4", [128, NT, 32]); Bsel4 = Buf("sel4")
            sel = sb("sel", [128, NT, 8]); Bsel = Buf("sel")
            sel2 = sb("sel2", [128, NT, 8]); Bsel2 = Buf("sel2")
            oh1 = sb("oh1", [128, NT, 8]); Boh1 = Buf("oh1")
            oh2 = sb("oh2", [128, NT, 8]); Boh2 = Buf("oh2")
            m1 = sb("m1", [128, NT]); m2 = sb("m2", [128, NT]); Bm1, Bm2 = Buf("m1"), Buf("m2")
            w1 = sb("w1", [128, NT]); w2 = sb("w2", [128, NT]); Bw1, Bw2 = Buf("w1"), Buf("w2")
            sg = [sb(f"sg{i}", [128, 256]) for i in range(2)]
            Bsg = [Buf(f"sg{i}") for i in range(2)]
            actb = [sb(f"actb{i}", [128, 256], BF16) for i in range(2)]
            Bact = [Buf(f"act{i}") for i in range(2)]
            actT = [sb(f"actT{i}", [128, 2, 128], BF16) for i in range(3)]
            BactT = [Buf(f"actT{i}") for i in range(3)]

            for ex in range(first_load, 4):
                moe_load(l, ex)

            gl = lg[:, :, 0:4]
            S.op("dve", lambda e: e.tensor_reduce(out=gl_m[:], in_=gl, axis=AX.X, op=ALU.max), reads=[Blg], writes=[Bglm])
            S.op("dve", lambda e: e.tensor_tensor(out=goh[:], in0=gl, in1=bc3(gl_m[:], 4), op=ALU.is_equal),
                 reads=[Blg, Bglm], writes=[Bgoh])
            S.op("dve", lambda e: e.tensor_tensor(out=gex[:], in0=gl, in1=bc3(gl_m[:], 4), op=ALU.subtract),
                 reads=[Blg, Bglm], writes=[Bgex])
            S.op("act", lambda e: e.activation(out=gex[:], in_=gex[:], func=AF.Exp), reads=[Bgex], writes=[Bgex])
            S.op("dve", lambda e: e.tensor_reduce(out=gp[:], in_=gex[:], axis=AX.X, op=ALU.add), reads=[Bgex], writes=[Bgp])
            S.op("dve", lambda e: e.reciprocal(out=gp[:], in_=gp[:]), reads=[Bgp], writes=[Bgp])
            S.op("dve", lambda e: e.tensor_tensor(
                out=sel4[:].rearrange("p t (g e) -> p t g e", g=4), in0=lg[:, :, 4:36].rearrange("p t (g e) -> p t g e", g=4),
                in1=goh[:].unsqueeze(3).to_broadcast([128, NT, 4, 8]), op=ALU.mult),
                 reads=[Blg, Bgoh], writes=[Bsel4])
            S.op("dve", lambda e: e.tensor_reduce(out=sel[:], in_=sel4[:].rearrange("p t (g e) -> p t e g", g=4),
                                                  axis=AX.X, op=ALU.add), reads=[Bsel4], writes=[Bsel])
            S.op("dve", lambda e: e.tensor_reduce(out=m1[:], in_=sel[:], axis=AX.X, op=ALU.max), reads=[Bsel], writes=[Bm1])
            S.op("dve", lambda e: e.tensor_tensor(out=oh1[:], in0=sel[:], in1=bc3(m1[:], 8), op=ALU.is_equal),
                 reads=[Bsel, Bm1], writes=[Boh1])
            S.op("dve", lambda e: e.scalar_tensor_tensor(out=sel2[:], in0=oh1[:], scalar=-1e30, in1=sel[:],
                                                         op0=ALU.mult, op1=ALU.add), reads=[Boh1, Bsel], writes=[Bsel2])
            S.op("dve", lambda e: e.tensor_reduce(out=m2[:], in_=sel2[:], axis=AX.X, op=ALU.max), reads=[Bsel2], writes=[Bm2])
            S.op("dve", lambda e: e.tensor_tensor(out=oh2[:], in0=sel2[:], in1=bc3(m2[:], 8), op=ALU.is_equal),
                 reads=[Bsel2, Bm2], writes=[Boh2])
            S.op("dve", lambda e: e.tensor_tensor(out=w2[:], in0=m2[:], in1=m1[:], op=ALU.subtract),
                 reads=[Bm1, Bm2], writes=[Bw2])
            S.op("act", lambda e: e.activation(out=w2[:], in_=w2[:], func=AF.Exp), reads=[Bw2], writes=[Bw2])
            S.op("dve", lambda e: e.tensor_scalar(out=w2[:], in0=w2[:], scalar1=1.0, scalar2=None, op0=ALU.add),
                 reads=[Bw2], writes=[Bw2])
            S.op("dve", lambda e: e.reciprocal(out=w1[:], in_=w2[:]), reads=[Bw2], writes=[Bw1])
            S.op("dve", lambda e: e.tensor_tensor(out=w1[:], in0=w1[:], in1=gp[:], op=ALU.mult), reads=[Bw1, Bgp], writes=[Bw1])
            S.op("dve", lambda e: e.tensor_tensor(out=w2[:], in0=gp[:], in1=w1[:], op=ALU.subtract),
                 reads=[Bgp, Bw1], writes=[Bw2])
            S.op("dve", lambda e: e.tensor_tensor(out=oh1[:], in0=oh1[:], in1=bc3(w1[:], 8), op=ALU.mult),
                 reads=[Boh1, Bw1], writes=[Boh1])
            S.op("dve", lambda e: e.tensor_tensor(out=oh2[:], in0=oh2[:], in1=bc3(w2[:], 8), op=ALU.mult),
                 reads=[Boh2, Bw2], writes=[Boh2])
            S.op("dve", lambda e: e.tensor_tensor(out=oh1[:], in0=oh1[:], in1=oh2[:], op=ALU.add),
                 reads=[Boh1, Boh2], writes=[Boh1])
            S.op("dve", lambda e: e.tensor_tensor(
                out=comb[:].rearrange("p t (g e) -> p t g e", g=4),
                in0=goh[:].unsqueeze(3).to_broadcast([128, NT, 4, 8]),
                in1=oh1[:].unsqueeze(2).to_broadcast([128, NT, 4, 8]), op=ALU.mult),
                 reads=[Bgoh, Boh1], writes=[Bcomb])

            GUB = [0, 1]; TB = [2, 3]; YB = [[4, 5], [6, 7]]
            steps = []
            for c in range(nchunks):
                for t in range(NT):
                    for e2 in range(2):
                        steps.append((c, t, e2))
            n = len(steps)

            def GU(i):
                c, t, e2 = steps[i]
                s = (2 * c + e2) % 4
                wgu, _ = moe_views(s)
                bk = GUB[i % 2]
                for k in range(8):
                    S.op("pe", (lambda k=k: lambda e: e.matmul(
                        banks[bk][:], hT[:, k, t * 128:(t + 1) * 128], wgu[:, k, :], start=(k == 0), stop=(k == 7)))(),
                         reads=[BH[t], BW[s]], writes=[PB[bk]])
                si = i % 2
                S.op("act", lambda e: e.activation(out=sg[si][:], in_=banks[bk][:, 0:256], func=AF.Silu),
                     reads=[PB[bk]], writes=[Bsg[si]])
                ex = 2 * c + e2
                S.op("dve", lambda e: e.scalar_tensor_tensor(
                    out=actb[si][:], in0=sg[si][:], scalar=comb[:, t, ex:ex + 1], in1=banks[bk][:, 256:512],
                    op0=ALU.mult, op1=ALU.mult), reads=[Bsg[si], Bcomb, PB[bk]], writes=[Bact[si]])

            def TR(i):
                si = i % 2
                bk = TB[i % 2]
                ti = i % 3
                for j in range(2):
                    S.op("pe", (lambda j=j: lambda e: e.matmul(
                        banks[bk][:, j * 128:(j + 1) * 128], actb[si][:, j * 128:(j + 1) * 128], ident[:],
                        start=True, stop=True))(),
                         reads=[Bact[si], Bident], writes=[PB[bk]])
                S.op("act", lambda e: e.activation(out=actT[ti][:], in_=banks[bk][:, 0:256].rearrange("p (j c) -> p j c", j=2),
                                                   func=AF.Copy), reads=[PB[bk]], writes=[BactT[ti]])

            def DN(i):
                c, t, e2 = steps[i]
                s = (2 * c + e2) % 4
                _, wdn = moe_views(s)
                ti = i % 3
                yb = YB[t % 2]
                for nb in range(2):
                    for j in range(2):
                        S.op("pe", (lambda nb=nb, j=j: lambda e: e.matmul(
                            banks[yb[nb]][:], actT[ti][:, j, :], wdn[:, j, nb * 512:(nb + 1) * 512],
                            start=(e2 == 0 and j == 0), stop=(e2 == 1 and j == 1)))(),
                             reads=[BactT[ti], BW[s]], writes=[PB[yb[nb]]])
                if e2 == 1:
                    for nb in range(2):
                        S.op("dve", (lambda nb=nb: lambda e: e.tensor_tensor(
                            out=x[:, t, nb * 512:(nb + 1) * 512], in0=x[:, t, nb * 512:(nb + 1) * 512],
                            in1=banks[yb[nb]][:], op=ALU.add))(), reads=[BX[t], PB[yb[nb]]], writes=[BX[t]])
                    if tile_hook is not None:
                        tile_hook(c, t)
                    if end_hook is not None and c == nchunks - 1:
                        end_hook(t)
                    if t == NT - 1:
                        if c + 2 < 16 and c + 2 < nchunks:
                            moe_load(l, 2 * (c + 2)); moe_load(l, 2 * (c + 2) + 1)
                        if after_chunk is not None:
                            after_chunk(c)

            for i in range(n + 2):
                if i < n:
                    GU(i)
                if 1 <= i <= n:
                    TR(i - 1)
                if 2 <= i <= n + 1:
                    DN(i - 2)

        def mod_jobs_moe0():
            vecT, BvecT = MS["vecT"], MS["BvecT"]
            yield from compute_mod_gen(1, 1, vecT, BvecT, True, lag=12)
            to_fm(vecT, BvecT, fmt, Bfmt)
            yield from compute_mod_gen(1, 0, vecT, BvecT, False, lag=12)
            to_fm(vecT, BvecT, fmt2, Bfmt2)
            haff_combine(2, 1)
            yield from compute_mod_gen(1, 2, gtbm, Bgtbm, True, lag=12)
            make_aset(1, bout_d, gtbm, Bgtbm)
            yield from compute_mod_gen(1, 4, vecT, BvecT, True, lag=12)
            to_fm(vecT, BvecT, fmt, Bfmt)
            yield from compute_mod_gen(1, 3, vecT, BvecT, False, lag=12)
            to_fm(vecT, BvecT, fmt2, Bfmt2)
            haff_combine(3, 2)
            yield from compute_mod_gen(1, 5, MS["gtmp"], MS["Bgtmp"], True, lag=12)

        moe0_job = [None]

        def tile_hook_moe0(c, t):
            if c < 1:
                return
            if moe0_job[0] is None:
                moe0_job[0] = mod_jobs_moe0()
            next(moe0_job[0], None)

        es2 = ExitStack()
        cur_es[0] = es2
        bank_pool[0] = [0, 1, 2, 3]
        with es2:
            alloc_mod_scratch()
            MS["gtmp"] = sb("gtmp", [128, D]); MS["Bgtmp"] = Buf("gtmp")
            eh0, ef0 = make_end_hook("mid", haff=2)
            moe_phase(0, None, nchunks=(NCHUNK_DBG or 16), tile_hook=tile_hook_moe0, end_hook=(eh0 if USE_END_HOOK else None), first_load=2)
            for _ in (moe0_job[0] or ()):
                pass
            if USE_END_HOOK:
                ef0()
            else:
                finalize_seq("mid", haff=2)
            dma("pool", "dw", W[:, 4096:8192].rearrange("p (k n) -> p k n", k=8),
                win_d[:, 2048:2560].rearrange("(k p) n -> p k n", p=128), writes=[BW[0], BW[1]])
            if stop_after == "moe0":
                dump_and_end()
                return nc
            S.op("pool", lambda e: e.tensor_copy(out=gtbf[:], in_=MS["gtmp"][:]), reads=[MS["Bgtmp"]], writes=[Bgtbf])
            make_aset(2, None, None, None)
        S.barrier()
        cur_es[0] = es

        es3 = ExitStack()
        cur_es[0] = es3
        bank_pool[0] = list(range(8))
        with es3:
            gst = sb("gst", [128, NT, 4, 6]); Bgst = Buf("gst")
            mvg = sb("mvg", [128, NT, 2]); Bmvg = Buf("mvg")
            sdg = sb("sdg", [128, NT]); rstdg = sb("rstdg", [128, NT]); nmrg = sb("nmrg", [128, NT])
            Bsdg, Brstdg, Bnmrg = Buf("sdg"), Buf("rstdg"), Buf("nmrg")
            u32 = [sb(f"u32_{i}", [128, 512]) for i in range(2)]; Bu32 = [Buf(f"u32_{i}") for i in range(2)]
            v32 = [sb(f"v32_{i}", [128, 512]) for i in range(2)]; Bv32 = [Buf(f"v32_{i}") for i in range(2)]
            vln = [sb(f"vln{i}", [128, 512], BF16) for i in range(2)]; Bvln = [Buf(f"vln{i}") for i in range(2)]
            gated = [sb(f"gated{i}", [128, 512], BF16) for i in range(2)]; Bgated = [Buf(f"gated{i}") for i in range(2)]
            gatedT = [sb(f"gatedT{i}", [128, 4, 128], BF16) for i in range(3)]; BgatedT = [Buf(f"gatedT{i}") for i in range(3)]
            lngb = [sb(f"lngb{i}", [128, 2, 512]) for i in range(2)]; Blngb = [Buf(f"lngb{i}") for i in range(2)]
            binr = [sb(f"binr{i}", [128, 2, 512], BF16) for i in range(2)]; Bbinr = [Buf(f"binr{i}") for i in range(2)]
            for i in range(2):
                S.op("pool", (lambda i=i: lambda e: e.memset(binr[i][:], 0.0))(), writes=[Bbinr[i]])
            wsTm = sb("wsTm", [128, 8, 128], BF16); BwsT = Buf("wsTm")
            trilb = sb("trilb", [128, 128], BF16); Btril = Buf("tril")
            bsT = sb("bsT", [128, 8]); BbsT = Buf("bsT")
            dma("pool", "dw", wsTm[:], wsT_d.rearrange("g s t -> s g t"), writes=[BwsT])
            dma("pool", "dw", trilb[:], tril_d, writes=[Btril])
            dma("sp", "dc", bsT[:], bsT_d, writes=[BbsT])
            S.op("dve", lambda e: e.tensor_tensor(out=wsTm[:], in0=wsTm[:], in1=trilb[:].unsqueeze(1).to_broadcast([128, 8, 128]),
                                                  op=ALU.mult), reads=[BwsT, Btril], writes=[BwsT])

            def gviews(b):
                base = b * 12288
                wu = W[:, base:base + 4096].rearrange("p (k n) -> p k n", k=8)
                wv = W[:, base + 4096:base + 8192].rearrange("p (k n) -> p k n", k=8)
                wo2 = W[:, base + 8192:base + 12288].rearrange("p (j n) -> p j n", j=4)
                return wu, wv, wo2

            def gfold(b):
                _, _, wo2 = gviews(b)
                bw = [BW[2 * b], BW[2 * b + 1]]
                S.op("pool", lambda e: e.tensor_tensor(out=wo2, in0=wo2,
                                                       in1=gtbm[:].unsqueeze(1).to_broadcast([128, 4, D]), op=ALU.mult),
                     reads=bw + [Bgtbm], writes=bw)

            def gload(cb, b, main, skip_wv=False, fold_now=True, part=None):
                wu, wv, wo2 = gviews(b)
                bw = [BW[2 * b], BW[2 * b + 1]]
                if part in (None, 0):
                    if not skip_wv:
                        dma("pool", "dw", wv,
                            win_d[:, 2048 + cb * 512:2048 + (cb + 1) * 512].rearrange("(k p) n -> p k n", p=128), writes=bw)
                    dma("pool", "dw", binr[b][0:1, 1, :], bin_d[:, 2048 + cb * 512:2048 + (cb + 1) * 512], writes=[Bbinr[b]])
                    if main:
                        dma("pool", "dw", binr[b][0:1, 0, :], bin_d[:, cb * 512:(cb + 1) * 512], writes=[Bbinr[b]])
                if main and part in (None, 1):
                    dma("pool", "dw", wu, win_d[:, cb * 512:(cb + 1) * 512].rearrange("(k p) n -> p k n", p=128), writes=bw)
                if main and part in (None, 2):
                    dma("pool", "dw", wo2, wout_d[cb * 512:(cb + 1) * 512, :].rearrange("(j p) n -> p j n", p=128), writes=bw)
                    if fold_now:
                        gfold(b)
                if main and part in (None, 0):
                    pass
                    dma("sp", "dc", lngb[b][:, 0, :], glng_d[:, cb * 512:(cb + 1) * 512].to_broadcast([128, 512]),
                        writes=[Blngb[b]])
                    dma("sp", "dc", lngb[b][:, 1, :], glnb_d[:, cb * 512:(cb + 1) * 512].to_broadcast([128, 512]),
                        writes=[Blngb[b]])

            gload(0, 0, False, skip_wv=True)
            pi = 0
            for cb in range(4):
                b = cb % 2
                if cb + 1 < 4:
                    gload(cb + 1, (cb + 1) % 2, False)
                _, wv, _ = gviews(b)
                bw = [BW[2 * b], BW[2 * b + 1]]
                for t in range(NT):
                    bk = next_bank()
                    i2 = pi % 2
                    pi += 1
                    for k in range(8):
                        S.op("pe", (lambda k=k, bk=bk, t=t, wv=wv: lambda e: e.matmul(
                            banks[bk][:], hT[:, k, t * 128:(t + 1) * 128], wv[:, k, :], start=(k == 0), stop=False))(),
                             reads=[BH[t]] + bw, writes=[PB[bk]])
                    S.op("pe", (lambda bk=bk, b=b: lambda e: e.matmul(banks[bk][:], onesrow[:], binr[b][:, 1, :],
                                                                      start=False, stop=True))(),
                         reads=[Bonesrow, Bbinr[b]], writes=[PB[bk]])
                    S.op("act", (lambda bk=bk, i2=i2: lambda e: e.activation(out=v32[i2][:], in_=banks[bk][:], func=AF.Gelu))(),
                         reads=[PB[bk]], writes=[Bv32[i2]])
                    S.op("dve", (lambda i2=i2, t=t, cb=cb: lambda e: e.bn_stats(out=gst[:, t, cb, :], in_=v32[i2][:]))(),
                         reads=[Bv32[i2]], writes=[Bgst])
            for t in range(NT):
                S.op("dve", (lambda t=t: lambda e: e.bn_aggr(out=mvg[:, t, :], in_=gst[:, t, :, :]))(),
                     reads=[Bgst], writes=[Bmvg])
            S.op("act", lambda e: e.activation(out=sdg[:], in_=mvg[:, :, 1], func=AF.Ln, bias=eps_t[:]),
                 reads=[Bmvg, Beps], writes=[Bsdg])
            S.op("act", lambda e: e.activation(out=rstdg[:], in_=sdg[:], func=AF.Exp, scale=-0.5),
                 reads=[Bsdg], writes=[Brstdg])
            S.op("dve", lambda e: e.scalar_tensor_tensor(out=nmrg[:], in0=mvg[:, :, 0], scalar=-1.0, in1=rstdg[:],
                                                         op0=ALU.mult, op1=ALU.mult), reads=[Bmvg, Brstdg], writes=[Bnmrg])

            gsteps = [(cb, t) for cb in range(4) for t in range(NT)]
            ng = len(gsteps)

            def gA(i):
                cb, t = gsteps[i]
                b = cb % 2
                if cb + 1 < 4:
                    if t == 3:
                        gload(cb + 1, (cb + 1) % 2, True, fold_now=False, part=0)
                    if t == 6:
                        gload(cb + 1, (cb + 1) % 2, True, fold_now=False, part=1)
                    if t == 9:
                        gload(cb + 1, (cb + 1) % 2, True, fold_now=False, part=2)
                    if t == 13:
                        gfold((cb + 1) % 2)
                wu, wv, _ = gviews(b)
                bw = [BW[2 * b], BW[2 * b + 1]]
                i2 = i % 2
                for which, wmat, dst, bdst in ((0, wu, u32[i2], Bu32[i2]), (1, wv, v32[i2], Bv32[i2])):
                    bk = next_bank()
                    for k in range(8):
                        S.op("pe", (lambda k=k, bk=bk, wmat=wmat: lambda e: e.matmul(
                            banks[bk][:], hT[:, k, t * 128:(t + 1) * 128], wmat[:, k, :], start=(k == 0), stop=False))(),
                             reads=[BH[t]] + bw, writes=[PB[bk]])
                    S.op("pe", (lambda bk=bk, which=which: lambda e: e.matmul(banks[bk][:], onesrow[:], binr[b][:, which, :],
                                                                              start=False, stop=True))(),
                         reads=[Bonesrow, Bbinr[b]], writes=[PB[bk]])
                    S.op("act", (lambda bk=bk, dst=dst: lambda e: e.activation(out=dst[:], in_=banks[bk][:], func=AF.Gelu))(),
                         reads=[PB[bk]], writes=[bdst])
                S.op("dve", lambda e: e.tensor_scalar(out=v32[i2][:], in0=v32[i2][:], scalar1=rstdg[:, t:t + 1],
                                                      scalar2=nmrg[:, t:t + 1], op0=ALU.mult, op1=ALU.add),
                     reads=[Bv32[i2], Brstdg, Bnmrg], writes=[Bv32[i2]])
                S.op("pool", lambda e: e.tensor_tensor(out=v32[i2][:], in0=v32[i2][:], in1=lngb[b][:, 0, :], op=ALU.mult),
                     reads=[Bv32[i2], Blngb[b]], writes=[Bv32[i2]])
                S.op("pool", lambda e: e.tensor_tensor(out=vln[i2][:], in0=v32[i2][:], in1=lngb[b][:, 1, :], op=ALU.add),
                     reads=[Bv32[i2], Blngb[b]], writes=[Bvln[i2]])

            def gB(i):
                cb, t = gsteps[i]
                i2 = i % 2
                bk = next_bank()
                for gi in range(2):
                    g = 2 * cb + gi
                    S.op("pe", (lambda gi=gi, g=g: lambda e: e.matmul(
                        banks[bk][:, gi * 256:(gi + 1) * 256], wsTm[:, g, :], vln[i2][:, gi * 256:(gi + 1) * 256],
                        start=True, stop=True))(), reads=[BwsT, Bvln[i2]], writes=[PB[bk]])
                for gi in range(2):
                    g = 2 * cb + gi
                    S.op("dve", (lambda gi=gi, g=g: lambda e: e.scalar_tensor_tensor(
                        out=gated[i2][:, gi * 256:(gi + 1) * 256], in0=banks[bk][:, gi * 256:(gi + 1) * 256],
                        scalar=bsT[:, g:g + 1], in1=u32[i2][:, gi * 256:(gi + 1) * 256], op0=ALU.add, op1=ALU.mult))(),
                         reads=[PB[bk], BbsT, Bu32[i2]], writes=[Bgated[i2]])

            def gC(i):
                i2 = i % 2
                bt = next_bank()
                i3 = i % 3
                for j in range(4):
                    S.op("pe", (lambda j=j: lambda e: e.matmul(
                        banks[bt][:, j * 128:(j + 1) * 128], gated[i2][:, j * 128:(j + 1) * 128], ident[:],
                        start=True, stop=True))(),
                         reads=[Bgated[i2], Bident], writes=[PB[bt]])
                S.op("act", lambda e: e.activation(out=gatedT[i3][:], in_=banks[bt][:].rearrange("p (j c) -> p j c", j=4),
                                                   func=AF.Copy), reads=[PB[bt]], writes=[BgatedT[i3]])

            def gD(i):
                cb, t = gsteps[i]
                b = cb % 2
                _, _, wo2 = gviews(b)
                bw = [BW[2 * b], BW[2 * b + 1]]
                i3 = i % 3
                for nb in range(2):
                    bk = next_bank()
                    for j in range(4):
                        S.op("pe", (lambda j=j, nb=nb, bk=bk: lambda e: e.matmul(
                            banks[bk][:], gatedT[i3][:, j, :], wo2[:, j, nb * 512:(nb + 1) * 512],
                            start=(j == 0), stop=(j == 3)))(), reads=[BgatedT[i3]] + bw, writes=[PB[bk]])
                    S.op("dve", (lambda nb=nb, bk=bk: lambda e: e.tensor_tensor(
                        out=x[:, t, nb * 512:(nb + 1) * 512], in0=x[:, t, nb * 512:(nb + 1) * 512], in1=banks[bk][:],
                        op=ALU.add))(), reads=[BX[t], PB[bk]], writes=[BX[t]])
                if cb == 3:
                    if t == 3:
                        moe_load_gu(1, 0); moe_load_gu(1, 1)
                    if t == 8:
                        moe_load_dn(1, 0); moe_load_dn(1, 1)
                    if t == 12:
                        moe_fold(1, 0); moe_fold(1, 1)
                    if USE_END_HOOK:
                        ehg(t)

            ehg, efg = make_end_hook("mid", haff=3, route_l=1)
            gload(0, 0, True)
            for i in range(ng + 3):
                if i < ng:
                    gA(i)
                if 1 <= i <= ng:
                    gB(i - 1)
                if 2 <= i <= ng + 1:
                    gC(i - 2)
                if 3 <= i <= ng + 2:
                    gD(i - 3)
            if USE_END_HOOK:
                efg()
            else:
                finalize_seq("mid", haff=3, route_l=1)
            if stop_after == "gmlp":
                dump_and_end()
                return nc
        S.barrier()
        cur_es[0] = es

        es4 = ExitStack()
        cur_es[0] = es4
        bank_pool[0] = [0, 1, 2, 3]
        with es4:
            make_aset(3, None, None, None, scale=1.0)
            eh1, ef1 = make_end_hook("out")
            moe_phase(1, None, nchunks=(NCHUNK_DBG or 16), first_load=2, end_hook=(eh1 if USE_END_HOOK else None))
            if USE_END_HOOK:
                ef1()
            else:
                finalize_seq("out")
            S.wait_all("sp", [Bout])
            S.emit()
    return nc


NCHUNK_DBG = None
USE_END_HOOK = False
ATT_DBG = [NT, None]


_CACHE = {}


def _host_inputs(inputs):
    f32 = np.float32
    x = np.asarray(inputs["x"], f32)
    c = np.asarray(inputs["c"], f32)
    pos = np.asarray(inputs["positions"], np.int32)
    shared = {}
    shared["ident"] = np.eye(128, dtype=f32)
    s = np.arange(128)[:, None]
    q = np.arange(128)[None, :]
    m_cur = np.where(s <= q, 0.0, NEG).astype(f32)
    m_prev = np.where(s > q, 0.0, NEG).astype(f32)
    m_none = np.full((128, 128), NEG, f32)
    inv_freq = (10000.0 ** (-np.arange(0, 64, 2, dtype=f32) / f32(64))).astype(f32)
    shared["invf"] = np.ascontiguousarray(np.broadcast_to(inv_freq[None, :], (128, 32))).astype(f32)
    shared["tril"] = (s <= q).astype(f32)
    shared["ada_w"] = np.ascontiguousarray(inputs["ada_w"], f32)
    shared["ada_b"] = np.ascontiguousarray(inputs["ada_b"], f32)
    g = np.asarray(inputs["post_ln_g"], f32).reshape(4, D)
    b = np.asarray(inputs["post_ln_b"], f32).reshape(4, D)
    shared["post_ln_g"] = np.ascontiguousarray(g)
    shared["post_ln_b"] = np.ascontiguousarray(b)
    shared["post_ln_gT"] = np.ascontiguousarray(g.reshape(4, 8, 128).transpose(0, 2, 1))
    shared["post_ln_bT"] = np.ascontiguousarray(b.reshape(4, 8, 128).transpose(0, 2, 1))
    shared["attn_w_qkv"] = np.ascontiguousarray(inputs["attn_w_qkv"][0], f32)
    shared["attn_b_qkv"] = np.ascontiguousarray(inputs["attn_b_qkv"], f32).reshape(1, 1536)
    shared["attn_sinks"] = np.ascontiguousarray(inputs["attn_sinks"], f32).reshape(1, 16)
    shared["attn_w_o"] = np.ascontiguousarray(inputs["attn_w_o"][0], f32)
    shared["attn_b_o"] = np.ascontiguousarray(inputs["attn_b_o"], f32).reshape(1, D)
    shared["gmlp_w_in"] = np.ascontiguousarray(inputs["gmlp_w_in"][0], f32)
    shared["gmlp_b_in"] = np.ascontiguousarray(inputs["gmlp_b_in"], f32).reshape(1, 4096)
    shared["gmlp_ln_g"] = np.ascontiguousarray(inputs["gmlp_sgu_ln_g"], f32).reshape(1, 2048)
    shared["gmlp_ln_b"] = np.ascontiguousarray(inputs["gmlp_sgu_ln_b"], f32).reshape(1, 2048)
    shared["gmlp_w_sT"] = np.ascontiguousarray(np.asarray(inputs["gmlp_w_s"][0], f32).transpose(0, 2, 1))
    shared["gmlp_b_sT"] = np.ascontiguousarray(np.asarray(inputs["gmlp_b_s"][0], f32).T)
    shared["gmlp_w_out"] = np.ascontiguousarray(inputs["gmlp_w_out"][0], f32)
    shared["gmlp_b_out"] = np.ascontiguousarray(inputs["gmlp_b_out"], f32).reshape(1, D)
    shared["moe_wr"] = np.ascontiguousarray(np.concatenate(
        [np.asarray(inputs["moe_w_group_router"], f32), np.asarray(inputs["moe_w_expert_router"], f32)], axis=-1))
    shared["moe_br"] = np.ascontiguousarray(np.concatenate(
        [np.asarray(inputs["moe_b_group_router"], f32), np.asarray(inputs["moe_b_expert_router"], f32)], axis=-1)
    ).reshape(2, 1, 36)
    shared["moe_w_gate_up"] = np.ascontiguousarray(inputs["moe_w_gate_up"], f32).reshape(2, 32, D, 512)
    shared["moe_w_down"] = np.ascontiguousarray(inputs["moe_w_down"], f32).reshape(2, 32, 256, D)
    in_maps = []
    for r in range(8):
        bi, qi = r // 4, r % 4
        s0 = qi * 2048
        m = dict(shared)
        xc = np.zeros((17 * 128, D), f32)
        pc = np.zeros((17 * 128,), np.int32)
        if qi > 0:
            xc[:] = x[bi, s0 - 128:s0 + 2048]
            pc[:] = pos[bi, s0 - 128:s0 + 2048]
        else:
            xc[128:] = x[bi, 0:2048]
            pc[128:] = pos[bi, 0:2048]
        m["xin"] = xc
        m["pos"] = np.ascontiguousarray(pc.reshape(17, 128).T)
        m["cT"] = np.ascontiguousarray(c[bi].reshape(8, 128).T)
        mk = np.stack([np.tile(m_cur, (1, 4)), np.tile(m_prev, (1, 4)),
                       np.tile(m_none if qi == 0 else m_prev, (1, 4))]).astype(f32)
        m["masks"] = np.ascontiguousarray(mk)
        in_maps.append(m)
    return in_maps


def kernel(**inputs):
    in_maps = _host_inputs(inputs)
    if "nc" not in _CACHE:
        _CACHE["nc"] = build()
    res = run_bass_kernel_spmd(_CACHE["nc"], in_maps, core_ids=list(range(8)))
    out = np.empty((2, 8192, D), np.float32)
    for r in range(8):
        bi, qi = r // 4, r % 4
        out[bi, qi * 2048:(qi + 1) * 2048] = res.results[r]["out"]
    return out
```

```python
import numpy as np
from contextlib import ExitStack
import concourse.bass as bass
import concourse.mybir as mybir
from concourse.bass_utils import run_bass_kernel_spmd

F32 = mybir.dt.float32
BF16 = mybir.dt.bfloat16
I32 = mybir.dt.int32
AF = mybir.ActivationFunctionType
ALU = mybir.AluOpType
AX = mybir.AxisListType

NT = 16
D = 1024
ALPHA = 4.0 ** 0.25
LN_EPS = 1e-5
TWO_PI = 6.283185307179586
C1 = 6.28125
C2 = TWO_PI - C1
NEG = -30000.0


class Buf:
    __slots__ = ("name", "w", "r")

    def __init__(self, name):
        self.name = name
        self.w = None
        self.r = {}


class Sched:
    ENG = ("pe", "act", "dve", "pool", "sp")
    NSLOT = 8

    def __init__(self, nc, es):
        self.nc = nc
        self.es = es
        self.E = {"pe": nc.tensor, "act": nc.scalar, "dve": nc.vector,
                  "pool": nc.gpsimd, "sp": nc.sync}
        self.cnt = {}
        self.isdma = {}
        self.waited = {e: {} for e in self.ENG}
        self.ops = []
        self.needed = {}
        for e in self.ENG:
            self.cnt[e] = 0
            self.isdma[e] = False
            self.needed[e] = set()

    def dma_proc(self, name):
        self.cnt[name] = 0
        self.isdma[name] = True
        self.needed[name] = set()

    def _add_dep(self, deps, p, v):
        if self.isdma[p]:
            key = (p, (v - 1) % self.NSLOT)
            deps[key] = max(deps.get(key, 0), (v - 1) // self.NSLOT + 1)
        else:
            deps[p] = max(deps.get(p, 0), v)

    def _mk_waits(self, eng, deps):
        waits = []
        wd = self.waited[eng]
        for key, v in deps.items():
            if wd.get(key, 0) >= v:
                continue
            wd[key] = v
            waits.append((key, v))
            if not isinstance(key, tuple):
                self.needed[key].add(v)
        return waits

    def op(self, eng, fn, reads=(), writes=(), proc=None):
        proc = proc or eng
        deps = {}
        for b in reads:
            if b.w is not None:
                p, v = b.w
                if p == eng and eng == "pe":
                    continue
                self._add_dep(deps, p, v)
        for b in writes:
            if b.w is not None:
                p, v = b.w
                if p != eng or eng != "pe":
                    self._add_dep(deps, p, v)
            for p, v in b.r.items():
                if p != eng or eng != "pe":
                    self._add_dep(deps, p, v)
        self.cnt[proc] += 1
        c = self.cnt[proc]
        if self.isdma[proc] and c > self.NSLOT:
            self._add_dep(deps, proc, c - self.NSLOT)
        waits = self._mk_waits(eng, deps)
        self.ops.append((eng, fn, waits, proc, c))
        for b in reads:
            b.r[proc] = c
        for b in writes:
            b.w = (proc, c)
            b.r = {}
        return c

    def _all_deps(self):
        deps = {}
        for p, v in self.cnt.items():
            if v <= 0:
                continue
            if self.isdma[p]:
                for i in range(max(1, v - self.NSLOT + 1), v + 1):
                    self._add_dep(deps, p, i)
            else:
                deps[p] = v
        return deps

    def barrier(self):
        for eng in self.ENG:
            deps = {k: v for k, v in self._all_deps().items() if k != eng}
            waits = self._mk_waits(eng, deps)
            if waits:
                self.ops.append((eng, None, waits, None, 0))

    def wait_all(self, eng, bufs):
        deps = {}
        for b in bufs:
            if b.w is not None:
                self._add_dep(deps, b.w[0], b.w[1])
        for b in bufs:
            if b.w is not None and self.isdma[b.w[0]]:
                p = b.w[0]
                v = self.cnt[p]
                for i in range(max(1, v - self.NSLOT + 1), v + 1):
                    self._add_dep(deps, p, i)
        waits = self._mk_waits(eng, deps)
        self.ops.append((eng, None, waits, None, 0))

    def emit(self):
        nc = self.nc
        sems = {}
        for p in self.cnt:
            if self.isdma[p]:
                for sl in range(self.NSLOT):
                    sems[(p, sl)] = self.es.enter_context(nc.semaphore(f"s_{p}{sl}"))
            else:
                sems[p] = self.es.enter_context(nc.semaphore("s_" + p))
        last_inc = {p: 0 for p in self.cnt}
        for eng, fn, waits, proc, c in self.ops:
            e = self.E[eng]
            for key, v in waits:
                e.wait_ge(sems[key], v * 16 if isinstance(key, tuple) else v)
            if fn is None:
                continue
            ins = fn(e)
            if self.isdma[proc]:
                ins.then_inc(sems[(proc, (c - 1) % self.NSLOT)], 16)
            elif c in self.needed[proc]:
                ins.then_inc(sems[proc], c - last_inc[proc])
                last_inc[proc] = c


def build(stop_after=None):
    nc = bass.Bass("TRN2", target_bir_lowering=False)

    def din(name, shape, dt=F32):
        return nc.dram_tensor(name, list(shape), dt, kind="ExternalInput").ap()

    xin = din("xin", [17 * 128, D])
    pos_d = din("pos", [128, 17], I32)
    cT_d = din("cT", [128, 8])
    ident_d = din("ident", [128, 128])
    masks_d = din("masks", [3, 128, 512])
    invf_d = din("invf", [128, 32])
    tril_d = din("tril", [128, 128])
    ada_w = din("ada_w", [2, D, 6 * D])
    ada_b = din("ada_b", [2, 6 * D])
    lng_d = din("post_ln_g", [4, D])
    lnb_d = din("post_ln_b", [4, D])
    lngT_d = din("post_ln_gT", [4, 128, 8])
    lnbT_d = din("post_ln_bT", [4, 128, 8])
    wqkv_d = din("attn_w_qkv", [D, 1536])
    bqkv_d = din("attn_b_qkv", [1, 1536])
    sinks_d = din("attn_sinks", [1, 16])
    wo_d = din("attn_w_o", [D, D])
    bo_d = din("attn_b_o", [1, D])
    win_d = din("gmlp_w_in", [D, 4096])
    bin_d = din("gmlp_b_in", [1, 4096])
    glng_d = din("gmlp_ln_g", [1, 2048])
    glnb_d = din("gmlp_ln_b", [1, 2048])
    wsT_d = din("gmlp_w_sT", [8, 128, 128])
    bsT_d = din("gmlp_b_sT", [128, 8])
    wout_d = din("gmlp_w_out", [2048, D])
    bout_d = din("gmlp_b_out", [1, D])
    wr_d = din("moe_wr", [2, D, 36])
    br_d = din("moe_br", [2, 1, 36])
    wgu_d = din("moe_w_gate_up", [2, 32, D, 512])
    wdn_d = din("moe_w_down", [2, 32, 256, D])
    out_d = nc.dram_tensor("out", [NT * 128, D], F32, kind="ExternalOutput").ap()

    es = ExitStack()
    with es:
        S = Sched(nc, es)
        for p in ("dx", "dw", "dc", "do"):
            S.dma_proc(p)

        cur_es = [es]

        sb_n = [0]

        def sb(name, shape, dt=F32):
            sb_n[0] += 1
            return cur_es[0].enter_context(nc.sbuf_tensor(f"sb{sb_n[0]}_{name}", list(shape), dt))

        banks = [es.enter_context(nc.psum_tensor(f"bank{i}", [128, 512], F32)) for i in range(8)]
        bankbf = [b[:].bitcast(BF16) for b in banks]
        PB = [Buf(f"bank{i}") for i in range(8)]
        bank_rr = [0]
        bank_pool = [list(range(8))]

        def next_bank():
            bank_rr[0] += 1
            return bank_pool[0][bank_rr[0] % len(bank_pool[0])]

        x = sb("x", [128, NT, D])
        BX = [Buf(f"x{t}") for t in range(NT)]
        hT = sb("hT", [128, 8, NT * 128], BF16)
        BH = [Buf(f"hT{t}") for t in range(NT)]
        W = sb("W", [128, 24576], BF16)
        BW = [Buf(f"W{s}") for s in range(4)]
        A1 = sb("A1", [128, D]); A0 = sb("A0", [128, D])
        BA1, BA0 = Buf("A1"), Buf("A0")
        gtbm = sb("gtbm", [128, D]); gtbf = sb("gtbf", [128, D])
        Bgtbm, Bgtbf = Buf("gtbm"), Buf("gtbf")
        ident = sb("ident", [128, 128], BF16); Bident = Buf("ident")
        ident32 = sb("ident32", [128, 128]); Bident32 = Buf("ident32")
        onesrow = sb("onesrow", [128, 128], BF16); Bonesrow = Buf("onesrow")
        cact_rep = sb("cact_rep", [128, 8, 128], BF16); Bcact = Buf("cact")
        ctmp = sb("ctmp", [128, 8]); Bctmp = Buf("ctmp")
        xnb = [sb(f"xnb{i}", [128, D], BF16) for i in range(2)]
        Bxnb = [Buf(f"xnb{i}") for i in range(2)]
        st_ = [sb(f"st{i}", [128, 2, 6]) for i in range(2)]; mv_ = [sb(f"mv{i}", [128, 2]) for i in range(2)]
        sd_ = [sb(f"sd{i}", [128, 1]) for i in range(2)]
        rstd_ = [sb(f"rstd{i}", [128, 1]) for i in range(2)]; nmr_ = [sb(f"nmr{i}", [128, 1]) for i in range(2)]
        Bst_ = [Buf(f"st{i}") for i in range(2)]; Bmv_ = [Buf(f"mv{i}") for i in range(2)]
        Bsd_ = [Buf(f"sd{i}") for i in range(2)]; Brstd_ = [Buf(f"rstd{i}") for i in range(2)]
        Bnmr_ = [Buf(f"nmr{i}") for i in range(2)]
        fin_i = [0]
        eps_t = sb("eps_t", [128, 1]); Beps = Buf("eps")
        H1 = [sb(f"H1_{i}", [128, 8]) for i in range(4)]
        H0 = [sb(f"H0_{i}", [128, 8]) for i in range(4)]
        BHa = [Buf(f"Haff{i}") for i in range(4)]
        fmt = sb("fmt", [128, 8]); fmt2 = sb("fmt2", [128, 8]); fmg = sb("fmg", [128, 8]); fmb = sb("fmb", [128, 8])
        Bfmt, Bfmt2, Bfmg, Bfmb = Buf("fmt"), Buf("fmt2"), Buf("fmg"), Buf("fmb")
        wr = [sb(f"wr{l}", [128, 8, 36], BF16) for l in range(2)]
        brr = [sb(f"brr{l}", [128, 36], BF16) for l in range(2)]
        Bwr = [Buf(f"wr{l}") for l in range(2)]
        lg = sb("lg", [128, NT, 36]); Blg = Buf("lg")
        comb = sb("comb", [128, NT, 32]); Bcomb = Buf("comb")
        Bout = Buf("out")

        def dma(eng, proc, out, in_, reads=(), writes=()):
            S.op(eng, lambda e: e.dma_start(out=out, in_=in_), reads=reads, writes=writes, proc=proc)

        def bc_load(dst, bdst, row_ap):
            n = row_ap.shape[-1]
            dma("sp", "dc", dst[:, 0:n], row_ap.to_broadcast([128, n]), writes=[bdst])

        MS = {}

        def alloc_mod_scratch():
            MS["vecT"] = sb("vecT", [128, D]); MS["BvecT"] = Buf("vecT")
            MS["bcT"] = sb("bcT", [128, D]); MS["BbcT"] = Buf("bcT")
            MS["stg"] = [sb(f"stg{i}", [128, 8, 256], BF16) for i in range(2)]
            MS["Bstg"] = [Buf(f"stg{i}") for i in range(2)]

        dma("pool", "dw", ident[:], ident_d, writes=[Bident])
        dma("sp", "dc", ident32[:], ident_d, writes=[Bident32])
        S.op("dve", lambda e: e.memset(eps_t[:], LN_EPS), writes=[Beps])
        S.op("dve", lambda e: e.memset(onesrow[:], 0.0), writes=[Bonesrow])
        S.op("dve", lambda e: e.memset(onesrow[0:1, :], 1.0), writes=[Bonesrow])
        dma("sp", "dc", ctmp[:], cT_d, writes=[Bctmp])
        S.op("act", lambda e: e.activation(out=ctmp[:], in_=ctmp[:], func=AF.Silu), reads=[Bctmp], writes=[Bctmp])
        S.op("dve", lambda e: e.tensor_copy(out=cact_rep[:], in_=ctmp[:].unsqueeze(2).to_broadcast([128, 8, 128])),
             reads=[Bctmp], writes=[Bcact])
        for l in range(2):
            dma("pool", "dw", wr[l][:], wr_d[l].rearrange("(k p) n -> p k n", p=128), writes=[Bwr[l]])
            S.op("pool", (lambda l=l: lambda e: e.memset(brr[l][:], 0.0))(), writes=[Bwr[l]])
            dma("pool", "dw", brr[l][0:1, :], br_d[l], writes=[Bwr[l]])

        def compute_mod_gen(l, j, dst, bdst, add_one, lag=0):
            bcT, BbcT, stg, Bstg = MS["bcT"], MS["BbcT"], MS["stg"], MS["Bstg"]
            bc_load(bcT, BbcT, ada_b[l:l + 1, j * D:(j + 1) * D])

            def issue(nb):
                si = nb % 2
                col = j * D + nb * 256
                dma("pool", "dw", stg[si][:], ada_w[l][:, col:col + 256].rearrange("(k p) n -> p k n", p=128),
                    writes=[Bstg[si]])

            def consume(nb):
                si = nb % 2
                bk = next_bank()
                for k in range(8):
                    S.op("pe", (lambda k=k: lambda e: e.matmul(
                        banks[bk][:, 0:256], cact_rep[:, k, :], stg[si][:, k, :], start=(k == 0), stop=(k == 7)))(),
                         reads=[Bcact, Bstg[si]], writes=[PB[bk]])
                S.op("dve", lambda e: e.scalar_tensor_tensor(
                    out=dst[:, nb * 256:(nb + 1) * 256], in0=banks[bk][:, 0:256], scalar=(1.0 if add_one else 0.0),
                    in1=bcT[:, nb * 256:(nb + 1) * 256], op0=ALU.add, op1=ALU.add),
                     reads=[PB[bk], BbcT], writes=[bdst])

            issue(0); issue(1)
            for _ in range(lag):
                yield
            consume(0); consume(1)
            issue(2); issue(3)
            for _ in range(lag):
                yield
            consume(2); consume(3)

        def compute_mod(l, j, dst, bdst, add_one):
            for _ in compute_mod_gen(l, j, dst, bdst, add_one):
                pass

        def to_fm(src, bsrc, dst, bdst):
            tmp, Btmp = MS["bcT"], MS["BbcT"]
            S.op("dve", lambda e: e.tensor_tensor(
                out=tmp[:].rearrange("p (k c) -> p k c", k=8), in0=src[:].rearrange("p (k c) -> p k c", k=8),
                in1=ident32[:].unsqueeze(1).to_broadcast([128, 8, 128]), op=ALU.mult),
                 reads=[bsrc, Bident32], writes=[Btmp])
            S.op("dve", lambda e: e.tensor_reduce(out=dst[:], in_=tmp[:].rearrange("p (k c) -> p k c", k=8),
                                                  axis=AX.X, op=ALU.add),
                 reads=[Btmp], writes=[bdst])

        def make_haff(idx, l, j_sh, j_sc, ln_idx):
            vecT, BvecT = MS["vecT"], MS["BvecT"]
            compute_mod(l, j_sc, vecT, BvecT, True)
            to_fm(vecT, BvecT, fmt, Bfmt)
            compute_mod(l, j_sh, vecT, BvecT, False)
            to_fm(vecT, BvecT, fmt2, Bfmt2)
            haff_combine(idx, ln_idx)

        def haff_combine(idx, ln_idx):
            if ln_idx is None:
                S.op("dve", lambda e: e.tensor_copy(out=H1[idx][:], in_=fmt[:]), reads=[Bfmt], writes=[BHa[idx]])
                S.op("dve", lambda e: e.tensor_copy(out=H0[idx][:], in_=fmt2[:]), reads=[Bfmt2], writes=[BHa[idx]])
            else:
                dma("sp", "dc", fmg[:], lngT_d[ln_idx], writes=[Bfmg])
                dma("sp", "dc", fmb[:], lnbT_d[ln_idx], writes=[Bfmb])
                S.op("dve", lambda e: e.tensor_tensor(out=H1[idx][:], in0=fmt[:], in1=fmg[:], op=ALU.mult),
                     reads=[Bfmt, Bfmg], writes=[BHa[idx]])
                S.op("dve", lambda e: e.tensor_tensor(out=fmb[:], in0=fmt[:], in1=fmb[:], op=ALU.mult),
                     reads=[Bfmt, Bfmb], writes=[Bfmb])
                S.op("dve", lambda e: e.tensor_tensor(out=H0[idx][:], in0=fmb[:], in1=fmt2[:], op=ALU.add),
                     reads=[Bfmb, Bfmt2], writes=[BHa[idx]])

        def make_aset(ln_idx, bias_row, gtb, bgtb, scale=ALPHA):
            bc_load(A1, BA1, lng_d[ln_idx:ln_idx + 1, :])
            bc_load(A0, BA0, lnb_d[ln_idx:ln_idx + 1, :])
            if scale != 1.0:
                S.op("dve", lambda e: e.tensor_scalar(out=A1[:], in0=A1[:], scalar1=scale, scalar2=None, op0=ALU.mult),
                     reads=[BA1], writes=[BA1])
            if bias_row is None:
                if scale != 1.0:
                    S.op("dve", lambda e: e.tensor_scalar(out=A0[:], in0=A0[:], scalar1=scale, scalar2=None, op0=ALU.mult),
                         reads=[BA0], writes=[BA0])
            else:
                vt, bvt = MS["vecT"], MS["BvecT"]
                bc_load(vt, bvt, bias_row)
                S.op("dve", lambda e: e.tensor_tensor(out=vt[:], in0=vt[:], in1=gtb[:], op=ALU.mult),
                     reads=[bvt, bgtb], writes=[bvt])
                S.op("dve", lambda e: e.scalar_tensor_tensor(out=A0[:], in0=A0[:], scalar=scale, in1=vt[:],
                                                             op0=ALU.mult, op1=ALU.add),
                     reads=[BA0, bvt], writes=[BA0])

        def router_logits(l, t):
            bk = next_bank()
            for k in range(8):
                S.op("pe", (lambda k=k: lambda e: e.matmul(
                    banks[bk][:, 0:36], hT[:, k, t * 128:(t + 1) * 128], wr[l][:, k, :], start=(k == 0), stop=False))(),
                     reads=[BH[t], Bwr[l]], writes=[PB[bk]])
            S.op("pe", lambda e: e.matmul(banks[bk][:, 0:36], onesrow[:], brr[l][:], start=False, stop=True),
                 reads=[Bonesrow, Bwr[l]], writes=[PB[bk]])
            S.op("dve", lambda e: e.tensor_copy(out=lg[:, t, :], in_=banks[bk][:, 0:36]), reads=[PB[bk]], writes=[Blg])

        def finalize_a(t, mode, src=None, bsrc=None, part=0):
            xa = x[:, t, :] if src is None else src
            bxa = BX[t] if bsrc is None else bsrc
            i = fin_i[0] % 2
            fin_i[0] += 1
            st, mv, sd, rstd, nmr = st_[i], mv_[i], sd_[i], rstd_[i], nmr_[i]
            Bst, Bmv, Bsd, Brstd, Bnmr = Bst_[i], Bmv_[i], Bsd_[i], Brstd_[i], Bnmr_[i]
            if mode == "pro":
                S.op("act", lambda e: e.activation(out=xnb[i][:], in_=xa, func=AF.Copy), reads=[bxa], writes=[Bxnb[i]])
                if src is None:
                    S.op("dve", lambda e: e.scalar_tensor_tensor(out=xa, in0=xa, scalar=ALPHA, in1=A0[:],
                                                                 op0=ALU.mult, op1=ALU.add),
                         reads=[bxa, BA0], writes=[bxa])
                return i
            S.op("dve", lambda e: e.bn_stats(out=st[:, 0, :], in_=xa[:, 0:512]), reads=[bxa], writes=[Bst])
            S.op("dve", lambda e: e.bn_stats(out=st[:, 1, :], in_=xa[:, 512:1024]), reads=[bxa], writes=[Bst])
            S.op("dve", lambda e: e.bn_aggr(out=mv[:], in_=st[:]), reads=[Bst], writes=[Bmv])
            if part == 1:
                return i
            return finalize_a2(t, mode, i, src=src, bsrc=bsrc)

        def finalize_a2(t, mode, i, src=None, bsrc=None, stage=None):
            xa = x[:, t, :] if src is None else src
            bxa = BX[t] if bsrc is None else bsrc
            st, mv, sd, rstd, nmr = st_[i], mv_[i], sd_[i], rstd_[i], nmr_[i]
            Bst, Bmv, Bsd, Brstd, Bnmr = Bst_[i], Bmv_[i], Bsd_[i], Brstd_[i], Bnmr_[i]
            if stage in (None, "act"):
                finalize_a2_act(mv, sd, rstd, nmr, Bmv, Bsd, Brstd, Bnmr)
            if stage == "act":
                return i
            if mode == "mid":
                S.op("act", lambda e: e.activation(out=xnb[i][:], in_=xa, func=AF.Identity, scale=rstd[:], bias=nmr[:]),
                     reads=[bxa, Brstd, Bnmr], writes=[Bxnb[i]])
            S.op("dve", lambda e: e.tensor_scalar(out=xa, in0=xa, scalar1=rstd[:], scalar2=nmr[:],
                                                  op0=ALU.mult, op1=ALU.add),
                 reads=[bxa, Brstd, Bnmr], writes=[bxa])
            S.op("dve" if mode == "out" else "pool", lambda e: e.tensor_tensor(out=xa, in0=xa, in1=A1[:], op=ALU.mult),
                 reads=[bxa, BA1], writes=[bxa])
            S.op("pool", lambda e: e.tensor_tensor(out=xa, in0=xa, in1=A0[:], op=ALU.add),
                 reads=[bxa, BA0], writes=[bxa])
            if mode == "out":
                dma("sp", "do", out_d[t * 128:(t + 1) * 128, :], xa, reads=[bxa], writes=[Bout])
            return i

        def finalize_a2_act(mv, sd, rstd, nmr, Bmv, Bsd, Brstd, Bnmr):
            S.op("act", lambda e: e.activation(out=sd[:], in_=mv[:, 1:2], func=AF.Ln, bias=eps_t[:]),
                 reads=[Bmv, Beps], writes=[Bsd])
            S.op("act", lambda e: e.activation(out=rstd[:], in_=sd[:], func=AF.Exp, scale=-0.5), reads=[Bsd], writes=[Brstd])
            S.op("act", lambda e: e.activation(out=nmr[:], in_=mv[:, 0:1], func=AF.Copy, scale=rstd[:]),
                 reads=[Bmv, Brstd], writes=[Bnmr])
            S.op("act", lambda e: e.activation(out=nmr[:], in_=nmr[:], func=AF.Copy, scale=-1.0),
                 reads=[Bnmr], writes=[Bnmr])

        def finalize_b(t, i, haff, hdst=None, bhdst=None):
            bks = [next_bank(), next_bank()]
            for k in range(8):
                S.op("pe", (lambda k=k: lambda e: e.matmul(
                    banks[bks[k // 4]][:, (k % 4) * 128:(k % 4 + 1) * 128], xnb[i][:, k * 128:(k + 1) * 128], ident[:],
                    start=True, stop=True))(),
                     reads=[Bxnb[i], Bident], writes=[PB[bks[k // 4]]])
            hd = hT[:, :, t * 128:(t + 1) * 128] if hdst is None else hdst
            bhd = BH[t] if bhdst is None else bhdst
            for k in range(8):
                if k % 2 == 0:
                    S.op("act", (lambda k=k: lambda e: e.activation(
                        out=hd[:, k, :], in_=banks[bks[k // 4]][:, (k % 4) * 128:(k % 4 + 1) * 128], func=AF.Identity,
                        scale=H1[haff][:, k:k + 1], bias=H0[haff][:, k:k + 1]))(),
                         reads=[PB[bks[k // 4]], BHa[haff]], writes=[bhd])
                else:
                    S.op("dve", (lambda k=k: lambda e: e.tensor_scalar(
                        out=hd[:, k, :], in0=banks[bks[k // 4]][:, (k % 4) * 128:(k % 4 + 1) * 128],
                        scalar1=H1[haff][:, k:k + 1], scalar2=H0[haff][:, k:k + 1], op0=ALU.mult, op1=ALU.add))(),
                         reads=[PB[bks[k // 4]], BHa[haff]], writes=[bhd])

        def finalize(t, mode, haff=None, hdst=None, bhdst=None, route_l=None, src=None, bsrc=None):
            i = finalize_a(t, mode, src=src, bsrc=bsrc)
            if mode == "out":
                return
            finalize_b(t, i, haff, hdst=hdst, bhdst=bhdst)
            if route_l is not None:
                router_logits(route_l, t)

        def finalize_seq(mode, haff=None, route_l=None, hook=None):
            idx = {}
            if mode == "pro":
                idx[0] = finalize_a(0, mode)
                for t in range(NT):
                    if hook is not None:
                        hook(t)
                    if t + 1 < NT:
                        idx[t + 1] = finalize_a(t + 1, mode)
                    finalize_b(t, idx[t], haff)
                return
            idx[0] = finalize_a(0, mode, part=1)
            idx[1] = finalize_a(1, mode, part=1)
            finalize_a2(0, mode, idx[0])
            for t in range(NT):
                if hook is not None:
                    hook(t)
                if t + 2 < NT:
                    idx[t + 2] = finalize_a(t + 2, mode, part=1)
                if t + 1 < NT:
                    finalize_a2(t + 1, mode, idx[t + 1])
                if mode != "out":
                    finalize_b(t, idx[t], haff)
                    if route_l is not None and t >= 1:
                        router_logits(route_l, t - 1)
            if mode != "out" and route_l is not None:
                router_logits(route_l, NT - 1)

        def make_end_hook(mode, haff=None, route_l=None):
            st = {}

            def stage_b(t):
                if mode != "out":
                    finalize_b(t, st[t], haff)
                    if route_l is not None:
                        router_logits(route_l, t)

            def hook_out(t):
                st[t] = finalize_a(t, mode, part=1)
                if t >= 1:
                    finalize_a2(t - 1, mode, st[t - 1], stage="act")
                if t >= 2:
                    finalize_a2(t - 2, mode, st[t - 2], stage="rest")

            def flush_out():
                finalize_a2(NT - 1, mode, st[NT - 1], stage="act")
                finalize_a2(NT - 2, mode, st[NT - 2], stage="rest")
                finalize_a2(NT - 1, mode, st[NT - 1], stage="rest")

            if mode == "out":
                return hook_out, flush_out

            def hook(t):
                st[t] = finalize_a(t, mode, part=1)
                if t >= 1:
                    finalize_a2(t - 1, mode, st[t - 1])
                if t >= 2:
                    stage_b(t - 2)

            def flush():
                finalize_a2(NT - 1, mode, st[NT - 1])
                stage_b(NT - 2)
                stage_b(NT - 1)
            return hook, flush

        def dump_and_end():
            for t in range(NT):
                dma("sp", "do", out_d[t * 128:(t + 1) * 128, :], x[:, t, :], reads=[BX[t]], writes=[Bout])
            S.wait_all("sp", [Bout])
            S.emit()

        def moe_views(s):
            base = s * 6144
            wgu = W[:, base:base + 4096].rearrange("p (k n) -> p k n", k=8)
            wdn = W[:, base + 4096:base + 6144].rearrange("p (j n) -> p j n", j=2)
            return wgu, wdn

        def moe_load_gu(l, ex):
            s_ = ex % 4
            wgu, _ = moe_views(s_)
            dma("pool", "dw", wgu, wgu_d[l, ex].rearrange("(k p) n -> p k n", p=128), writes=[BW[s_]])

        def moe_load_dn(l, ex):
            s_ = ex % 4
            _, wdn = moe_views(s_)
            dma("pool", "dw", wdn, wdn_d[l, ex].rearrange("(j p) n -> p j n", p=128), writes=[BW[s_]])

        def moe_fold(l, ex):
            s_ = ex % 4
            _, wdn = moe_views(s_)
            S.op("pool", lambda e: e.tensor_tensor(out=wdn, in0=wdn, in1=gtbf[:].unsqueeze(1).to_broadcast([128, 2, D]),
                                                   op=ALU.mult), reads=[BW[s_], Bgtbf], writes=[BW[s_]])

        def moe_load(l, ex):
            moe_load_gu(l, ex); moe_load_dn(l, ex); moe_fold(l, ex)

        esA = ExitStack()
        cur_es[0] = esA
        with esA:
            cosT = sb("cosT", [128, 17, 32]); sinT = sb("sinT", [128, 17, 32])
            Bcos, Bsin = Buf("cos"), Buf("sin")
            hTh = sb("hTh", [128, 8, 128], BF16); BhTh = Buf("hTh")
            bqkv = sb("bqkv", [128, 1536], BF16); Bbqkv = Buf("bqkv")
            esink = sb("esink", [128, 16]); Besink = Buf("esink")
            maskb = sb("maskb", [128, 3, 512], BF16); Bmask = Buf("maskb")
            Wqkv = W[:, 0:12288].rearrange("p (k n) -> p k n", k=8)
            Wo = W[:, 12288:20480].rearrange("p (k n) -> p k n", k=8)

            es0 = ExitStack()
            cur_es[0] = es0
            with es0:
                alloc_mod_scratch()
                posi = sb("posi", [128, 17], I32); posf = sb("posf", [128, 17]); invf = sb("invf", [128, 32])
                ang = sb("ang", [128, 17, 32]); ki = sb("ki", [128, 17, 32], I32)
                kf = W[:, 22528:22528 + 1088].bitcast(F32).rearrange("p (t f) -> p t f", t=17)
                Bpos, Binvf, Bang, Bkf, Bki = Buf("pos"), Buf("invf"), Buf("ang"), Buf("kf"), Buf("ki")
                xh = W[:, 20480:22528].bitcast(F32); Bxh = Buf("xh")
                dma("sp", "dx", xh, xin[0:128, :], writes=[Bxh])
                for t in range(NT):
                    dma("sp", "dx", x[:, t, :], xin[(t + 1) * 128:(t + 2) * 128, :], writes=[BX[t]])

                make_haff(0, 0, 0, 1, None)
                compute_mod(0, 2, gtbm, Bgtbm, True)
                bc_load(A0, BA0, bo_d)
                S.op("dve", lambda e: e.tensor_tensor(out=A0[:], in0=A0[:], in1=gtbm[:], op=ALU.mult),
                     reads=[BA0, Bgtbm], writes=[BA0])
                dma("pool", "dw", Wqkv, wqkv_d.rearrange("(k p) n -> p k n", p=128), writes=[BW[0], BW[1]])
                dma("pool", "dw", Wo, wo_d.rearrange("(k p) n -> p k n", p=128), writes=[BW[2], BW[3]])
                S.op("pool", lambda e: e.memset(bqkv[:], 0.0), writes=[Bbqkv])
                dma("pool", "dw", bqkv[0:1, :], bqkv_d, writes=[Bbqkv])
                bc_load(esink, Besink, sinks_d)
                S.op("act", lambda e: e.activation(out=esink[:], in_=esink[:], func=AF.Exp), reads=[Besink], writes=[Besink])
                dma("pool", "dw", maskb[:], masks_d.rearrange("m p n -> p m n"), writes=[Bmask])
                dma("sp", "dc", posi[:], pos_d, writes=[Bpos])
                dma("sp", "dc", invf[:], invf_d, writes=[Binvf])
                S.op("dve", lambda e: e.tensor_copy(out=posf[:], in_=posi[:]), reads=[Bpos], writes=[Bpos])
                S.op("dve", lambda e: e.tensor_tensor(out=ang[:], in0=posf[:].unsqueeze(2).to_broadcast([128, 17, 32]),
                                                      in1=invf[:].unsqueeze(1).to_broadcast([128, 17, 32]), op=ALU.mult),
                     reads=[Bpos, Binvf], writes=[Bang])

                def sin_of(dst, bdst, shift):
                    if shift != 0.0:
                        S.op("dve", lambda e: e.tensor_scalar(out=dst[:], in0=ang[:], scalar1=shift, scalar2=None, op0=ALU.add),
                             reads=[Bang], writes=[bdst])
                        a_src, ba = dst, bdst
                    else:
                        a_src, ba = ang, Bang
                    S.op("dve", lambda e: e.tensor_scalar(out=ki[:], in0=a_src[:], scalar1=1.0 / TWO_PI, scalar2=None,
                                                          op0=ALU.mult), reads=[ba], writes=[Bki])
                    S.op("dve", lambda e: e.tensor_copy(out=kf, in_=ki[:]), reads=[Bki], writes=[Bkf])
                    S.op("dve", lambda e: e.scalar_tensor_tensor(out=dst[:], in0=kf, scalar=-C1, in1=a_src[:],
                                                                 op0=ALU.mult, op1=ALU.add), reads=[Bkf, ba], writes=[bdst])
                    S.op("dve", lambda e: e.scalar_tensor_tensor(out=dst[:], in0=kf, scalar=-C2, in1=dst[:],
                                                                 op0=ALU.mult, op1=ALU.add), reads=[Bkf, bdst], writes=[bdst])
                    S.op("dve", lambda e: e.tensor_scalar(out=dst[:], in0=dst[:], scalar1=3.1415925, scalar2=-3.1415925,
                                                          op0=ALU.min, op1=ALU.max), reads=[bdst], writes=[bdst])
                    S.op("act", lambda e: e.activation(out=dst[:], in_=dst[:], func=AF.Sin), reads=[bdst], writes=[bdst])

                sin_of(sinT, Bsin, 0.0)
                sin_of(cosT, Bcos, TWO_PI / 4)

                finalize(0, "pro", haff=0, hdst=hTh, bhdst=BhTh, src=xh, bsrc=Bxh)
                def mods_b():
                    vecT, BvecT = MS["vecT"], MS["BvecT"]
                    yield from compute_mod_gen(0, 4, vecT, BvecT, True, lag=3)
                    to_fm(vecT, BvecT, fmt, Bfmt)
                    yield from compute_mod_gen(0, 3, vecT, BvecT, False, lag=3)
                    to_fm(vecT, BvecT, fmt2, Bfmt2)
                    haff_combine(1, 0)
                    yield from compute_mod_gen(0, 5, gtbf, Bgtbf, True, lag=2)

                job_b = mods_b()
                finalize_seq("pro", haff=0, hook=lambda t: next(job_b, None))
                for _ in job_b:
                    pass
                S.op("dve", lambda e: e.tensor_tensor(out=Wo, in0=Wo, in1=gtbm[:].unsqueeze(1).to_broadcast([128, 8, D]),
                                                      op=ALU.mult),
                     reads=[BW[2], BW[3], Bgtbm], writes=[BW[2], BW[3]])
                if stop_after == "pro":
                    dump_and_end()
                    return nc
                make_aset(0, None, None, None)
            S.barrier()

            es1 = ExitStack()
            cur_es[0] = es1
            bank_pool[0] = [3, 6, 7]
            with es1:
                qk32 = sb("qk32", [128, 20, 64]); Bqk32 = Buf("qk32")
                rot = sb("rot", [128, 20, 64], BF16); Brot = Buf("rot")
                rot2 = rot[:].rearrange("p h d -> p (h d)")
                tA = sb("tA", [128, 20, 32]); tB = sb("tB", [128, 20, 32])
                BtA, BtB = Buf("tA"), Buf("tB")
                kpad = sb("kpad", [128, 4, 2, 128], BF16); Bkpad = Buf("kpad")
                qT = sb("qT", [128, 8, 128], BF16); BqT = Buf("qT")
                Vaug = [sb(f"Vaug{i}", [128, 4, 65], BF16) for i in range(3)]
                BV = [Buf(f"V{i}") for i in range(3)]
                ao = sb("ao", [128, 16, 64], BF16); Bao = Buf("ao")
                ao2 = ao[:].rearrange("p h d -> p (h d)")
                aoT = sb("aoT", [128, 8, 128], BF16); BaoT = Buf("aoT")
                dent = sb("dent", [128, 16]); Bdent = Buf("dent")
                kT = [W[:, 20480 + i * 1024:20480 + (i + 1) * 1024].rearrange("p (v c) -> p v c", v=8) for i in range(2)]
                BkT = [Buf(f"kT{i}") for i in range(2)]
                PT = [W[:, 22528 + i * 1024:22528 + (i + 1) * 1024].rearrange("p (b c) -> p b c", b=2) for i in range(2)]
                BPT = [Buf(f"PT{i}") for i in range(2)]
                S.op("pool", lambda e: e.memset(kpad[:], 0.0), writes=[Bkpad])
                for i in range(3):
                    S.op("pool", (lambda i=i: lambda e: e.memset(Vaug[i][:], 1.0))(), writes=[BV[i]])

                def X1(t):
                    halo = t < 0
                    tt = t + 1
                    hsrc = hTh if halo else hT[:, :, t * 128:(t + 1) * 128]
                    bh = BhTh if halo else BH[t]
                    qb = []
                    for nb in ([2] if halo else [0, 1, 2]):
                        bk = nb
                        qb.append((nb, bk))
                        for k in range(8):
                            S.op("pe", (lambda k=k, nb=nb, bk=bk: lambda e: e.matmul(
                                banks[bk][:], hsrc[:, k, :], Wqkv[:, k, nb * 512:(nb + 1) * 512], start=(k == 0), stop=False))(),
                                 reads=[bh, BW[0], BW[1]], writes=[PB[bk]])
                        S.op("pe", (lambda nb=nb, bk=bk: lambda e: e.matmul(
                            banks[bk][:], onesrow[:], bqkv[:, nb * 512:(nb + 1) * 512], start=False, stop=True))(),
                             reads=[Bonesrow, Bbqkv], writes=[PB[bk]])
                    vcur = Vaug[tt % 3]; bvcur = BV[tt % 3]
                    for nb, bk in qb:
                        if nb < 2:
                            S.op("act", (lambda nb=nb, bk=bk: lambda e: e.activation(
                                out=qk32[:, nb * 8:(nb + 1) * 8, :], in_=banks[bk][:].rearrange("p (h d) -> p h d", d=64),
                                func=AF.Copy))(), reads=[PB[bk]], writes=[Bqk32])
                        else:
                            S.op("act", (lambda bk=bk: lambda e: e.activation(
                                out=qk32[:, 16:20, :], in_=banks[bk][:, 0:256].rearrange("p (h d) -> p h d", d=64),
                                func=AF.Copy))(), reads=[PB[bk]], writes=[Bqk32])
                            S.op("act", (lambda bk=bk: lambda e: e.activation(
                                out=vcur[:, :, 0:64], in_=banks[bk][:, 256:512].rearrange("p (h d) -> p h d", d=64),
                                func=AF.Copy))(), reads=[PB[bk]], writes=[bvcur])
                    h0 = 16 if halo else 0
                    nh = 20 - h0
                    x1 = qk32[:, h0:20, 0:32]; x2 = qk32[:, h0:20, 32:64]
                    cb = cosT[:, tt, :].unsqueeze(1).to_broadcast([128, nh, 32])
                    sbc = sinT[:, tt, :].unsqueeze(1).to_broadcast([128, nh, 32])
                    S.op("dve", lambda e: e.tensor_tensor(out=tA[:, h0:20, :], in0=x1, in1=cb, op=ALU.mult),
                         reads=[Bqk32, Bcos], writes=[BtA])
                    S.op("dve", lambda e: e.tensor_tensor(out=tB[:, h0:20, :], in0=x2, in1=sbc, op=ALU.mult),
                         reads=[Bqk32, Bsin], writes=[BtB])
                    if not halo:
                        S.op("dve", lambda e: e.tensor_tensor(out=rot[:, 0:16, 0:32], in0=tA[:, 0:16, :], in1=tB[:, 0:16, :],
                                                              op=ALU.subtract), reads=[BtA, BtB], writes=[Brot])
                    for half in range(2):
                        S.op("dve", (lambda half=half: lambda e: e.tensor_tensor(
                            out=kpad[:, :, half, half * 64:half * 64 + 32], in0=tA[:, 16:20, :], in1=tB[:, 16:20, :],
                            op=ALU.subtract))(), reads=[BtA, BtB], writes=[Bkpad])
                    S.op("dve", lambda e: e.tensor_tensor(out=tA[:, h0:20, :], in0=x2, in1=cb, op=ALU.mult),
                         reads=[Bqk32, Bcos], writes=[BtA])
                    S.op("dve", lambda e: e.tensor_tensor(out=tB[:, h0:20, :], in0=x1, in1=sbc, op=ALU.mult),
                         reads=[Bqk32, Bsin], writes=[BtB])
                    if not halo:
                        S.op("dve", lambda e: e.tensor_tensor(out=rot[:, 0:16, 32:64], in0=tA[:, 0:16, :], in1=tB[:, 0:16, :],
                                                              op=ALU.add), reads=[BtA, BtB], writes=[Brot])
                    for half in range(2):
                        S.op("dve", (lambda half=half: lambda e: e.tensor_tensor(
                            out=kpad[:, :, half, half * 64 + 32:half * 64 + 64], in0=tA[:, 16:20, :], in1=tB[:, 16:20, :],
                            op=ALU.add))(), reads=[BtA, BtB], writes=[Bkpad])

                tb = [3, 6]

                def X2(t):
                    halo = t < 0
                    tt = t + 1
                    cur = tt % 2
                    for v in range(8):
                        S.op("pe", (lambda v=v: lambda e: e.matmul(
                            banks[tb[v // 4]][:, (v % 4) * 128:(v % 4 + 1) * 128], kpad[:, v // 2, v % 2, :], ident[:],
                            start=True, stop=True))(),
                             reads=[Bkpad, Bident], writes=[PB[tb[v // 4]]])
                    for hb in range(2):
                        S.op("act", (lambda hb=hb: lambda e: e.activation(
                            out=kT[cur][:, hb * 4:(hb + 1) * 4, :], in_=banks[tb[hb]][:].rearrange("p (v c) -> p v c", v=4),
                            func=AF.Copy))(), reads=[PB[tb[hb]]], writes=[BkT[cur]])
                    if halo:
                        return
                    for j in range(8):
                        S.op("pe", (lambda j=j: lambda e: e.matmul(
                            banks[tb[j // 4]][:, (j % 4) * 128:(j % 4 + 1) * 128], rot2[:, j * 128:(j + 1) * 128], ident[:],
                            start=True, stop=True))(),
                             reads=[Brot, Bident], writes=[PB[tb[j // 4]]])
                    for hb in range(2):
                        S.op("dve", (lambda hb=hb: lambda e: e.tensor_copy(
                            out=qT[:, hb * 4:(hb + 1) * 4, :], in_=banks[tb[hb]][:].rearrange("p (v c) -> p v c", v=4)))(),
                             reads=[PB[tb[hb]]], writes=[BqT])

                def Y1(t):
                    tt = t + 1
                    cur = tt % 2
                    prv = 1 - cur
                    vcur, bvcur = Vaug[tt % 3], BV[tt % 3]
                    vprv, bvprv = Vaug[(tt - 1) % 3], BV[(tt - 1) % 3]
                    ob = [0, 1, 2]
                    sc_i = [0]
                    first_tile = (t == 0)

                    def scores(g):
                        pi = g % 2
                        for blk, (ktile, bkt, mi) in enumerate([(kT[prv], BkT[prv], 2 if first_tile else 1),
                                                                (kT[cur], BkT[cur], 0)]):
                            bk = 4 + (sc_i[0] % 2)
                            sc_i[0] += 1
                            S.op("pe", (lambda bk=bk, mi=mi: lambda e: e.matmul(
                                banks[bk][:], ident[:], maskb[:, mi, :], start=True, stop=False))(),
                                 reads=[Bident, Bmask], writes=[PB[bk]])
                            for i in range(4):
                                h = 4 * g + i
                                S.op("pe", (lambda bk=bk, i=i, h=h, ktile=ktile, g=g: lambda e: e.matmul(
                                    banks[bk][:, i * 128:(i + 1) * 128], ktile[:, g * 2 + (h % 2), :], qT[:, h // 2, :],
                                    start=False, stop=(i == 3)))(),
                                     reads=[bkt, BqT], writes=[PB[bk]])
                            S.op("act", (lambda bk=bk, blk=blk, pi=pi: lambda e: e.activation(
                                out=PT[pi][:, blk, :], in_=banks[bk][:], func=AF.Exp, scale=0.125))(),
                                 reads=[PB[bk]], writes=[BPT[pi]])

                    def pv(g):
                        pi = g % 2
                        for i in range(4):
                            h = 4 * g + i
                            obk = ob[h // 7]
                            oc = (h % 7) * 65
                            for blk, (vt, bvt) in enumerate([(vprv, bvprv), (vcur, bvcur)]):
                                S.op("pe", (lambda obk=obk, oc=oc, blk=blk, pi=pi, i=i, vt=vt, g=g: lambda e: e.matmul(
                                    banks[obk][:, oc:oc + 65], PT[pi][:, blk, i * 128:(i + 1) * 128], vt[:, g, :],
                                    start=(blk == 0), stop=(blk == 1)))(),
                                     reads=[BPT[pi], bvt], writes=[PB[obk]])

                    scores(0)
                    for g in range(4):
                        if g + 1 < 4:
                            scores(g + 1)
                        pv(g)
                    for b3 in range(3):
                        hs = 7 * b3
                        n = min(7, 16 - hs)
                        ov = banks[ob[b3]][:, 0:n * 65].rearrange("p (h d) -> p h d", d=65)
                        S.op("dve", (lambda ov=ov, hs=hs, n=n: lambda e: e.tensor_tensor(
                            out=dent[:, hs:hs + n], in0=ov[:, :, 64], in1=esink[:, hs:hs + n], op=ALU.add))(),
                             reads=[PB[ob[b3]], Besink], writes=[Bdent])
                        S.op("dve", (lambda hs=hs, n=n: lambda e: e.reciprocal(out=dent[:, hs:hs + n], in_=dent[:, hs:hs + n]))(),
                             reads=[Bdent], writes=[Bdent])
                        S.op("dve", (lambda ov=ov, hs=hs, n=n: lambda e: e.tensor_tensor(
                            out=ao[:, hs:hs + n, :], in0=ov[:, :, 0:64],
                            in1=dent[:, hs:hs + n].unsqueeze(2).to_broadcast([128, n, 64]), op=ALU.mult))(),
                             reads=[PB[ob[b3]], Bdent], writes=[Bao])

                def Y2a(t):
                    for j in range(8):
                        S.op("pe", (lambda j=j: lambda e: e.matmul(
                            banks[tb[j // 4]][:, (j % 4) * 128:(j % 4 + 1) * 128], ao2[:, j * 128:(j + 1) * 128], ident[:],
                            start=True, stop=True))(),
                             reads=[Bao, Bident], writes=[PB[tb[j // 4]]])
                    for hb in range(2):
                        S.op("act", (lambda hb=hb: lambda e: e.activation(
                            out=aoT[:, hb * 4:(hb + 1) * 4, :], in_=banks[tb[hb]][:].rearrange("p (v c) -> p v c", v=4),
                            func=AF.Copy))(), reads=[PB[tb[hb]]], writes=[BaoT])

                def Y2b(t):
                    for nb in range(2):
                        bk = 4 + nb
                        for k in range(8):
                            S.op("pe", (lambda k=k, nb=nb, bk=bk: lambda e: e.matmul(
                                banks[bk][:], aoT[:, k, :], Wo[:, k, nb * 512:(nb + 1) * 512], start=(k == 0), stop=(k == 7)))(),
                                 reads=[BaoT, BW[2], BW[3]], writes=[PB[bk]])
                        S.op("dve", (lambda nb=nb, bk=bk: lambda e: e.tensor_tensor(
                            out=x[:, t, nb * 512:(nb + 1) * 512], in0=x[:, t, nb * 512:(nb + 1) * 512], in1=banks[bk][:],
                            op=ALU.add))(), reads=[BX[t], PB[bk]], writes=[BX[t]])

                X1(-1); X2(-1)
                X1(0); X2(0)
                fi = {}
                for t in range(NT + 3):
                    if 0 <= t - 2 < NT:
                        fi[t - 2] = finalize_a(t - 2, "mid")
                    if t + 1 < NT:
                        X1(t + 1)
                    if t == NT - 1:
                        moe_load_gu(0, 0); moe_load_gu(0, 1)
                    if t == NT:
                        moe_load_dn(0, 0); moe_load_dn(0, 1)
                    if t == NT + 2:
                        moe_fold(0, 0); moe_fold(0, 1)
                    if 0 <= t - 1 < NT:
                        Y2a(t - 1)
                    if t < NT:
                        Y1(t)
                    if 0 <= t - 1 < NT:
                        Y2b(t - 1)
                    if 0 <= t - 2 < NT:
                        finalize_b(t - 2, fi[t - 2], 1)
                    if t + 1 < NT:
                        X2(t + 1)
                    if 0 <= t - 3 < NT:
                        router_logits(0, t - 3)
                if stop_after == "attn":
                    dump_and_end()
                    return nc
            S.barrier()
        cur_es[0] = es

        def bc3(ap2, n):
            return ap2.unsqueeze(2).to_broadcast([128, NT, n])

        def moe_phase(l, after_chunk=None, nchunks=16, tile_hook=None, first_load=0, end_hook=None):
            gl_m = sb("gl_m", [128, NT]); Bglm = Buf("gl_m")
            goh = sb("goh", [128, NT, 4]); Bgoh = Buf("goh")
            gex = sb("gex", [128, NT, 4]); Bgex = Buf("gex")
            gp = sb("gp", [128, NT]); Bgp = Buf("gp")
            sel4 = sb("sel4", [128, NT, 32]); Bsel4 = Buf("sel4")
            sel = sb("sel", [128, NT, 8]); Bsel = Buf("sel")
            sel2 = sb("sel2", [128, NT, 8]); Bsel2 = Buf("sel2")
            oh1 = sb("oh1", [128, NT, 8]); Boh1 = Buf("oh1")
            oh2 = sb("oh2", [128, NT, 8]); Boh2 = Buf("oh2")
            m1 = sb("m1", [128, NT]); m2 = sb("m2", [128, NT]); Bm1, Bm2 = Buf("m1"), Buf("m2")
            w1 = sb("w1", [128, NT]); w2 = sb("w2", [128, NT]); Bw1, Bw2 = Buf("w1"), Buf("w2")
            sg = [sb(f"sg{i}", [128, 256]) for i in range(2)]
            Bsg = [Buf(f"sg{i}") for i in range(2)]
            actb = [sb(f"actb{i}", [128, 256], BF16) for i in range(2)]
            Bact = [Buf(f"act{i}") for i in range(2)]
            actT = [sb(f"actT{i}", [128, 2, 128], BF16) for i in range(3)]
            BactT = [Buf(f"actT{i}") for i in range(3)]

            for ex in range(first_load, 4):
                moe_load(l, ex)

            gl = lg[:, :, 0:4]
            S.op("dve", lambda e: e.tensor_reduce(out=gl_m[:], in_=gl, axis=AX.X, op=ALU.max), reads=[Blg], writes=[Bglm])
            S.op("dve", lambda e: e.tensor_tensor(out=goh[:], in0=gl, in1=bc3(gl_m[:], 4), op=ALU.is_equal),
                 reads=[Blg, Bglm], writes=[Bgoh])
            S.op("dve", lambda e: e.tensor_tensor(out=gex[:], in0=gl, in1=bc3(gl_m[:], 4), op=ALU.subtract),
                 reads=[Blg, Bglm], writes=[Bgex])
            S.op("act", lambda e: e.activation(out=gex[:], in_=gex[:], func=AF.Exp), reads=[Bgex], writes=[Bgex])
            S.op("dve", lambda e: e.tensor_reduce(out=gp[:], in_=gex[:], axis=AX.X, op=ALU.add), reads=[Bgex], writes=[Bgp])
            S.op("dve", lambda e: e.reciprocal(out=gp[:], in_=gp[:]), reads=[Bgp], writes=[Bgp])
            S.op("dve", lambda e: e.tensor_tensor(
                out=sel4[:].rearrange("p t (g e) -> p t g e", g=4), in0=lg[:, :, 4:36].rearrange("p t (g e) -> p t g e", g=4),
                in1=goh[:].unsqueeze(3).to_broadcast([128, NT, 4, 8]), op=ALU.mult),
                 reads=[Blg, Bgoh], writes=[Bsel4])
            S.op("dve", lambda e: e.tensor_reduce(out=sel[:], in_=sel4[:].rearrange("p t (g e) -> p t e g", g=4),
                                                  axis=AX.X, op=ALU.add), reads=[Bsel4], writes=[Bsel])
            S.op("dve", lambda e: e.tensor_reduce(out=m1[:], in_=sel[:], axis=AX.X, op=ALU.max), reads=[Bsel], writes=[Bm1])
            S.op("dve", lambda e: e.tensor_tensor(out=oh1[:], in0=sel[:], in1=bc3(m1[:], 8), op=ALU.is_equal),
                 reads=[Bsel, Bm1], writes=[Boh1])
            S.op("dve", lambda e: e.scalar_tensor_tensor(out=sel2[:], in0=oh1[:], scalar=-1e30, in1=sel[:],
                                                         op0=ALU.mult, op1=ALU.add), reads=[Boh1, Bsel], writes=[Bsel2])
            S.op("dve", lambda e: e.tensor_reduce(out=m2[:], in_=sel2[:], axis=AX.X, op=ALU.max), reads=[Bsel2], writes=[Bm2])
            S.op("dve", lambda e: e.tensor_tensor(out=oh2[:], in0=sel2[:], in1=bc3(m2[:], 8), op=ALU.is_equal),
                 reads=[Bsel2, Bm2], writes=[Boh2])
            S.op("dve", lambda e: e.tensor_tensor(out=w2[:], in0=m2[:], in1=m1[:], op=ALU.subtract),
                 reads=[Bm1, Bm2], writes=[Bw2])
            S.op("act", lambda e: e.activation(out=w2[:], in_=w2[:], func=AF.Exp), reads=[Bw2], writes=[Bw2])
            S.op("dve", lambda e: e.tensor_scalar(out=w2[:], in0=w2[:], scalar1=1.0, scalar2=None, op0=ALU.add),
                 reads=[Bw2], writes=[Bw2])
            S.op("dve", lambda e: e.reciprocal(out=w1[:], in_=w2[:]), reads=[Bw2], writes=[Bw1])
            S.op("dve", lambda e: e.tensor_tensor(out=w1[:], in0=w1[:], in1=gp[:], op=ALU.mult), reads=[Bw1, Bgp], writes=[Bw1])
            S.op("dve", lambda e: e.tensor_tensor(out=w2[:], in0=gp[:], in1=w1[:], op=ALU.subtract),
                 reads=[Bgp, Bw1], writes=[Bw2])
            S.op("dve", lambda e: e.tensor_tensor(out=oh1[:], in0=oh1[:], in1=bc3(w1[:], 8), op=ALU.mult),
                 reads=[Boh1, Bw1], writes=[Boh1])
            S.op("dve", lambda e: e.tensor_tensor(out=oh2[:], in0=oh2[:], in1=bc3(w2[:], 8), op=ALU.mult),
                 reads=[Boh2, Bw2], writes=[Boh2])
            S.op("dve", lambda e: e.tensor_tensor(out=oh1[:], in0=oh1[:], in1=oh2[:], op=ALU.add),
                 reads=[Boh1, Boh2], writes=[Boh1])
            S.op("dve", lambda e: e.tensor_tensor(
                out=comb[:].rearrange("p t (g e) -> p t g e", g=4),
                in0=goh[:].unsqueeze(3).to_broadcast([128, NT, 4, 8]),
                in1=oh1[:].unsqueeze(2).to_broadcast([128, NT, 4, 8]), op=ALU.mult),
                 reads=[Bgoh, Boh1], writes=[Bcomb])

            GUB = [0, 1]; TB = [2, 3]; YB = [[4, 5], [6, 7]]
            steps = []
            for c in range(nchunks):
                for t in range(NT):
                    for e2 in range(2):
                        steps.append((c, t, e2))
            n = len(steps)

            def GU(i):
                c, t, e2 = steps[i]
                s = (2 * c + e2) % 4
                wgu, _ = moe_views(s)
                bk = GUB[i % 2]
                for k in range(8):
                    S.op("pe", (lambda k=k: lambda e: e.matmul(
                        banks[bk][:], hT[:, k, t * 128:(t + 1) * 128], wgu[:, k, :], start=(k == 0), stop=(k == 7)))(),
                         reads=[BH[t], BW[s]], writes=[PB[bk]])
                si = i % 2
                S.op("act", lambda e: e.activation(out=sg[si][:], in_=banks[bk][:, 0:256], func=AF.Silu),
                     reads=[PB[bk]], writes=[Bsg[si]])
                ex = 2 * c + e2
                S.op("dve", lambda e: e.scalar_tensor_tensor(
                    out=actb[si][:], in0=sg[si][:], scalar=comb[:, t, ex:ex + 1], in1=banks[bk][:, 256:512],
                    op0=ALU.mult, op1=ALU.mult), reads=[Bsg[si], Bcomb, PB[bk]], writes=[Bact[si]])

            def TR(i):
                si = i % 2
                bk = TB[i % 2]
                ti = i % 3
                for j in range(2):
                    S.op("pe", (lambda j=j: lambda e: e.matmul(
                        banks[bk][:, j * 128:(j + 1) * 128], actb[si][:, j * 128:(j + 1) * 128], ident[:],
                        start=True, stop=True))(),
                         reads=[Bact[si], Bident], writes=[PB[bk]])
                S.op("act", lambda e: e.activation(out=actT[ti][:], in_=banks[bk][:, 0:256].rearrange("p (j c) -> p j c", j=2),
                                                   func=AF.Copy), reads=[PB[bk]], writes=[BactT[ti]])

            def DN(i):
                c, t, e2 = steps[i]
                s = (2 * c + e2) % 4
                _, wdn = moe_views(s)
                ti = i % 3
                yb = YB[t % 2]
                for nb in range(2):
                    for j in range(2):
                        S.op("pe", (lambda nb=nb, j=j: lambda e: e.matmul(
                            banks[yb[nb]][:], actT[ti][:, j, :], wdn[:, j, nb * 512:(nb + 1) * 512],
                            start=(e2 == 0 and j == 0), stop=(e2 == 1 and j == 1)))(),
                             reads=[BactT[ti], BW[s]], writes=[PB[yb[nb]]])
                if e2 == 1:
                    for nb in range(2):
                        S.op("dve", (lambda nb=nb: lambda e: e.tensor_tensor(
                            out=x[:, t, nb * 512:(nb + 1) * 512], in0=x[:, t, nb * 512:(nb + 1) * 512],
                            in1=banks[yb[nb]][:], op=ALU.add))(), reads=[BX[t], PB[yb[nb]]], writes=[BX[t]])
                    if tile_hook is not None:
                        tile_hook(c, t)
                    if end_hook is not None and c == nchunks - 1:
                        end_hook(t)
                    if t == NT - 1:
                        if c + 2 < 16 and c + 2 < nchunks:
                            moe_load(l, 2 * (c + 2)); moe_load(l, 2 * (c + 2) + 1)
                        if after_chunk is not None:
                            after_chunk(c)

            for i in range(n + 2):
                if i < n:
                    GU(i)
                if 1 <= i <= n:
                    TR(i - 1)
                if 2 <= i <= n + 1:
                    DN(i - 2)

        def mod_jobs_moe0():
            vecT, BvecT = MS["vecT"], MS["BvecT"]
            yield from compute_mod_gen(1, 1, vecT, BvecT, True, lag=12)
            to_fm(vecT, BvecT, fmt, Bfmt)
            yield from compute_mod_gen(1, 0, vecT, BvecT, False, lag=12)
            to_fm(vecT, BvecT, fmt2, Bfmt2)
            haff_combine(2, 1)
            yield from compute_mod_gen(1, 2, gtbm, Bgtbm, True, lag=12)
            make_aset(1, bout_d, gtbm, Bgtbm)
            yield from compute_mod_gen(1, 4, vecT, BvecT, True, lag=12)
            to_fm(vecT, BvecT, fmt, Bfmt)
            yield from compute_mod_gen(1, 3, vecT, BvecT, False, lag=12)
            to_fm(vecT, BvecT, fmt2, Bfmt2)
            haff_combine(3, 2)
            yield from compute_mod_gen(1, 5, MS["gtmp"], MS["Bgtmp"], True, lag=12)

        moe0_job = [None]

        def tile_hook_moe0(c, t):
            if c < 1:
                return
            if moe0_job[0] is None:
                moe0_job[0] = mod_jobs_moe0()
            next(moe0_job[0], None)

        es2 = ExitStack()
        cur_es[0] = es2
        bank_pool[0] = [0, 1, 2, 3]
        with es2:
            alloc_mod_scratch()
            MS["gtmp"] = sb("gtmp", [128, D]); MS["Bgtmp"] = Buf("gtmp")
            eh0, ef0 = make_end_hook("mid", haff=2)
            moe_phase(0, None, nchunks=(NCHUNK_DBG or 16), tile_hook=tile_hook_moe0, end_hook=(eh0 if USE_END_HOOK else None), first_load=2)
            for _ in (moe0_job[0] or ()):
                pass
            if USE_END_HOOK:
                ef0()
            else:
                finalize_seq("mid", haff=2)
            dma("pool", "dw", W[:, 4096:8192].rearrange("p (k n) -> p k n", k=8),
                win_d[:, 2048:2560].rearrange("(k p) n -> p k n", p=128), writes=[BW[0], BW[1]])
            if stop_after == "moe0":
                dump_and_end()
                return nc
            S.op("pool", lambda e: e.tensor_copy(out=gtbf[:], in_=MS["gtmp"][:]), reads=[MS["Bgtmp"]], writes=[Bgtbf])
            make_aset(2, None, None, None)
        S.barrier()
        cur_es[0] = es

        es3 = ExitStack()
        cur_es[0] = es3
        bank_pool[0] = list(range(8))
        with es3:
            gst = sb("gst", [128, NT, 4, 6]); Bgst = Buf("gst")
            mvg = sb("mvg", [128, NT, 2]); Bmvg = Buf("mvg")
            sdg = sb("sdg", [128, NT]); rstdg = sb("rstdg", [128, NT]); nmrg = sb("nmrg", [128, NT])
            Bsdg, Brstdg, Bnmrg = Buf("sdg"), Buf("rstdg"), Buf("nmrg")
            u32 = [sb(f"u32_{i}", [128, 512]) for i in range(2)]; Bu32 = [Buf(f"u32_{i}") for i in range(2)]
            v32 = [sb(f"v32_{i}", [128, 512]) for i in range(2)]; Bv32 = [Buf(f"v32_{i}") for i in range(2)]
            vln = [sb(f"vln{i}", [128, 512], BF16) for i in range(2)]; Bvln = [Buf(f"vln{i}") for i in range(2)]
            gated = [sb(f"gated{i}", [128, 512], BF16) for i in range(2)]; Bgated = [Buf(f"gated{i}") for i in range(2)]
            gatedT = [sb(f"gatedT{i}", [128, 4, 128], BF16) for i in range(3)]; BgatedT = [Buf(f"gatedT{i}") for i in range(3)]
            lngb = [sb(f"lngb{i}", [128, 2, 512]) for i in range(2)]; Blngb = [Buf(f"lngb{i}") for i in range(2)]
            binr = [sb(f"binr{i}", [128, 2, 512], BF16) for i in range(2)]; Bbinr = [Buf(f"binr{i}") for i in range(2)]
            for i in range(2):
                S.op("pool", (lambda i=i: lambda e: e.memset(binr[i][:], 0.0))(), writes=[Bbinr[i]])
            wsTm = sb("wsTm", [128, 8, 128], BF16); BwsT = Buf("wsTm")
            trilb = sb("trilb", [128, 128], BF16); Btril = Buf("tril")
            bsT = sb("bsT", [128, 8]); BbsT = Buf("bsT")
            dma("pool", "dw", wsTm[:], wsT_d.rearrange("g s t -> s g t"), writes=[BwsT])
            dma("pool", "dw", trilb[:], tril_d, writes=[Btril])
            dma("sp", "dc", bsT[:], bsT_d, writes=[BbsT])
            S.op("dve", lambda e: e.tensor_tensor(out=wsTm[:], in0=wsTm[:], in1=trilb[:].unsqueeze(1).to_broadcast([128, 8, 128]),
                                                  op=ALU.mult), reads=[BwsT, Btril], writes=[BwsT])

            def gviews(b):
                base = b * 12288
                wu = W[:, base:base + 4096].rearrange("p (k n) -> p k n", k=8)
                wv = W[:, base + 4096:base + 8192].rearrange("p (k n) -> p k n", k=8)
                wo2 = W[:, base + 8192:base + 12288].rearrange("p (j n) -> p j n", j=4)
                return wu, wv, wo2

            def gfold(b):
                _, _, wo2 = gviews(b)
                bw = [BW[2 * b], BW[2 * b + 1]]
                S.op("pool", lambda e: e.tensor_tensor(out=wo2, in0=wo2,
                                                       in1=gtbm[:].unsqueeze(1).to_broadcast([128, 4, D]), op=ALU.mult),
                     reads=bw + [Bgtbm], writes=bw)

            def gload(cb, b, main, skip_wv=False, fold_now=True, part=None):
                wu, wv, wo2 = gviews(b)
                bw = [BW[2 * b], BW[2 * b + 1]]
                if part in (None, 0):
                    if not skip_wv:
                        dma("pool", "dw", wv,
                            win_d[:, 2048 + cb * 512:2048 + (cb + 1) * 512].rearrange("(k p) n -> p k n", p=128), writes=bw)
                    dma("pool", "dw", binr[b][0:1, 1, :], bin_d[:, 2048 + cb * 512:2048 + (cb + 1) * 512], writes=[Bbinr[b]])
                    if main:
                        dma("pool", "dw", binr[b][0:1, 0, :], bin_d[:, cb * 512:(cb + 1) * 512], writes=[Bbinr[b]])
                if main and part in (None, 1):
                    dma("pool", "dw", wu, win_d[:, cb * 512:(cb + 1) * 512].rearrange("(k p) n -> p k n", p=128), writes=bw)
                if main and part in (None, 2):
                    dma("pool", "dw", wo2, wout_d[cb * 512:(cb + 1) * 512, :].rearrange("(j p) n -> p j n", p=128), writes=bw)
                    if fold_now:
                        gfold(b)
                if main and part in (None, 0):
                    pass
                    dma("sp", "dc", lngb[b][:, 0, :], glng_d[:, cb * 512:(cb + 1) * 512].to_broadcast([128, 512]),
                        writes=[Blngb[b]])
                    dma("sp", "dc", lngb[b][:, 1, :], glnb_d[:, cb * 512:(cb + 1) * 512].to_broadcast([128, 512]),
                        writes=[Blngb[b]])

            gload(0, 0, False, skip_wv=True)
            pi = 0
            for cb in range(4):
                b = cb % 2
                if cb + 1 < 4:
                    gload(cb + 1, (cb + 1) % 2, False)
                _, wv, _ = gviews(b)
                bw = [BW[2 * b], BW[2 * b + 1]]
                for t in range(NT):
                    bk = next_bank()
                    i2 = pi % 2
                    pi += 1
                    for k in range(8):
                        S.op("pe", (lambda k=k, bk=bk, t=t, wv=wv: lambda e: e.matmul(
                            banks[bk][:], hT[:, k, t * 128:(t + 1) * 128], wv[:, k, :], start=(k == 0), stop=False))(),
                             reads=[BH[t]] + bw, writes=[PB[bk]])
                    S.op("pe", (lambda bk=bk, b=b: lambda e: e.matmul(banks[bk][:], onesrow[:], binr[b][:, 1, :],
                                                                      start=False, stop=True))(),
                         reads=[Bonesrow, Bbinr[b]], writes=[PB[bk]])
                    S.op("act", (lambda bk=bk, i2=i2: lambda e: e.activation(out=v32[i2][:], in_=banks[bk][:], func=AF.Gelu))(),
                         reads=[PB[bk]], writes=[Bv32[i2]])
                    S.op("dve", (lambda i2=i2, t=t, cb=cb: lambda e: e.bn_stats(out=gst[:, t, cb, :], in_=v32[i2][:]))(),
                         reads=[Bv32[i2]], writes=[Bgst])
            for t in range(NT):
                S.op("dve", (lambda t=t: lambda e: e.bn_aggr(out=mvg[:, t, :], in_=gst[:, t, :, :]))(),
                     reads=[Bgst], writes=[Bmvg])
            S.op("act", lambda e: e.activation(out=sdg[:], in_=mvg[:, :, 1], func=AF.Ln, bias=eps_t[:]),
                 reads=[Bmvg, Beps], writes=[Bsdg])
            S.op("act", lambda e: e.activation(out=rstdg[:], in_=sdg[:], func=AF.Exp, scale=-0.5),
                 reads=[Bsdg], writes=[Brstdg])
            S.op("dve", lambda e: e.scalar_tensor_tensor(out=nmrg[:], in0=mvg[:, :, 0], scalar=-1.0, in1=rstdg[:],
                                                         op0=ALU.mult, op1=ALU.mult), reads=[Bmvg, Brstdg], writes=[Bnmrg])

            gsteps = [(cb, t) for cb in range(4) for t in range(NT)]
            ng = len(gsteps)

            def gA(i):
                cb, t = gsteps[i]
                b = cb % 2
                if cb + 1 < 4:
                    if t == 3:
                        gload(cb + 1, (cb + 1) % 2, True, fold_now=False, part=0)
                    if t == 6:
                        gload(cb + 1, (cb + 1) % 2, True, fold_now=False, part=1)
                    if t == 9:
                        gload(cb + 1, (cb + 1) % 2, True, fold_now=False, part=2)
                    if t == 13:
                        gfold((cb + 1) % 2)
                wu, wv, _ = gviews(b)
                bw = [BW[2 * b], BW[2 * b + 1]]
                i2 = i % 2
                for which, wmat, dst, bdst in ((0, wu, u32[i2], Bu32[i2]), (1, wv, v32[i2], Bv32[i2])):
                    bk = next_bank()
                    for k in range(8):
                        S.op("pe", (lambda k=k, bk=bk, wmat=wmat: lambda e: e.matmul(
                            banks[bk][:], hT[:, k, t * 128:(t + 1) * 128], wmat[:, k, :], start=(k == 0), stop=False))(),
                             reads=[BH[t]] + bw, writes=[PB[bk]])
                    S.op("pe", (lambda bk=bk, which=which: lambda e: e.matmul(banks[bk][:], onesrow[:], binr[b][:, which, :],
                                                                              start=False, stop=True))(),
                         reads=[Bonesrow, Bbinr[b]], writes=[PB[bk]])
                    S.op("act", (lambda bk=bk, dst=dst: lambda e: e.activation(out=dst[:], in_=banks[bk][:], func=AF.Gelu))(),
                         reads=[PB[bk]], writes=[bdst])
                S.op("dve", lambda e: e.tensor_scalar(out=v32[i2][:], in0=v32[i2][:], scalar1=rstdg[:, t:t + 1],
                                                      scalar2=nmrg[:, t:t + 1], op0=ALU.mult, op1=ALU.add),
                     reads=[Bv32[i2], Brstdg, Bnmrg], writes=[Bv32[i2]])
                S.op("pool", lambda e: e.tensor_tensor(out=v32[i2][:], in0=v32[i2][:], in1=lngb[b][:, 0, :], op=ALU.mult),
                     reads=[Bv32[i2], Blngb[b]], writes=[Bv32[i2]])
                S.op("pool", lambda e: e.tensor_tensor(out=vln[i2][:], in0=v32[i2][:], in1=lngb[b][:, 1, :], op=ALU.add),
                     reads=[Bv32[i2], Blngb[b]], writes=[Bvln[i2]])

            def gB(i):
                cb, t = gsteps[i]
                i2 = i % 2
                bk = next_bank()
                for gi in range(2):
                    g = 2 * cb + gi
                    S.op("pe", (lambda gi=gi, g=g: lambda e: e.matmul(
                        banks[bk][:, gi * 256:(gi + 1) * 256], wsTm[:, g, :], vln[i2][:, gi * 256:(gi + 1) * 256],
                        start=True, stop=True))(), reads=[BwsT, Bvln[i2]], writes=[PB[bk]])
                for gi in range(2):
                    g = 2 * cb + gi
                    S.op("dve", (lambda gi=gi, g=g: lambda e: e.scalar_tensor_tensor(
                        out=gated[i2][:, gi * 256:(gi + 1) * 256], in0=banks[bk][:, gi * 256:(gi + 1) * 256],
                        scalar=bsT[:, g:g + 1], in1=u32[i2][:, gi * 256:(gi + 1) * 256], op0=ALU.add, op1=ALU.mult))(),
                         reads=[PB[bk], BbsT, Bu32[i2]], writes=[Bgated[i2]])

            def gC(i):
                i2 = i % 2
                bt = next_bank()
                i3 = i % 3
                for j in range(4):
                    S.op("pe", (lambda j=j: lambda e: e.matmul(
                        banks[bt][:, j * 128:(j + 1) * 128], gated[i2][:, j * 128:(j + 1) * 128], ident[:],
                        start=True, stop=True))(),
                         reads=[Bgated[i2], Bident], writes=[PB[bt]])
                S.op("act", lambda e: e.activation(out=gatedT[i3][:], in_=banks[bt][:].rearrange("p (j c) -> p j c", j=4),
                                                   func=AF.Copy), reads=[PB[bt]], writes=[BgatedT[i3]])

            def gD(i):
                cb, t = gsteps[i]
                b = cb % 2
                _, _, wo2 = gviews(b)
                bw = [BW[2 * b], BW[2 * b + 1]]
                i3 = i % 3
                for nb in range(2):
                    bk = next_bank()
                    for j in range(4):
                        S.op("pe", (lambda j=j, nb=nb, bk=bk: lambda e: e.matmul(
                            banks[bk][:], gatedT[i3][:, j, :], wo2[:, j, nb * 512:(nb + 1) * 512],
                            start=(j == 0), stop=(j == 3)))(), reads=[BgatedT[i3]] + bw, writes=[PB[bk]])
                    S.op("dve", (lambda nb=nb, bk=bk: lambda e: e.tensor_tensor(
                        out=x[:, t, nb * 512:(nb + 1) * 512], in0=x[:, t, nb * 512:(nb + 1) * 512], in1=banks[bk][:],
                        op=ALU.add))(), reads=[BX[t], PB[bk]], writes=[BX[t]])
                if cb == 3:
                    if t == 3:
                        moe_load_gu(1, 0); moe_load_gu(1, 1)
                    if t == 8:
                        moe_load_dn(1, 0); moe_load_dn(1, 1)
                    if t == 12:
                        moe_fold(1, 0); moe_fold(1, 1)
                    if USE_END_HOOK:
                        ehg(t)

            ehg, efg = make_end_hook("mid", haff=3, route_l=1)
            gload(0, 0, True)
            for i in range(ng + 3):
                if i < ng:
                    gA(i)
                if 1 <= i <= ng:
                    gB(i - 1)
                if 2 <= i <= ng + 1:
                    gC(i - 2)
                if 3 <= i <= ng + 2:
                    gD(i - 3)
            if USE_END_HOOK:
                efg()
            else:
                finalize_seq("mid", haff=3, route_l=1)
            if stop_after == "gmlp":
                dump_and_end()
                return nc
        S.barrier()
        cur_es[0] = es

        es4 = ExitStack()
        cur_es[0] = es4
        bank_pool[0] = [0, 1, 2, 3]
        with es4:
            make_aset(3, None, None, None, scale=1.0)
            eh1, ef1 = make_end_hook("out")
            moe_phase(1, None, nchunks=(NCHUNK_DBG or 16), first_load=2, end_hook=(eh1 if USE_END_HOOK_OUT else None))
            if USE_END_HOOK_OUT:
                ef1()
            else:
                finalize_seq("out")
            S.wait_all("sp", [Bout])
            S.emit()
    return nc


NCHUNK_DBG = None
USE_END_HOOK = False
USE_END_HOOK_OUT = True
ATT_DBG = [NT, None]


_CACHE = {}


def _host_inputs(inputs):
    f32 = np.float32
    x = np.asarray(inputs["x"], f32)
    c = np.asarray(inputs["c"], f32)
    pos = np.asarray(inputs["positions"], np.int32)
    shared = {}
    shared["ident"] = np.eye(128, dtype=f32)
    s = np.arange(128)[:, None]
    q = np.arange(128)[None, :]
    m_cur = np.where(s <= q, 0.0, NEG).astype(f32)
    m_prev = np.where(s > q, 0.0, NEG).astype(f32)
    m_none = np.full((128, 128), NEG, f32)
    inv_freq = (10000.0 ** (-np.arange(0, 64, 2, dtype=f32) / f32(64))).astype(f32)
    shared["invf"] = np.ascontiguousarray(np.broadcast_to(inv_freq[None, :], (128, 32))).astype(f32)
    shared["tril"] = (s <= q).astype(f32)
    shared["ada_w"] = np.ascontiguousarray(inputs["ada_w"], f32)
    shared["ada_b"] = np.ascontiguousarray(inputs["ada_b"], f32)
    g = np.asarray(inputs["post_ln_g"], f32).reshape(4, D)
    b = np.asarray(inputs["post_ln_b"], f32).reshape(4, D)
    shared["post_ln_g"] = np.ascontiguousarray(g)
    shared["post_ln_b"] = np.ascontiguousarray(b)
    shared["post_ln_gT"] = np.ascontiguousarray(g.reshape(4, 8, 128).transpose(0, 2, 1))
    shared["post_ln_bT"] = np.ascontiguousarray(b.reshape(4, 8, 128).transpose(0, 2, 1))
    shared["attn_w_qkv"] = np.ascontiguousarray(inputs["attn_w_qkv"][0], f32)
    shared["attn_b_qkv"] = np.ascontiguousarray(inputs["attn_b_qkv"], f32).reshape(1, 1536)
    shared["attn_sinks"] = np.ascontiguousarray(inputs["attn_sinks"], f32).reshape(1, 16)
    shared["attn_w_o"] = np.ascontiguousarray(inputs["attn_w_o"][0], f32)
    shared["attn_b_o"] = np.ascontiguousarray(inputs["attn_b_o"], f32).reshape(1, D)
    shared["gmlp_w_in"] = np.ascontiguousarray(inputs["gmlp_w_in"][0], f32)
    shared["gmlp_b_in"] = np.ascontiguousarray(inputs["gmlp_b_in"], f32).reshape(1, 4096)
    shared["gmlp_ln_g"] = np.ascontiguousarray(inputs["gmlp_sgu_ln_g"], f32).reshape(1, 2048)
    shared["gmlp_ln_b"] = np.ascontiguousarray(inputs["gmlp_sgu_ln_b"], f32).reshape(1, 2048)
    shared["gmlp_w_sT"] = np.ascontiguousarray(np.asarray(inputs["gmlp_w_s"][0], f32).transpose(0, 2, 1))
    shared["gmlp_b_sT"] = np.ascontiguousarray(np.asarray(inputs["gmlp_b_s"][0], f32).T)
    shared["gmlp_w_out"] = np.ascontiguousarray(inputs["gmlp_w_out"][0], f32)
    shared["gmlp_b_out"] = np.ascontiguousarray(inputs["gmlp_b_out"], f32).reshape(1, D)
    shared["moe_wr"] = np.ascontiguousarray(np.concatenate(
        [np.asarray(inputs["moe_w_group_router"], f32), np.asarray(inputs["moe_w_expert_router"], f32)], axis=-1))
    shared["moe_br"] = np.ascontiguousarray(np.concatenate(
        [np.asarray(inputs["moe_b_group_router"], f32), np.asarray(inputs["moe_b_expert_router"], f32)], axis=-1)
    ).reshape(2, 1, 36)
    shared["moe_w_gate_up"] = np.ascontiguousarray(inputs["moe_w_gate_up"], f32).reshape(2, 32, D, 512)
    shared["moe_w_down"] = np.ascontiguousarray(inputs["moe_w_down"], f32).reshape(2, 32, 256, D)
    in_maps = []
    for r in range(8):
        bi, qi = r // 4, r % 4
        s0 = qi * 2048
        m = dict(shared)
        xc = np.zeros((17 * 128, D), f32)
        pc = np.zeros((17 * 128,), np.int32)
        if qi > 0:
            xc[:] = x[bi, s0 - 128:s0 + 2048]
            pc[:] = pos[bi, s0 - 128:s0 + 2048]
        else:
            xc[128:] = x[bi, 0:2048]
            pc[128:] = pos[bi, 0:2048]
        m["xin"] = xc
        m["pos"] = np.ascontiguousarray(pc.reshape(17, 128).T)
        m["cT"] = np.ascontiguousarray(c[bi].reshape(8, 128).T)
        mk = np.stack([np.tile(m_cur, (1, 4)), np.tile(m_prev, (1, 4)),
                       np.tile(m_none if qi == 0 else m_prev, (1, 4))]).astype(f32)
        m["masks"] = np.ascontiguousarray(mk)
        in_maps.append(m)
    return in_maps


def kernel(**inputs):
    in_maps = _host_inputs(inputs)
    if "nc" not in _CACHE:
        _CACHE["nc"] = build()
    res = run_bass_kernel_spmd(_CACHE["nc"], in_maps, core_ids=list(range(8)))
    out = np.empty((2, 8192, D), np.float32)
    for r in range(8):
        bi, qi = r // 4, r % 4
        out[bi, qi * 2048:(qi + 1) * 2048] = res.results[r]["out"]
    return out
```

```python
import numpy as np
from contextlib import ExitStack
import concourse.bass as bass
import concourse.mybir as mybir
from concourse.bass_utils import run_bass_kernel_spmd

F32 = mybir.dt.float32
BF16 = mybir.dt.bfloat16
I32 = mybir.dt.int32
AF = mybir.ActivationFunctionType
ALU = mybir.AluOpType
AX = mybir.AxisListType

NT = 16
D = 1024
ALPHA = 4.0 ** 0.25
LN_EPS = 1e-5
TWO_PI = 6.283185307179586
C1 = 6.28125
C2 = TWO_PI - C1
NEG = -30000.0


class Buf:
    __slots__ = ("name", "w", "r")

    def __init__(self, name):
        self.name = name
        self.w = None
        self.r = {}


class Sched:
    ENG = ("pe", "act", "dve", "pool", "sp")
    NSLOT = 8

    def __init__(self, nc, es):
        self.nc = nc
        self.es = es
        self.E = {"pe": nc.tensor, "act": nc.scalar, "dve": nc.vector,
                  "pool": nc.gpsimd, "sp": nc.sync}
        self.cnt = {}
        self.isdma = {}
        self.waited = {e: {} for e in self.ENG}
        self.ops = []
        self.needed = {}
        for e in self.ENG:
            self.cnt[e] = 0
            self.isdma[e] = False
            self.needed[e] = set()

    def dma_proc(self, name):
        self.cnt[name] = 0
        self.isdma[name] = True
        self.needed[name] = set()

    def _add_dep(self, deps, p, v):
        if self.isdma[p]:
            key = (p, (v - 1) % self.NSLOT)
            deps[key] = max(deps.get(key, 0), (v - 1) // self.NSLOT + 1)
        else:
            deps[p] = max(deps.get(p, 0), v)

    def _mk_waits(self, eng, deps):
        waits = []
        wd = self.waited[eng]
        for key, v in deps.items():
            if wd.get(key, 0) >= v:
                continue
            wd[key] = v
            waits.append((key, v))
            if not isinstance(key, tuple):
                self.needed[key].add(v)
        return waits

    def op(self, eng, fn, reads=(), writes=(), proc=None):
        proc = proc or eng
        deps = {}
        for b in reads:
            if b.w is not None:
                p, v = b.w
                if p == eng and eng == "pe":
                    continue
                self._add_dep(deps, p, v)
        for b in writes:
            if b.w is not None:
                p, v = b.w
                if p != eng or eng != "pe":
                    self._add_dep(deps, p, v)
            for p, v in b.r.items():
                if p != eng or eng != "pe":
                    self._add_dep(deps, p, v)
        self.cnt[proc] += 1
        c = self.cnt[proc]
        if self.isdma[proc] and c > self.NSLOT:
            self._add_dep(deps, proc, c - self.NSLOT)
        waits = self._mk_waits(eng, deps)
        self.ops.append((eng, fn, waits, proc, c))
        for b in reads:
            b.r[proc] = c
        for b in writes:
            b.w = (proc, c)
            b.r = {}
        return c

    def _all_deps(self):
        deps = {}
        for p, v in self.cnt.items():
            if v <= 0:
                continue
            if self.isdma[p]:
                for i in range(max(1, v - self.NSLOT + 1), v + 1):
                    self._add_dep(deps, p, i)
            else:
                deps[p] = v
        return deps

    def barrier(self):
        for eng in self.ENG:
            deps = {k: v for k, v in self._all_deps().items() if k != eng}
            waits = self._mk_waits(eng, deps)
            if waits:
                self.ops.append((eng, None, waits, None, 0))

    def wait_all(self, eng, bufs):
        deps = {}
        for b in bufs:
            if b.w is not None:
                self._add_dep(deps, b.w[0], b.w[1])
        for b in bufs:
            if b.w is not None and self.isdma[b.w[0]]:
                p = b.w[0]
                v = self.cnt[p]
                for i in range(max(1, v - self.NSLOT + 1), v + 1):
                    self._add_dep(deps, p, i)
        waits = self._mk_waits(eng, deps)
        self.ops.append((eng, None, waits, None, 0))

    def emit(self):
        nc = self.nc
        sems = {}
        for p in self.cnt:
            if self.isdma[p]:
                for sl in range(self.NSLOT):
                    sems[(p, sl)] = self.es.enter_context(nc.semaphore(f"s_{p}{sl}"))
            else:
                sems[p] = self.es.enter_context(nc.semaphore("s_" + p))
        last_inc = {p: 0 for p in self.cnt}
        for eng, fn, waits, proc, c in self.ops:
            e = self.E[eng]
            for key, v in waits:
                e.wait_ge(sems[key], v * 16 if isinstance(key, tuple) else v)
            if fn is None:
                continue
            ins = fn(e)
            if self.isdma[proc]:
                ins.then_inc(sems[(proc, (c - 1) % self.NSLOT)], 16)
            elif c in self.needed[proc]:
                ins.then_inc(sems[proc], c - last_inc[proc])
                last_inc[proc] = c


def build(stop_after=None):
    nc = bass.Bass("TRN2", target_bir_lowering=False)

    def din(name, shape, dt=F32):
        return nc.dram_tensor(name, list(shape), dt, kind="ExternalInput").ap()

    xin = din("xin", [17 * 128, D])
    pos_d = din("pos", [128, 17], I32)
    cT_d = din("cT", [128, 8])
    ident_d = din("ident", [128, 128])
    masks_d = din("masks", [3, 128, 512])
    invf_d = din("invf", [128, 32])
    tril_d = din("tril", [128, 128])
    ada_w = din("ada_w", [2, D, 6 * D])
    ada_b = din("ada_b", [2, 6 * D])
    lng_d = din("post_ln_g", [4, D])
    lnb_d = din("post_ln_b", [4, D])
    lngT_d = din("post_ln_gT", [4, 128, 8])
    lnbT_d = din("post_ln_bT", [4, 128, 8])
    wqkv_d = din("attn_w_qkv", [D, 1536])
    bqkv_d = din("attn_b_qkv", [1, 1536])
    sinks_d = din("attn_sinks", [1, 16])
    wo_d = din("attn_w_o", [D, D])
    bo_d = din("attn_b_o", [1, D])
    win_d = din("gmlp_w_in", [D, 4096])
    bin_d = din("gmlp_b_in", [1, 4096])
    glng_d = din("gmlp_ln_g", [1, 2048])
    glnb_d = din("gmlp_ln_b", [1, 2048])
    wsT_d = din("gmlp_w_sT", [8, 128, 128])
    bsT_d = din("gmlp_b_sT", [128, 8])
    wout_d = din("gmlp_w_out", [2048, D])
    bout_d = din("gmlp_b_out", [1, D])
    wr_d = din("moe_wr", [2, D, 36])
    br_d = din("moe_br", [2, 1, 36])
    wgu_d = din("moe_w_gate_up", [2, 32, D, 512])
    wdn_d = din("moe_w_down", [2, 32, 256, D])
    out_d = nc.dram_tensor("out", [NT * 128, D], F32, kind="ExternalOutput").ap()

    es = ExitStack()
    with es:
        S = Sched(nc, es)
        for p in ("dx", "dw", "dc", "do"):
            S.dma_proc(p)

        cur_es = [es]

        sb_n = [0]

        def sb(name, shape, dt=F32):
            sb_n[0] += 1
            return cur_es[0].enter_context(nc.sbuf_tensor(f"sb{sb_n[0]}_{name}", list(shape), dt))

        banks = [es.enter_context(nc.psum_tensor(f"bank{i}", [128, 512], F32)) for i in range(8)]
        bankbf = [b[:].bitcast(BF16) for b in banks]
        PB = [Buf(f"bank{i}") for i in range(8)]
        bank_rr = [0]
        bank_pool = [list(range(8))]

        def next_bank():
            bank_rr[0] += 1
            return bank_pool[0][bank_rr[0] % len(bank_pool[0])]

        x = sb("x", [128, NT, D])
        BX = [Buf(f"x{t}") for t in range(NT)]
        hT = sb("hT", [128, 8, NT * 128], BF16)
        BH = [Buf(f"hT{t}") for t in range(NT)]
        W = sb("W", [128, 24576], BF16)
        BW = [Buf(f"W{s}") for s in range(4)]
        A1 = sb("A1", [128, D]); A0 = sb("A0", [128, D])
        BA1, BA0 = Buf("A1"), Buf("A0")
        gtbm = sb("gtbm", [128, D]); gtbf = sb("gtbf", [128, D])
        Bgtbm, Bgtbf = Buf("gtbm"), Buf("gtbf")
        ident = sb("ident", [128, 128], BF16); Bident = Buf("ident")
        ident32 = sb("ident32", [128, 128]); Bident32 = Buf("ident32")
        onesrow = sb("onesrow", [128, 128], BF16); Bonesrow = Buf("onesrow")
        cact_rep = sb("cact_rep", [128, 8, 128], BF16); Bcact = Buf("cact")
        ctmp = sb("ctmp", [128, 8]); Bctmp = Buf("ctmp")
        xnb = [sb(f"xnb{i}", [128, D], BF16) for i in range(2)]
        Bxnb = [Buf(f"xnb{i}") for i in range(2)]
        st_ = [sb(f"st{i}", [128, 2, 6]) for i in range(2)]; mv_ = [sb(f"mv{i}", [128, 2]) for i in range(2)]
        sd_ = [sb(f"sd{i}", [128, 1]) for i in range(2)]
        rstd_ = [sb(f"rstd{i}", [128, 1]) for i in range(2)]; nmr_ = [sb(f"nmr{i}", [128, 1]) for i in range(2)]
        Bst_ = [Buf(f"st{i}") for i in range(2)]; Bmv_ = [Buf(f"mv{i}") for i in range(2)]
        Bsd_ = [Buf(f"sd{i}") for i in range(2)]; Brstd_ = [Buf(f"rstd{i}") for i in range(2)]
        Bnmr_ = [Buf(f"nmr{i}") for i in range(2)]
        fin_i = [0]
        eps_t = sb("eps_t", [128, 1]); Beps = Buf("eps")
        H1 = [sb(f"H1_{i}", [128, 8]) for i in range(4)]
        H0 = [sb(f"H0_{i}", [128, 8]) for i in range(4)]
        BHa = [Buf(f"Haff{i}") for i in range(4)]
        fmt = sb("fmt", [128, 8]); fmt2 = sb("fmt2", [128, 8]); fmg = sb("fmg", [128, 8]); fmb = sb("fmb", [128, 8])
        Bfmt, Bfmt2, Bfmg, Bfmb = Buf("fmt"), Buf("fmt2"), Buf("fmg"), Buf("fmb")
        wr = [sb(f"wr{l}", [128, 8, 36], BF16) for l in range(2)]
        brr = [sb(f"brr{l}", [128, 36], BF16) for l in range(2)]
        Bwr = [Buf(f"wr{l}") for l in range(2)]
        lg = sb("lg", [128, NT, 36]); Blg = Buf("lg")
        comb = sb("comb", [128, NT, 32]); Bcomb = Buf("comb")
        Bout = Buf("out")

        def dma(eng, proc, out, in_, reads=(), writes=()):
            S.op(eng, lambda e: e.dma_start(out=out, in_=in_), reads=reads, writes=writes, proc=proc)

        def bc_load(dst, bdst, row_ap):
            n = row_ap.shape[-1]
            dma("sp", "dc", dst[:, 0:n], row_ap.to_broadcast([128, n]), writes=[bdst])

        MS = {}

        def alloc_mod_scratch():
            MS["vecT"] = sb("vecT", [128, D]); MS["BvecT"] = Buf("vecT")
            MS["bcT"] = sb("bcT", [128, D]); MS["BbcT"] = Buf("bcT")
            MS["stg"] = [sb(f"stg{i}", [128, 8, 256], BF16) for i in range(2)]
            MS["Bstg"] = [Buf(f"stg{i}") for i in range(2)]

        dma("pool", "dw", ident[:], ident_d, writes=[Bident])
        dma("sp", "dc", ident32[:], ident_d, writes=[Bident32])
        S.op("dve", lambda e: e.memset(eps_t[:], LN_EPS), writes=[Beps])
        S.op("dve", lambda e: e.memset(onesrow[:], 0.0), writes=[Bonesrow])
        S.op("dve", lambda e: e.memset(onesrow[0:1, :], 1.0), writes=[Bonesrow])
        dma("sp", "dc", ctmp[:], cT_d, writes=[Bctmp])
        S.op("act", lambda e: e.activation(out=ctmp[:], in_=ctmp[:], func=AF.Silu), reads=[Bctmp], writes=[Bctmp])
        S.op("dve", lambda e: e.tensor_copy(out=cact_rep[:], in_=ctmp[:].unsqueeze(2).to_broadcast([128, 8, 128])),
             reads=[Bctmp], writes=[Bcact])
        for l in range(2):
            dma("pool", "dw", wr[l][:], wr_d[l].rearrange("(k p) n -> p k n", p=128), writes=[Bwr[l]])
            S.op("pool", (lambda l=l: lambda e: e.memset(brr[l][:], 0.0))(), writes=[Bwr[l]])
            dma("pool", "dw", brr[l][0:1, :], br_d[l], writes=[Bwr[l]])

        def compute_mod_gen(l, j, dst, bdst, add_one, lag=0):
            bcT, BbcT, stg, Bstg = MS["bcT"], MS["BbcT"], MS["stg"], MS["Bstg"]
            bc_load(bcT, BbcT, ada_b[l:l + 1, j * D:(j + 1) * D])

            def issue(nb):
                si = nb % 2
                col = j * D + nb * 256
                dma("pool", "dw", stg[si][:], ada_w[l][:, col:col + 256].rearrange("(k p) n -> p k n", p=128),
                    writes=[Bstg[si]])

            def consume(nb):
                si = nb % 2
                bk = next_bank()
                for k in range(8):
                    S.op("pe", (lambda k=k: lambda e: e.matmul(
                        banks[bk][:, 0:256], cact_rep[:, k, :], stg[si][:, k, :], start=(k == 0), stop=(k == 7)))(),
                         reads=[Bcact, Bstg[si]], writes=[PB[bk]])
                S.op("dve", lambda e: e.scalar_tensor_tensor(
                    out=dst[:, nb * 256:(nb + 1) * 256], in0=banks[bk][:, 0:256], scalar=(1.0 if add_one else 0.0),
                    in1=bcT[:, nb * 256:(nb + 1) * 256], op0=ALU.add, op1=ALU.add),
                     reads=[PB[bk], BbcT], writes=[bdst])

            issue(0); issue(1)
            for _ in range(lag):
                yield
            consume(0); consume(1)
            issue(2); issue(3)
            for _ in range(lag):
                yield
            consume(2); consume(3)

        def compute_mod(l, j, dst, bdst, add_one):
            for _ in compute_mod_gen(l, j, dst, bdst, add_one):
                pass

        def to_fm(src, bsrc, dst, bdst):
            tmp, Btmp = MS["bcT"], MS["BbcT"]
            S.op("dve", lambda e: e.tensor_tensor(
                out=tmp[:].rearrange("p (k c) -> p k c", k=8), in0=src[:].rearrange("p (k c) -> p k c", k=8),
                in1=ident32[:].unsqueeze(1).to_broadcast([128, 8, 128]), op=ALU.mult),
                 reads=[bsrc, Bident32], writes=[Btmp])
            S.op("dve", lambda e: e.tensor_reduce(out=dst[:], in_=tmp[:].rearrange("p (k c) -> p k c", k=8),
                                                  axis=AX.X, op=ALU.add),
                 reads=[Btmp], writes=[bdst])

        def make_haff(idx, l, j_sh, j_sc, ln_idx):
            vecT, BvecT = MS["vecT"], MS["BvecT"]
            compute_mod(l, j_sc, vecT, BvecT, True)
            to_fm(vecT, BvecT, fmt, Bfmt)
            compute_mod(l, j_sh, vecT, BvecT, False)
            to_fm(vecT, BvecT, fmt2, Bfmt2)
            haff_combine(idx, ln_idx)

        def haff_combine(idx, ln_idx):
            if ln_idx is None:
                S.op("dve", lambda e: e.tensor_copy(out=H1[idx][:], in_=fmt[:]), reads=[Bfmt], writes=[BHa[idx]])
                S.op("dve", lambda e: e.tensor_copy(out=H0[idx][:], in_=fmt2[:]), reads=[Bfmt2], writes=[BHa[idx]])
            else:
                dma("sp", "dc", fmg[:], lngT_d[ln_idx], writes=[Bfmg])
                dma("sp", "dc", fmb[:], lnbT_d[ln_idx], writes=[Bfmb])
                S.op("dve", lambda e: e.tensor_tensor(out=H1[idx][:], in0=fmt[:], in1=fmg[:], op=ALU.mult),
                     reads=[Bfmt, Bfmg], writes=[BHa[idx]])
                S.op("dve", lambda e: e.tensor_tensor(out=fmb[:], in0=fmt[:], in1=fmb[:], op=ALU.mult),
                     reads=[Bfmt, Bfmb], writes=[Bfmb])
                S.op("dve", lambda e: e.tensor_tensor(out=H0[idx][:], in0=fmb[:], in1=fmt2[:], op=ALU.add),
                     reads=[Bfmb, Bfmt2], writes=[BHa[idx]])

        def make_aset(ln_idx, bias_row, gtb, bgtb, scale=ALPHA):
            bc_load(A1, BA1, lng_d[ln_idx:ln_idx + 1, :])
            bc_load(A0, BA0, lnb_d[ln_idx:ln_idx + 1, :])
            if scale != 1.0:
                S.op("dve", lambda e: e.tensor_scalar(out=A1[:], in0=A1[:], scalar1=scale, scalar2=None, op0=ALU.mult),
                     reads=[BA1], writes=[BA1])
            if bias_row is None:
                if scale != 1.0:
                    S.op("dve", lambda e: e.tensor_scalar(out=A0[:], in0=A0[:], scalar1=scale, scalar2=None, op0=ALU.mult),
                         reads=[BA0], writes=[BA0])
            else:
                vt, bvt = MS["vecT"], MS["BvecT"]
                bc_load(vt, bvt, bias_row)
                S.op("dve", lambda e: e.tensor_tensor(out=vt[:], in0=vt[:], in1=gtb[:], op=ALU.mult),
                     reads=[bvt, bgtb], writes=[bvt])
                S.op("dve", lambda e: e.scalar_tensor_tensor(out=A0[:], in0=A0[:], scalar=scale, in1=vt[:],
                                                             op0=ALU.mult, op1=ALU.add),
                     reads=[BA0, bvt], writes=[BA0])

        def router_logits(l, t):
            bk = next_bank()
            for k in range(8):
                S.op("pe", (lambda k=k: lambda e: e.matmul(
                    banks[bk][:, 0:36], hT[:, k, t * 128:(t + 1) * 128], wr[l][:, k, :], start=(k == 0), stop=False))(),
                     reads=[BH[t], Bwr[l]], writes=[PB[bk]])
            S.op("pe", lambda e: e.matmul(banks[bk][:, 0:36], onesrow[:], brr[l][:], start=False, stop=True),
                 reads=[Bonesrow, Bwr[l]], writes=[PB[bk]])
            S.op("dve", lambda e: e.tensor_copy(out=lg[:, t, :], in_=banks[bk][:, 0:36]), reads=[PB[bk]], writes=[Blg])

        def finalize_a(t, mode, src=None, bsrc=None, part=0):
            xa = x[:, t, :] if src is None else src
            bxa = BX[t] if bsrc is None else bsrc
            i = fin_i[0] % 2
            fin_i[0] += 1
            st, mv, sd, rstd, nmr = st_[i], mv_[i], sd_[i], rstd_[i], nmr_[i]
            Bst, Bmv, Bsd, Brstd, Bnmr = Bst_[i], Bmv_[i], Bsd_[i], Brstd_[i], Bnmr_[i]
            if mode == "pro":
                S.op("act", lambda e: e.activation(out=xnb[i][:], in_=xa, func=AF.Copy), reads=[bxa], writes=[Bxnb[i]])
                if src is None:
                    S.op("dve", lambda e: e.scalar_tensor_tensor(out=xa, in0=xa, scalar=ALPHA, in1=A0[:],
                                                                 op0=ALU.mult, op1=ALU.add),
                         reads=[bxa, BA0], writes=[bxa])
                return i
            S.op("dve", lambda e: e.bn_stats(out=st[:, 0, :], in_=xa[:, 0:512]), reads=[bxa], writes=[Bst])
            S.op("dve", lambda e: e.bn_stats(out=st[:, 1, :], in_=xa[:, 512:1024]), reads=[bxa], writes=[Bst])
            S.op("dve", lambda e: e.bn_aggr(out=mv[:], in_=st[:]), reads=[Bst], writes=[Bmv])
            if part == 1:
                return i
            return finalize_a2(t, mode, i, src=src, bsrc=bsrc)

        def finalize_a2(t, mode, i, src=None, bsrc=None, stage=None):
            xa = x[:, t, :] if src is None else src
            bxa = BX[t] if bsrc is None else bsrc
            st, mv, sd, rstd, nmr = st_[i], mv_[i], sd_[i], rstd_[i], nmr_[i]
            Bst, Bmv, Bsd, Brstd, Bnmr = Bst_[i], Bmv_[i], Bsd_[i], Brstd_[i], Bnmr_[i]
            if stage in (None, "act"):
                finalize_a2_act(mv, sd, rstd, nmr, Bmv, Bsd, Brstd, Bnmr)
            if stage == "act":
                if mode == "mid":
                    S.op("act", lambda e: e.activation(out=hT[:, :, t * 128:(t + 1) * 128],
                                                       in_=xa.rearrange("p (k c) -> p k c", k=8), func=AF.Identity,
                                                       scale=rstd[:], bias=nmr[:]),
                         reads=[bxa, Brstd, Bnmr], writes=[BH[t]])
                return i
            if mode == "mid" and stage is None:
                S.op("act", lambda e: e.activation(out=xnb[i][:], in_=xa, func=AF.Identity, scale=rstd[:], bias=nmr[:]),
                     reads=[bxa, Brstd, Bnmr], writes=[Bxnb[i]])
            S.op("dve", lambda e: e.tensor_scalar(out=xa, in0=xa, scalar1=rstd[:], scalar2=nmr[:],
                                                  op0=ALU.mult, op1=ALU.add),
                 reads=[bxa, Brstd, Bnmr], writes=[bxa])
            S.op("dve" if mode == "out" else "pool", lambda e: e.tensor_tensor(out=xa, in0=xa, in1=A1[:], op=ALU.mult),
                 reads=[bxa, BA1], writes=[bxa])
            S.op("pool", lambda e: e.tensor_tensor(out=xa, in0=xa, in1=A0[:], op=ALU.add),
                 reads=[bxa, BA0], writes=[bxa])
            if mode == "out":
                dma("sp", "do", out_d[t * 128:(t + 1) * 128, :], xa, reads=[bxa], writes=[Bout])
            return i

        def finalize_a2_act(mv, sd, rstd, nmr, Bmv, Bsd, Brstd, Bnmr):
            S.op("act", lambda e: e.activation(out=sd[:], in_=mv[:, 1:2], func=AF.Ln, bias=eps_t[:]),
                 reads=[Bmv, Beps], writes=[Bsd])
            S.op("act", lambda e: e.activation(out=rstd[:], in_=sd[:], func=AF.Exp, scale=-0.5), reads=[Bsd], writes=[Brstd])
            S.op("act", lambda e: e.activation(out=nmr[:], in_=mv[:, 0:1], func=AF.Copy, scale=rstd[:]),
                 reads=[Bmv, Brstd], writes=[Bnmr])
            S.op("act", lambda e: e.activation(out=nmr[:], in_=nmr[:], func=AF.Copy, scale=-1.0),
                 reads=[Bnmr], writes=[Bnmr])

        def finalize_b(t, i, haff, hdst=None, bhdst=None, parked=False):
            bks = [next_bank(), next_bank()]
            for k in range(8):
                if parked:
                    S.op("pe", (lambda k=k: lambda e: e.matmul(
                        banks[bks[k // 4]][:, (k % 4) * 128:(k % 4 + 1) * 128], hT[:, k, t * 128:(t + 1) * 128], ident[:],
                        start=True, stop=True))(),
                         reads=[BH[t], Bident], writes=[PB[bks[k // 4]]])
                    continue
                S.op("pe", (lambda k=k: lambda e: e.matmul(
                    banks[bks[k // 4]][:, (k % 4) * 128:(k % 4 + 1) * 128], xnb[i][:, k * 128:(k + 1) * 128], ident[:],
                    start=True, stop=True))(),
                     reads=[Bxnb[i], Bident], writes=[PB[bks[k // 4]]])
            hd = hT[:, :, t * 128:(t + 1) * 128] if hdst is None else hdst
            bhd = BH[t] if bhdst is None else bhdst
            for k in range(8):
                if k % 2 == 0:
                    S.op("act", (lambda k=k: lambda e: e.activation(
                        out=hd[:, k, :], in_=banks[bks[k // 4]][:, (k % 4) * 128:(k % 4 + 1) * 128], func=AF.Identity,
                        scale=H1[haff][:, k:k + 1], bias=H0[haff][:, k:k + 1]))(),
                         reads=[PB[bks[k // 4]], BHa[haff]], writes=[bhd])
                else:
                    S.op("dve", (lambda k=k: lambda e: e.tensor_scalar(
                        out=hd[:, k, :], in0=banks[bks[k // 4]][:, (k % 4) * 128:(k % 4 + 1) * 128],
                        scalar1=H1[haff][:, k:k + 1], scalar2=H0[haff][:, k:k + 1], op0=ALU.mult, op1=ALU.add))(),
                         reads=[PB[bks[k // 4]], BHa[haff]], writes=[bhd])

        def finalize(t, mode, haff=None, hdst=None, bhdst=None, route_l=None, src=None, bsrc=None):
            i = finalize_a(t, mode, src=src, bsrc=bsrc)
            if mode == "out":
                return
            finalize_b(t, i, haff, hdst=hdst, bhdst=bhdst)
            if route_l is not None:
                router_logits(route_l, t)

        def finalize_seq(mode, haff=None, route_l=None, hook=None):
            idx = {}
            if mode == "pro":
                idx[0] = finalize_a(0, mode)
                for t in range(NT):
                    if hook is not None:
                        hook(t)
                    if t + 1 < NT:
                        idx[t + 1] = finalize_a(t + 1, mode)
                    finalize_b(t, idx[t], haff)
                return
            idx[0] = finalize_a(0, mode, part=1)
            idx[1] = finalize_a(1, mode, part=1)
            finalize_a2(0, mode, idx[0])
            for t in range(NT):
                if hook is not None:
                    hook(t)
                if t + 2 < NT:
                    idx[t + 2] = finalize_a(t + 2, mode, part=1)
                if t + 1 < NT:
                    finalize_a2(t + 1, mode, idx[t + 1])
                if mode != "out":
                    finalize_b(t, idx[t], haff)
                    if route_l is not None and t >= 1:
                        router_logits(route_l, t - 1)
            if mode != "out" and route_l is not None:
                router_logits(route_l, NT - 1)

        def make_end_hook(mode, haff=None, route_l=None):
            st = {}

            def stage_b(t):
                if mode != "out":
                    finalize_b(t, st[t], haff)
                    if route_l is not None:
                        router_logits(route_l, t)

            def hook_out(t):
                st[t] = finalize_a(t, mode, part=1)
                if t >= 1:
                    finalize_a2(t - 1, mode, st[t - 1], stage="act")
                if t >= 2:
                    finalize_a2(t - 2, mode, st[t - 2], stage="rest")

            def flush_out():
                finalize_a2(NT - 1, mode, st[NT - 1], stage="act")
                finalize_a2(NT - 2, mode, st[NT - 2], stage="rest")
                finalize_a2(NT - 1, mode, st[NT - 1], stage="rest")

            if mode == "out":
                return hook_out, flush_out

            def hook_mid(t):
                st[t] = finalize_a(t, mode, part=1)
                if t >= 1:
                    finalize_a2(t - 1, mode, st[t - 1], stage="act")
                if t >= 2:
                    finalize_a2(t - 2, mode, st[t - 2], stage="rest")

            def flush_mid():
                finalize_a2(NT - 1, mode, st[NT - 1], stage="act")
                finalize_a2(NT - 2, mode, st[NT - 2], stage="rest")
                finalize_a2(NT - 1, mode, st[NT - 1], stage="rest")
                for t in range(NT):
                    finalize_b(t, None, haff, parked=True)
                    if route_l is not None and t >= 1:
                        router_logits(route_l, t - 1)
                if route_l is not None:
                    router_logits(route_l, NT - 1)

            if USE_END_HOOK_MID:
                return hook_mid, flush_mid

            def hook(t):
                st[t] = finalize_a(t, mode, part=1)
                if t >= 1:
                    finalize_a2(t - 1, mode, st[t - 1])
                if t >= 2:
                    stage_b(t - 2)

            def flush():
                finalize_a2(NT - 1, mode, st[NT - 1])
                stage_b(NT - 2)
                stage_b(NT - 1)
            return hook, flush

        def dump_and_end():
            for t in range(NT):
                dma("sp", "do", out_d[t * 128:(t + 1) * 128, :], x[:, t, :], reads=[BX[t]], writes=[Bout])
            S.wait_all("sp", [Bout])
            S.emit()

        def moe_views(s):
            base = s * 6144
            wgu = W[:, base:base + 4096].rearrange("p (k n) -> p k n", k=8)
            wdn = W[:, base + 4096:base + 6144].rearrange("p (j n) -> p j n", j=2)
            return wgu, wdn

        def moe_load_gu(l, ex):
            s_ = ex % 4
            wgu, _ = moe_views(s_)
            dma("pool", "dw", wgu, wgu_d[l, ex].rearrange("(k p) n -> p k n", p=128), writes=[BW[s_]])

        def moe_load_dn(l, ex):
            s_ = ex % 4
            _, wdn = moe_views(s_)
            dma("pool", "dw", wdn, wdn_d[l, ex].rearrange("(j p) n -> p j n", p=128), writes=[BW[s_]])

        def moe_fold(l, ex):
            s_ = ex % 4
            _, wdn = moe_views(s_)
            S.op("pool", lambda e: e.tensor_tensor(out=wdn, in0=wdn, in1=gtbf[:].unsqueeze(1).to_broadcast([128, 2, D]),
                                                   op=ALU.mult), reads=[BW[s_], Bgtbf], writes=[BW[s_]])

        def moe_load(l, ex):
            moe_load_gu(l, ex); moe_load_dn(l, ex); moe_fold(l, ex)

        esA = ExitStack()
        cur_es[0] = esA
        with esA:
            cosT = sb("cosT", [128, 17, 32]); sinT = sb("sinT", [128, 17, 32])
            Bcos, Bsin = Buf("cos"), Buf("sin")
            hTh = sb("hTh", [128, 8, 128], BF16); BhTh = Buf("hTh")
            bqkv = sb("bqkv", [128, 1536], BF16); Bbqkv = Buf("bqkv")
            esink = sb("esink", [128, 16]); Besink = Buf("esink")
            maskb = sb("maskb", [128, 3, 512], BF16); Bmask = Buf("maskb")
            Wqkv = W[:, 0:12288].rearrange("p (k n) -> p k n", k=8)
            Wo = W[:, 12288:20480].rearrange("p (k n) -> p k n", k=8)

            es0 = ExitStack()
            cur_es[0] = es0
            with es0:
                alloc_mod_scratch()
                posi = sb("posi", [128, 17], I32); posf = sb("posf", [128, 17]); invf = sb("invf", [128, 32])
                ang = sb("ang", [128, 17, 32]); ki = sb("ki", [128, 17, 32], I32)
                kf = W[:, 22528:22528 + 1088].bitcast(F32).rearrange("p (t f) -> p t f", t=17)
                Bpos, Binvf, Bang, Bkf, Bki = Buf("pos"), Buf("invf"), Buf("ang"), Buf("kf"), Buf("ki")
                xh = W[:, 20480:22528].bitcast(F32); Bxh = Buf("xh")
                dma("sp", "dx", xh, xin[0:128, :], writes=[Bxh])
                for t in range(NT):
                    dma("sp", "dx", x[:, t, :], xin[(t + 1) * 128:(t + 2) * 128, :], writes=[BX[t]])

                make_haff(0, 0, 0, 1, None)
                compute_mod(0, 2, gtbm, Bgtbm, True)
                bc_load(A0, BA0, bo_d)
                S.op("dve", lambda e: e.tensor_tensor(out=A0[:], in0=A0[:], in1=gtbm[:], op=ALU.mult),
                     reads=[BA0, Bgtbm], writes=[BA0])
                dma("pool", "dw", Wqkv, wqkv_d.rearrange("(k p) n -> p k n", p=128), writes=[BW[0], BW[1]])
                dma("pool", "dw", Wo, wo_d.rearrange("(k p) n -> p k n", p=128), writes=[BW[2], BW[3]])
                S.op("pool", lambda e: e.memset(bqkv[:], 0.0), writes=[Bbqkv])
                dma("pool", "dw", bqkv[0:1, :], bqkv_d, writes=[Bbqkv])
                bc_load(esink, Besink, sinks_d)
                S.op("act", lambda e: e.activation(out=esink[:], in_=esink[:], func=AF.Exp), reads=[Besink], writes=[Besink])
                dma("pool", "dw", maskb[:], masks_d.rearrange("m p n -> p m n"), writes=[Bmask])
                dma("sp", "dc", posi[:], pos_d, writes=[Bpos])
                dma("sp", "dc", invf[:], invf_d, writes=[Binvf])
                S.op("dve", lambda e: e.tensor_copy(out=posf[:], in_=posi[:]), reads=[Bpos], writes=[Bpos])
                S.op("dve", lambda e: e.tensor_tensor(out=ang[:], in0=posf[:].unsqueeze(2).to_broadcast([128, 17, 32]),
                                                      in1=invf[:].unsqueeze(1).to_broadcast([128, 17, 32]), op=ALU.mult),
                     reads=[Bpos, Binvf], writes=[Bang])

                def sin_of(dst, bdst, shift):
                    if shift != 0.0:
                        S.op("dve", lambda e: e.tensor_scalar(out=dst[:], in0=ang[:], scalar1=shift, scalar2=None, op0=ALU.add),
                             reads=[Bang], writes=[bdst])
                        a_src, ba = dst, bdst
                    else:
                        a_src, ba = ang, Bang
                    S.op("dve", lambda e: e.tensor_scalar(out=ki[:], in0=a_src[:], scalar1=1.0 / TWO_PI, scalar2=None,
                                                          op0=ALU.mult), reads=[ba], writes=[Bki])
                    S.op("dve", lambda e: e.tensor_copy(out=kf, in_=ki[:]), reads=[Bki], writes=[Bkf])
                    S.op("dve", lambda e: e.scalar_tensor_tensor(out=dst[:], in0=kf, scalar=-C1, in1=a_src[:],
                                                                 op0=ALU.mult, op1=ALU.add), reads=[Bkf, ba], writes=[bdst])
                    S.op("dve", lambda e: e.scalar_tensor_tensor(out=dst[:], in0=kf, scalar=-C2, in1=dst[:],
                                                                 op0=ALU.mult, op1=ALU.add), reads=[Bkf, bdst], writes=[bdst])
                    S.op("dve", lambda e: e.tensor_scalar(out=dst[:], in0=dst[:], scalar1=3.1415925, scalar2=-3.1415925,
                                                          op0=ALU.min, op1=ALU.max), reads=[bdst], writes=[bdst])
                    S.op("act", lambda e: e.activation(out=dst[:], in_=dst[:], func=AF.Sin), reads=[bdst], writes=[bdst])

                sin_of(sinT, Bsin, 0.0)
                sin_of(cosT, Bcos, TWO_PI / 4)

                finalize(0, "pro", haff=0, hdst=hTh, bhdst=BhTh, src=xh, bsrc=Bxh)
                def mods_b():
                    vecT, BvecT = MS["vecT"], MS["BvecT"]
                    yield from compute_mod_gen(0, 4, vecT, BvecT, True, lag=3)
                    to_fm(vecT, BvecT, fmt, Bfmt)
                    yield from compute_mod_gen(0, 3, vecT, BvecT, False, lag=3)
                    to_fm(vecT, BvecT, fmt2, Bfmt2)
                    haff_combine(1, 0)
                    yield from compute_mod_gen(0, 5, gtbf, Bgtbf, True, lag=2)

                job_b = mods_b()
                finalize_seq("pro", haff=0, hook=lambda t: next(job_b, None))
                for _ in job_b:
                    pass
                S.op("dve", lambda e: e.tensor_tensor(out=Wo, in0=Wo, in1=gtbm[:].unsqueeze(1).to_broadcast([128, 8, D]),
                                                      op=ALU.mult),
                     reads=[BW[2], BW[3], Bgtbm], writes=[BW[2], BW[3]])
                if stop_after == "pro":
                    dump_and_end()
                    return nc
                make_aset(0, None, None, None)
            S.barrier()

            es1 = ExitStack()
            cur_es[0] = es1
            bank_pool[0] = [3, 6, 7]
            with es1:
                qk32 = sb("qk32", [128, 20, 64]); Bqk32 = Buf("qk32")
                rot = sb("rot", [128, 20, 64], BF16); Brot = Buf("rot")
                rot2 = rot[:].rearrange("p h d -> p (h d)")
                tA = sb("tA", [128, 20, 32]); tB = sb("tB", [128, 20, 32])
                BtA, BtB = Buf("tA"), Buf("tB")
                kpad = sb("kpad", [128, 4, 2, 128], BF16); Bkpad = Buf("kpad")
                qT = sb("qT", [128, 8, 128], BF16); BqT = Buf("qT")
                Vaug = [sb(f"Vaug{i}", [128, 4, 65], BF16) for i in range(3)]
                BV = [Buf(f"V{i}") for i in range(3)]
                ao = sb("ao", [128, 16, 64], BF16); Bao = Buf("ao")
                ao2 = ao[:].rearrange("p h d -> p (h d)")
                aoT = sb("aoT", [128, 8, 128], BF16); BaoT = Buf("aoT")
                dent = sb("dent", [128, 16]); Bdent = Buf("dent")
                kT = [W[:, 20480 + i * 1024:20480 + (i + 1) * 1024].rearrange("p (v c) -> p v c", v=8) for i in range(2)]
                BkT = [Buf(f"kT{i}") for i in range(2)]
                PT = [W[:, 22528 + i * 1024:22528 + (i + 1) * 1024].rearrange("p (b c) -> p b c", b=2) for i in range(2)]
                BPT = [Buf(f"PT{i}") for i in range(2)]
                S.op("pool", lambda e: e.memset(kpad[:], 0.0), writes=[Bkpad])
                for i in range(3):
                    S.op("pool", (lambda i=i: lambda e: e.memset(Vaug[i][:], 1.0))(), writes=[BV[i]])

                def X1(t):
                    halo = t < 0
                    tt = t + 1
                    hsrc = hTh if halo else hT[:, :, t * 128:(t + 1) * 128]
                    bh = BhTh if halo else BH[t]
                    qb = []
                    for nb in ([2] if halo else [0, 1, 2]):
                        bk = nb
                        qb.append((nb, bk))
                        for k in range(8):
                            S.op("pe", (lambda k=k, nb=nb, bk=bk: lambda e: e.matmul(
                                banks[bk][:], hsrc[:, k, :], Wqkv[:, k, nb * 512:(nb + 1) * 512], start=(k == 0), stop=False))(),
                                 reads=[bh, BW[0], BW[1]], writes=[PB[bk]])
                        S.op("pe", (lambda nb=nb, bk=bk: lambda e: e.matmul(
                            banks[bk][:], onesrow[:], bqkv[:, nb * 512:(nb + 1) * 512], start=False, stop=True))(),
                             reads=[Bonesrow, Bbqkv], writes=[PB[bk]])
                    vcur = Vaug[tt % 3]; bvcur = BV[tt % 3]
                    for nb, bk in qb:
                        if nb < 2:
                            S.op("act", (lambda nb=nb, bk=bk: lambda e: e.activation(
                                out=qk32[:, nb * 8:(nb + 1) * 8, :], in_=banks[bk][:].rearrange("p (h d) -> p h d", d=64),
                                func=AF.Copy))(), reads=[PB[bk]], writes=[Bqk32])
                        else:
                            S.op("act", (lambda bk=bk: lambda e: e.activation(
                                out=qk32[:, 16:20, :], in_=banks[bk][:, 0:256].rearrange("p (h d) -> p h d", d=64),
                                func=AF.Copy))(), reads=[PB[bk]], writes=[Bqk32])
                            S.op("act", (lambda bk=bk: lambda e: e.activation(
                                out=vcur[:, :, 0:64], in_=banks[bk][:, 256:512].rearrange("p (h d) -> p h d", d=64),
                                func=AF.Copy))(), reads=[PB[bk]], writes=[bvcur])
                    h0 = 16 if halo else 0
                    nh = 20 - h0
                    x1 = qk32[:, h0:20, 0:32]; x2 = qk32[:, h0:20, 32:64]
                    cb = cosT[:, tt, :].unsqueeze(1).to_broadcast([128, nh, 32])
                    sbc = sinT[:, tt, :].unsqueeze(1).to_broadcast([128, nh, 32])
                    S.op("dve", lambda e: e.tensor_tensor(out=tA[:, h0:20, :], in0=x1, in1=cb, op=ALU.mult),
                         reads=[Bqk32, Bcos], writes=[BtA])
                    S.op("dve", lambda e: e.tensor_tensor(out=tB[:, h0:20, :], in0=x2, in1=sbc, op=ALU.mult),
                         reads=[Bqk32, Bsin], writes=[BtB])
                    if not halo:
                        S.op("dve", lambda e: e.tensor_tensor(out=rot[:, 0:16, 0:32], in0=tA[:, 0:16, :], in1=tB[:, 0:16, :],
                                                              op=ALU.subtract), reads=[BtA, BtB], writes=[Brot])
                    for half in range(2):
                        S.op("dve", (lambda half=half: lambda e: e.tensor_tensor(
                            out=kpad[:, :, half, half * 64:half * 64 + 32], in0=tA[:, 16:20, :], in1=tB[:, 16:20, :],
                            op=ALU.subtract))(), reads=[BtA, BtB], writes=[Bkpad])
                    S.op("dve", lambda e: e.tensor_tensor(out=tA[:, h0:20, :], in0=x2, in1=cb, op=ALU.mult),
                         reads=[Bqk32, Bcos], writes=[BtA])
                    S.op("dve", lambda e: e.tensor_tensor(out=tB[:, h0:20, :], in0=x1, in1=sbc, op=ALU.mult),
                         reads=[Bqk32, Bsin], writes=[BtB])
                    if not halo:
                        S.op("dve", lambda e: e.tensor_tensor(out=rot[:, 0:16, 32:64], in0=tA[:, 0:16, :], in1=tB[:, 0:16, :],
                                                              op=ALU.add), reads=[BtA, BtB], writes=[Brot])
                    for half in range(2):
                        S.op("dve", (lambda half=half: lambda e: e.tensor_tensor(
                            out=kpad[:, :, half, half * 64 + 32:half * 64 + 64], in0=tA[:, 16:20, :], in1=tB[:, 16:20, :],
                            op=ALU.add))(), reads=[BtA, BtB], writes=[Bkpad])

                tb = [3, 6]

                def X2(t):
                    halo = t < 0
                    tt = t + 1
                    cur = tt % 2
                    for v in range(8):
                        S.op("pe", (lambda v=v: lambda e: e.matmul(
                            banks[tb[v // 4]][:, (v % 4) * 128:(v % 4 + 1) * 128], kpad[:, v // 2, v % 2, :], ident[:],
                            start=True, stop=True))(),
                             reads=[Bkpad, Bident], writes=[PB[tb[v // 4]]])
                    for hb in range(2):
                        S.op("act", (lambda hb=hb: lambda e: e.activation(
                            out=kT[cur][:, hb * 4:(hb + 1) * 4, :], in_=banks[tb[hb]][:].rearrange("p (v c) -> p v c", v=4),
                            func=AF.Copy))(), reads=[PB[tb[hb]]], writes=[BkT[cur]])
                    if halo:
                        return
                    for j in range(8):
                        S.op("pe", (lambda j=j: lambda e: e.matmul(
                            banks[tb[j // 4]][:, (j % 4) * 128:(j % 4 + 1) * 128], rot2[:, j * 128:(j + 1) * 128], ident[:],
                            start=True, stop=True))(),
                             reads=[Brot, Bident], writes=[PB[tb[j // 4]]])
                    for hb in range(2):
                        S.op("dve", (lambda hb=hb: lambda e: e.tensor_copy(
                            out=qT[:, hb * 4:(hb + 1) * 4, :], in_=banks[tb[hb]][:].rearrange("p (v c) -> p v c", v=4)))(),
                             reads=[PB[tb[hb]]], writes=[BqT])

                def Y1(t):
                    tt = t + 1
                    cur = tt % 2
                    prv = 1 - cur
                    vcur, bvcur = Vaug[tt % 3], BV[tt % 3]
                    vprv, bvprv = Vaug[(tt - 1) % 3], BV[(tt - 1) % 3]
                    ob = [0, 1, 2]
                    sc_i = [0]
                    first_tile = (t == 0)

                    def scores(g):
                        pi = g % 2
                        for blk, (ktile, bkt, mi) in enumerate([(kT[prv], BkT[prv], 2 if first_tile else 1),
                                                                (kT[cur], BkT[cur], 0)]):
                            bk = 4 + (sc_i[0] % 2)
                            sc_i[0] += 1
                            S.op("pe", (lambda bk=bk, mi=mi: lambda e: e.matmul(
                                banks[bk][:], ident[:], maskb[:, mi, :], start=True, stop=False))(),
                                 reads=[Bident, Bmask], writes=[PB[bk]])
                            for i in range(4):
                                h = 4 * g + i
                                S.op("pe", (lambda bk=bk, i=i, h=h, ktile=ktile, g=g: lambda e: e.matmul(
                                    banks[bk][:, i * 128:(i + 1) * 128], ktile[:, g * 2 + (h % 2), :], qT[:, h // 2, :],
                                    start=False, stop=(i == 3)))(),
                                     reads=[bkt, BqT], writes=[PB[bk]])
                            S.op("act", (lambda bk=bk, blk=blk, pi=pi: lambda e: e.activation(
                                out=PT[pi][:, blk, :], in_=banks[bk][:], func=AF.Exp, scale=0.125))(),
                                 reads=[PB[bk]], writes=[BPT[pi]])

                    def pv(g):
                        pi = g % 2
                        for i in range(4):
                            h = 4 * g + i
                            obk = ob[h // 7]
                            oc = (h % 7) * 65
                            for blk, (vt, bvt) in enumerate([(vprv, bvprv), (vcur, bvcur)]):
                                S.op("pe", (lambda obk=obk, oc=oc, blk=blk, pi=pi, i=i, vt=vt, g=g: lambda e: e.matmul(
                                    banks[obk][:, oc:oc + 65], PT[pi][:, blk, i * 128:(i + 1) * 128], vt[:, g, :],
                                    start=(blk == 0), stop=(blk == 1)))(),
                                     reads=[BPT[pi], bvt], writes=[PB[obk]])

                    scores(0)
                    for g in range(4):
                        if g + 1 < 4:
                            scores(g + 1)
                        pv(g)
                    for b3 in range(3):
                        hs = 7 * b3
                        n = min(7, 16 - hs)
                        ov = banks[ob[b3]][:, 0:n * 65].rearrange("p (h d) -> p h d", d=65)
                        S.op("dve", (lambda ov=ov, hs=hs, n=n: lambda e: e.tensor_tensor(
                            out=dent[:, hs:hs + n], in0=ov[:, :, 64], in1=esink[:, hs:hs + n], op=ALU.add))(),
                             reads=[PB[ob[b3]], Besink], writes=[Bdent])
                        S.op("dve", (lambda hs=hs, n=n: lambda e: e.reciprocal(out=dent[:, hs:hs + n], in_=dent[:, hs:hs + n]))(),
                             reads=[Bdent], writes=[Bdent])
                        S.op("dve", (lambda ov=ov, hs=hs, n=n: lambda e: e.tensor_tensor(
                            out=ao[:, hs:hs + n, :], in0=ov[:, :, 0:64],
                            in1=dent[:, hs:hs + n].unsqueeze(2).to_broadcast([128, n, 64]), op=ALU.mult))(),
                             reads=[PB[ob[b3]], Bdent], writes=[Bao])

                def Y2a(t):
                    for j in range(8):
                        S.op("pe", (lambda j=j: lambda e: e.matmul(
                            banks[tb[j // 4]][:, (j % 4) * 128:(j % 4 + 1) * 128], ao2[:, j * 128:(j + 1) * 128], ident[:],
                            start=True, stop=True))(),
                             reads=[Bao, Bident], writes=[PB[tb[j // 4]]])
                    for hb in range(2):
                        S.op("act", (lambda hb=hb: lambda e: e.activation(
                            out=aoT[:, hb * 4:(hb + 1) * 4, :], in_=banks[tb[hb]][:].rearrange("p (v c) -> p v c", v=4),
                            func=AF.Copy))(), reads=[PB[tb[hb]]], writes=[BaoT])

                def Y2b(t):
                    for nb in range(2):
                        bk = 4 + nb
                        for k in range(8):
                            S.op("pe", (lambda k=k, nb=nb, bk=bk: lambda e: e.matmul(
                                banks[bk][:], aoT[:, k, :], Wo[:, k, nb * 512:(nb + 1) * 512], start=(k == 0), stop=(k == 7)))(),
                                 reads=[BaoT, BW[2], BW[3]], writes=[PB[bk]])
                        S.op("dve", (lambda nb=nb, bk=bk: lambda e: e.tensor_tensor(
                            out=x[:, t, nb * 512:(nb + 1) * 512], in0=x[:, t, nb * 512:(nb + 1) * 512], in1=banks[bk][:],
                            op=ALU.add))(), reads=[BX[t], PB[bk]], writes=[BX[t]])

                X1(-1); X2(-1)
                X1(0); X2(0)
                fi = {}
                for t in range(NT + 3):
                    if 0 <= t - 2 < NT:
                        fi[t - 2] = finalize_a(t - 2, "mid")
                    if t + 1 < NT:
                        X1(t + 1)
                    if t == NT - 1:
                        moe_load_gu(0, 0); moe_load_gu(0, 1)
                    if t == NT:
                        moe_load_dn(0, 0); moe_load_dn(0, 1)
                    if t == NT + 2:
                        moe_fold(0, 0); moe_fold(0, 1)
                    if 0 <= t - 1 < NT:
                        Y2a(t - 1)
                    if t < NT:
                        Y1(t)
                    if 0 <= t - 1 < NT:
                        Y2b(t - 1)
                    if 0 <= t - 2 < NT:
                        finalize_b(t - 2, fi[t - 2], 1)
                    if t + 1 < NT:
                        X2(t + 1)
                    if 0 <= t - 3 < NT:
                        router_logits(0, t - 3)
                if stop_after == "attn":
                    dump_and_end()
                    return nc
            S.barrier()
        cur_es[0] = es

        def bc3(ap2, n):
            return ap2.unsqueeze(2).to_broadcast([128, NT, n])

        def moe_phase(l, after_chunk=None, nchunks=16, tile_hook=None, first_load=0, end_hook=None):
            gl_m = sb("gl_m", [128, NT]); Bglm = Buf("gl_m")
            goh = sb("goh", [128, NT, 4]); Bgoh = Buf("goh")
            gex = sb("gex", [128, NT, 4]); Bgex = Buf("gex")
            gp = sb("gp", [128, NT]); Bgp = Buf("gp")
            sel4 = sb("sel4", [128, NT, 32]); Bsel4 = Buf("sel4")
            sel = sb("sel", [128, NT, 8]); Bsel = Buf("sel")
            sel2 = sb("sel2", [128, NT, 8]); Bsel2 = Buf("sel2")
            oh1 = sb("oh1", [128, NT, 8]); Boh1 = Buf("oh1")
            oh2 = sb("oh2", [128, NT, 8]); Boh2 = Buf("oh2")
            m1 = sb("m1", [128, NT]); m2 = sb("m2", [128, NT]); Bm1, Bm2 = Buf("m1"), Buf("m2")
            w1 = sb("w1", [128, NT]); w2 = sb("w2", [128, NT]); Bw1, Bw2 = Buf("w1"), Buf("w2")
            sg = [sb(f"sg{i}", [128, 256]) for i in range(2)]
            Bsg = [Buf(f"sg{i}") for i in range(2)]
            actb = [sb(f"actb{i}", [128, 256], BF16) for i in range(2)]
            Bact = [Buf(f"act{i}") for i in range(2)]
            actT = [sb(f"actT{i}", [128, 2, 128], BF16) for i in range(3)]
            BactT = [Buf(f"actT{i}") for i in range(3)]

            for ex in range(first_load, 4):
                moe_load(l, ex)

            gl = lg[:, :, 0:4]
            S.op("dve", lambda e: e.tensor_reduce(out=gl_m[:], in_=gl, axis=AX.X, op=ALU.max), reads=[Blg], writes=[Bglm])
            S.op("dve", lambda e: e.tensor_tensor(out=goh[:], in0=gl, in1=bc3(gl_m[:], 4), op=ALU.is_equal),
                 reads=[Blg, Bglm], writes=[Bgoh])
            S.op("dve", lambda e: e.tensor_tensor(out=gex[:], in0=gl, in1=bc3(gl_m[:], 4), op=ALU.subtract),
                 reads=[Blg, Bglm], writes=[Bgex])
            S.op("act", lambda e: e.activation(out=gex[:], in_=gex[:], func=AF.Exp), reads=[Bgex], writes=[Bgex])
            S.op("dve", lambda e: e.tensor_reduce(out=gp[:], in_=gex[:], axis=AX.X, op=ALU.add), reads=[Bgex], writes=[Bgp])
            S.op("dve", lambda e: e.reciprocal(out=gp[:], in_=gp[:]), reads=[Bgp], writes=[Bgp])
            S.op("dve", lambda e: e.tensor_tensor(
                out=sel4[:].rearrange("p t (g e) -> p t g e", g=4), in0=lg[:, :, 4:36].rearrange("p t (g e) -> p t g e", g=4),
                in1=goh[:].unsqueeze(3).to_broadcast([128, NT, 4, 8]), op=ALU.mult),
                 reads=[Blg, Bgoh], writes=[Bsel4])
            S.op("dve", lambda e: e.tensor_reduce(out=sel[:], in_=sel4[:].rearrange("p t (g e) -> p t e g", g=4),
                                                  axis=AX.X, op=ALU.add), reads=[Bsel4], writes=[Bsel])
            S.op("dve", lambda e: e.tensor_reduce(out=m1[:], in_=sel[:], axis=AX.X, op=ALU.max), reads=[Bsel], writes=[Bm1])
            S.op("dve", lambda e: e.tensor_tensor(out=oh1[:], in0=sel[:], in1=bc3(m1[:], 8), op=ALU.is_equal),
                 reads=[Bsel, Bm1], writes=[Boh1])
            S.op("dve", lambda e: e.scalar_tensor_tensor(out=sel2[:], in0=oh1[:], scalar=-1e30, in1=sel[:],
                                                         op0=ALU.mult, op1=ALU.add), reads=[Boh1, Bsel], writes=[Bsel2])
            S.op("dve", lambda e: e.tensor_reduce(out=m2[:], in_=sel2[:], axis=AX.X, op=ALU.max), reads=[Bsel2], writes=[Bm2])
            S.op("dve", lambda e: e.tensor_tensor(out=oh2[:], in0=sel2[:], in1=bc3(m2[:], 8), op=ALU.is_equal),
                 reads=[Bsel2, Bm2], writes=[Boh2])
            S.op("dve", lambda e: e.tensor_tensor(out=w2[:], in0=m2[:], in1=m1[:], op=ALU.subtract),
                 reads=[Bm1, Bm2], writes=[Bw2])
            S.op("act", lambda e: e.activation(out=w2[:], in_=w2[:], func=AF.Exp), reads=[Bw2], writes=[Bw2])
            S.op("dve", lambda e: e.tensor_scalar(out=w2[:], in0=w2[:], scalar1=1.0, scalar2=None, op0=ALU.add),
                 reads=[Bw2], writes=[Bw2])
            S.op("dve", lambda e: e.reciprocal(out=w1[:], in_=w2[:]), reads=[Bw2], writes=[Bw1])
            S.op("dve", lambda e: e.tensor_tensor(out=w1[:], in0=w1[:], in1=gp[:], op=ALU.mult), reads=[Bw1, Bgp], writes=[Bw1])
            S.op("dve", lambda e: e.tensor_tensor(out=w2[:], in0=gp[:], in1=w1[:], op=ALU.subtract),
                 reads=[Bgp, Bw1], writes=[Bw2])
            S.op("dve", lambda e: e.tensor_tensor(out=oh1[:], in0=oh1[:], in1=bc3(w1[:], 8), op=ALU.mult),
                 reads=[Boh1, Bw1], writes=[Boh1])
            S.op("dve", lambda e: e.tensor_tensor(out=oh2[:], in0=oh2[:], in1=bc3(w2[:], 8), op=ALU.mult),
                 reads=[Boh2, Bw2], writes=[Boh2])
            S.op("dve", lambda e: e.tensor_tensor(out=oh1[:], in0=oh1[:], in1=oh2[:], op=ALU.add),
                 reads=[Boh1, Boh2], writes=[Boh1])
            S.op("dve", lambda e: e.tensor_tensor(
                out=comb[:].rearrange("p t (g e) -> p t g e", g=4),
                in0=goh[:].unsqueeze(3).to_broadcast([128, NT, 4, 8]),
                in1=oh1[:].unsqueeze(2).to_broadcast([128, NT, 4, 8]), op=ALU.mult),
                 reads=[Bgoh, Boh1], writes=[Bcomb])

            GUB = [0, 1]; TB = [2, 3]; YB = [[4, 5], [6, 7]]
            steps = []
            for c in range(nchunks):
                for t in range(NT):
                    for e2 in range(2):
                        steps.append((c, t, e2))
            n = len(steps)

            def GU(i):
                c, t, e2 = steps[i]
                s = (2 * c + e2) % 4
                wgu, _ = moe_views(s)
                bk = GUB[i % 2]
                for k in range(8):
                    S.op("pe", (lambda k=k: lambda e: e.matmul(
                        banks[bk][:], hT[:, k, t * 128:(t + 1) * 128], wgu[:, k, :], start=(k == 0), stop=(k == 7)))(),
                         reads=[BH[t], BW[s]], writes=[PB[bk]])
                si = i % 2
                S.op("act", lambda e: e.activation(out=sg[si][:], in_=banks[bk][:, 0:256], func=AF.Silu),
                     reads=[PB[bk]], writes=[Bsg[si]])
                ex = 2 * c + e2
                S.op("dve", lambda e: e.scalar_tensor_tensor(
                    out=actb[si][:], in0=sg[si][:], scalar=comb[:, t, ex:ex + 1], in1=banks[bk][:, 256:512],
                    op0=ALU.mult, op1=ALU.mult), reads=[Bsg[si], Bcomb, PB[bk]], writes=[Bact[si]])

            def TR(i):
                si = i % 2
                bk = TB[i % 2]
                ti = i % 3
                for j in range(2):
                    S.op("pe", (lambda j=j: lambda e: e.matmul(
                        banks[bk][:, j * 128:(j + 1) * 128], actb[si][:, j * 128:(j + 1) * 128], ident[:],
                        start=True, stop=True))(),
                         reads=[Bact[si], Bident], writes=[PB[bk]])
                S.op("act", lambda e: e.activation(out=actT[ti][:], in_=banks[bk][:, 0:256].rearrange("p (j c) -> p j c", j=2),
                                                   func=AF.Copy), reads=[PB[bk]], writes=[BactT[ti]])

            def DN(i):
                c, t, e2 = steps[i]
                s = (2 * c + e2) % 4
                _, wdn = moe_views(s)
                ti = i % 3
                yb = YB[t % 2]
                for nb in range(2):
                    for j in range(2):
                        S.op("pe", (lambda nb=nb, j=j: lambda e: e.matmul(
                            banks[yb[nb]][:], actT[ti][:, j, :], wdn[:, j, nb * 512:(nb + 1) * 512],
                            start=(e2 == 0 and j == 0), stop=(e2 == 1 and j == 1)))(),
                             reads=[BactT[ti], BW[s]], writes=[PB[yb[nb]]])
                if e2 == 1:
                    for nb in range(2):
                        S.op("dve", (lambda nb=nb: lambda e: e.tensor_tensor(
                            out=x[:, t, nb * 512:(nb + 1) * 512], in0=x[:, t, nb * 512:(nb + 1) * 512],
                            in1=banks[yb[nb]][:], op=ALU.add))(), reads=[BX[t], PB[yb[nb]]], writes=[BX[t]])
                    if tile_hook is not None:
                        tile_hook(c, t)
                    if end_hook is not None and c == nchunks - 1:
                        end_hook(t)
                    if t == NT - 1:
                        if c + 2 < 16 and c + 2 < nchunks:
                            moe_load(l, 2 * (c + 2)); moe_load(l, 2 * (c + 2) + 1)
                        if after_chunk is not None:
                            after_chunk(c)

            for i in range(n + 2):
                if i < n:
                    GU(i)
                if 1 <= i <= n:
                    TR(i - 1)
                if 2 <= i <= n + 1:
                    DN(i - 2)

        def mod_jobs_moe0():
            vecT, BvecT = MS["vecT"], MS["BvecT"]
            yield from compute_mod_gen(1, 1, vecT, BvecT, True, lag=12)
            to_fm(vecT, BvecT, fmt, Bfmt)
            yield from compute_mod_gen(1, 0, vecT, BvecT, False, lag=12)
            to_fm(vecT, BvecT, fmt2, Bfmt2)
            haff_combine(2, 1)
            yield from compute_mod_gen(1, 2, gtbm, Bgtbm, True, lag=12)
            make_aset(1, bout_d, gtbm, Bgtbm)
            yield from compute_mod_gen(1, 4, vecT, BvecT, True, lag=12)
            to_fm(vecT, BvecT, fmt, Bfmt)
            yield from compute_mod_gen(1, 3, vecT, BvecT, False, lag=12)
            to_fm(vecT, BvecT, fmt2, Bfmt2)
            haff_combine(3, 2)
            yield from compute_mod_gen(1, 5, MS["gtmp"], MS["Bgtmp"], True, lag=12)

        moe0_job = [None]

        def tile_hook_moe0(c, t):
            if c < 1:
                return
            if moe0_job[0] is None:
                moe0_job[0] = mod_jobs_moe0()
            next(moe0_job[0], None)

        es2 = ExitStack()
        cur_es[0] = es2
        bank_pool[0] = [0, 1, 2, 3]
        with es2:
            alloc_mod_scratch()
            MS["gtmp"] = sb("gtmp", [128, D]); MS["Bgtmp"] = Buf("gtmp")
            eh0, ef0 = make_end_hook("mid", haff=2)
            moe_phase(0, None, nchunks=(NCHUNK_DBG or 16), tile_hook=tile_hook_moe0, end_hook=(eh0 if (USE_END_HOOK or USE_END_HOOK_MID) else None), first_load=2)
            for _ in (moe0_job[0] or ()):
                pass
            if USE_END_HOOK or USE_END_HOOK_MID:
                ef0()
            else:
                finalize_seq("mid", haff=2)
            dma("pool", "dw", W[:, 4096:8192].rearrange("p (k n) -> p k n", k=8),
                win_d[:, 2048:2560].rearrange("(k p) n -> p k n", p=128), writes=[BW[0], BW[1]])
            if stop_after == "moe0":
                dump_and_end()
                return nc
            S.op("pool", lambda e: e.tensor_copy(out=gtbf[:], in_=MS["gtmp"][:]), reads=[MS["Bgtmp"]], writes=[Bgtbf])
            make_aset(2, None, None, None)
        S.barrier()
        cur_es[0] = es

        es3 = ExitStack()
        cur_es[0] = es3
        bank_pool[0] = list(range(8))
        with es3:
            gst = sb("gst", [128, NT, 4, 6]); Bgst = Buf("gst")
            mvg = sb("mvg", [128, NT, 2]); Bmvg = Buf("mvg")
            sdg = sb("sdg", [128, NT]); rstdg = sb("rstdg", [128, NT]); nmrg = sb("nmrg", [128, NT])
            Bsdg, Brstdg, Bnmrg = Buf("sdg"), Buf("rstdg"), Buf("nmrg")
            u32 = [sb(f"u32_{i}", [128, 512]) for i in range(2)]; Bu32 = [Buf(f"u32_{i}") for i in range(2)]
            v32 = [sb(f"v32_{i}", [128, 512]) for i in range(2)]; Bv32 = [Buf(f"v32_{i}") for i in range(2)]
            vln = [sb(f"vln{i}", [128, 512], BF16) for i in range(2)]; Bvln = [Buf(f"vln{i}") for i in range(2)]
            gated = [sb(f"gated{i}", [128, 512], BF16) for i in range(2)]; Bgated = [Buf(f"gated{i}") for i in range(2)]
            gatedT = [sb(f"gatedT{i}", [128, 4, 128], BF16) for i in range(3)]; BgatedT = [Buf(f"gatedT{i}") for i in range(3)]
            lngb = [sb(f"lngb{i}", [128, 2, 512]) for i in range(2)]; Blngb = [Buf(f"lngb{i}") for i in range(2)]
            binr = [sb(f"binr{i}", [128, 2, 512], BF16) for i in range(2)]; Bbinr = [Buf(f"binr{i}") for i in range(2)]
            for i in range(2):
                S.op("pool", (lambda i=i: lambda e: e.memset(binr[i][:], 0.0))(), writes=[Bbinr[i]])
            wsTm = sb("wsTm", [128, 8, 128], BF16); BwsT = Buf("wsTm")
            trilb = sb("trilb", [128, 128], BF16); Btril = Buf("tril")
            bsT = sb("bsT", [128, 8]); BbsT = Buf("bsT")
            dma("pool", "dw", wsTm[:], wsT_d.rearrange("g s t -> s g t"), writes=[BwsT])
            dma("pool", "dw", trilb[:], tril_d, writes=[Btril])
            dma("sp", "dc", bsT[:], bsT_d, writes=[BbsT])
            S.op("dve", lambda e: e.tensor_tensor(out=wsTm[:], in0=wsTm[:], in1=trilb[:].unsqueeze(1).to_broadcast([128, 8, 128]),
                                                  op=ALU.mult), reads=[BwsT, Btril], writes=[BwsT])

            def gviews(b):
                base = b * 12288
                wu = W[:, base:base + 4096].rearrange("p (k n) -> p k n", k=8)
                wv = W[:, base + 4096:base + 8192].rearrange("p (k n) -> p k n", k=8)
                wo2 = W[:, base + 8192:base + 12288].rearrange("p (j n) -> p j n", j=4)
                return wu, wv, wo2

            def gfold(b):
                _, _, wo2 = gviews(b)
                bw = [BW[2 * b], BW[2 * b + 1]]
                S.op("pool", lambda e: e.tensor_tensor(out=wo2, in0=wo2,
                                                       in1=gtbm[:].unsqueeze(1).to_broadcast([128, 4, D]), op=ALU.mult),
                     reads=bw + [Bgtbm], writes=bw)

            def gload(cb, b, main, skip_wv=False, fold_now=True, part=None):
                wu, wv, wo2 = gviews(b)
                bw = [BW[2 * b], BW[2 * b + 1]]
                if part in (None, 0):
                    if not skip_wv:
                        dma("pool", "dw", wv,
                            win_d[:, 2048 + cb * 512:2048 + (cb + 1) * 512].rearrange("(k p) n -> p k n", p=128), writes=bw)
                    dma("pool", "dw", binr[b][0:1, 1, :], bin_d[:, 2048 + cb * 512:2048 + (cb + 1) * 512], writes=[Bbinr[b]])
                    if main:
                        dma("pool", "dw", binr[b][0:1, 0, :], bin_d[:, cb * 512:(cb + 1) * 512], writes=[Bbinr[b]])
                if main and part in (None, 1):
                    dma("pool", "dw", wu, win_d[:, cb * 512:(cb + 1) * 512].rearrange("(k p) n -> p k n", p=128), writes=bw)
                if main and part in (None, 2):
                    dma("pool", "dw", wo2, wout_d[cb * 512:(cb + 1) * 512, :].rearrange("(j p) n -> p j n", p=128), writes=bw)
                    if fold_now:
                        gfold(b)
                if main and part in (None, 0):
                    pass
                    dma("sp", "dc", lngb[b][:, 0, :], glng_d[:, cb * 512:(cb + 1) * 512].to_broadcast([128, 512]),
                        writes=[Blngb[b]])
                    dma("sp", "dc", lngb[b][:, 1, :], glnb_d[:, cb * 512:(cb + 1) * 512].to_broadcast([128, 512]),
                        writes=[Blngb[b]])

            gload(0, 0, False, skip_wv=True)
            pi = 0
            for cb in range(4):
                b = cb % 2
                if cb + 1 < 4:
                    gload(cb + 1, (cb + 1) % 2, False)
                _, wv, _ = gviews(b)
                bw = [BW[2 * b], BW[2 * b + 1]]
                for t in range(NT):
                    bk = next_bank()
                    i2 = pi % 2
                    pi += 1
                    for k in range(8):
                        S.op("pe", (lambda k=k, bk=bk, t=t, wv=wv: lambda e: e.matmul(
                            banks[bk][:], hT[:, k, t * 128:(t + 1) * 128], wv[:, k, :], start=(k == 0), stop=False))(),
                             reads=[BH[t]] + bw, writes=[PB[bk]])
                    S.op("pe", (lambda bk=bk, b=b: lambda e: e.matmul(banks[bk][:], onesrow[:], binr[b][:, 1, :],
                                                                      start=False, stop=True))(),
                         reads=[Bonesrow, Bbinr[b]], writes=[PB[bk]])
                    S.op("act", (lambda bk=bk, i2=i2: lambda e: e.activation(out=v32[i2][:], in_=banks[bk][:], func=AF.Gelu))(),
                         reads=[PB[bk]], writes=[Bv32[i2]])
                    S.op("dve", (lambda i2=i2, t=t, cb=cb: lambda e: e.bn_stats(out=gst[:, t, cb, :], in_=v32[i2][:]))(),
                         reads=[Bv32[i2]], writes=[Bgst])
            for t in range(NT):
                S.op("dve", (lambda t=t: lambda e: e.bn_aggr(out=mvg[:, t, :], in_=gst[:, t, :, :]))(),
                     reads=[Bgst], writes=[Bmvg])
            S.op("act", lambda e: e.activation(out=sdg[:], in_=mvg[:, :, 1], func=AF.Ln, bias=eps_t[:]),
                 reads=[Bmvg, Beps], writes=[Bsdg])
            S.op("act", lambda e: e.activation(out=rstdg[:], in_=sdg[:], func=AF.Exp, scale=-0.5),
                 reads=[Bsdg], writes=[Brstdg])
            S.op("dve", lambda e: e.scalar_tensor_tensor(out=nmrg[:], in0=mvg[:, :, 0], scalar=-1.0, in1=rstdg[:],
                                                         op0=ALU.mult, op1=ALU.mult), reads=[Bmvg, Brstdg], writes=[Bnmrg])

            gsteps = [(cb, t) for cb in range(4) for t in range(NT)]
            ng = len(gsteps)

            def gA(i):
                cb, t = gsteps[i]
                b = cb % 2
                if cb + 1 < 4:
                    if t == 3:
                        gload(cb + 1, (cb + 1) % 2, True, fold_now=False, part=0)
                    if t == 6:
                        gload(cb + 1, (cb + 1) % 2, True, fold_now=False, part=1)
                    if t == 9:
                        gload(cb + 1, (cb + 1) % 2, True, fold_now=False, part=2)
                    if t == 13:
                        gfold((cb + 1) % 2)
                wu, wv, _ = gviews(b)
                bw = [BW[2 * b], BW[2 * b + 1]]
                i2 = i % 2
                for which, wmat, dst, bdst in ((0, wu, u32[i2], Bu32[i2]), (1, wv, v32[i2], Bv32[i2])):
                    bk = next_bank()
                    for k in range(8):
                        S.op("pe", (lambda k=k, bk=bk, wmat=wmat: lambda e: e.matmul(
                            banks[bk][:], hT[:, k, t * 128:(t + 1) * 128], wmat[:, k, :], start=(k == 0), stop=False))(),
                             reads=[BH[t]] + bw, writes=[PB[bk]])
                    S.op("pe", (lambda bk=bk, which=which: lambda e: e.matmul(banks[bk][:], onesrow[:], binr[b][:, which, :],
                                                                              start=False, stop=True))(),
                         reads=[Bonesrow, Bbinr[b]], writes=[PB[bk]])
                    S.op("act", (lambda bk=bk, dst=dst: lambda e: e.activation(out=dst[:], in_=banks[bk][:], func=AF.Gelu))(),
                         reads=[PB[bk]], writes=[bdst])
                S.op("dve", lambda e: e.tensor_scalar(out=v32[i2][:], in0=v32[i2][:], scalar1=rstdg[:, t:t + 1],
                                                      scalar2=nmrg[:, t:t + 1], op0=ALU.mult, op1=ALU.add),
                     reads=[Bv32[i2], Brstdg, Bnmrg], writes=[Bv32[i2]])
                S.op("pool", lambda e: e.tensor_tensor(out=v32[i2][:], in0=v32[i2][:], in1=lngb[b][:, 0, :], op=ALU.mult),
                     reads=[Bv32[i2], Blngb[b]], writes=[Bv32[i2]])
                S.op("pool", lambda e: e.tensor_tensor(out=vln[i2][:], in0=v32[i2][:], in1=lngb[b][:, 1, :], op=ALU.add),
                     reads=[Bv32[i2], Blngb[b]], writes=[Bvln[i2]])

            def gB(i):
                cb, t = gsteps[i]
                i2 = i % 2
                bk = next_bank()
                for gi in range(2):
                    g = 2 * cb + gi
                    S.op("pe", (lambda gi=gi, g=g: lambda e: e.matmul(
                        banks[bk][:, gi * 256:(gi + 1) * 256], wsTm[:, g, :], vln[i2][:, gi * 256:(gi + 1) * 256],
                        start=True, stop=True))(), reads=[BwsT, Bvln[i2]], writes=[PB[bk]])
                for gi in range(2):
                    g = 2 * cb + gi
                    S.op("dve", (lambda gi=gi, g=g: lambda e: e.scalar_tensor_tensor(
                        out=gated[i2][:, gi * 256:(gi + 1) * 256], in0=banks[bk][:, gi * 256:(gi + 1) * 256],
                        scalar=bsT[:, g:g + 1], in1=u32[i2][:, gi * 256:(gi + 1) * 256], op0=ALU.add, op1=ALU.mult))(),
                         reads=[PB[bk], BbsT, Bu32[i2]], writes=[Bgated[i2]])

            def gC(i):
                i2 = i % 2
                bt = next_bank()
                i3 = i % 3
                for j in range(4):
                    S.op("pe", (lambda j=j: lambda e: e.matmul(
                        banks[bt][:, j * 128:(j + 1) * 128], gated[i2][:, j * 128:(j + 1) * 128], ident[:],
                        start=True, stop=True))(),
                         reads=[Bgated[i2], Bident], writes=[PB[bt]])
                S.op("act", lambda e: e.activation(out=gatedT[i3][:], in_=banks[bt][:].rearrange("p (j c) -> p j c", j=4),
                                                   func=AF.Copy), reads=[PB[bt]], writes=[BgatedT[i3]])

            def gD(i):
                cb, t = gsteps[i]
                b = cb % 2
                _, _, wo2 = gviews(b)
                bw = [BW[2 * b], BW[2 * b + 1]]
                i3 = i % 3
                for nb in range(2):
                    bk = next_bank()
                    for j in range(4):
                        S.op("pe", (lambda j=j, nb=nb, bk=bk: lambda e: e.matmul(
                            banks[bk][:], gatedT[i3][:, j, :], wo2[:, j, nb * 512:(nb + 1) * 512],
                            start=(j == 0), stop=(j == 3)))(), reads=[BgatedT[i3]] + bw, writes=[PB[bk]])
                    S.op("dve", (lambda nb=nb, bk=bk: lambda e: e.tensor_tensor(
                        out=x[:, t, nb * 512:(nb + 1) * 512], in0=x[:, t, nb * 512:(nb + 1) * 512], in1=banks[bk][:],
                        op=ALU.add))(), reads=[BX[t], PB[bk]], writes=[BX[t]])
                if cb == 3:
                    if t == 3:
                        moe_load_gu(1, 0); moe_load_gu(1, 1)
                    if t == 8:
                        moe_load_dn(1, 0); moe_load_dn(1, 1)
                    if t == 12:
                        moe_fold(1, 0); moe_fold(1, 1)
                    if USE_END_HOOK or USE_END_HOOK_MID:
                        ehg(t)

            ehg, efg = make_end_hook("mid", haff=3, route_l=1)
            gload(0, 0, True)
            for i in range(ng + 3):
                if i < ng:
                    gA(i)
                if 1 <= i <= ng:
                    gB(i - 1)
                if 2 <= i <= ng + 1:
                    gC(i - 2)
                if 3 <= i <= ng + 2:
                    gD(i - 3)
            if USE_END_HOOK or USE_END_HOOK_MID:
                efg()
            else:
                finalize_seq("mid", haff=3, route_l=1)
            if stop_after == "gmlp":
                dump_and_end()
                return nc
        S.barrier()
        cur_es[0] = es

        es4 = ExitStack()
        cur_es[0] = es4
        bank_pool[0] = [0, 1, 2, 3]
        with es4:
            make_aset(3, None, None, None, scale=1.0)
            eh1, ef1 = make_end_hook("out")
            moe_phase(1, None, nchunks=(NCHUNK_DBG or 16), first_load=2, end_hook=(eh1 if USE_END_HOOK_OUT else None))
            if USE_END_HOOK_OUT:
                ef1()
            else:
                finalize_seq("out")
            S.wait_all("sp", [Bout])
            S.emit()
    return nc


NCHUNK_DBG = None
USE_END_HOOK = False
USE_END_HOOK_OUT = True
USE_END_HOOK_MID = True
ATT_DBG = [NT, None]


_CACHE = {}


def _host_inputs(inputs):
    f32 = np.float32
    x = np.asarray(inputs["x"], f32)
    c = np.asarray(inputs["c"], f32)
    pos = np.asarray(inputs["positions"], np.int32)
    shared = {}
    shared["ident"] = np.eye(128, dtype=f32)
    s = np.arange(128)[:, None]
    q = np.arange(128)[None, :]
    m_cur = np.where(s <= q, 0.0, NEG).astype(f32)
    m_prev = np.where(s > q, 0.0, NEG).astype(f32)
    m_none = np.full((128, 128), NEG, f32)
    inv_freq = (10000.0 ** (-np.arange(0, 64, 2, dtype=f32) / f32(64))).astype(f32)
    shared["invf"] = np.ascontiguousarray(np.broadcast_to(inv_freq[None, :], (128, 32))).astype(f32)
    shared["tril"] = (s <= q).astype(f32)
    shared["ada_w"] = np.ascontiguousarray(inputs["ada_w"], f32)
    shared["ada_b"] = np.ascontiguousarray(inputs["ada_b"], f32)
    g = np.asarray(inputs["post_ln_g"], f32).reshape(4, D)
    b = np.asarray(inputs["post_ln_b"], f32).reshape(4, D)
    shared["post_ln_g"] = np.ascontiguousarray(g)
    shared["post_ln_b"] = np.ascontiguousarray(b)
    shared["post_ln_gT"] = np.ascontiguousarray(g.reshape(4, 8, 128).transpose(0, 2, 1))
    shared["post_ln_bT"] = np.ascontiguousarray(b.reshape(4, 8, 128).transpose(0, 2, 1))
    shared["attn_w_qkv"] = np.ascontiguousarray(inputs["attn_w_qkv"][0], f32)
    shared["attn_b_qkv"] = np.ascontiguousarray(inputs["attn_b_qkv"], f32).reshape(1, 1536)
    shared["attn_sinks"] = np.ascontiguousarray(inputs["attn_sinks"], f32).reshape(1, 16)
    shared["attn_w_o"] = np.ascontiguousarray(inputs["attn_w_o"][0], f32)
    shared["attn_b_o"] = np.ascontiguousarray(inputs["attn_b_o"], f32).reshape(1, D)
    shared["gmlp_w_in"] = np.ascontiguousarray(inputs["gmlp_w_in"][0], f32)
    shared["gmlp_b_in"] = np.ascontiguousarray(inputs["gmlp_b_in"], f32).reshape(1, 4096)
    shared["gmlp_ln_g"] = np.ascontiguousarray(inputs["gmlp_sgu_ln_g"], f32).reshape(1, 2048)
    shared["gmlp_ln_b"] = np.ascontiguousarray(inputs["gmlp_sgu_ln_b"], f32).reshape(1, 2048)
    shared["gmlp_w_sT"] = np.ascontiguousarray(np.asarray(inputs["gmlp_w_s"][0], f32).transpose(0, 2, 1))
    shared["gmlp_b_sT"] = np.ascontiguousarray(np.asarray(inputs["gmlp_b_s"][0], f32).T)
    shared["gmlp_w_out"] = np.ascontiguousarray(inputs["gmlp_w_out"][0], f32)
    shared["gmlp_b_out"] = np.ascontiguousarray(inputs["gmlp_b_out"], f32).reshape(1, D)
    shared["moe_wr"] = np.ascontiguousarray(np.concatenate(
        [np.asarray(inputs["moe_w_group_router"], f32), np.asarray(inputs["moe_w_expert_router"], f32)], axis=-1))
    shared["moe_br"] = np.ascontiguousarray(np.concatenate(
        [np.asarray(inputs["moe_b_group_router"], f32), np.asarray(inputs["moe_b_expert_router"], f32)], axis=-1)
    ).reshape(2, 1, 36)
    shared["moe_w_gate_up"] = np.ascontiguousarray(inputs["moe_w_gate_up"], f32).reshape(2, 32, D, 512)
    shared["moe_w_down"] = np.ascontiguousarray(inputs["moe_w_down"], f32).reshape(2, 32, 256, D)
    in_maps = []
    for r in range(8):
        bi, qi = r // 4, r % 4
        s0 = qi * 2048
        m = dict(shared)
        xc = np.zeros((17 * 128, D), f32)
        pc = np.zeros((17 * 128,), np.int32)
        if qi > 0:
            xc[:] = x[bi, s0 - 128:s0 + 2048]
            pc[:] = pos[bi, s0 - 128:s0 + 2048]
        else:
            xc[128:] = x[bi, 0:2048]
            pc[128:] = pos[bi, 0:2048]
        m["xin"] = xc
        m["pos"] = np.ascontiguousarray(pc.reshape(17, 128).T)
        m["cT"] = np.ascontiguousarray(c[bi].reshape(8, 128).T)
        mk = np.stack([np.tile(m_cur, (1, 4)), np.tile(m_prev, (1, 4)),
                       np.tile(m_none if qi == 0 else m_prev, (1, 4))]).astype(f32)
        m["masks"] = np.ascontiguousarray(mk)
        in_maps.append(m)
    return in_maps


def kernel(**inputs):
    in_maps = _host_inputs(inputs)
    if "nc" not in _CACHE:
        _CACHE["nc"] = build()
    res = run_bass_kernel_spmd(_CACHE["nc"], in_maps, core_ids=list(range(8)))
    out = np.empty((2, 8192, D), np.float32)
    for r in range(8):
        bi, qi = r // 4, r % 4
        out[bi, qi * 2048:(qi + 1) * 2048] = res.results[r]["out"]
    return out
```
